# Optimizing a Trainium2 kernel written in Bass

```python
import jax, jax.numpy as jnp
from jax import lax
import numpy as np

D_MODEL = 1024
BATCH = 8
SEQ = 2048
DEPTH = 4

GRID_W = 64
CTX_LEN = 256
N_MIXERS = 2
ML_INNER = 2 * D_MODEL
ML_HEADS = 4
ML_HEAD_DIM = ML_INNER // ML_HEADS
ML_QKV_BLOCK = 4
ML_CONV_W = 5
ML_CHUNK = 64
NA_HEAD_DIM = 64
NA_HEADS = D_MODEL // NA_HEAD_DIM
NA_KH = 8
NA_KW = 16
FF_HIDDEN = ((8 * D_MODEL // 3 + 127) // 128) * 128
FF_CONV_W = 3
EPS = 1e-6

kernel_name = "hybrid_mlstm_natten_convffn_diffusion"


def rmsnorm(x, g):
    xf = x.astype(jnp.float32)
    y = xf * lax.rsqrt(jnp.mean(xf * xf, axis=-1, keepdims=True) + EPS)
    return (y * g.astype(jnp.float32)).astype(x.dtype)


def modulate(h, shift, scale):
    return h * (1 + scale) + shift


def dwconv(x, w, b):
    k = w.shape[0]
    y = lax.conv_general_dilated(x, w[:, None, :].astype(x.dtype), window_strides=(1,),
                                 padding=[(k // 2, k // 2)],
                                 dimension_numbers=('NWC', 'WIO', 'NWC'),
                                 feature_group_count=x.shape[-1])
    return y + b


def split_heads(t, n_heads):
    bsz, t_len, _ = t.shape
    return t.reshape(bsz, t_len, n_heads, -1).transpose(0, 2, 1, 3)


def merge_heads(t):
    bsz, n_heads, t_len, dh = t.shape
    return t.transpose(0, 2, 1, 3).reshape(bsz, t_len, n_heads * dh)


def blockdiag(u, w):
    bsz, t_len, _ = u.shape
    ub = u.reshape(bsz, t_len, w.shape[0], w.shape[1])
    return jnp.einsum('btni,nio->btno', ub, w).reshape(bsz, t_len, -1)


def mlstm_chunkwise(q, k, v, log_i, log_f, state):
    bsz, n_heads, t_len, dh = q.shape
    nc = t_len // ML_CHUNK

    def chunks(a):
        return jnp.moveaxis(a.reshape(bsz, n_heads, nc, ML_CHUNK, *a.shape[3:]), 2, 0)

    tril = jnp.tril(jnp.ones((ML_CHUNK, ML_CHUNK), dtype=bool))

    def step(carry, inp):
        c_mem, n_mem, m_prev = carry
        qc, kc, vc, li, lf = inp
        b = jnp.cumsum(lf, axis=-1)
        log_d = jnp.where(tril, b[..., :, None] - b[..., None, :] + li[..., None, :], -jnp.inf)
        inter = b + m_prev[..., None]
        m_t = jnp.maximum(inter, jnp.max(log_d, axis=-1))
        s = jnp.einsum('bhtd,bhsd->bhts', qc, kc) * jnp.exp(log_d - m_t[..., None])
        w_inter = jnp.exp(inter - m_t)
        num = (jnp.einsum('bhts,bhsd->bhtd', s, vc)
               + w_inter[..., None] * jnp.einsum('bhvk,bhtk->bhtv', c_mem, qc))
        den = jnp.sum(s, axis=-1) + w_inter * jnp.einsum('bhk,bhtk->bht', n_mem, qc)
        h = num / jnp.maximum(jnp.abs(den), jnp.exp(-m_t))[..., None]
        g = b[..., -1:] - b + li
        m_new = jnp.maximum(b[..., -1] + m_prev, jnp.max(g, axis=-1))
        decay = jnp.exp(b[..., -1] + m_prev - m_new)
        wk = jnp.exp(g - m_new[..., None])
        c_new = decay[..., None, None] * c_mem + jnp.einsum('bhsv,bhsk->bhvk', vc * wk[..., None], kc)
        n_new = decay[..., None] * n_mem + jnp.einsum('bhs,bhsk->bhk', wk, kc)
        return (c_new, n_new, m_new), h

    state, h = lax.scan(step, state, (chunks(q), chunks(k), chunks(v), chunks(log_i), chunks(log_f)))
    return jnp.moveaxis(h, 0, 2).reshape(bsz, n_heads, t_len, dh), state


def mlstm_mixer(hc, hx, w_in, conv_w, conv_b, wq, wk, wv, gate_w, gate_b, skip, hnorm_g, w_out, need_ctx):
    f32 = jnp.float32

    def project(h):
        u, z = jnp.split(h @ w_in, 2, axis=-1)
        uc = jax.nn.silu(dwconv(u, conv_w, conv_b))
        q = blockdiag(uc, wq)
        k = blockdiag(uc, wk) * (ML_HEAD_DIM ** -0.5)
        v = blockdiag(u, wv)
        gates = jnp.einsum('btc,zcg->zbtg', jnp.concatenate([q, k, v], axis=-1), gate_w) + gate_b[:, None, None, :]
        gates = jnp.moveaxis(gates.astype(f32), -1, 2)
        log_i = gates[:, :, :ML_HEADS]
        log_f = jax.nn.log_sigmoid(gates[:, :, ML_HEADS:])
        heads = lambda t: split_heads(t, ML_HEADS).astype(f32)
        return uc, z, heads(q), heads(k), heads(v), log_i, log_f

    pc = project(hc)
    px = project(hx)
    bsz = hx.shape[0]
    dh = ML_HEAD_DIM
    hs_c, hs_x = [], []
    for d in range(2):
        fl = (lambda a: jnp.flip(a, axis=2)) if d == 1 else (lambda a: a)
        state = (jnp.zeros((bsz, ML_HEADS, dh, dh), f32), jnp.zeros((bsz, ML_HEADS, dh), f32),
                 jnp.zeros((bsz, ML_HEADS), f32))
        h_c, state = mlstm_chunkwise(fl(pc[2]), fl(pc[3]), fl(pc[4]), fl(pc[5][d]), fl(pc[6][d]), state)
        h_x, _ = mlstm_chunkwise(fl(px[2]), fl(px[3]), fl(px[4]), fl(px[5][d]), fl(px[6][d]), state)
        hs_c.append(fl(h_c))
        hs_x.append(fl(h_x))

    def output(hsum, uc, z):
        mu = jnp.mean(hsum, axis=-1, keepdims=True)
        var = jnp.mean(jnp.square(hsum - mu), axis=-1, keepdims=True)
        hn = merge_heads((hsum - mu) * lax.rsqrt(var + EPS)).astype(uc.dtype) * hnorm_g
        return ((hn + skip * uc) * jax.nn.silu(z)) @ w_out

    yx = output(hs_x[0] + hs_x[1], px[0], px[1])
    yc = output(hs_c[0] + hs_c[1], pc[0], pc[1]) if need_ctx else None
    return yc, yx


def na_mixer(hc, hx, w_qkv, b_qkv, rpb, w_out, b_out, need_ctx):
    f32 = jnp.float32
    scale = NA_HEAD_DIM ** -0.5

    def qkv(h):
        return [split_heads(a, NA_HEADS) for a in jnp.split(h @ w_qkv + b_qkv, 3, axis=-1)]

    q_c, k_c, v_c = qkv(hc)
    q_x, k_x, v_x = qkv(hx)
    yc = None
    if need_ctx:
        s = jnp.einsum('bhqd,bhkd->bhqk', q_c, k_c).astype(f32) * scale
        yc = merge_heads(jnp.einsum('bhqk,bhkd->bhqd', jax.nn.softmax(s, axis=-1).astype(v_c.dtype), v_c)) @ w_out + b_out

    bsz, n_heads, s_len, dh = q_x.shape
    rows = s_len // GRID_W
    kh = min(NA_KH, rows)
    kg = k_x.reshape(bsz, n_heads, rows, GRID_W, dh)
    vg = v_x.reshape(bsz, n_heads, rows, GRID_W, dh)
    qg = jnp.moveaxis(q_x.reshape(bsz, n_heads, rows, GRID_W, dh), 2, 0)
    col = jnp.arange(GRID_W)
    col_start = jnp.clip(col - NA_KW // 2, 0, GRID_W - NA_KW)
    col_ok = (col[None, :] >= col_start[:, None]) & (col[None, :] < col_start[:, None] + NA_KW)
    dc_idx = jnp.clip(col[None, :] - col[:, None] + NA_KW - 1, 0, 2 * NA_KW - 2)
    rpb_cols = rpb.astype(f32)[:, :, dc_idx]
    n_loc = kh * GRID_W

    def row_block(args):
        r, q_r = args
        rs = jnp.clip(r - kh // 2, 0, rows - kh)
        kb = lax.dynamic_slice_in_dim(kg, rs, kh, axis=2)
        vb = lax.dynamic_slice_in_dim(vg, rs, kh, axis=2)
        bias = jnp.take(rpb_cols, rs + jnp.arange(kh) - r + NA_KH - 1, axis=1)
        s_loc = (jnp.einsum('bhqd,bhjkd->bhqjk', q_r, kb).astype(f32) * scale
                 + jnp.transpose(bias, (0, 2, 1, 3))[None])
        s_loc = jnp.where(col_ok[:, None, :], s_loc, -jnp.inf).reshape(bsz, n_heads, GRID_W, n_loc)
        s_ctx = jnp.einsum('bhqd,bhcd->bhqc', q_r, k_c).astype(f32) * scale
        p = jax.nn.softmax(jnp.concatenate([s_loc, s_ctx], axis=-1), axis=-1).astype(vb.dtype)
        return (jnp.einsum('bhqn,bhnd->bhqd', p[..., :n_loc], vb.reshape(bsz, n_heads, n_loc, dh))
                + jnp.einsum('bhqc,bhcd->bhqd', p[..., n_loc:], v_c))

    o = lax.map(row_block, (jnp.arange(rows), qg))
    o = jnp.transpose(o, (1, 0, 3, 2, 4)).reshape(bsz, s_len, n_heads * dh)
    return yc, o @ w_out + b_out


def conv_ffn(h, w_up, b_up, conv_w, conv_b, w_down, b_down):
    u = dwconv(h @ w_up + b_up, conv_w, conv_b)
    a, g = jnp.split(u, 2, axis=-1)
    return (a * jax.nn.silu(g)) @ w_down + b_down


def setup_inputs(seed: int = 0) -> dict:
    key = jax.random.key(seed)
    ks = jax.random.split(key, 40)
    n_a = (DEPTH + 1) // 2
    n_b = DEPTH // 2
    nrm = lambda i, shape, s: jax.random.normal(ks[i], shape, jnp.float32) * s
    d = D_MODEL
    gate_b_i = nrm(20, (n_a, 2, ML_HEADS), 0.1)
    gate_b_f = jnp.linspace(3.0, 6.0, ML_HEADS, dtype=jnp.float32) + nrm(21, (n_a, 2, ML_HEADS), 0.1)
    return {
        'x': nrm(0, (BATCH, SEQ, d), 1.0),
        'c': nrm(1, (BATCH, d), 1.0),
        'ctx': nrm(2, (BATCH, CTX_LEN, d), 1.0),
        'c_ctx': nrm(3, (d,), 1.0),
        'ada_w': nrm(4, (DEPTH, d, 6 * d), 0.5 * d ** -0.5),
        'ada_b': nrm(5, (DEPTH, 6 * d), 0.02),
        'norm1_g': 1.0 + nrm(6, (DEPTH, d), 0.02),
        'norm2_g': 1.0 + nrm(7, (DEPTH, d), 0.02),
        'final_g': 1.0 + nrm(8, (d,), 0.02),
        'ml_w_in': nrm(9, (n_a, d, 2 * ML_INNER), d ** -0.5),
        'ml_conv_w': nrm(10, (n_a, ML_CONV_W, ML_INNER), ML_CONV_W ** -0.5),
        'ml_conv_b': nrm(11, (n_a, ML_INNER), 0.02),
        'ml_wq': nrm(12, (n_a, ML_INNER // ML_QKV_BLOCK, ML_QKV_BLOCK, ML_QKV_BLOCK), ML_QKV_BLOCK ** -0.5),
        'ml_wk': nrm(13, (n_a, ML_INNER // ML_QKV_BLOCK, ML_QKV_BLOCK, ML_QKV_BLOCK), ML_QKV_BLOCK ** -0.5),
        'ml_wv': nrm(14, (n_a, ML_INNER // ML_QKV_BLOCK, ML_QKV_BLOCK, ML_QKV_BLOCK), ML_QKV_BLOCK ** -0.5),
        'ml_gate_w': nrm(15, (n_a, 2, 3 * ML_INNER, 2 * ML_HEADS), 0.5 * (3 * ML_INNER) ** -0.5),
        'ml_gate_b': jnp.concatenate([gate_b_i, gate_b_f], axis=-1),
        'ml_skip': 1.0 + nrm(16, (n_a, ML_INNER), 0.02),
        'ml_hnorm_g': 1.0 + nrm(17, (n_a, ML_INNER), 0.02),
        'ml_w_out': nrm(18, (n_a, ML_INNER, d), ML_INNER ** -0.5),
        'na_w_qkv': nrm(22, (n_b, d, 3 * d), d ** -0.5),
        'na_b_qkv': nrm(23, (n_b, 3 * d), 0.02),
        'na_rpb': nrm(24, (n_b, NA_HEADS, 2 * NA_KH - 1, 2 * NA_KW - 1), 0.1),
        'na_w_out': nrm(25, (n_b, d, d), d ** -0.5),
        'na_b_out': nrm(26, (n_b, d), 0.02),
        'ff_w_up': nrm(27, (DEPTH, d, 2 * FF_HIDDEN), d ** -0.5),
        'ff_b_up': nrm(28, (DEPTH, 2 * FF_HIDDEN), 0.02),
        'ff_conv_w': nrm(29, (DEPTH, FF_CONV_W, 2 * FF_HIDDEN), FF_CONV_W ** -0.5),
        'ff_conv_b': nrm(30, (DEPTH, 2 * FF_HIDDEN), 0.02),
        'ff_w_down': nrm(31, (DEPTH, FF_HIDDEN, d), FF_HIDDEN ** -0.5),
        'ff_b_down': nrm(32, (DEPTH, d), 0.02),
    }


def reference(x, c, ctx, c_ctx, ada_w, ada_b, norm1_g, norm2_g, final_g,
              ml_w_in, ml_conv_w, ml_conv_b, ml_wq, ml_wk, ml_wv, ml_gate_w, ml_gate_b,
              ml_skip, ml_hnorm_g, ml_w_out,
              na_w_qkv, na_b_qkv, na_rpb, na_w_out, na_b_out,
              ff_w_up, ff_b_up, ff_conv_w, ff_conv_b, ff_w_down, ff_b_down):
    xx, xc = x, ctx
    silu_c = jax.nn.silu(c)
    silu_cc = jax.nn.silu(c_ctx)
    for i in range(DEPTH):
        need_ctx = i < DEPTH - 1
        mod_x = jnp.split((silu_c @ ada_w[i] + ada_b[i])[:, None, :], 6, axis=-1)
        mod_c = jnp.split(silu_cc @ ada_w[i] + ada_b[i], 6, axis=-1)
        hx = modulate(rmsnorm(xx, norm1_g[i]), mod_x[0], mod_x[1])
        hc = modulate(rmsnorm(xc, norm1_g[i]), mod_c[0], mod_c[1])
        j = i // N_MIXERS
        if i % N_MIXERS == 0:
            yc, yx = mlstm_mixer(hc, hx, ml_w_in[j], ml_conv_w[j], ml_conv_b[j], ml_wq[j], ml_wk[j],
                                 ml_wv[j], ml_gate_w[j], ml_gate_b[j], ml_skip[j], ml_hnorm_g[j],
                                 ml_w_out[j], need_ctx)
        else:
            yc, yx = na_mixer(hc, hx, na_w_qkv[j], na_b_qkv[j], na_rpb[j], na_w_out[j], na_b_out[j], need_ctx)
        xx = xx + mod_x[2] * yx
        hx = modulate(rmsnorm(xx, norm2_g[i]), mod_x[3], mod_x[4])
        xx = xx + mod_x[5] * conv_ffn(hx, ff_w_up[i], ff_b_up[i], ff_conv_w[i], ff_conv_b[i],
                                      ff_w_down[i], ff_b_down[i])
        if need_ctx:
            xc = xc + mod_c[2] * yc
            hc = modulate(rmsnorm(xc, norm2_g[i]), mod_c[3], mod_c[4])
            xc = xc + mod_c[5] * conv_ffn(hc, ff_w_up[i], ff_b_up[i], ff_conv_w[i], ff_conv_b[i],
                                          ff_w_down[i], ff_b_down[i])
    return rmsnorm(xx, final_g)
```

```python
import contextlib
import numpy as np
import concourse.bass as bass
import concourse.mybir as mybir
from concourse.bass_utils import run_bass_kernel_spmd

F32 = mybir.dt.float32
BF16 = mybir.dt.bfloat16
AF = mybir.ActivationFunctionType
ALU = mybir.AluOpType
AX = mybir.AxisListType

D = 1024
SEQ = 2048
CTX = 256
TOK = SEQ + CTX
NT = TOK // 128
EPS = 1e-6
FF = 2816
NEGV = -30000.0
TBLK = [(0, 256), (256, 512), (768, 512), (1280, 512), (1792, 512)]


class Prog:
    ENGS = ("pe", "act", "dve", "pool", "sp")
    NDMA = {"sp": 40, "pool": 16, "act": 8}

    def __init__(self, nc):
        self.nc = nc
        self.ops = []
        self.last_w = {}
        self.readers = {}
        self.sb_off = 16512
        self.ndma = {q: 0 for q in self.NDMA}
        self.fence_idx = None
        self.nname = 0

    def sb(self, name, shape, dtype):
        esz = 4 if dtype == F32 else 2
        n = 1
        for s in shape[1:]:
            n *= s
        nbytes = (n * esz + 63) // 64 * 64
        off = self.sb_off
        self.sb_off += nbytes
        assert self.sb_off <= 229344, (name, self.sb_off)
        self.nname += 1
        return self.nc.alloc_sbuf_tensor_at("%s_%d" % (name, self.nname), list(shape), dtype, offset=off)

    def add(self, eng, fn, reads=(), writes=(), dma=False):
        idx = len(self.ops)
        deps = set()
        if self.fence_idx is not None:
            deps.add(self.fence_idx)
        for k in reads:
            if k in self.last_w:
                deps.add(self.last_w[k])
        for k in writes:
            if k in self.last_w:
                deps.add(self.last_w[k])
            for r in self.readers.get(k, ()):
                deps.add(r)
        deps.discard(idx)
        for k in reads:
            self.readers.setdefault(k, []).append(idx)
        for k in writes:
            self.last_w[k] = idx
            self.readers[k] = []
        op = dict(eng=eng, fn=fn, deps=deps, dma=dma, sig=False)
        if dma:
            q = eng
            k = self.ndma[q]
            self.ndma[q] += 1
            op["dq"] = (q, k % self.NDMA[q])
            op["dval"] = 16 * (k // self.NDMA[q] + 1)
            op["dprev"] = 16 * (k // self.NDMA[q])
        self.ops.append(op)
        return idx

    def fence(self, scratch):
        deps = set(self.last_w.values())
        for rs in self.readers.values():
            deps.update(rs)
        idx = len(self.ops)
        if self.fence_idx is not None:
            deps.add(self.fence_idx)
        self.ops.append(dict(eng="pool", fn=lambda e: e.memset(scratch, 0.0), deps=deps, dma=False, sig=True))
        self.last_w = {}
        self.readers = {}
        self.fence_idx = idx

    def pe(self, fn, reads=(), writes=()):
        return self.add("pe", fn, reads, writes)

    def act(self, fn, reads=(), writes=()):
        return self.add("act", fn, reads, writes)

    def dve(self, fn, reads=(), writes=()):
        return self.add("dve", fn, reads, writes)

    def pool(self, fn, reads=(), writes=()):
        return self.add("pool", fn, reads, writes)

    def dma(self, out, in_, reads=(), writes=(), q="sp", **kw):
        return self.add(q, lambda e: e.dma_start(out=out, in_=in_, **kw), reads, writes, dma=True)

    def mm(self, out_ap, pairs, reads, writes):
        def fn(e):
            n = len(pairs)
            ins = None
            for i, (l, r) in enumerate(pairs):
                ins = e.matmul(out_ap, lhsT=l, rhs=r, start=(i == 0), stop=(i == n - 1))
            return ins

        return self.pe(fn, reads, writes)

    def emit(self):
        nc = self.nc
        ops = self.ops
        for op in ops:
            for d in op["deps"]:
                if ops[d]["eng"] == "pe" and op["eng"] == "pe" and not ops[d]["dma"]:
                    continue
                ops[d]["sig"] = True
        last_on = {}
        for i, op in enumerate(ops):
            if not op["dma"]:
                last_on[op["eng"]] = i
        for e, i in last_on.items():
            ops[i]["sig"] = True
        cnt = {e: 0 for e in self.ENGS}
        for op in ops:
            if op["sig"] and not op["dma"]:
                cnt[op["eng"]] += 1
                op["val"] = cnt[op["eng"]]
        with contextlib.ExitStack() as st:
            esem = {e: st.enter_context(nc.semaphore("c_" + e)) for e in self.ENGS}
            dsem = {
                q: [st.enter_context(nc.semaphore("d_%s%d" % (q, i))) for i in range(n)]
                for q, n in self.NDMA.items()
            }
            block = st.enter_context(nc.Block())

            def stream(ename):
                def body(e):
                    waited = {}

                    def wait(sem, key, val):
                        if waited.get(key, 0) >= val:
                            return
                        waited[key] = val
                        e.wait_ge(sem, val)

                    for op in ops:
                        if op["eng"] != ename:
                            continue
                        for d in sorted(op["deps"]):
                            dop = ops[d]
                            if dop["dma"]:
                                q, si = dop["dq"]
                                wait(dsem[q][si], ("d", q, si), dop["dval"])
                            else:
                                if dop["eng"] == "pe" and ename == "pe":
                                    continue
                                wait(esem[dop["eng"]], ("e", dop["eng"]), dop["val"])
                        if op["dma"]:
                            q, si = op["dq"]
                            if op["dprev"] > 0:
                                wait(dsem[q][si], ("d", q, si), op["dprev"])
                            ins = op["fn"](e)
                            ins.then_inc(dsem[q][si], 16)
                        else:
                            ins = op["fn"](e)
                            if op["sig"]:
                                ins.then_inc(esem[ename], 1)
                    if ename == "sp":
                        for q, n in self.NDMA.items():
                            tot = self.ndma[q]
                            for si in range(min(n, tot)):
                                k_last = ((tot - 1 - si) // n) * n + si
                                wait(dsem[q][si], ("d", q, si), 16 * (k_last // n + 1))
                        for en in self.ENGS:
                            if en != "sp" and cnt[en] > 0:
                                wait(esem[en], ("e", en), cnt[en])

                return body

            block.tensor(stream("pe"))
            block.scalar(stream("act"))
            block.vector(stream("dve"))
            block.gpsimd(stream("pool"))
            block.sync(stream("sp"))


def tkeys(prefix, start, n):
    return ["%s%d" % (prefix, t) for t in range(start // 128, (start + n + 127) // 128)]


def pcol(t):
    return 1 + t if t < CTX else 2 + t


class Builder:
    def __init__(self, depth=4, dbg=None):
        self.depth = depth
        self.dbg = dbg
        nc = self.nc = bass.Bass("TRN2", target_bir_lowering=False)
        self.P = Prog(nc)
        self.I = {}
        self.PS = [nc.alloc_psum_tensor("psb%d" % i, [128, 512], F32) for i in range(8)]

    def inp(self, name, shape, dtype=F32):
        self.I[name] = self.nc.dram_tensor(name, list(shape), dtype, kind="ExternalInput").ap()
        return self.I[name]

    def scratch(self, name, shape, dtype):
        return self.nc.dram_tensor(name, list(shape), dtype, kind="Internal").ap()

    def declare(self):
        n_a, n_b = 2, 2
        inp = self.inp
        inp("x", [SEQ, D]); inp("ctx", [CTX, D]); inp("cvec", [2, D])
        inp("ada_w", [4, D, 6 * D]); inp("ada_b", [4, 6 * D])
        inp("norm1_g", [4, D]); inp("norm2_g", [4, D]); inp("final_g", [D])
        inp("ml_w_in", [n_a, D, 4096]); inp("ml_conv_w", [n_a, 5, 2048]); inp("ml_conv_b", [n_a, 2048])
        inp("bdq", [n_a, 16, 128, 128]); inp("bdk", [n_a, 16, 128, 128]); inp("bdv", [n_a, 16, 128, 128])
        inp("bdqT", [n_a, 16, 128, 128]); inp("bdkT", [n_a, 16, 128, 128]); inp("bdvT", [n_a, 16, 128, 128])
        inp("gw", [n_a, 3, 16, 128, 16]); inp("gb", [n_a, 16])
        inp("ml_skip", [n_a, 2048]); inp("ml_hnorm_g", [n_a, 2048]); inp("ml_w_out", [n_a, 2048, D])
        inp("na_w_qkv", [n_b, D, 3 * D]); inp("na_b_qkv", [n_b, 3 * D]); inp("nab", [n_b, 16, 5, 128, 576])
        inp("na_w_out", [n_b, D, D]); inp("na_b_out", [n_b, D])
        inp("ff_w_up", [4, D, 2 * FF]); inp("ff_b_up", [4, 2 * FF]); inp("ff_conv_w", [4, 3, 2 * FF])
        inp("ff_conv_b", [4, 2 * FF]); inp("ff_w_down", [4, FF, D]); inp("ff_b_down", [4, D])
        inp("cmask", [2, 3, 128, 128]); inp("identf", [128, 128]); inp("halfm", [128, 2])
        self.out = self.nc.dram_tensor("out", [SEQ, D], F32, kind="ExternalOutput").ap()
        sc = self.scratch
        self.Xs = sc("Xs", [TOK, D], F32)
        self.modd = sc("modd", [4, 2, 6 * D], F32)
        self.d_u = sc("d_u", [2048, TOK], BF16)
        self.d_uc = sc("d_uc", [2048, TOK], BF16)
        self.d_sz = sc("d_sz", [2048, TOK], BF16)
        self.d_act = sc("d_act", [FF, TOK], BF16)

    def consts(self):
        P, I = self.P, self.I
        self.identf = P.sb("identf", [128, 128], F32)
        self.identb = P.sb("identb", [128, 128], BF16)
        self.cm = P.sb("cm", [128, 2, 3, 128], F32)
        self.halfm = P.sb("halfm", [128, 2], F32)
        self.onesf = P.sb("onesf", [128, 128], F32)
        self.onesb = P.sb("onesb", [128, 2], BF16)
        self.epsc = P.sb("epsc", [128, 1], F32)
        self.fsc = P.sb("fsc", [128, 16], F32)
        self.junk = P.sb("junk", [128, 1024], BF16)
        P.dma(self.identf[:], I["identf"], writes=["identf"])
        P.dma(self.cm[:], I["cmask"].rearrange("z m p c -> p z m c"), writes=["cm"])
        P.dma(self.halfm[:], I["halfm"], writes=["halfm"])
        P.dve(lambda e: e.tensor_copy(out=self.identb[:], in_=self.identf[:]), ["identf"], ["identb"])
        P.dve(lambda e: e.memset(self.onesf[:], 1.0), [], ["onesf"])
        P.dve(lambda e: e.memset(self.onesb[:], 1.0), [], ["onesb"])
        P.dve(lambda e: e.memset(self.epsc[:], EPS), [], ["epsc"])
        self.const_keys = ["identf", "identb", "cm", "halfm", "onesf", "onesb", "epsc"]

    def refence(self):
        self.P.fence(self.fsc[:, 0:1])

    def mods(self):
        P, I, PS = self.P, self.I, self.PS
        m0 = P.sb_off
        s2 = P.sb("s2", [128, 8, 2], F32)
        stg = [P.sb("adastg%d" % i, [128, 8, 512], F32) for i in range(2)]
        modrow = P.sb("modrow", [2, 6 * D], F32)
        adab = P.sb("adab", [2, 6 * D], F32)
        g1b = P.sb("g1b", [2, D], F32)
        g2b = P.sb("g2b", [2, D], F32)
        for jj in range(2):
            P.dma(s2[:, :, jj], I["cvec"][jj].rearrange("(k p) -> p k", p=128), writes=["s2"], allow_slow_non_contiguous=True)
        P.act(lambda e: e.activation(out=s2[:], in_=s2[:], func=AF.Silu), ["s2"], ["s2"])
        n = 0
        for i in range(self.depth):
            P.dma(adab[:], I["ada_b"][i].partition_broadcast(2), writes=["adab"])
            P.dma(g1b[:], I["norm1_g"][i].partition_broadcast(2), writes=["g1b"])
            P.dma(g2b[:], I["norm2_g"][i].partition_broadcast(2), writes=["g2b"])
            for cb in range(12):
                sl = n % 2
                bank = 6 + (n % 2)
                n += 1
                P.dma(stg[sl][:], I["ada_w"][i][:, cb * 512:(cb + 1) * 512].rearrange("(k p) c -> p k c", p=128),
                      writes=["adastg%d" % sl])
                P.mm(PS[bank][0:2, :], [(s2[:, k, :], stg[sl][:, k, :]) for k in range(8)],
                     ["s2", "adastg%d" % sl], ["ps%d" % bank])
                P.dve(lambda e, bank=bank, cb=cb: e.tensor_tensor(out=modrow[:, cb * 512:(cb + 1) * 512], in0=PS[bank][0:2, :],
                                                                  in1=adab[:, cb * 512:(cb + 1) * 512], op=ALU.add),
                      ["ps%d" % bank, "adab"], ["modrow"])
            P.dve(lambda e: e.scalar_tensor_tensor(out=modrow[:, D:2 * D], in0=modrow[:, D:2 * D], scalar=1.0, in1=g1b[:],
                                                   op0=ALU.add, op1=ALU.mult), ["modrow", "g1b"], ["modrow"])
            P.dve(lambda e: e.scalar_tensor_tensor(out=modrow[:, 4 * D:5 * D], in0=modrow[:, 4 * D:5 * D], scalar=1.0, in1=g2b[:],
                                                   op0=ALU.add, op1=ALU.mult), ["modrow", "g2b"], ["modrow"])
            P.dma(self.modd[i], modrow[:], reads=["modrow"], writes=["modd%d" % i])
        self.refence()
        P.sb_off = m0

    def load_cols(self, dst, src1d, key):
        self.P.dma(dst, src1d.rearrange("(k p) -> p k", p=128), writes=[key], allow_slow_non_contiguous=True)

    def persist(self):
        P = self.P
        self.colm = P.sb("colm", [128, 4, 2, 6, 8], F32)
        self.grow_off = P.sb_off
        self.grow = P.sb("grow", [128, 2, 2, D], F32)
        self.nst = [P.sb("nst%d" % s, [128, 8], F32) for s in range(2)]
        self.xn = [P.sb("xn%d" % s, [128, D], BF16) for s in range(2)]
        for i in range(self.depth):
            for v in range(2):
                for s in (0, 1, 3, 4):
                    P.dma(self.colm[:, i, v, s, :], self.modd[i, v, s * D:(s + 1) * D].rearrange("(k p) -> p k", p=128),
                          reads=["modd%d" % i], writes=["colm"], allow_slow_non_contiguous=True)
        self.hA = P.sb("hA", [128, 8, TOK], BF16)
        self.hA_off = P.sb_off - 8 * TOK * 2
        self.top = P.sb_off

    def load_grow(self, i):
        P = self.P
        for v in range(2):
            P.dma(self.grow[:, v, 0, :], self.modd[i, v, 2 * D:3 * D].partition_broadcast(128), reads=["modd%d" % i], writes=["grow"])
            P.dma(self.grow[:, v, 1, :], self.modd[i, v, 5 * D:6 * D].partition_broadcast(128), reads=["modd%d" % i], writes=["grow"])

    def rms_stats(self, xt, xkey, sl):
        P = self.P
        st = self.nst[sl]
        ks = "nst%d" % sl
        P.act(lambda e: e.activation(out=self.junk[:], in_=xt, func=AF.Square, accum_out=st[:, 0:1]), [xkey], ["junk", ks])
        P.act(lambda e: e.activation(out=st[:, 1:2], in_=st[:, 0:1], func=AF.Sqrt, scale=1.0 / D, bias=self.epsc[:, 0:1]), [ks], [ks])
        P.dve(lambda e: e.reciprocal(out=st[:, 2:3], in_=st[:, 1:2]), [ks], [ks])
        return st, ks

    def norm_tile(self, xt, xkey, tt, sl, li, which, hname, bank):
        P, PS = self.P, self.PS
        v = 1 if tt < 2 else 0
        xn = self.xn[sl]
        kx = "xn%d" % sl
        st, ks = self.rms_stats(xt, xkey, sl)
        P.dve(lambda e: e.tensor_scalar(out=xn[:], in0=xt, scalar1=st[:, 2:3], scalar2=None, op0=ALU.mult), [xkey, ks], [kx])
        pv = PS[bank][:].bitcast(BF16)

        def tr(e):
            ins = None
            for c in range(8):
                ins = e.transpose(pv[:, c * 128:(c + 1) * 128], xn[:, c * 128:(c + 1) * 128], self.identb[:])
            return ins

        P.pe(tr, [kx], ["ps%d" % bank])
        sa, sb_ = (1, 0) if which == 0 else (4, 3)
        hk = "%s%d" % (hname, tt)
        for c in range(8):
            A = self.colm[:, li, v, sa, c:c + 1]
            B = self.colm[:, li, v, sb_, c:c + 1]
            dst = self.hA[:, c, tt * 128:(tt + 1) * 128]
            src = pv[:, c * 128:(c + 1) * 128]
            if c % 2 == 0:
                P.act(lambda e, A=A, B=B, dst=dst, src=src: e.activation(out=dst, in_=src, func=AF.Identity, scale=A, bias=B),
                      ["ps%d" % bank, "colm"], [hk])
            else:
                P.dve(lambda e, A=A, B=B, dst=dst, src=src: e.tensor_scalar(out=dst, in0=src, scalar1=A, scalar2=B, op0=ALU.mult, op1=ALU.add),
                      ["ps%d" % bank, "colm"], [hk])

    def first_norm(self):
        P, I = self.P, self.I
        P.sb_off = self.top
        xt = [P.sb("fxt%d" % s, [128, D], F32) for s in range(2)]
        for tt in range(NT):
            sl = tt % 2
            src = I["ctx"][tt * 128:(tt + 1) * 128, :] if tt < 2 else I["x"][(tt - 2) * 128:(tt - 1) * 128, :]
            P.dma(xt[sl][:], src, writes=["fxt%d" % sl])
            P.dma(self.Xs[tt * 128:(tt + 1) * 128, :], xt[sl][:], reads=["fxt%d" % sl], writes=["X%d" % tt])
            self.norm_tile(xt[sl][:], "fxt%d" % sl, tt, sl, 0, 0, "h1_", 6 + sl)
        self.refence()

    def stager(self, name, nbuf, cols):
        P = self.P
        bufs = [P.sb("%s%d" % (name, i), [128, cols], F32) for i in range(nbuf)]
        state = dict(n=0)

        def load(dst, srcs, dkeys, eng="pool", shape=None):
            sl = state["n"] % nbuf
            state["n"] += 1
            key = "%s%d" % (name, sl)
            tot = 0
            for off, ncols, ap, pat in srcs:
                d = bufs[sl][:, off:off + ncols]
                if pat is not None:
                    d = d.rearrange(pat[0], **pat[1])
                P.dma(d, ap, writes=[key], q="pool")
                tot = max(tot, off + ncols)
            src = bufs[sl][:, 0:tot]
            if shape is not None:
                src = src.rearrange(shape[0], **shape[1])
            P.add(eng, lambda e: e.tensor_copy(out=dst, in_=src), [key], dkeys)

        return load

    def ffn(self, i, last):
        P, I, PS = self.P, self.I, self.PS
        hT = self.hA
        P.sb_off = self.top
        tiles = list(range(NT)) if not last else list(range(2, NT))
        blks = TBLK if not last else TBLK[1:]
        W = 2307
        L = W - 2
        load = self.stager("fstg", 2, 4096)
        bcol = P.sb("fbcol", [128, 5, 44], F32)
        wup = [P.sb("wup%d" % s, [128, 8, 2, 256], BF16) for s in range(2)]
        upad = [[P.sb("upad%d%d" % (s, g), [128, W], BF16) for g in range(2)] for s in range(2)]
        acc = [[P.sb("facc%d%d" % (s, g), [128, L], F32) for g in range(2)] for s in range(2)]
        actc = [P.sb("actc%d" % s, [128, L], BF16) for s in range(2)]
        self.load_cols(bcol[:, 0, :], I["ff_b_up"][i], "fbcol")
        for k in range(3):
            self.load_cols(bcol[:, 1 + k, :], I["ff_conv_w"][i, k], "fbcol")
        self.load_cols(bcol[:, 4, :], I["ff_conv_b"][i], "fbcol")
        for s in range(2):
            for g in range(2):
                P.pool(lambda e, s=s, g=g: e.memset(upad[s][g][:], 0.0), [], ["upad%d%d" % (s, g)])
        nb = 0
        for cg in range(11):
            ws = cg % 2
            load(wup[ws][:], [
                (0, 2048, I["ff_w_up"][i][:, cg * 256:(cg + 1) * 256].rearrange("(k p) c -> p k c", p=128),
                 ("p (k c) -> p k c", dict(k=8))),
                (2048, 2048, I["ff_w_up"][i][:, FF + cg * 256:FF + (cg + 1) * 256].rearrange("(k p) c -> p k c", p=128),
                 ("p (k c) -> p k c", dict(k=8))),
            ], ["wup%d" % ws], shape=("p (g k c) -> p k g c", dict(g=2, k=8)))
            for cc in range(2):
                c = cg * 2 + cc
                s = c % 2
                for g in range(2):
                    col = c + 22 * g
                    ku, ka = "upad%d%d" % (s, g), "facc%d%d" % (s, g)
                    u, a = upad[s][g], acc[s][g]
                    for (t0, n) in blks:
                        bank = nb % 4
                        nb += 1
                        P.mm(PS[bank][:, 0:n], [(wup[ws][:, k, g, cc * 128:(cc + 1) * 128], hT[:, k, t0:t0 + n]) for k in range(8)],
                             ["wup%d" % ws] + tkeys("h2_", t0, n), ["ps%d" % bank])
                        P.act(lambda e, bank=bank, n=n, u=u, t0=t0, col=col: e.activation(
                            out=u[:, pcol(t0):pcol(t0) + n], in_=PS[bank][:, 0:n], func=AF.Identity,
                            bias=bcol[:, 0, col:col + 1], scale=1.0), ["ps%d" % bank, "fbcol"], [ku])
                    P.pool(lambda e, u=u, a=a, col=col: e.tensor_scalar(out=a[:], in0=u[:, 1:1 + L], scalar1=bcol[:, 2, col:col + 1],
                                                                        scalar2=bcol[:, 4, col:col + 1], op0=ALU.mult, op1=ALU.add),
                           [ku, "fbcol"], [ka])
                    P.dve(lambda e, u=u, a=a, col=col: e.scalar_tensor_tensor(out=a[:], in0=u[:, 0:L], scalar=bcol[:, 1, col:col + 1],
                                                                               in1=a[:], op0=ALU.mult, op1=ALU.add), [ku, ka, "fbcol"], [ka])
                    P.dve(lambda e, u=u, a=a, col=col: e.scalar_tensor_tensor(out=a[:], in0=u[:, 2:2 + L], scalar=bcol[:, 3, col:col + 1],
                                                                              in1=a[:], op0=ALU.mult, op1=ALU.add), [ku, ka, "fbcol"], [ka])
                P.act(lambda e, s=s: e.activation(out=acc[s][1][:], in_=acc[s][1][:], func=AF.Silu), ["facc%d1" % s], ["facc%d1" % s])
                P.dve(lambda e, s=s: e.tensor_tensor(out=actc[s][:], in0=acc[s][0][:], in1=acc[s][1][:], op=ALU.mult),
                      ["facc%d0" % s, "facc%d1" % s], ["actc%d" % s])
                if not last:
                    P.dma(self.d_act[c * 128:(c + 1) * 128, 0:CTX], actc[s][:, 0:CTX], reads=["actc%d" % s], writes=["dact%d" % c])
                P.dma(self.d_act[c * 128:(c + 1) * 128, CTX:TOK], actc[s][:, CTX + 1:TOK + 1], reads=["actc%d" % s], writes=["dact%d" % c])
        self.refence()
        P.sb_off = self.top
        load = self.stager("dstg", 2, 1024)
        wdn = P.sb("wdn", [128, 22, D], BF16)
        bdn = P.sb("bdn", [128, D], F32)
        P.dma(bdn[:], I["ff_b_down"][i].partition_broadcast(128), writes=["bdn"])
        for c in range(22):
            load(wdn[:, c, :], [(0, D, I["ff_w_down"][i][c * 128:(c + 1) * 128, :], None)], ["wdn"])
        at = [P.sb("at%d" % s, [128, 22, 128], BF16) for s in range(2)]
        xt = [P.sb("xt%d" % s, [128, D], F32) for s in range(2)]
        tmp = [P.sb("ftmp%d" % s, [128, 512], F32) for s in range(2)]
        fg = None
        if last:
            fg = P.sb("fg", [128, D], F32)
            P.dma(fg[:], I["final_g"].partition_broadcast(128), writes=["fg"])
        for n_, tt in enumerate(tiles):
            sl = n_ % 2
            v = 1 if tt < 2 else 0
            P.dma(at[sl][:], self.d_act[:, tt * 128:(tt + 1) * 128].rearrange("(c p) t -> p c t", p=128), writes=["at%d" % sl])
            P.dma(xt[sl][:], self.Xs[tt * 128:(tt + 1) * 128, :], writes=["xt%d" % sl])
            for hf in range(2):
                bank = 2 * sl + hf
                P.mm(PS[bank][:, :], [(at[sl][:, c, :], wdn[:, c, hf * 512:(hf + 1) * 512]) for c in range(22)],
                     ["at%d" % sl, "wdn"], ["ps%d" % bank])
                tk = "ftmp%d" % hf
                P.dve(lambda e, bank=bank, hf=hf: e.tensor_tensor(out=tmp[hf][:], in0=PS[bank][:, :], in1=bdn[:, hf * 512:(hf + 1) * 512], op=ALU.add),
                      ["ps%d" % bank, "bdn"], [tk])
                P.pool(lambda e, hf=hf, v=v: e.tensor_tensor(out=tmp[hf][:], in0=tmp[hf][:], in1=self.grow[:, v, 1, hf * 512:(hf + 1) * 512], op=ALU.mult),
                       [tk, "grow"], [tk])
                P.pool(lambda e, hf=hf, sl=sl: e.tensor_tensor(out=xt[sl][:, hf * 512:(hf + 1) * 512], in0=xt[sl][:, hf * 512:(hf + 1) * 512], in1=tmp[hf][:], op=ALU.add),
                       [tk, "xt%d" % sl], ["xt%d" % sl])
            if last:
                st_, ks = self.rms_stats(xt[sl][:], "xt%d" % sl, sl)
                P.dve(lambda e, sl=sl, st_=st_: e.scalar_tensor_tensor(out=xt[sl][:], in0=xt[sl][:], scalar=st_[:, 2:3], in1=fg[:], op0=ALU.mult, op1=ALU.mult),
                      ["xt%d" % sl, ks, "fg"], ["xt%d" % sl])
                P.dma(self.out[(tt - 2) * 128:(tt - 1) * 128, :], xt[sl][:], reads=["xt%d" % sl], writes=["out%d" % tt])
            else:
                P.dma(self.Xs[tt * 128:(tt + 1) * 128, :], xt[sl][:], reads=["xt%d" % sl], writes=["X%d" % tt])
                self.norm_tile(xt[sl][:], "xt%d" % sl, tt, sl, i + 1, 0, "h1_", 6 + sl)
        self.refence()

    def outproj(self, i, nk, w_src, b_src, tiles):
        P, I, PS = self.P, self.I, self.PS
        P.sb_off = self.top
        self.load_grow(i)
        load = self.stager("ostg", 2, 1024)
        wo = P.sb("wo", [128, nk, D], BF16)
        at = [P.sb("oat%d" % s, [128, nk, 128], BF16) for s in range(2)]
        xt = [P.sb("oxt%d" % s, [128, D], F32) for s in range(2)]
        tmp = [P.sb("otmp%d" % s, [128, 512], F32) for s in range(2)]
        bo = None
        if b_src is not None:
            bo = P.sb("bo", [128, D], F32)
            P.dma(bo[:], b_src.partition_broadcast(128), writes=["bo"])
        for c in range(nk):
            load(wo[:, c, :], [(0, D, w_src[c * 128:(c + 1) * 128, :], None)], ["wo"])
        for n_, tt in enumerate(tiles):
            sl = n_ % 2
            v = 1 if tt < 2 else 0
            P.dma(at[sl][:], self.d_act[0:nk * 128, tt * 128:(tt + 1) * 128].rearrange("(c p) t -> p c t", p=128), writes=["oat%d" % sl])
            P.dma(xt[sl][:], self.Xs[tt * 128:(tt + 1) * 128, :], writes=["oxt%d" % sl])
            for hf in range(2):
                bank = 2 * sl + hf
                P.mm(PS[bank][:, :], [(at[sl][:, k, :], wo[:, k, hf * 512:(hf + 1) * 512]) for k in range(nk)], ["oat%d" % sl, "wo"], ["ps%d" % bank])
                tk = "otmp%d" % hf
                if bo is not None:
                    P.dve(lambda e, bank=bank, hf=hf: e.tensor_tensor(out=tmp[hf][:], in0=PS[bank][:, :], in1=bo[:, hf * 512:(hf + 1) * 512], op=ALU.add),
                          ["ps%d" % bank, "bo"], [tk])
                    P.pool(lambda e, hf=hf, v=v: e.tensor_tensor(out=tmp[hf][:], in0=tmp[hf][:], in1=self.grow[:, v, 0, hf * 512:(hf + 1) * 512], op=ALU.mult),
                           [tk, "grow"], [tk])
                else:
                    P.dve(lambda e, bank=bank, hf=hf, v=v: e.tensor_tensor(out=tmp[hf][:], in0=PS[bank][:, :], in1=self.grow[:, v, 0, hf * 512:(hf + 1) * 512], op=ALU.mult),
                          ["ps%d" % bank, "grow"], [tk])
                P.pool(lambda e, hf=hf, sl=sl: e.tensor_tensor(out=xt[sl][:, hf * 512:(hf + 1) * 512], in0=xt[sl][:, hf * 512:(hf + 1) * 512], in1=tmp[hf][:], op=ALU.add),
                       [tk, "oxt%d" % sl], ["oxt%d" % sl])
            P.dma(self.Xs[tt * 128:(tt + 1) * 128, :], xt[sl][:], reads=["oxt%d" % sl], writes=["X%d" % tt])
            self.norm_tile(xt[sl][:], "oxt%d" % sl, tt, sl, i, 1, "h2_", 6 + sl)
        self.refence()

    def mlstm(self, i, j):
        P, I, PS = self.P, self.I, self.PS
        hT = self.hA
        idf = self.identf
        P.sb_off = self.top
        gcol = P.sb("gcol", [128, NT, 16], F32)
        emc = P.sb("emc", [128, 8], F32)
        mcol = P.sb("mcol", [128, 2, 16], F32)
        bdb = P.sb("bdb", [128, 3, 16, 128], BF16)
        keepA = P.sb_off
        load = self.stager("mstg", 2, 4096)
        win = [P.sb("win%d" % s, [128, 8, 512], BF16) for s in range(2)]
        bdT_off = P.sb_off
        bdT = P.sb("bdT", [128, 3, 16, 128], F32)
        P.sb("bdTpad", [128, 1024], F32)
        gwt = P.sb("gwt", [128, 3, 16, 16], F32)
        wgf = P.sb("wgf", [128, 2, 16, 16], BF16)
        ccol = P.sb("ccol", [128, 6, 16], F32)
        gbc = P.sb("gbc", [40, 1], F32)
        W5 = 2310
        L5 = W5 - 4
        upad = [P.sb("mupad%d" % s, [128, W5], BF16) for s in range(2)]
        ucp = [P.sb("mucp%d" % s, [128, W5], BF16) for s in range(2)]
        macc = [P.sb("macc%d" % s, [128, L5], F32) for s in range(2)]
        szb = [P.sb("szb%d" % s, [128, 512], BF16) for s in range(3)]
        mx = P.sb("mx", [8, 12], F32)
        _save = P.sb_off
        P.sb_off = bdT_off
        gsb = P.sb("gsb", [40, TOK], F32)
        gt1 = P.sb("gt1", [40, TOK], F32)
        gt2 = P.sb("gt2", [40, TOK], F32)
        P.sb_off = max(_save, P.sb_off)

        self.load_cols(mcol[:, 0, :], I["ml_hnorm_g"][j], "mcol")
        self.load_cols(mcol[:, 1, :], I["ml_skip"][j], "mcol")
        for k in range(5):
            self.load_cols(ccol[:, k, :], I["ml_conv_w"][j, k], "ccol")
        self.load_cols(ccol[:, 5, :], I["ml_conv_b"][j], "ccol")
        P.dma(gbc[0:8, :], I["gb"][j, 0:8].rearrange("(p o) -> p o", o=1), writes=["gbc"])
        P.dma(gbc[32:40, :], I["gb"][j, 8:16].rearrange("(p o) -> p o", o=1), writes=["gbc"])
        for m, nm in enumerate(("bdq", "bdk", "bdv")):
            for half in range(2):
                load(bdb[:, m, half * 8:(half + 1) * 8, :], [(0, 1024, I[nm][j, half * 8:(half + 1) * 8].rearrange("c p o -> p c o"),
                                                            ("p (c o) -> p c o", dict(c=8)))], ["bdb"],
                     shape=("p (c o) -> p c o", dict(c=8)))
            P.dma(bdT[:, m, :, :], I[nm + "T"][j].rearrange("c p o -> p c o"), writes=["bdT"])
            P.dma(gwt[:, m, :, :], I["gw"][j, m].rearrange("c p o -> p c o"), writes=["gwt"])
        P.dve(lambda e: e.tensor_scalar(out=gwt[:, 1, :, :], in0=gwt[:, 1, :, :], scalar1=float(512 ** -0.5), scalar2=None, op0=ALU.mult), ["gwt"], ["gwt"])
        for c in range(16):
            P.mm(PS[5][:, c * 16:(c + 1) * 16], [(bdT[:, 0, c, :], gwt[:, 0, c, :]), (bdT[:, 1, c, :], gwt[:, 1, c, :])], ["bdT", "gwt"], ["ps5"])
            P.mm(PS[6][:, c * 16:(c + 1) * 16], [(bdT[:, 2, c, :], gwt[:, 2, c, :])], ["bdT", "gwt"], ["ps6"])
        P.dve(lambda e: e.tensor_copy(out=wgf[:, 0, :, :], in_=PS[5][:, 0:256].rearrange("p (c o) -> p c o", c=16)), ["ps5"], ["wgf"])
        P.dve(lambda e: e.tensor_copy(out=wgf[:, 1, :, :], in_=PS[6][:, 0:256].rearrange("p (c o) -> p c o", c=16)), ["ps6"], ["wgf"])
        for s in range(2):
            P.pool(lambda e, s=s: e.memset(upad[s][:], 0.0), [], ["mupad%d" % s])

        def p5(t):
            return 2 + t if t < CTX else 4 + t

        nb = 0
        nz = 0
        for og in range(8):
            ws = og % 2
            load(win[ws][:], [(0, 4096, I["ml_w_in"][j][:, og * 512:(og + 1) * 512].rearrange("(k p) c -> p k c", p=128),
                               ("p (k c) -> p k c", dict(k=8)))], ["win%d" % ws], shape=("p (k c) -> p k c", dict(k=8)))
            for oo in range(4):
                oc = og * 4 + oo
                s = oc % 2
                for (t0, n) in TBLK:
                    bank = 5 + nb % 3
                    nb += 1
                    P.mm(PS[bank][:, 0:n], [(win[ws][:, k, oo * 128:(oo + 1) * 128], hT[:, k, t0:t0 + n]) for k in range(8)],
                         ["win%d" % ws] + tkeys("h1_", t0, n), ["ps%d" % bank])
                    if oc < 16:
                        P.act(lambda e, bank=bank, n=n, s=s, t0=t0: e.copy(out=upad[s][:, p5(t0):p5(t0) + n], in_=PS[bank][:, 0:n]),
                              ["ps%d" % bank], ["mupad%d" % s])
                    else:
                        zs = nz % 3
                        nz += 1
                        P.act(lambda e, bank=bank, n=n, zs=zs: e.activation(out=szb[zs][:, 0:n], in_=PS[bank][:, 0:n], func=AF.Silu),
                              ["ps%d" % bank], ["szb%d" % zs])
                        P.dma(self.d_sz[(oc - 16) * 128:(oc - 15) * 128, t0:t0 + n], szb[zs][:, 0:n], reads=["szb%d" % zs], writes=["dsz%d" % (oc - 16)])
                if oc < 16:
                    u, a, uc = upad[s], macc[s], ucp[s]
                    ku, ka, kc = "mupad%d" % s, "macc%d" % s, "mucp%d" % s
                    P.pool(lambda e, u=u, a=a, oc=oc: e.tensor_scalar(out=a[:], in0=u[:, 0:L5], scalar1=ccol[:, 0, oc:oc + 1], scalar2=ccol[:, 5, oc:oc + 1],
                                                                      op0=ALU.mult, op1=ALU.add), [ku, "ccol"], [ka])
                    for k in range(1, 5):
                        eng = "dve"
                        P.add(eng, lambda e, u=u, a=a, oc=oc, k=k: e.scalar_tensor_tensor(out=a[:], in0=u[:, k:k + L5], scalar=ccol[:, k, oc:oc + 1], in1=a[:],
                                                                                        op0=ALU.mult, op1=ALU.add), [ku, ka, "ccol"], [ka])
                    P.act(lambda e, a=a, uc=uc: e.activation(out=uc[:, 2:2 + L5], in_=a[:], func=AF.Silu), [ka], [kc])
                    for b, (t0, n) in enumerate(TBLK):
                        def gm(e, b=b, t0=t0, n=n, oc=oc, u=u, uc=uc):
                            c0 = p5(t0)
                            e.matmul(PS[b][0:8, 0:n], lhsT=wgf[:, 0, oc, 0:8], rhs=uc[:, c0:c0 + n], start=(oc == 0), stop=False)
                            e.matmul(PS[b][0:8, 0:n], lhsT=wgf[:, 1, oc, 0:8], rhs=u[:, c0:c0 + n], start=False, stop=(oc == 15))
                            e.matmul(PS[b][32:40, 0:n], lhsT=wgf[:, 0, oc, 8:16], rhs=uc[:, c0:c0 + n], start=(oc == 0), stop=False)
                            return e.matmul(PS[b][32:40, 0:n], lhsT=wgf[:, 1, oc, 8:16], rhs=u[:, c0:c0 + n], start=False, stop=(oc == 15))
                        P.pe(gm, [ku, kc, "wgf"], ["ps%d" % b])
                    for (src, dst, kk) in ((u, self.d_u, "du"), (uc, self.d_uc, "duc")):
                        P.dma(dst[oc * 128:(oc + 1) * 128, 0:CTX], src[:, 2:2 + CTX], reads=[ku if src is u else kc], writes=["%s%d" % (kk, oc)])
                        P.dma(dst[oc * 128:(oc + 1) * 128, CTX:TOK], src[:, 4 + CTX:4 + TOK], reads=[ku if src is u else kc], writes=["%s%d" % (kk, oc)])
        for b, (t0, n) in enumerate(TBLK):
            P.act(lambda e, b=b, t0=t0, n=n: e.activation(out=gsb[0:40, t0:t0 + n], in_=PS[b][0:40, 0:n], func=AF.Identity, bias=gbc[0:40, 0:1], scale=1.0),
                  ["ps%d" % b, "gbc"], ["gsb"])
        R = slice(32, 40)
        P.dve(lambda e: e.tensor_scalar(out=gt2[R, :], in0=gsb[R, :], scalar1=-1.0, scalar2=None, op0=ALU.mult), ["gsb"], ["gt2"])
        P.dve(lambda e: e.tensor_tensor(out=gt1[R, :], in0=gsb[R, :], in1=gt2[R, :], op=ALU.max), ["gsb", "gt2"], ["gt1"])
        P.act(lambda e: e.activation(out=gt1[R, :], in_=gt1[R, :], func=AF.Exp, scale=-1.0), ["gt1"], ["gt1"])
        P.act(lambda e: e.activation(out=gt1[R, :], in_=gt1[R, :], func=AF.Ln, bias=self.onesf[R, 0:1], scale=1.0), ["gt1"], ["gt1"])
        P.dve(lambda e: e.tensor_scalar(out=gt2[R, :], in0=gt2[R, :], scalar1=0.0, scalar2=None, op0=ALU.max), ["gt2"], ["gt2"])
        P.dve(lambda e: e.tensor_tensor(out=gsb[R, :], in0=gt1[R, :], in1=gt2[R, :], op=ALU.add), ["gt1", "gt2", "gsb"], ["gsb"])
        P.dve(lambda e: e.tensor_reduce(out=mx[0:8, 0:1], in_=gsb[0:8, :], axis=AX.X, op=ALU.max), ["gsb"], ["mx"])
        P.dve(lambda e: e.tensor_scalar(out=gsb[0:8, :], in0=gsb[0:8, :], scalar1=mx[0:8, 0:1], scalar2=None, op0=ALU.subtract), ["gsb", "mx"], ["gsb"])
        P.dve(lambda e: e.tensor_scalar(out=mx[0:8, 4:12], in0=idf[0:8, 0:8], scalar1=mx[0:8, 0:1], scalar2=None, op0=ALU.mult), ["mx"], ["mx"])
        P.mm(PS[5][:, 0:8], [(self.onesf[0:8, :], mx[0:8, 4:12])], ["mx"], ["ps5"])
        P.act(lambda e: e.activation(out=emc[:], in_=PS[5][:, 0:8], func=AF.Exp, scale=-1.0), ["ps5"], ["emc"])

        def gtr(e):
            ins = None
            for tt in range(NT):
                e.transpose(PS[6][:, tt * 16:tt * 16 + 8], gsb[0:8, tt * 128:(tt + 1) * 128], idf[0:8, 0:8])
                ins = e.transpose(PS[6][:, tt * 16 + 8:tt * 16 + 16], gsb[32:40, tt * 128:(tt + 1) * 128], idf[32:40, 32:40])
            return ins

        P.pe(gtr, ["gsb"], ["ps6"])
        P.dve(lambda e: e.tensor_copy(out=gcol[:].rearrange("p t g -> p (t g)"), in_=PS[6][:, 0:NT * 16]), ["ps6"], ["gcol"])
        self.refence()

        cm = self.cm
        order = {0: [0, 1] + list(range(2, NT)), 1: [1, 0] + list(range(NT - 1, 1, -1))}
        for hd in range(4):
            P.sb_off = self.hA_off
            H = P.sb("H", [128, NT, 512], BF16)
            C32 = [P.sb("C32_%d" % z, [128, 4, 512], F32) for z in range(2)]
            assert P.sb_off <= self.top
            P.sb_off = keepA
            qT = P.sb("qT", [128, 4, TOK], BF16)
            kT = P.sb("kT", [128, 4, TOK], BF16)
            ktm = P.sb("ktm", [128, NT, 512], BF16)
            vtm = P.sb("vtm", [128, NT, 512], BF16)
            Cbf = [P.sb("Cbf%d" % z, [128, 4, 512], BF16) for z in range(2)]
            nst = P.sb("nstate", [128, 2, 4], F32)
            nbf = P.sb("nbf", [128, 2, 4], BF16)
            keepB = P.sb_off
            ucT = P.sb("ucT", [128, 4, TOK], BF16)
            uT = P.sb("uT", [128, 4, TOK], BF16)
            P.dma(ucT[:], self.d_uc[hd * 512:(hd + 1) * 512, :].rearrange("(c p) t -> p c t", p=128), writes=["ucT"])
            P.dma(uT[:], self.d_u[hd * 512:(hd + 1) * 512, :].rearrange("(c p) t -> p c t", p=128), writes=["uT"])
            nb = 0
            sk = float(512 ** -0.5)
            for c in range(4):
                cc = hd * 4 + c
                for (t0, n) in TBLK:
                    for m, dst in ((0, qT), (1, kT)):
                        bank = nb % 4
                        nb += 1
                        P.mm(PS[bank][:, 0:n], [(bdb[:, m, cc, :], ucT[:, c, t0:t0 + n])], ["ucT"], ["ps%d" % bank])
                        if m == 0:
                            P.act(lambda e, bank=bank, n=n, dst=dst, c=c, t0=t0: e.copy(out=dst[:, c, t0:t0 + n], in_=PS[bank][:, 0:n]), ["ps%d" % bank], ["qT"])
                        else:
                            P.dve(lambda e, bank=bank, n=n, dst=dst, c=c, t0=t0: e.tensor_scalar(out=dst[:, c, t0:t0 + n], in0=PS[bank][:, 0:n], scalar1=sk, scalar2=None, op0=ALU.mult),
                                  ["ps%d" % bank], ["kT"])
            for tt in range(NT):
                tk = slice(tt * 128, (tt + 1) * 128)
                for m, src, dst in ((1, ucT, ktm), (2, uT, vtm)):
                    bank = 4 + nb % 4
                    nb += 1

                    def tm(e, bank=bank, m=m, src=src, tk=tk, hd=hd):
                        ins = None
                        for c in range(4):
                            ins = e.matmul(PS[bank][:, c * 128:(c + 1) * 128], lhsT=src[:, c, tk], rhs=bdb[:, m, hd * 4 + c, :], start=True, stop=True)
                        return ins

                    P.pe(tm, ["ucT", "uT"], ["ps%d" % bank])
                    if m == 1:
                        P.act(lambda e, bank=bank, tt=tt: e.activation(out=ktm[:, tt, :], in_=PS[bank][:, :], func=AF.Copy, scale=sk), ["ps%d" % bank], ["ktm%d" % tt])
                    else:
                        P.dve(lambda e, bank=bank, tt=tt: e.tensor_copy(out=vtm[:, tt, :], in_=PS[bank][:, :]), ["ps%d" % bank], ["vtm%d" % tt])
            for z in range(2):
                P.pool(lambda e, z=z: e.memset(C32[z][:], 0.0), [], ["C32_%d" % z])
                P.pool(lambda e, z=z: e.memset(Cbf[z][:], 0.0), [], ["Cbf%d" % z])
            P.pool(lambda e: e.memset(nst[:], 0.0), [], ["nstate0", "nstate1"])
            P.pool(lambda e: e.memset(nbf[:], 0.0), [], ["nbf0", "nbf1"])
            self.refence()
            P.sb_off = keepB
            DT = P.sb("DT", [128, 2, NT, 128], F32)
            LFm = P.sb("LFm", [128, NT, 128], F32)
            T2 = P.sb("T2", [128, NT, 128], F32)
            eb = P.sb("eb", [128, 2, NT], F32)
            wkh = P.sb("wkh", [128, 2, 2, NT], F32)
            wkh16 = P.sb("wkh16", [128, 2, 2, NT], BF16)
            dec = P.sb("dec", [128, 2, NT, 2], F32)
            r2 = P.sb("r2", [128, NT, 2], F32)
            tc_ = P.sb("tcol", [128, NT], F32)
            wkc = P.sb("wkc", [128, NT], F32)
            PTt = [P.sb("PT%d" % s, [128, 128], BF16) for s in range(2)]
            vw = [P.sb("vw%d" % s, [128, 512], BF16) for s in range(2)]
            _sv = P.sb_off
            P.sb_off = self.grow_off
            tB = [P.sb("tB%d" % s, [128, 512], F32) for s in range(2)]
            hs = [P.sb("hs%d" % s, [128, 512], F32) for s in range(2)]
            hn = [P.sb("hn%d" % s, [128, 512], BF16) for s in range(2)]
            oa = [P.sb("oa%d" % s, [128, 4, 128], F32) for s in range(2)]
            assert P.sb_off <= self.grow_off + 16384
            P.sb_off = _sv
            dn = [P.sb("dn%d" % s, [128, 8], F32) for s in range(2)]
            ost = [P.sb("ost%d" % s, [128, 8], F32) for s in range(2)]
            uct = [P.sb("uct%d" % s, [128, 4, 128], BF16) for s in range(2)]
            szt = [P.sb("szt%d" % s, [128, 4, 128], BF16) for s in range(2)]
            actt = [P.sb("actt%d" % s, [128, 4, 128], BF16) for s in range(2)]
            for z in range(2):
                lic = gcol[:, :, z * 4 + hd]
                nlfc = gcol[:, :, 8 + z * 4 + hd]
                P.dve(lambda e, z=z, nlfc=nlfc: e.tensor_tensor(out=LFm[:], in0=cm[:, z, 1, :].unsqueeze(1).to_broadcast([128, NT, 128]),
                                                                in1=nlfc.unsqueeze(2).to_broadcast([128, NT, 128]), op=ALU.mult), ["gcol"], ["LFm"])
                P.pool(lambda e, lic=lic: e.tensor_tensor(out=T2[:], in0=idf[:].unsqueeze(1).to_broadcast([128, NT, 128]),
                                                          in1=lic.unsqueeze(2).to_broadcast([128, NT, 128]), op=ALU.mult), ["gcol"], ["T2"])
                P.dve(lambda e: e.tensor_tensor(out=LFm[:], in0=LFm[:], in1=T2[:], op=ALU.add), ["LFm", "T2"], ["LFm"])
                for g0 in range(0, NT, 4):
                    bank = (g0 // 4) % 2
                    ng = min(4, NT - g0)

                    def dm(e, g0=g0, ng=ng, bank=bank, z=z):
                        ins = None
                        for q in range(ng):
                            e.matmul(PS[bank][:, q * 128:(q + 1) * 128], lhsT=LFm[:, g0 + q, :], rhs=cm[:, z, 0, :], start=True, stop=False)
                            ins = e.matmul(PS[bank][:, q * 128:(q + 1) * 128], lhsT=idf[:], rhs=cm[:, z, 2, :], start=False, stop=True)
                        return ins

                    P.pe(dm, ["LFm"], ["ps%d" % bank])
                    P.act(lambda e, g0=g0, ng=ng, bank=bank, z=z: e.activation(out=DT[:, z, g0:g0 + ng, :].rearrange("p t s -> p (t s)"), in_=PS[bank][:, 0:ng * 128], func=AF.Exp),
                          ["ps%d" % bank], ["DT%d" % z])
                P.mm(PS[2][:, 0:NT], [(cm[:, z, 0, :], nlfc)], ["gcol"], ["ps2"])
                P.act(lambda e, z=z: e.activation(out=eb[:, z, :], in_=PS[2][:, 0:NT], func=AF.Exp, scale=-1.0), ["ps2"], ["eb%d" % z])
                P.mm(PS[3][:, 0:NT], [(cm[:, z, 1, :], nlfc)], ["gcol"], ["ps3"])
                P.dve(lambda e, lic=lic: e.tensor_tensor(out=tc_[:], in0=PS[3][:, 0:NT], in1=lic, op=ALU.add), ["ps3", "gcol"], ["tcol"])
                P.act(lambda e: e.activation(out=wkc[:], in_=tc_[:], func=AF.Exp), ["tcol"], ["wkc"])
                for hf in range(2):
                    P.dve(lambda e, z=z, hf=hf: e.tensor_scalar(out=wkh[:, z, hf, :], in0=wkc[:], scalar1=self.halfm[:, hf:hf + 1], scalar2=None, op0=ALU.mult),
                          ["wkc"], ["wkh%d" % z])
                P.dve(lambda e, z=z: e.tensor_copy(out=wkh16[:, z, :, :], in_=wkh[:, z, :, :]), ["wkh%d" % z], ["wkh16_%d" % z])
                P.dve(lambda e, nlfc=nlfc: e.tensor_tensor(out=r2[:], in0=nlfc.unsqueeze(2).to_broadcast([128, NT, 2]),
                                                           in1=self.halfm[:].unsqueeze(1).to_broadcast([128, NT, 2]), op=ALU.mult), ["gcol"], ["r2"])
                P.mm(PS[2][:, 64:64 + 2 * NT], [(self.onesf[:], r2[:].rearrange("p t h -> p (t h)"))], ["r2"], ["ps2"])
                P.act(lambda e, z=z: e.activation(out=dec[:, z, :, :].rearrange("p t h -> p (t h)"), in_=PS[2][:, 64:64 + 2 * NT], func=AF.Exp, scale=-1.0),
                      ["ps2"], ["dec%d" % z])
            self.refence()
            visited = set()
            nC = 0
            for step in range(NT):
                for z in range(2):
                    tt = order[z][step]
                    tk = slice(tt * 128, (tt + 1) * 128)
                    sl = (2 * step + z) % 2
                    kz = str(z)
                    bS, bA, bD = 0, 1, 6 + z
                    P.mm(PS[bS][:, 0:128], [(kT[:, c, tk], qT[:, c, tk]) for c in range(4)], ["kT", "qT"], ["psS"])
                    P.dve(lambda e, sl=sl, z=z, tt=tt: e.tensor_tensor(out=PTt[sl][:], in0=PS[0][:, 0:128], in1=DT[:, z, tt, :], op=ALU.mult),
                          ["psS", "DT%d" % z], ["PT%d" % sl])
                    P.mm(PS[bA][:, :], [(PTt[sl][:], vtm[:, tt, :])], ["PT%d" % sl, "vtm%d" % tt], ["psA"])
                    P.mm(PS[bD][:, 0:1], [(PTt[sl][:], self.onesb[:, 0:1])], ["PT%d" % sl], ["psD%d_0" % z])
                    halves = (0, 1) if z == 0 else (1, 0)
                    for hi, hf in enumerate(halves):
                        bB = 2 + hi
                        P.mm(PS[bB][:, :], [(qT[:, c, tk], Cbf[z][:, c, :]) for c in range(4)], ["qT", "Cbf" + kz], ["psB%d" % hi])
                        P.mm(PS[bD][:, 1 + hi:2 + hi], [(qT[:, c, tk], nbf[:, z, c:c + 1]) for c in range(4)], ["qT", "nbf" + kz], ["psD%d_%d" % (z, 1 + hi)])
                        P.dve(lambda e, sl=sl, z=z, hf=hf, tt=tt, hi=hi: e.tensor_scalar(out=vw[hi][:], in0=vtm[:, tt, :], scalar1=wkh[:, z, hf, tt:tt + 1], scalar2=None, op0=ALU.mult),
                              ["vtm%d" % tt, "wkh" + kz], ["vw%d" % hi])
                        dcol = dec[:, z, tt, hf:hf + 1]
                        for c in range(4):
                            bC = 4 + nC % 2
                            nC += 1
                            P.mm(PS[bC][:, :], [(ktm[:, tt, c * 128:(c + 1) * 128], vw[hi][:])], ["ktm%d" % tt, "vw%d" % hi], ["psC%d" % (bC - 4)])
                            P.dve(lambda e, z=z, c=c, bC=bC, dcol=dcol: e.scalar_tensor_tensor(out=C32[z][:, c, :], in0=C32[z][:, c, :], scalar=dcol, in1=PS[bC][:, :],
                                                                                           op0=ALU.mult, op1=ALU.add), ["psC%d" % (bC - 4), "dec" + kz, "C32_%s_%d" % (kz, c)], ["C32_%s_%d" % (kz, c)])
                            P.act(lambda e, z=z, c=c: e.copy(out=Cbf[z][:, c, :], in_=C32[z][:, c, :]), ["C32_%s_%d" % (kz, c)], ["Cbf" + kz])

                        def nm(e, z=z, tt=tt, hf=hf, bD=bD, hi=hi):
                            ins = None
                            for c in range(4):
                                ins = e.matmul(PS[bD][:, 8 + hi * 4 + c:9 + hi * 4 + c], lhsT=ktm[:, tt, c * 128:(c + 1) * 128], rhs=wkh16[:, z, hf, tt:tt + 1], start=True, stop=True)
                            return ins

                        P.pe(nm, ["ktm%d" % tt, "wkh16_" + kz], ["psD%d_n%d" % (z, hi)])
                        P.dve(lambda e, z=z, hi=hi, bD=bD, dcol=dcol: e.scalar_tensor_tensor(out=nst[:, z, :], in0=nst[:, z, :], scalar=dcol, in1=PS[bD][:, 8 + hi * 4:12 + hi * 4],
                                                                                          op0=ALU.mult, op1=ALU.add), ["psD%d_n%d" % (z, hi), "dec" + kz, "nstate" + kz], ["nstate" + kz])
                        P.act(lambda e, z=z: e.copy(out=nbf[:, z, :], in_=nst[:, z, :]), ["nstate" + kz], ["nbf" + kz])
                    ebc = eb[:, z, tt:tt + 1]
                    d = dn[sl]
                    kd = "dn%d" % sl
                    for hi, hf in enumerate(halves):
                        rows = slice(hf * 64, hf * 64 + 64)
                        P.act(lambda e, sl=sl, hi=hi, rows=rows, z=z, tt=tt: e.activation(out=tB[sl][rows, :], in_=PS[2 + hi][rows, :], func=AF.Copy, scale=eb[rows, z, tt:tt + 1]),
                              ["psB%d" % hi, "eb" + kz], ["tB%d" % sl])
                        P.dve(lambda e, d=d, rows=rows, bD=bD, hi=hi: e.tensor_copy(out=d[rows, 0:1], in_=PS[bD][rows, 1 + hi:2 + hi]), ["psD%d_%d" % (z, 1 + hi)], [kd])
                    P.dve(lambda e, sl=sl: e.tensor_tensor(out=tB[sl][:], in0=tB[sl][:], in1=PS[1][:, :], op=ALU.add), ["tB%d" % sl, "psA"], ["tB%d" % sl])
                    P.dve(lambda e, d=d, bD=bD, ebc=ebc: e.scalar_tensor_tensor(out=d[:, 1:2], in0=d[:, 0:1], scalar=ebc, in1=PS[bD][:, 0:1], op0=ALU.mult, op1=ALU.add),
                          [kd, "eb" + kz, "psD%d_0" % z], [kd])
                    P.dve(lambda e, d=d: e.tensor_scalar(out=d[:, 5:6], in0=d[:, 1:2], scalar1=-1.0, scalar2=None, op0=ALU.mult), [kd], [kd])
                    P.dve(lambda e, d=d: e.tensor_tensor(out=d[:, 2:3], in0=d[:, 1:2], in1=d[:, 5:6], op=ALU.max), [kd], [kd])
                    P.dve(lambda e, d=d, z=z, hd=hd: e.tensor_tensor(out=d[:, 3:4], in0=d[:, 2:3], in1=emc[:, z * 4 + hd:z * 4 + hd + 1], op=ALU.max), [kd], [kd])
                    P.dve(lambda e, d=d: e.reciprocal(out=d[:, 4:5], in_=d[:, 3:4]), [kd], [kd])
                    if tt not in visited:
                        visited.add(tt)
                        P.act(lambda e, sl=sl, d=d, tt=tt: e.activation(out=H[:, tt, :], in_=tB[sl][:], func=AF.Copy, scale=d[:, 4:5]), ["tB%d" % sl, kd], ["H%d" % tt])
                        continue
                    osl = tt % 2
                    P.dve(lambda e, sl=sl, d=d, tt=tt, osl=osl: e.scalar_tensor_tensor(out=hs[osl][:], in0=tB[sl][:], scalar=d[:, 4:5], in1=H[:, tt, :], op0=ALU.mult, op1=ALU.add),
                          ["tB%d" % sl, kd, "H%d" % tt], ["hs%d" % osl])
                    o = ost[osl]
                    ko = "ost%d" % osl
                    P.dve(lambda e, o=o, osl=osl: e.tensor_reduce(out=o[:, 0:1], in_=hs[osl][:], axis=AX.X, op=ALU.add), ["hs%d" % osl], [ko])
                    P.act(lambda e, o=o, osl=osl: e.activation(out=self.junk[:, 0:512], in_=hs[osl][:], func=AF.Square, accum_out=o[:, 1:2]), ["hs%d" % osl], ["junk", ko])
                    P.dve(lambda e, o=o: e.tensor_scalar(out=o[:, 2:3], in0=o[:, 0:1], scalar1=1.0 / 512, scalar2=None, op0=ALU.mult), [ko], [ko])
                    P.dve(lambda e, o=o: e.tensor_tensor(out=o[:, 3:4], in0=o[:, 2:3], in1=o[:, 2:3], op=ALU.mult), [ko], [ko])
                    P.dve(lambda e, o=o: e.scalar_tensor_tensor(out=o[:, 4:5], in0=o[:, 1:2], scalar=1.0 / 512, in1=o[:, 3:4], op0=ALU.mult, op1=ALU.subtract), [ko], [ko])
                    P.act(lambda e, o=o: e.activation(out=o[:, 5:6], in_=o[:, 4:5], func=AF.Sqrt, bias=self.epsc[:, 0:1], scale=1.0), [ko], [ko])
                    P.dve(lambda e, o=o: e.reciprocal(out=o[:, 6:7], in_=o[:, 5:6]), [ko], [ko])
                    P.dve(lambda e, o=o: e.scalar_tensor_tensor(out=o[:, 7:8], in0=o[:, 2:3], scalar=-1.0, in1=o[:, 6:7], op0=ALU.mult, op1=ALU.mult), [ko], [ko])
                    P.act(lambda e, o=o, osl=osl: e.activation(out=hn[osl][:], in_=hs[osl][:], func=AF.Identity, scale=o[:, 6:7], bias=o[:, 7:8]), ["hs%d" % osl, ko], ["hn%d" % osl])
                    pv = PS[0][:].bitcast(BF16)

                    def otr(e, osl=osl, pv=pv):
                        ins = None
                        for c in range(4):
                            ins = e.transpose(pv[:, 512 + c * 128:512 + (c + 1) * 128], hn[osl][:, c * 128:(c + 1) * 128], self.identb[:])
                        return ins

                    P.pe(otr, ["hn%d" % osl], ["psS"])
                    P.dma(uct[osl][:], self.d_uc[hd * 512:(hd + 1) * 512, tk].rearrange("(c p) t -> p c t", p=128), writes=["uct%d" % osl])
                    P.dma(szt[osl][:], self.d_sz[hd * 512:(hd + 1) * 512, tk].rearrange("(c p) t -> p c t", p=128), writes=["szt%d" % osl])
                    gb_ = mcol[:, 0, hd * 4:(hd + 1) * 4].unsqueeze(2).to_broadcast([128, 4, 128])
                    sb_ = mcol[:, 1, hd * 4:(hd + 1) * 4].unsqueeze(2).to_broadcast([128, 4, 128])
                    P.pool(lambda e, osl=osl, sb_=sb_: e.tensor_tensor(out=oa[osl][:], in0=uct[osl][:], in1=sb_, op=ALU.mult), ["uct%d" % osl, "mcol"], ["oa%d" % osl])
                    P.dve(lambda e, osl=osl, gb_=gb_, pv=pv: e.tensor_tensor(out=actt[osl][:], in0=pv[:, 512:1024].rearrange("p (c t) -> p c t", c=4), in1=gb_, op=ALU.mult),
                          ["psS", "mcol"], ["actt%d" % osl])
                    P.pool(lambda e, osl=osl: e.tensor_tensor(out=oa[osl][:], in0=oa[osl][:], in1=actt[osl][:], op=ALU.add), ["oa%d" % osl, "actt%d" % osl], ["oa%d" % osl])
                    P.pool(lambda e, osl=osl: e.tensor_tensor(out=actt[osl][:], in0=oa[osl][:], in1=szt[osl][:], op=ALU.mult), ["oa%d" % osl, "szt%d" % osl], ["actt%d" % osl])
                    P.dma(self.d_act[hd * 512:(hd + 1) * 512, tk].rearrange("(c p) t -> p c t", p=128), actt[osl][:], reads=["actt%d" % osl], writes=["dact_h%d_%d" % (hd, tt)])
            self.refence()
        self.outproj(i, 16, I["ml_w_out"][j], None, list(range(NT)))

    def natten(self, i, j, last):
        P, I, PS = self.P, self.I, self.PS
        hT = self.hA
        P.sb_off = self.top
        load = self.stager("nstg", 2, 3072)
        wq = [P.sb("nwq%d" % s, [128, 8, 3, 128], BF16) for s in range(2)]
        bcol = P.sb("nbcol", [128, 2, 8], F32)
        bvr = P.sb("nbvr", [128, D], F32)
        qc = [P.sb("nqc%d" % s, [128, TOK], BF16) for s in range(2)]
        kc = [P.sb("nkc%d" % s, [128, TOK], BF16) for s in range(2)]
        vc = [P.sb("nvc%d" % s, [128, NT, 128], BF16) for s in range(2)]
        oc_ = [P.sb("noc%d" % s, [128, TOK], BF16) for s in range(2)]
        nabt = [P.sb("nabt%d" % s, [128, 2, 5, 576], F32) for s in range(2)]
        S = [P.sb("nS%d" % s, [128, 832], F32) for s in range(2)]
        Pm = [P.sb("nPm%d" % s, [128, 832], BF16) for s in range(2)]
        PT = [P.sb("nPT%d" % s, [128, 7, 128], BF16) for s in range(2)]
        otm = [P.sb("notm%d" % s, [128, 128], BF16) for s in range(2)]
        sst = [P.sb("nsst%d" % s, [128, 4], F32) for s in range(2)]
        self.load_cols(bcol[:, 0, :], I["na_b_qkv"][j, 0:D], "nbcol")
        self.load_cols(bcol[:, 1, :], I["na_b_qkv"][j, D:2 * D], "nbcol")
        P.dve(lambda e: e.tensor_scalar(out=bcol[:, 0, :], in0=bcol[:, 0, :], scalar1=0.125, scalar2=None, op0=ALU.mult), ["nbcol"], ["nbcol"])
        P.dma(bvr[:], I["na_b_qkv"][j, 2 * D:3 * D].partition_broadcast(128), writes=["nbvr"])
        Wq = I["na_w_qkv"][j]
        nb = 0
        nu = 0
        qtiles = list(range(NT)) if not last else list(range(2, NT))
        for c in range(8):
            s = c % 2
            load(wq[s][:], [(m * 1024, 1024, Wq[:, m * D + c * 128:m * D + (c + 1) * 128].rearrange("(k p) o -> p k o", p=128),
                             ("p (k o) -> p k o", dict(k=8))) for m in range(3)], ["nwq%d" % s],
                 shape=("p (m k o) -> p k m o", dict(m=3, k=8)))
            P.dma(nabt[s][:], I["nab"][j, 2 * c:2 * c + 2].rearrange("h r q n -> q h r n"), writes=["nabt%d" % s])
            for (t0, n) in TBLK:
                for m, dst, kk in ((0, qc[s], "nqc%d" % s), (1, kc[s], "nkc%d" % s)):
                    bank = nb % 4
                    nb += 1
                    P.mm(PS[bank][:, 0:n], [(wq[s][:, k, m, :], hT[:, k, t0:t0 + n]) for k in range(8)], ["nwq%d" % s] + tkeys("h1_", t0, n), ["ps%d" % bank])
                    P.act(lambda e, bank=bank, n=n, dst=dst, t0=t0, m=m, c=c: e.activation(out=dst[:, t0:t0 + n], in_=PS[bank][:, 0:n], func=AF.Identity,
                                                                                       bias=bcol[:, m, c:c + 1], scale=(0.125 if m == 0 else 1.0)),
                          ["ps%d" % bank, "nbcol"], [kk])
            for g0 in range(0, NT, 4):
                ng = min(4, NT - g0)
                bank = nb % 4
                nb += 1

                def vm(e, g0=g0, ng=ng, bank=bank, s=s):
                    ins = None
                    for q in range(ng):
                        tk = slice((g0 + q) * 128, (g0 + q + 1) * 128)
                        for k in range(8):
                            ins = e.matmul(PS[bank][:, q * 128:(q + 1) * 128], lhsT=hT[:, k, tk], rhs=wq[s][:, k, 2, :], start=(k == 0), stop=(k == 7))
                    return ins

                P.pe(vm, ["nwq%d" % s] + tkeys("h1_", g0 * 128, ng * 128), ["ps%d" % bank])
                P.dve(lambda e, g0=g0, ng=ng, bank=bank, s=s, c=c: e.tensor_tensor(out=vc[s][:, g0:g0 + ng, :], in0=PS[bank][:, 0:ng * 128].rearrange("p (t o) -> p t o", t=ng),
                                                                                  in1=bvr[:, c * 128:(c + 1) * 128].unsqueeze(1).to_broadcast([128, ng, 128]), op=ALU.add),
                      ["ps%d" % bank, "nbvr"], ["nvc%d" % s])
            for tt in qtiles:
                tq = slice(tt * 128, (tt + 1) * 128)
                if tt >= 2:
                    r = 2 * (tt - 2)
                    if r <= 2:
                        rs0, nkr, cls = 0, 8, r // 2
                    elif r >= 28:
                        rs0, nkr, cls = 24, 8, 3 + (r - 28) // 2
                    else:
                        rs0, nkr, cls = r - 4, 9, 2
                    nl = nkr * 64
                    ks = CTX + rs0 * 64
                else:
                    nl, ks, cls = 0, 0, 0
                ntot = nl + CTX
                osl = (nu // 2) % 2
                for hh in range(2):
                    pb = slice(hh * 64, hh * 64 + 64)
                    u = nu % 2
                    ub = 4 + 2 * (nu % 2)
                    nu += 1
                    kS, kP, kPT, kst = "nS%d" % u, "nPm%d" % u, "nPT%d" % u, "nsst%d" % u
                    q_ap = qc[s][pb, tq]
                    if nl > 0:
                        P.mm(PS[ub][:, 0:512], [(q_ap, kc[s][pb, ks:ks + 512])], ["nqc%d" % s, "nkc%d" % s], ["ps%d" % ub])
                        P.dve(lambda e, u=u, ub=ub, hh=hh, cls=cls, s=s: e.tensor_tensor(out=S[u][:, 0:512], in0=PS[ub][:, 0:512], in1=nabt[s][:, hh, cls, 0:512], op=ALU.add),
                              ["ps%d" % ub, "nabt%d" % s], [kS])

                    def sc2(e, q_ap=q_ap, ub=ub, nl=nl, ks=ks, s=s, pb=pb):
                        if nl > 512:
                            e.matmul(PS[ub + 1][:, 0:64], lhsT=q_ap, rhs=kc[s][pb, ks + 512:ks + 576], start=True, stop=True)
                        return e.matmul(PS[ub + 1][:, 64:320], lhsT=q_ap, rhs=kc[s][pb, 0:CTX], start=True, stop=True)

                    P.pe(sc2, ["nqc%d" % s, "nkc%d" % s], ["ps%d" % (ub + 1)])
                    if nl > 512:
                        P.dve(lambda e, u=u, ub=ub, hh=hh, cls=cls, s=s: e.tensor_tensor(out=S[u][:, 512:576], in0=PS[ub + 1][:, 0:64], in1=nabt[s][:, hh, cls, 512:576], op=ALU.add),
                              ["ps%d" % (ub + 1), "nabt%d" % s], [kS])
                    P.act(lambda e, u=u, ub=ub, nl=nl: e.copy(out=S[u][:, nl:nl + CTX], in_=PS[ub + 1][:, 64:320]), ["ps%d" % (ub + 1)], [kS])
                    st = sst[u]
                    P.dve(lambda e, u=u, st=st, ntot=ntot: e.tensor_reduce(out=st[:, 0:1], in_=S[u][:, 0:ntot], axis=AX.X, op=ALU.max), [kS], [kst])
                    P.dve(lambda e, st=st: e.tensor_scalar(out=st[:, 1:2], in0=st[:, 0:1], scalar1=-1.0, scalar2=None, op0=ALU.mult), [kst], [kst])
                    P.act(lambda e, u=u, st=st, ntot=ntot: e.activation(out=Pm[u][:, 0:ntot], in_=S[u][:, 0:ntot], func=AF.Exp, bias=st[:, 1:2], scale=1.0, accum_out=st[:, 2:3]),
                          [kS, kst], [kP, kst])
                    P.dve(lambda e, st=st: e.reciprocal(out=st[:, 3:4], in_=st[:, 2:3]), [kst], [kst])
                    chunks = []
                    off = 0
                    while off < nl:
                        kn = min(128, nl - off)
                        chunks.append((off, kn, vc[s][0:kn, (ks + off) // 128, hh * 64:(hh + 1) * 64]))
                        off += kn
                    for q in range(2):
                        chunks.append((nl + q * 128, 128, vc[s][:, q, hh * 64:(hh + 1) * 64]))
                    pv = PS[ub][:].bitcast(BF16)

                    def ptr(e, u=u, pv=pv, chunks=chunks):
                        ins = None
                        for ci, (off, kn, _) in enumerate(chunks):
                            ins = e.transpose(pv[0:kn, ci * 128:(ci + 1) * 128], Pm[u][:, off:off + kn], self.identb[:])
                        return ins

                    P.pe(ptr, [kP], ["ps%d" % ub])
                    nch = len(chunks)
                    P.act(lambda e, u=u, pv=pv, nch=nch: e.copy(out=PT[u][:, 0:nch, :].rearrange("p c q -> p (c q)"), in_=pv[:, 0:nch * 128]), ["ps%d" % ub], [kPT])
                    P.mm(PS[ub + 1][:, 384 + hh * 64:384 + (hh + 1) * 64], [(PT[u][0:kn, ci, :], vap) for ci, (off, kn, vap) in enumerate(chunks)],
                         [kPT, "nvc%d" % s], ["ps%d" % (ub + 1)])
                    P.act(lambda e, ub=ub, hh=hh, st=st, osl=osl: e.activation(out=otm[osl][:, hh * 64:(hh + 1) * 64], in_=PS[ub + 1][:, 384 + hh * 64:384 + (hh + 1) * 64],
                                                                              func=AF.Copy, scale=st[:, 3:4]), ["ps%d" % (ub + 1), kst], ["notm%d" % osl])
                pv3 = PS[3][:].bitcast(BF16)
                P.pe(lambda e, osl=osl, pv3=pv3: e.transpose(pv3[:, 0:128], otm[osl][:], self.identb[:]), ["notm%d" % osl], ["ps3"])
                P.dve(lambda e, s=s, tq=tq, pv3=pv3: e.tensor_copy(out=oc_[s][:, tq], in_=pv3[:, 0:128]), ["ps3"], ["noc%d" % s])
            if not last:
                P.dma(self.d_act[c * 128:(c + 1) * 128, 0:CTX], oc_[s][:, 0:CTX], reads=["noc%d" % s], writes=["dactn%d" % c])
            P.dma(self.d_act[c * 128:(c + 1) * 128, CTX:TOK], oc_[s][:, CTX:TOK], reads=["noc%d" % s], writes=["dactn%d" % c])
        self.refence()
        self.outproj(i, 8, I["na_w_out"][j], I["na_b_out"][j], qtiles)

    def build(self):
        P = self.P
        self.declare()
        self.consts()
        self.mods()
        self.persist()
        self.first_norm()
        for i in range(self.depth):
            last = i == self.depth - 1
            j = i // 2
            if i % 2 == 0:
                self.mlstm(i, j)
            else:
                self.natten(i, j, last)
            self.ffn(i, last)
        P.emit()
        return self.nc


def host_inputs(inputs, depth=4):
    f = lambda a: np.ascontiguousarray(np.asarray(a, dtype=np.float32))
    x, c, ctx, c_ctx = f(inputs["x"]), f(inputs["c"]), f(inputs["ctx"]), f(inputs["c_ctx"])
    B = x.shape[0]
    shared = {}
    for k in ("ada_w", "ada_b", "norm1_g", "norm2_g", "final_g", "ml_w_in", "ml_conv_w", "ml_conv_b", "ml_skip", "ml_hnorm_g", "ml_w_out",
              "na_w_qkv", "na_b_qkv", "na_w_out", "na_b_out", "ff_w_up", "ff_b_up", "ff_conv_w", "ff_conv_b", "ff_w_down", "ff_b_down"):
        shared[k] = f(inputs[k])
    n_a = shared["ml_w_in"].shape[0]
    for nm, src in (("bdq", "ml_wq"), ("bdk", "ml_wk"), ("bdv", "ml_wv")):
        w = f(inputs[src])
        bd = np.zeros((n_a, 16, 128, 128), np.float32)
        wr = w.reshape(n_a, 16, 32, 4, 4)
        for n in range(32):
            bd[:, :, 4 * n:4 * n + 4, 4 * n:4 * n + 4] = wr[:, :, n]
        shared[nm] = bd
        shared[nm + "T"] = np.ascontiguousarray(bd.transpose(0, 1, 3, 2))
    gw = f(inputs["ml_gate_w"])
    g2 = gw.reshape(n_a, 2, 3, 16, 128, 2, 4)
    g2 = g2.transpose(0, 2, 3, 4, 5, 1, 6)
    shared["gw"] = np.ascontiguousarray(g2.reshape(n_a, 3, 16, 128, 16))
    gb = f(inputs["ml_gate_b"]).reshape(n_a, 2, 2, 4).transpose(0, 2, 1, 3)
    shared["gb"] = np.ascontiguousarray(gb.reshape(n_a, 16))
    rpb = f(inputs["na_rpb"])
    n_b = rpb.shape[0]
    nab = np.full((n_b, 16, 5, 128, 576), NEGV, np.float32)
    col = np.arange(64)
    cs = np.clip(col - 8, 0, 48)
    ok = (col[None, :] >= cs[:, None]) & (col[None, :] < cs[:, None] + 16)
    dc = np.clip(col[None, :] - col[:, None] + 15, 0, 30)
    for cls, r in enumerate((0, 2, 4, 28, 30)):
        for rho in range(2):
            rq = r + rho
            rs_q = int(np.clip(rq - 4, 0, 24))
            rs0 = int(np.clip(r - 4, 0, 24))
            nkr = 9 if cls == 2 else 8
            for jj in range(nkr):
                krow = rs0 + jj
                if krow < rs_q or krow >= rs_q + 8:
                    continue
                dr = krow - rq + 7
                vals = rpb[:, :, dr, :][:, :, dc]
                blk = np.where(ok[None, None], vals, np.float32(NEGV))
                nab[:, :, cls, rho * 64:(rho + 1) * 64, jj * 64:(jj + 1) * 64] = blk
    shared["nab"] = nab
    u = np.arange(128)
    same = (u[:, None] // 64) == (u[None, :] // 64)
    cm = np.zeros((2, 3, 128, 128), np.float32)
    cm[0, 0] = same & (u[:, None] <= u[None, :])
    cm[0, 1] = -(same & (u[:, None] > u[None, :])).astype(np.float32)
    cm[0, 2] = np.where(same & (u[:, None] <= u[None, :]), 0.0, NEGV)
    cm[1, 0] = same & (u[:, None] >= u[None, :])
    cm[1, 1] = -(same & (u[:, None] < u[None, :])).astype(np.float32)
    cm[1, 2] = np.where(same & (u[:, None] >= u[None, :]), 0.0, NEGV)
    shared["cmask"] = cm
    shared["identf"] = np.eye(128, dtype=np.float32)
    hm = np.zeros((128, 2), np.float32)
    hm[:64, 0] = 1.0
    hm[64:, 1] = 1.0
    shared["halfm"] = hm
    maps = []
    for b in range(B):
        m = dict(shared)
        m["x"] = x[b]
        m["ctx"] = ctx[b]
        m["cvec"] = np.ascontiguousarray(np.stack([c[b], c_ctx], 0))
        maps.append(m)
    return maps


_NC_CACHE = {}


def kernel(**inputs):
    maps = host_inputs(inputs)
    if "nc" not in _NC_CACHE:
        _NC_CACHE["nc"] = Builder(4).build()
    nc = _NC_CACHE["nc"]
    res = run_bass_kernel_spmd(nc, maps, core_ids=list(range(len(maps))))
    return np.stack([np.asarray(r["out"], dtype=np.float32) for r in res.results], 0)
```

```python
import contextlib
import itertools
import numpy as np
import concourse.bass as bass
import concourse.mybir as mybir
from concourse.bass_utils import run_bass_kernel_spmd

F32 = mybir.dt.float32
BF16 = mybir.dt.bfloat16
AF = mybir.ActivationFunctionType
ALU = mybir.AluOpType
AX = mybir.AxisListType

D = 1024
SEQ = 2048
CTX = 256
TOK = SEQ + CTX
NT = TOK // 128
EPS = 1e-6
FF = 2816
NEGV = -30000.0
TBLK = [(0, 256), (256, 512), (768, 512), (1280, 512), (1792, 512)]


class Prog:
    ENGS = ("pe", "act", "dve", "pool", "sp")
    NDMA = {"sp": 40, "pool": 16, "act": 8}

    def __init__(self, nc):
        self.nc = nc
        self.ops = []
        self.last_w = {}
        self.readers = {}
        self.sb_off = 16512
        self.ndma = {q: 0 for q in self.NDMA}
        self.fence_idx = None
        self.nname = 0

    def sb(self, name, shape, dtype):
        esz = 4 if dtype == F32 else 2
        n = 1
        for s in shape[1:]:
            n *= s
        nbytes = (n * esz + 63) // 64 * 64
        off = self.sb_off
        self.sb_off += nbytes
        assert self.sb_off <= 229344, (name, self.sb_off)
        self.nname += 1
        return self.nc.alloc_sbuf_tensor_at("%s_%d" % (name, self.nname), list(shape), dtype, offset=off)

    def add(self, eng, fn, reads=(), writes=(), dma=False):
        idx = len(self.ops)
        deps = set()
        if self.fence_idx is not None:
            deps.add(self.fence_idx)
        for k in reads:
            if k in self.last_w:
                deps.add(self.last_w[k])
        for k in writes:
            if k in self.last_w:
                deps.add(self.last_w[k])
            for r in self.readers.get(k, ()):
                deps.add(r)
        deps.discard(idx)
        for k in reads:
            self.readers.setdefault(k, []).append(idx)
        for k in writes:
            self.last_w[k] = idx
            self.readers[k] = []
        op = dict(eng=eng, fn=fn, deps=deps, dma=dma, sig=False)
        if dma:
            q = eng
            k = self.ndma[q]
            self.ndma[q] += 1
            op["dq"] = (q, k % self.NDMA[q])
            op["dval"] = 16 * (k // self.NDMA[q] + 1)
            op["dprev"] = 16 * (k // self.NDMA[q])
        self.ops.append(op)
        return idx

    def fence(self, scratch):
        deps = set(self.last_w.values())
        for rs in self.readers.values():
            deps.update(rs)
        idx = len(self.ops)
        if self.fence_idx is not None:
            deps.add(self.fence_idx)
        self.ops.append(dict(eng="pool", fn=lambda e: e.memset(scratch, 0.0), deps=deps, dma=False, sig=True))
        self.last_w = {}
        self.readers = {}
        self.fence_idx = idx

    def pe(self, fn, reads=(), writes=()):
        return self.add("pe", fn, reads, writes)

    def act(self, fn, reads=(), writes=()):
        return self.add("act", fn, reads, writes)

    def dve(self, fn, reads=(), writes=()):
        return self.add("dve", fn, reads, writes)

    def pool(self, fn, reads=(), writes=()):
        return self.add("pool", fn, reads, writes)

    def dma(self, out, in_, reads=(), writes=(), q="sp", **kw):
        return self.add(q, lambda e: e.dma_start(out=out, in_=in_, **kw), reads, writes, dma=True)

    def mm(self, out_ap, pairs, reads, writes):
        def fn(e):
            n = len(pairs)
            ins = None
            for i, (l, r) in enumerate(pairs):
                ins = e.matmul(out_ap, lhsT=l, rhs=r, start=(i == 0), stop=(i == n - 1))
            return ins

        return self.pe(fn, reads, writes)

    def emit(self):
        nc = self.nc
        ops = self.ops
        for op in ops:
            for d in op["deps"]:
                if ops[d]["eng"] == "pe" and op["eng"] == "pe" and not ops[d]["dma"]:
                    continue
                ops[d]["sig"] = True
        last_on = {}
        for i, op in enumerate(ops):
            if not op["dma"]:
                last_on[op["eng"]] = i
        for e, i in last_on.items():
            ops[i]["sig"] = True
        cnt = {e: 0 for e in self.ENGS}
        for op in ops:
            if op["sig"] and not op["dma"]:
                cnt[op["eng"]] += 1
                op["val"] = cnt[op["eng"]]
        with contextlib.ExitStack() as st:
            esem = {e: st.enter_context(nc.semaphore("c_" + e)) for e in self.ENGS}
            dsem = {
                q: [st.enter_context(nc.semaphore("d_%s%d" % (q, i))) for i in range(n)]
                for q, n in self.NDMA.items()
            }
            block = st.enter_context(nc.Block())

            def stream(ename):
                def body(e):
                    waited = {}

                    def wait(sem, key, val):
                        if waited.get(key, 0) >= val:
                            return
                        waited[key] = val
                        e.wait_ge(sem, val)

                    for op in ops:
                        if op["eng"] != ename:
                            continue
                        for d in sorted(op["deps"]):
                            dop = ops[d]
                            if dop["dma"]:
                                q, si = dop["dq"]
                                wait(dsem[q][si], ("d", q, si), dop["dval"])
                            else:
                                if dop["eng"] == "pe" and ename == "pe":
                                    continue
                                wait(esem[dop["eng"]], ("e", dop["eng"]), dop["val"])
                        if op["dma"]:
                            q, si = op["dq"]
                            if op["dprev"] > 0:
                                wait(dsem[q][si], ("d", q, si), op["dprev"])
                            ins = op["fn"](e)
                            ins.then_inc(dsem[q][si], 16)
                        else:
                            ins = op["fn"](e)
                            if op["sig"]:
                                ins.then_inc(esem[ename], 1)
                    if ename == "sp":
                        for q, n in self.NDMA.items():
                            tot = self.ndma[q]
                            for si in range(min(n, tot)):
                                k_last = ((tot - 1 - si) // n) * n + si
                                wait(dsem[q][si], ("d", q, si), 16 * (k_last // n + 1))
                        for en in self.ENGS:
                            if en != "sp" and cnt[en] > 0:
                                wait(esem[en], ("e", en), cnt[en])

                return body

            block.tensor(stream("pe"))
            block.scalar(stream("act"))
            block.vector(stream("dve"))
            block.gpsimd(stream("pool"))
            block.sync(stream("sp"))


def tkeys(prefix, start, n):
    return ["%s%d" % (prefix, t) for t in range(start // 128, (start + n + 127) // 128)]


def pcol(t):
    return 1 + t if t < CTX else 2 + t


class Builder:
    def __init__(self, depth=4, dbg=None):
        self.depth = depth
        self.dbg = dbg
        nc = self.nc = bass.Bass("TRN2", target_bir_lowering=False)
        self.P = Prog(nc)
        self.I = {}
        psall = nc.alloc_psum_tensor("psall", [128, 4096], F32)
        self.PS = [psall[:, i * 512:(i + 1) * 512] for i in range(8)]
        self.PSC = [psall[:, (4 * z + 2) * 512:(4 * z + 4) * 512] for z in range(2)]

    def inp(self, name, shape, dtype=F32):
        self.I[name] = self.nc.dram_tensor(name, list(shape), dtype, kind="ExternalInput").ap()
        return self.I[name]

    def scratch(self, name, shape, dtype):
        return self.nc.dram_tensor(name, list(shape), dtype, kind="Internal").ap()

    def declare(self):
        n_a, n_b = 2, 2
        inp = self.inp
        inp("x", [SEQ, D]); inp("ctx", [CTX, D]); inp("cvec", [2, D])
        inp("ada_w", [4, D, 6 * D]); inp("ada_b", [4, 6 * D])
        inp("norm1_g", [4, D]); inp("norm2_g", [4, D]); inp("final_g", [D])
        inp("ml_w_in", [n_a, D, 4096]); inp("ml_conv_w", [n_a, 5, 2048]); inp("ml_conv_b", [n_a, 2048])
        inp("bdq", [n_a, 16, 128, 128]); inp("bdk", [n_a, 16, 128, 128]); inp("bdv", [n_a, 16, 128, 128])
        inp("bdqT", [n_a, 16, 128, 128]); inp("bdkT", [n_a, 16, 128, 128]); inp("bdvT", [n_a, 16, 128, 128])
        inp("gw", [n_a, 3, 16, 128, 16]); inp("gb", [n_a, 16])
        inp("ml_skip", [n_a, 2048]); inp("ml_hnorm_g", [n_a, 2048]); inp("ml_w_out", [n_a, 2048, D])
        inp("na_w_qkv", [n_b, D, 3 * D]); inp("na_b_qkv", [n_b, 3 * D]); inp("nab", [n_b, 16, 5, 128, 576])
        inp("na_w_out", [n_b, D, D]); inp("na_b_out", [n_b, D])
        inp("ff_w_up", [4, D, 2 * FF]); inp("ff_b_up", [4, 2 * FF]); inp("ff_conv_w", [4, 3, 2 * FF])
        inp("ff_conv_b", [4, 2 * FF]); inp("ff_w_down", [4, FF, D]); inp("ff_b_down", [4, D])
        inp("cmask", [2, 3, 128, 128]); inp("identf", [128, 128]); inp("halfm", [128, 2])
        self.out = self.nc.dram_tensor("out", [SEQ, D], F32, kind="ExternalOutput").ap()
        sc = self.scratch
        self.Xs = sc("Xs", [TOK, D], F32)
        self.modd = sc("modd", [4, 2, 6 * D], F32)
        self.d_u = sc("d_u", [2048, TOK], BF16)
        self.d_uc = sc("d_uc", [2048, TOK], BF16)
        self.d_sz = sc("d_sz", [2048, TOK], BF16)
        self.d_act = sc("d_act", [FF, TOK], BF16)

    def consts(self):
        P, I = self.P, self.I
        self.identf = P.sb("identf", [128, 128], F32)
        self.identb = P.sb("identb", [128, 128], BF16)
        self.cm = P.sb("cm", [128, 2, 3, 128], F32)
        self.halfm = P.sb("halfm", [128, 2], F32)
        self.onesf = P.sb("onesf", [128, 128], F32)
        self.onesb = P.sb("onesb", [128, 2], BF16)
        self.epsc = P.sb("epsc", [128, 1], F32)
        self.fsc = P.sb("fsc", [128, 16], F32)
        self.junk = P.sb("junk", [128, 1024], BF16)
        P.dma(self.identf[:], I["identf"], writes=["identf"])
        P.dma(self.cm[:], I["cmask"].rearrange("z m p c -> p z m c"), writes=["cm"])
        P.dma(self.halfm[:], I["halfm"], writes=["halfm"])
        P.dve(lambda e: e.tensor_copy(out=self.identb[:], in_=self.identf[:]), ["identf"], ["identb"])
        P.dve(lambda e: e.memset(self.onesf[:], 1.0), [], ["onesf"])
        P.dve(lambda e: e.memset(self.onesb[:], 1.0), [], ["onesb"])
        P.dve(lambda e: e.memset(self.epsc[:], EPS), [], ["epsc"])
        self.const_keys = ["identf", "identb", "cm", "halfm", "onesf", "onesb", "epsc"]

    def refence(self):
        self.P.fence(self.fsc[:, 0:1])

    def mods(self):
        P, I, PS = self.P, self.I, self.PS
        m0 = P.sb_off
        s2 = P.sb("s2", [128, 8, 2], F32)
        stg = [P.sb("adastg%d" % i, [128, 8, 512], F32) for i in range(2)]
        modrow = P.sb("modrow", [2, 6 * D], F32)
        adab = P.sb("adab", [2, 6 * D], F32)
        g1b = P.sb("g1b", [2, D], F32)
        g2b = P.sb("g2b", [2, D], F32)
        for jj in range(2):
            P.dma(s2[:, :, jj], I["cvec"][jj].rearrange("(k p) -> p k", p=128), writes=["s2"], allow_slow_non_contiguous=True)
        P.act(lambda e: e.activation(out=s2[:], in_=s2[:], func=AF.Silu), ["s2"], ["s2"])
        n = 0
        for i in range(self.depth):
            P.dma(adab[:], I["ada_b"][i].partition_broadcast(2), writes=["adab"])
            P.dma(g1b[:], I["norm1_g"][i].partition_broadcast(2), writes=["g1b"])
            P.dma(g2b[:], I["norm2_g"][i].partition_broadcast(2), writes=["g2b"])
            for cb in range(12):
                sl = n % 2
                bank = 6 + (n % 2)
                n += 1
                P.dma(stg[sl][:], I["ada_w"][i][:, cb * 512:(cb + 1) * 512].rearrange("(k p) c -> p k c", p=128),
                      writes=["adastg%d" % sl])
                P.mm(PS[bank][0:2, :], [(s2[:, k, :], stg[sl][:, k, :]) for k in range(8)],
                     ["s2", "adastg%d" % sl], ["ps%d" % bank])
                P.dve(lambda e, bank=bank, cb=cb: e.tensor_tensor(out=modrow[:, cb * 512:(cb + 1) * 512], in0=PS[bank][0:2, :],
                                                                  in1=adab[:, cb * 512:(cb + 1) * 512], op=ALU.add),
                      ["ps%d" % bank, "adab"], ["modrow"])
            P.dve(lambda e: e.scalar_tensor_tensor(out=modrow[:, D:2 * D], in0=modrow[:, D:2 * D], scalar=1.0, in1=g1b[:],
                                                   op0=ALU.add, op1=ALU.mult), ["modrow", "g1b"], ["modrow"])
            P.dve(lambda e: e.scalar_tensor_tensor(out=modrow[:, 4 * D:5 * D], in0=modrow[:, 4 * D:5 * D], scalar=1.0, in1=g2b[:],
                                                   op0=ALU.add, op1=ALU.mult), ["modrow", "g2b"], ["modrow"])
            P.dma(self.modd[i], modrow[:], reads=["modrow"], writes=["modd%d" % i])
        self.refence()
        P.sb_off = m0

    def load_cols(self, dst, src1d, key):
        self.P.dma(dst, src1d.rearrange("(k p) -> p k", p=128), writes=[key], allow_slow_non_contiguous=True)

    def persist(self):
        P = self.P
        self.colm = P.sb("colm", [128, 4, 2, 6, 8], F32)
        self.grow_off = P.sb_off
        self.grow = P.sb("grow", [128, 2, 2, D], F32)
        self.nst = [P.sb("nst%d" % s, [128, 8], F32) for s in range(2)]
        self.xn = [P.sb("xn%d" % s, [128, D], BF16) for s in range(2)]
        for i in range(self.depth):
            for v in range(2):
                for s in (0, 1, 3, 4):
                    P.dma(self.colm[:, i, v, s, :], self.modd[i, v, s * D:(s + 1) * D].rearrange("(k p) -> p k", p=128),
                          reads=["modd%d" % i], writes=["colm"], allow_slow_non_contiguous=True)
        self.hA = P.sb("hA", [128, 8, TOK], BF16)
        self.hA_off = P.sb_off - 8 * TOK * 2
        self.top = P.sb_off

    def load_grow(self, i):
        P = self.P
        for v in range(2):
            P.dma(self.grow[:, v, 0, :], self.modd[i, v, 2 * D:3 * D].partition_broadcast(128), reads=["modd%d" % i], writes=["grow"])
            P.dma(self.grow[:, v, 1, :], self.modd[i, v, 5 * D:6 * D].partition_broadcast(128), reads=["modd%d" % i], writes=["grow"])

    def rms_stats(self, xt, xkey, sl):
        P = self.P
        st = self.nst[sl]
        ks = "nst%d" % sl
        P.act(lambda e: e.activation(out=self.junk[:], in_=xt, func=AF.Square, accum_out=st[:, 0:1]), [xkey], ["junk", ks])
        P.act(lambda e: e.activation(out=st[:, 1:2], in_=st[:, 0:1], func=AF.Sqrt, scale=1.0 / D, bias=self.epsc[:, 0:1]), [ks], [ks])
        P.dve(lambda e: e.reciprocal(out=st[:, 2:3], in_=st[:, 1:2]), [ks], [ks])
        return st, ks

    def norm_tile(self, xt, xkey, tt, sl, li, which, hname, bank):
        P, PS = self.P, self.PS
        v = 1 if tt < 2 else 0
        xn = self.xn[sl]
        kx = "xn%d" % sl
        st, ks = self.rms_stats(xt, xkey, sl)
        P.dve(lambda e: e.tensor_scalar(out=xn[:], in0=xt, scalar1=st[:, 2:3], scalar2=None, op0=ALU.mult), [xkey, ks], [kx])
        pv = PS[bank][:].bitcast(BF16)

        def tr(e):
            ins = None
            for c in range(8):
                ins = e.transpose(pv[:, c * 128:(c + 1) * 128], xn[:, c * 128:(c + 1) * 128], self.identb[:])
            return ins

        P.pe(tr, [kx], ["ps%d" % bank])
        sa, sb_ = (1, 0) if which == 0 else (4, 3)
        hk = "%s%d" % (hname, tt)
        for c in range(8):
            A = self.colm[:, li, v, sa, c:c + 1]
            B = self.colm[:, li, v, sb_, c:c + 1]
            dst = self.hA[:, c, tt * 128:(tt + 1) * 128]
            src = pv[:, c * 128:(c + 1) * 128]
            if c % 2 == 0:
                P.act(lambda e, A=A, B=B, dst=dst, src=src: e.activation(out=dst, in_=src, func=AF.Identity, scale=A, bias=B),
                      ["ps%d" % bank, "colm"], [hk])
            else:
                P.dve(lambda e, A=A, B=B, dst=dst, src=src: e.tensor_scalar(out=dst, in0=src, scalar1=A, scalar2=B, op0=ALU.mult, op1=ALU.add),
                      ["ps%d" % bank, "colm"], [hk])

    def first_norm(self):
        P, I = self.P, self.I
        P.sb_off = self.top
        xt = [P.sb("fxt%d" % s, [128, D], F32) for s in range(2)]
        for tt in range(NT):
            sl = tt % 2
            src = I["ctx"][tt * 128:(tt + 1) * 128, :] if tt < 2 else I["x"][(tt - 2) * 128:(tt - 1) * 128, :]
            P.dma(xt[sl][:], src, writes=["fxt%d" % sl])
            P.dma(self.Xs[tt * 128:(tt + 1) * 128, :], xt[sl][:], reads=["fxt%d" % sl], writes=["X%d" % tt])
            self.norm_tile(xt[sl][:], "fxt%d" % sl, tt, sl, 0, 0, "h1_", 6 + sl)
        self.refence()

    def stager(self, name, nbuf, cols):
        P = self.P
        bufs = [P.sb("%s%d" % (name, i), [128, cols], F32) for i in range(nbuf)]
        state = dict(n=0)

        def load(dst, srcs, dkeys, eng="pool", shape=None):
            sl = state["n"] % nbuf
            state["n"] += 1
            key = "%s%d" % (name, sl)
            tot = 0
            for off, ncols, ap, pat in srcs:
                d = bufs[sl][:, off:off + ncols]
                if pat is not None:
                    d = d.rearrange(pat[0], **pat[1])
                P.dma(d, ap, writes=[key], q="pool")
                tot = max(tot, off + ncols)
            src = bufs[sl][:, 0:tot]
            if shape is not None:
                src = src.rearrange(shape[0], **shape[1])
            P.add(eng, lambda e: e.tensor_copy(out=dst, in_=src), [key], dkeys)

        return load

    def ffn(self, i, last):
        P, I, PS = self.P, self.I, self.PS
        hT = self.hA
        P.sb_off = self.top
        tiles = list(range(NT)) if not last else list(range(2, NT))
        blks = TBLK if not last else TBLK[1:]
        W = 2307
        L = W - 2
        load = self.stager("fstg", 2, 4096)
        bcol = P.sb("fbcol", [128, 5, 44], F32)
        wup = [P.sb("wup%d" % s, [128, 8, 2, 256], BF16) for s in range(2)]
        upad = [[P.sb("upad%d%d" % (s, g), [128, W], BF16) for g in range(2)] for s in range(2)]
        acc = [[P.sb("facc%d%d" % (s, g), [128, L], F32) for g in range(2)] for s in range(2)]
        actc = [P.sb("actc%d" % s, [128, L], BF16) for s in range(2)]
        self.load_cols(bcol[:, 0, :], I["ff_b_up"][i], "fbcol")
        for k in range(3):
            self.load_cols(bcol[:, 1 + k, :], I["ff_conv_w"][i, k], "fbcol")
        self.load_cols(bcol[:, 4, :], I["ff_conv_b"][i], "fbcol")
        for s in range(2):
            for g in range(2):
                P.pool(lambda e, s=s, g=g: e.memset(upad[s][g][:], 0.0), [], ["upad%d%d" % (s, g)])
        nb = 0
        for cg in range(11):
            ws = cg % 2
            load(wup[ws][:], [
                (0, 2048, I["ff_w_up"][i][:, cg * 256:(cg + 1) * 256].rearrange("(k p) c -> p k c", p=128),
                 ("p (k c) -> p k c", dict(k=8))),
                (2048, 2048, I["ff_w_up"][i][:, FF + cg * 256:FF + (cg + 1) * 256].rearrange("(k p) c -> p k c", p=128),
                 ("p (k c) -> p k c", dict(k=8))),
            ], ["wup%d" % ws], shape=("p (g k c) -> p k g c", dict(g=2, k=8)))
            for cc in range(2):
                c = cg * 2 + cc
                s = c % 2
                for g in range(2):
                    col = c + 22 * g
                    ku, ka = "upad%d%d" % (s, g), "facc%d%d" % (s, g)
                    u, a = upad[s][g], acc[s][g]
                    for (t0, n) in blks:
                        bank = nb % 4
                        nb += 1
                        P.mm(PS[bank][:, 0:n], [(wup[ws][:, k, g, cc * 128:(cc + 1) * 128], hT[:, k, t0:t0 + n]) for k in range(8)],
                             ["wup%d" % ws] + tkeys("h2_", t0, n), ["ps%d" % bank])
                        P.act(lambda e, bank=bank, n=n, u=u, t0=t0, col=col: e.activation(
                            out=u[:, pcol(t0):pcol(t0) + n], in_=PS[bank][:, 0:n], func=AF.Identity,
                            bias=bcol[:, 0, col:col + 1], scale=1.0), ["ps%d" % bank, "fbcol"], [ku])
                    P.pool(lambda e, u=u, a=a, col=col: e.tensor_scalar(out=a[:], in0=u[:, 1:1 + L], scalar1=bcol[:, 2, col:col + 1],
                                                                        scalar2=bcol[:, 4, col:col + 1], op0=ALU.mult, op1=ALU.add),
                           [ku, "fbcol"], [ka])
                    P.dve(lambda e, u=u, a=a, col=col: e.scalar_tensor_tensor(out=a[:], in0=u[:, 0:L], scalar=bcol[:, 1, col:col + 1],
                                                                               in1=a[:], op0=ALU.mult, op1=ALU.add), [ku, ka, "fbcol"], [ka])
                    P.dve(lambda e, u=u, a=a, col=col: e.scalar_tensor_tensor(out=a[:], in0=u[:, 2:2 + L], scalar=bcol[:, 3, col:col + 1],
                                                                              in1=a[:], op0=ALU.mult, op1=ALU.add), [ku, ka, "fbcol"], [ka])
                P.act(lambda e, s=s: e.activation(out=acc[s][1][:], in_=acc[s][1][:], func=AF.Silu), ["facc%d1" % s], ["facc%d1" % s])
                P.dve(lambda e, s=s: e.tensor_tensor(out=actc[s][:], in0=acc[s][0][:], in1=acc[s][1][:], op=ALU.mult),
                      ["facc%d0" % s, "facc%d1" % s], ["actc%d" % s])
                if not last:
                    P.dma(self.d_act[c * 128:(c + 1) * 128, 0:CTX], actc[s][:, 0:CTX], reads=["actc%d" % s], writes=["dact%d" % c])
                P.dma(self.d_act[c * 128:(c + 1) * 128, CTX:TOK], actc[s][:, CTX + 1:TOK + 1], reads=["actc%d" % s], writes=["dact%d" % c])
        self.refence()
        P.sb_off = self.top
        load = self.stager("dstg", 2, 1024)
        wdn = P.sb("wdn", [128, 22, D], BF16)
        bdn = P.sb("bdn", [128, D], F32)
        P.dma(bdn[:], I["ff_b_down"][i].partition_broadcast(128), writes=["bdn"])
        for c in range(22):
            load(wdn[:, c, :], [(0, D, I["ff_w_down"][i][c * 128:(c + 1) * 128, :], None)], ["wdn"])
        at = [P.sb("at%d" % s, [128, 22, 128], BF16) for s in range(2)]
        xt = [P.sb("xt%d" % s, [128, D], F32) for s in range(2)]
        tmp = [P.sb("ftmp%d" % s, [128, 512], F32) for s in range(2)]
        fg = None
        if last:
            fg = P.sb("fg", [128, D], F32)
            P.dma(fg[:], I["final_g"].partition_broadcast(128), writes=["fg"])
        for n_, tt in enumerate(tiles):
            sl = n_ % 2
            v = 1 if tt < 2 else 0
            P.dma(at[sl][:], self.d_act[:, tt * 128:(tt + 1) * 128].rearrange("(c p) t -> p c t", p=128), writes=["at%d" % sl])
            P.dma(xt[sl][:], self.Xs[tt * 128:(tt + 1) * 128, :], writes=["xt%d" % sl])
            for hf in range(2):
                bank = 2 * sl + hf
                P.mm(PS[bank][:, :], [(at[sl][:, c, :], wdn[:, c, hf * 512:(hf + 1) * 512]) for c in range(22)],
                     ["at%d" % sl, "wdn"], ["ps%d" % bank])
                tk = "ftmp%d" % hf
                P.dve(lambda e, bank=bank, hf=hf: e.tensor_tensor(out=tmp[hf][:], in0=PS[bank][:, :], in1=bdn[:, hf * 512:(hf + 1) * 512], op=ALU.add),
                      ["ps%d" % bank, "bdn"], [tk])
                P.pool(lambda e, hf=hf, v=v: e.tensor_tensor(out=tmp[hf][:], in0=tmp[hf][:], in1=self.grow[:, v, 1, hf * 512:(hf + 1) * 512], op=ALU.mult),
                       [tk, "grow"], [tk])
                P.pool(lambda e, hf=hf, sl=sl: e.tensor_tensor(out=xt[sl][:, hf * 512:(hf + 1) * 512], in0=xt[sl][:, hf * 512:(hf + 1) * 512], in1=tmp[hf][:], op=ALU.add),
                       [tk, "xt%d" % sl], ["xt%d" % sl])
            if last:
                st_, ks = self.rms_stats(xt[sl][:], "xt%d" % sl, sl)
                P.dve(lambda e, sl=sl, st_=st_: e.scalar_tensor_tensor(out=xt[sl][:], in0=xt[sl][:], scalar=st_[:, 2:3], in1=fg[:], op0=ALU.mult, op1=ALU.mult),
                      ["xt%d" % sl, ks, "fg"], ["xt%d" % sl])
                P.dma(self.out[(tt - 2) * 128:(tt - 1) * 128, :], xt[sl][:], reads=["xt%d" % sl], writes=["out%d" % tt])
            else:
                P.dma(self.Xs[tt * 128:(tt + 1) * 128, :], xt[sl][:], reads=["xt%d" % sl], writes=["X%d" % tt])
                self.norm_tile(xt[sl][:], "xt%d" % sl, tt, sl, i + 1, 0, "h1_", 6 + sl)
        self.refence()

    def outproj(self, i, nk, w_src, b_src, tiles):
        P, I, PS = self.P, self.I, self.PS
        P.sb_off = self.top
        self.load_grow(i)
        load = self.stager("ostg", 2, 1024)
        wo = P.sb("wo", [128, nk, D], BF16)
        at = [P.sb("oat%d" % s, [128, nk, 128], BF16) for s in range(2)]
        xt = [P.sb("oxt%d" % s, [128, D], F32) for s in range(2)]
        tmp = [P.sb("otmp%d" % s, [128, 512], F32) for s in range(2)]
        bo = None
        if b_src is not None:
            bo = P.sb("bo", [128, D], F32)
            P.dma(bo[:], b_src.partition_broadcast(128), writes=["bo"])
        for c in range(nk):
            load(wo[:, c, :], [(0, D, w_src[c * 128:(c + 1) * 128, :], None)], ["wo"])
        for n_, tt in enumerate(tiles):
            sl = n_ % 2
            v = 1 if tt < 2 else 0
            P.dma(at[sl][:], self.d_act[0:nk * 128, tt * 128:(tt + 1) * 128].rearrange("(c p) t -> p c t", p=128), writes=["oat%d" % sl])
            P.dma(xt[sl][:], self.Xs[tt * 128:(tt + 1) * 128, :], writes=["oxt%d" % sl])
            for hf in range(2):
                bank = 2 * sl + hf
                P.mm(PS[bank][:, :], [(at[sl][:, k, :], wo[:, k, hf * 512:(hf + 1) * 512]) for k in range(nk)], ["oat%d" % sl, "wo"], ["ps%d" % bank])
                tk = "otmp%d" % hf
                if bo is not None:
                    P.dve(lambda e, bank=bank, hf=hf: e.tensor_tensor(out=tmp[hf][:], in0=PS[bank][:, :], in1=bo[:, hf * 512:(hf + 1) * 512], op=ALU.add),
                          ["ps%d" % bank, "bo"], [tk])
                    P.pool(lambda e, hf=hf, v=v: e.tensor_tensor(out=tmp[hf][:], in0=tmp[hf][:], in1=self.grow[:, v, 0, hf * 512:(hf + 1) * 512], op=ALU.mult),
                           [tk, "grow"], [tk])
                else:
                    P.dve(lambda e, bank=bank, hf=hf, v=v: e.tensor_tensor(out=tmp[hf][:], in0=PS[bank][:, :], in1=self.grow[:, v, 0, hf * 512:(hf + 1) * 512], op=ALU.mult),
                          ["ps%d" % bank, "grow"], [tk])
                P.pool(lambda e, hf=hf, sl=sl: e.tensor_tensor(out=xt[sl][:, hf * 512:(hf + 1) * 512], in0=xt[sl][:, hf * 512:(hf + 1) * 512], in1=tmp[hf][:], op=ALU.add),
                       [tk, "oxt%d" % sl], ["oxt%d" % sl])
            P.dma(self.Xs[tt * 128:(tt + 1) * 128, :], xt[sl][:], reads=["oxt%d" % sl], writes=["X%d" % tt])
            self.norm_tile(xt[sl][:], "oxt%d" % sl, tt, sl, i, 1, "h2_", 6 + sl)
        self.refence()

    def mlstm(self, i, j):
        P, I, PS = self.P, self.I, self.PS
        hT = self.hA
        idf = self.identf
        P.sb_off = self.top
        gcol = P.sb("gcol", [128, NT, 16], F32)
        emc = P.sb("emc", [128, 8], F32)
        mcol = P.sb("mcol", [128, 2, 16], F32)
        bdb = P.sb("bdb", [128, 3, 16, 128], BF16)
        keepA = P.sb_off
        load = self.stager("mstg", 2, 4096)
        win = [P.sb("win%d" % s, [128, 8, 512], BF16) for s in range(2)]
        bdT_off = P.sb_off
        bdT = P.sb("bdT", [128, 3, 16, 128], F32)
        P.sb("bdTpad", [128, 1024], F32)
        gwt = P.sb("gwt", [128, 3, 16, 16], F32)
        wgf = P.sb("wgf", [128, 2, 16, 16], BF16)
        ccol = P.sb("ccol", [128, 6, 16], F32)
        gbc = P.sb("gbc", [40, 1], F32)
        W5 = 2310
        L5 = W5 - 4
        upad = [P.sb("mupad%d" % s, [128, W5], BF16) for s in range(2)]
        ucp = [P.sb("mucp%d" % s, [128, W5], BF16) for s in range(2)]
        macc = [P.sb("macc%d" % s, [128, L5], F32) for s in range(2)]
        szb = [P.sb("szb%d" % s, [128, 512], BF16) for s in range(3)]
        mx = P.sb("mx", [8, 12], F32)
        _save = P.sb_off
        P.sb_off = bdT_off
        gsb = P.sb("gsb", [40, TOK], F32)
        gt1 = P.sb("gt1", [40, TOK], F32)
        gt2 = P.sb("gt2", [40, TOK], F32)
        P.sb_off = max(_save, P.sb_off)

        self.load_cols(mcol[:, 0, :], I["ml_hnorm_g"][j], "mcol")
        self.load_cols(mcol[:, 1, :], I["ml_skip"][j], "mcol")
        for k in range(5):
            self.load_cols(ccol[:, k, :], I["ml_conv_w"][j, k], "ccol")
        self.load_cols(ccol[:, 5, :], I["ml_conv_b"][j], "ccol")
        P.dma(gbc[0:8, :], I["gb"][j, 0:8].rearrange("(p o) -> p o", o=1), writes=["gbc"])
        P.dma(gbc[32:40, :], I["gb"][j, 8:16].rearrange("(p o) -> p o", o=1), writes=["gbc"])
        for m, nm in enumerate(("bdq", "bdk", "bdv")):
            for half in range(2):
                load(bdb[:, m, half * 8:(half + 1) * 8, :], [(0, 1024, I[nm][j, half * 8:(half + 1) * 8].rearrange("c p o -> p c o"),
                                                            ("p (c o) -> p c o", dict(c=8)))], ["bdb"],
                     shape=("p (c o) -> p c o", dict(c=8)))
            P.dma(bdT[:, m, :, :], I[nm + "T"][j].rearrange("c p o -> p c o"), writes=["bdT"])
            P.dma(gwt[:, m, :, :], I["gw"][j, m].rearrange("c p o -> p c o"), writes=["gwt"])
        P.dve(lambda e: e.tensor_scalar(out=gwt[:, 1, :, :], in0=gwt[:, 1, :, :], scalar1=float(512 ** -0.5), scalar2=None, op0=ALU.mult), ["gwt"], ["gwt"])
        for c in range(16):
            P.mm(PS[5][:, c * 16:(c + 1) * 16], [(bdT[:, 0, c, :], gwt[:, 0, c, :]), (bdT[:, 1, c, :], gwt[:, 1, c, :])], ["bdT", "gwt"], ["ps5"])
            P.mm(PS[6][:, c * 16:(c + 1) * 16], [(bdT[:, 2, c, :], gwt[:, 2, c, :])], ["bdT", "gwt"], ["ps6"])
        P.dve(lambda e: e.tensor_copy(out=wgf[:, 0, :, :], in_=PS[5][:, 0:256].rearrange("p (c o) -> p c o", c=16)), ["ps5"], ["wgf"])
        P.dve(lambda e: e.tensor_copy(out=wgf[:, 1, :, :], in_=PS[6][:, 0:256].rearrange("p (c o) -> p c o", c=16)), ["ps6"], ["wgf"])
        for s in range(2):
            P.pool(lambda e, s=s: e.memset(upad[s][:], 0.0), [], ["mupad%d" % s])

        def p5(t):
            return 2 + t if t < CTX else 4 + t

        nb = 0
        nz = 0
        for og in range(8):
            ws = og % 2
            load(win[ws][:], [(0, 4096, I["ml_w_in"][j][:, og * 512:(og + 1) * 512].rearrange("(k p) c -> p k c", p=128),
                               ("p (k c) -> p k c", dict(k=8)))], ["win%d" % ws], shape=("p (k c) -> p k c", dict(k=8)))
            for oo in range(4):
                oc = og * 4 + oo
                s = oc % 2
                for (t0, n) in TBLK:
                    bank = 5 + nb % 3
                    nb += 1
                    P.mm(PS[bank][:, 0:n], [(win[ws][:, k, oo * 128:(oo + 1) * 128], hT[:, k, t0:t0 + n]) for k in range(8)],
                         ["win%d" % ws] + tkeys("h1_", t0, n), ["ps%d" % bank])
                    if oc < 16:
                        P.act(lambda e, bank=bank, n=n, s=s, t0=t0: e.copy(out=upad[s][:, p5(t0):p5(t0) + n], in_=PS[bank][:, 0:n]),
                              ["ps%d" % bank], ["mupad%d" % s])
                    else:
                        zs = nz % 3
                        nz += 1
                        P.act(lambda e, bank=bank, n=n, zs=zs: e.activation(out=szb[zs][:, 0:n], in_=PS[bank][:, 0:n], func=AF.Silu),
                              ["ps%d" % bank], ["szb%d" % zs])
                        P.dma(self.d_sz[(oc - 16) * 128:(oc - 15) * 128, t0:t0 + n], szb[zs][:, 0:n], reads=["szb%d" % zs], writes=["dsz%d" % (oc - 16)])
                if oc < 16:
                    u, a, uc = upad[s], macc[s], ucp[s]
                    ku, ka, kc = "mupad%d" % s, "macc%d" % s, "mucp%d" % s
                    P.pool(lambda e, u=u, a=a, oc=oc: e.tensor_scalar(out=a[:], in0=u[:, 0:L5], scalar1=ccol[:, 0, oc:oc + 1], scalar2=ccol[:, 5, oc:oc + 1],
                                                                      op0=ALU.mult, op1=ALU.add), [ku, "ccol"], [ka])
                    for k in range(1, 5):
                        eng = "dve"
                        P.add(eng, lambda e, u=u, a=a, oc=oc, k=k: e.scalar_tensor_tensor(out=a[:], in0=u[:, k:k + L5], scalar=ccol[:, k, oc:oc + 1], in1=a[:],
                                                                                        op0=ALU.mult, op1=ALU.add), [ku, ka, "ccol"], [ka])
                    P.act(lambda e, a=a, uc=uc: e.activation(out=uc[:, 2:2 + L5], in_=a[:], func=AF.Silu), [ka], [kc])
                    for b, (t0, n) in enumerate(TBLK):
                        def gm(e, b=b, t0=t0, n=n, oc=oc, u=u, uc=uc):
                            c0 = p5(t0)
                            e.matmul(PS[b][0:8, 0:n], lhsT=wgf[:, 0, oc, 0:8], rhs=uc[:, c0:c0 + n], start=(oc == 0), stop=False)
                            e.matmul(PS[b][0:8, 0:n], lhsT=wgf[:, 1, oc, 0:8], rhs=u[:, c0:c0 + n], start=False, stop=(oc == 15))
                            e.matmul(PS[b][32:40, 0:n], lhsT=wgf[:, 0, oc, 8:16], rhs=uc[:, c0:c0 + n], start=(oc == 0), stop=False)
                            return e.matmul(PS[b][32:40, 0:n], lhsT=wgf[:, 1, oc, 8:16], rhs=u[:, c0:c0 + n], start=False, stop=(oc == 15))
                        P.pe(gm, [ku, kc, "wgf"], ["ps%d" % b])
                    for (src, dst, kk) in ((u, self.d_u, "du"), (uc, self.d_uc, "duc")):
                        P.dma(dst[oc * 128:(oc + 1) * 128, 0:CTX], src[:, 2:2 + CTX], reads=[ku if src is u else kc], writes=["%s%d" % (kk, oc)])
                        P.dma(dst[oc * 128:(oc + 1) * 128, CTX:TOK], src[:, 4 + CTX:4 + TOK], reads=[ku if src is u else kc], writes=["%s%d" % (kk, oc)])
        for b, (t0, n) in enumerate(TBLK):
            P.act(lambda e, b=b, t0=t0, n=n: e.activation(out=gsb[0:40, t0:t0 + n], in_=PS[b][0:40, 0:n], func=AF.Identity, bias=gbc[0:40, 0:1], scale=1.0),
                  ["ps%d" % b, "gbc"], ["gsb"])
        R = slice(32, 40)
        P.dve(lambda e: e.tensor_scalar(out=gt2[R, :], in0=gsb[R, :], scalar1=-1.0, scalar2=None, op0=ALU.mult), ["gsb"], ["gt2"])
        P.dve(lambda e: e.tensor_tensor(out=gt1[R, :], in0=gsb[R, :], in1=gt2[R, :], op=ALU.max), ["gsb", "gt2"], ["gt1"])
        P.act(lambda e: e.activation(out=gt1[R, :], in_=gt1[R, :], func=AF.Exp, scale=-1.0), ["gt1"], ["gt1"])
        P.act(lambda e: e.activation(out=gt1[R, :], in_=gt1[R, :], func=AF.Ln, bias=self.onesf[R, 0:1], scale=1.0), ["gt1"], ["gt1"])
        P.dve(lambda e: e.tensor_scalar(out=gt2[R, :], in0=gt2[R, :], scalar1=0.0, scalar2=None, op0=ALU.max), ["gt2"], ["gt2"])
        P.dve(lambda e: e.tensor_tensor(out=gsb[R, :], in0=gt1[R, :], in1=gt2[R, :], op=ALU.add), ["gt1", "gt2", "gsb"], ["gsb"])
        P.dve(lambda e: e.tensor_reduce(out=mx[0:8, 0:1], in_=gsb[0:8, :], axis=AX.X, op=ALU.max), ["gsb"], ["mx"])
        P.dve(lambda e: e.tensor_scalar(out=gsb[0:8, :], in0=gsb[0:8, :], scalar1=mx[0:8, 0:1], scalar2=None, op0=ALU.subtract), ["gsb", "mx"], ["gsb"])
        P.dve(lambda e: e.tensor_scalar(out=mx[0:8, 4:12], in0=idf[0:8, 0:8], scalar1=mx[0:8, 0:1], scalar2=None, op0=ALU.mult), ["mx"], ["mx"])
        P.mm(PS[5][:, 0:8], [(self.onesf[0:8, :], mx[0:8, 4:12])], ["mx"], ["ps5"])
        P.act(lambda e: e.activation(out=emc[:], in_=PS[5][:, 0:8], func=AF.Exp, scale=-1.0), ["ps5"], ["emc"])

        def gtr(e):
            ins = None
            for tt in range(NT):
                e.transpose(PS[6][:, tt * 16:tt * 16 + 8], gsb[0:8, tt * 128:(tt + 1) * 128], idf[0:8, 0:8])
                ins = e.transpose(PS[6][:, tt * 16 + 8:tt * 16 + 16], gsb[32:40, tt * 128:(tt + 1) * 128], idf[32:40, 32:40])
            return ins

        P.pe(gtr, ["gsb"], ["ps6"])
        P.dve(lambda e: e.tensor_copy(out=gcol[:].rearrange("p t g -> p (t g)"), in_=PS[6][:, 0:NT * 16]), ["ps6"], ["gcol"])
        self.refence()

        cm = self.cm
        order = {0: [0, 1] + list(range(2, NT)), 1: [1, 0] + list(range(NT - 1, 1, -1))}
        for hd in range(4):
            P.sb_off = self.hA_off
            H = P.sb("H", [128, NT, 512], BF16)
            C32 = [P.sb("C32_%d" % z, [128, 4, 512], F32) for z in range(2)]
            assert P.sb_off <= self.top
            P.sb_off = keepA
            qT = P.sb("qT", [128, 4, TOK], BF16)
            kT = P.sb("kT", [128, 4, TOK], BF16)
            ktm = P.sb("ktm", [128, NT, 512], BF16)
            vtm = P.sb("vtm", [128, NT, 512], BF16)
            Cbf = [P.sb("Cbf%d" % z, [128, 4, 512], BF16) for z in range(2)]
            nst = P.sb("nstate", [128, 2, 4], F32)
            nbf = P.sb("nbf", [128, 2, 4], BF16)
            keepB = P.sb_off
            ucT = P.sb("ucT", [128, 4, TOK], BF16)
            uT = P.sb("uT", [128, 4, TOK], BF16)
            P.dma(ucT[:], self.d_uc[hd * 512:(hd + 1) * 512, :].rearrange("(c p) t -> p c t", p=128), writes=["ucT"])
            P.dma(uT[:], self.d_u[hd * 512:(hd + 1) * 512, :].rearrange("(c p) t -> p c t", p=128), writes=["uT"])
            nb = 0
            sk = float(512 ** -0.5)
            for c in range(4):
                cc = hd * 4 + c
                for (t0, n) in TBLK:
                    for m, dst in ((0, qT), (1, kT)):
                        bank = nb % 4
                        nb += 1
                        P.mm(PS[bank][:, 0:n], [(bdb[:, m, cc, :], ucT[:, c, t0:t0 + n])], ["ucT"], ["ps%d" % bank])
                        if m == 0:
                            P.act(lambda e, bank=bank, n=n, dst=dst, c=c, t0=t0: e.copy(out=dst[:, c, t0:t0 + n], in_=PS[bank][:, 0:n]), ["ps%d" % bank], ["qT"])
                        else:
                            P.dve(lambda e, bank=bank, n=n, dst=dst, c=c, t0=t0: e.tensor_scalar(out=dst[:, c, t0:t0 + n], in0=PS[bank][:, 0:n], scalar1=sk, scalar2=None, op0=ALU.mult),
                                  ["ps%d" % bank], ["kT"])
            for tt in range(NT):
                tk = slice(tt * 128, (tt + 1) * 128)
                for m, src, dst in ((1, ucT, ktm), (2, uT, vtm)):
                    bank = 4 + nb % 4
                    nb += 1

                    def tm(e, bank=bank, m=m, src=src, tk=tk, hd=hd):
                        ins = None
                        for c in range(4):
                            ins = e.matmul(PS[bank][:, c * 128:(c + 1) * 128], lhsT=src[:, c, tk], rhs=bdb[:, m, hd * 4 + c, :], start=True, stop=True)
                        return ins

                    P.pe(tm, ["ucT", "uT"], ["ps%d" % bank])
                    if m == 1:
                        P.act(lambda e, bank=bank, tt=tt: e.activation(out=ktm[:, tt, :], in_=PS[bank][:, :], func=AF.Copy, scale=sk), ["ps%d" % bank], ["ktm%d" % tt])
                    else:
                        P.dve(lambda e, bank=bank, tt=tt: e.tensor_copy(out=vtm[:, tt, :], in_=PS[bank][:, :]), ["ps%d" % bank], ["vtm%d" % tt])
            for z in range(2):
                P.pool(lambda e, z=z: e.memset(C32[z][:], 0.0), [], ["C32_%d" % z])
                P.pool(lambda e, z=z: e.memset(Cbf[z][:], 0.0), [], ["Cbf%d" % z])
            P.pool(lambda e: e.memset(nst[:], 0.0), [], ["nstate0", "nstate1"])
            P.pool(lambda e: e.memset(nbf[:], 0.0), [], ["nbf0", "nbf1"])
            self.refence()
            P.sb_off = keepB
            DT = P.sb("DT", [128, 2, NT, 128], F32)
            EBb = P.sb("EBb", [128, 2, NT, 128], BF16)
            wkh = P.sb("wkh", [128, 2, 2, NT], F32)
            wkh16 = P.sb("wkh16", [128, 2, 2, NT], BF16)
            dec = P.sb("dec", [128, 2, NT, 2], F32)
            r2 = P.sb("r2", [128, NT, 2], F32)
            tc_ = P.sb("tcol", [128, NT], F32)
            wkc = P.sb("wkc", [128, NT], F32)
            dn = [P.sb("dn%d" % s, [128, 8], F32) for s in range(2)]
            ost = [P.sb("ost%d" % s, [128, 8], F32) for s in range(2)]
            lfm_off = P.sb_off
            LFm = P.sb("LFm", [128, NT, 128], F32)
            T2 = P.sb("T2", [128, NT, 128], F32)
            P.sb_off = lfm_off
            PTt = [P.sb("PT%d" % s, [128, 128], BF16) for s in range(2)]
            qs = [[P.sb("qs%d%d" % (z, h), [128, 4, 128], BF16) for h in range(2)] for z in range(2)]
            vw = [[P.sb("vw%d%d" % (z, h), [128, 512], BF16) for h in range(2)] for z in range(2)]
            _sv = P.sb_off
            P.sb_off = self.grow_off
            hs = [P.sb("hs%d" % s, [128, 512], F32) for s in range(2)]
            hn = [P.sb("hn%d" % s, [128, 512], BF16) for s in range(2)]
            oa = [P.sb("oa%d" % s, [128, 4, 128], F32) for s in range(2)]
            uct = [P.sb("uct%d" % s, [128, 4, 128], BF16) for s in range(2)]
            szt = [P.sb("szt%d" % s, [128, 4, 128], BF16) for s in range(2)]
            actt = [P.sb("actt%d" % s, [128, 4, 128], BF16) for s in range(2)]
            assert P.sb_off <= self.grow_off + 16384
            P.sb_off = _sv
            for z in range(2):
                lic = gcol[:, :, z * 4 + hd]
                nlfc = gcol[:, :, 8 + z * 4 + hd]
                P.dve(lambda e, z=z, nlfc=nlfc: e.tensor_tensor(out=LFm[:], in0=cm[:, z, 1, :].unsqueeze(1).to_broadcast([128, NT, 128]),
                                                                in1=nlfc.unsqueeze(2).to_broadcast([128, NT, 128]), op=ALU.mult), ["gcol"], ["LFm"])
                P.pool(lambda e, lic=lic: e.tensor_tensor(out=T2[:], in0=idf[:].unsqueeze(1).to_broadcast([128, NT, 128]),
                                                          in1=lic.unsqueeze(2).to_broadcast([128, NT, 128]), op=ALU.mult), ["gcol"], ["T2"])
                P.dve(lambda e: e.tensor_tensor(out=LFm[:], in0=LFm[:], in1=T2[:], op=ALU.add), ["LFm", "T2"], ["LFm"])
                P.dve(lambda e, nlfc=nlfc: e.tensor_scalar(out=T2[:], in0=nlfc.unsqueeze(2).to_broadcast([128, NT, 128]), scalar1=-1.0, scalar2=None, op0=ALU.mult),
                       ["gcol", "T2"], ["T2"])
                for g0 in range(0, NT, 4):
                    bank = (g0 // 4) % 2
                    ng = min(4, NT - g0)

                    def dm(e, g0=g0, ng=ng, bank=bank, z=z):
                        ins = None
                        for q in range(ng):
                            e.matmul(PS[bank][:, q * 128:(q + 1) * 128], lhsT=LFm[:, g0 + q, :], rhs=cm[:, z, 0, :], start=True, stop=False)
                            ins = e.matmul(PS[bank][:, q * 128:(q + 1) * 128], lhsT=idf[:], rhs=cm[:, z, 2, :], start=False, stop=True)
                        return ins

                    P.pe(dm, ["LFm"], ["ps%d" % bank])
                    P.act(lambda e, g0=g0, ng=ng, bank=bank, z=z: e.activation(out=DT[:, z, g0:g0 + ng, :].rearrange("p t s -> p (t s)"), in_=PS[bank][:, 0:ng * 128], func=AF.Exp),
                          ["ps%d" % bank], ["DT%d" % z])
                    bank2 = 4 + (g0 // 4) % 2

                    def em(e, g0=g0, ng=ng, bank2=bank2, z=z):
                        ins = None
                        for q in range(ng):
                            ins = e.matmul(PS[bank2][:, q * 128:(q + 1) * 128], lhsT=T2[:, g0 + q, :], rhs=cm[:, z, 0, :], start=True, stop=True)
                        return ins

                    P.pe(em, ["T2"], ["ps%d" % bank2])
                    P.act(lambda e, g0=g0, ng=ng, bank2=bank2, z=z: e.activation(out=EBb[:, z, g0:g0 + ng, :].rearrange("p t s -> p (t s)"), in_=PS[bank2][:, 0:ng * 128], func=AF.Exp),
                          ["ps%d" % bank2], ["EBb%d" % z])
                P.mm(PS[3][:, 0:NT], [(cm[:, z, 1, :], nlfc)], ["gcol"], ["ps3"])
                P.dve(lambda e, lic=lic: e.tensor_tensor(out=tc_[:], in0=PS[3][:, 0:NT], in1=lic, op=ALU.add), ["ps3", "gcol"], ["tcol"])
                P.act(lambda e: e.activation(out=wkc[:], in_=tc_[:], func=AF.Exp), ["tcol"], ["wkc"])
                for hf in range(2):
                    P.dve(lambda e, z=z, hf=hf: e.tensor_scalar(out=wkh[:, z, hf, :], in0=wkc[:], scalar1=self.halfm[:, hf:hf + 1], scalar2=None, op0=ALU.mult),
                          ["wkc"], ["wkh%d" % z])
                P.dve(lambda e, z=z: e.tensor_copy(out=wkh16[:, z, :, :], in_=wkh[:, z, :, :]), ["wkh%d" % z], ["wkh16_%d" % z])
                P.dve(lambda e, nlfc=nlfc: e.tensor_tensor(out=r2[:], in0=nlfc.unsqueeze(2).to_broadcast([128, NT, 2]),
                                                           in1=self.halfm[:].unsqueeze(1).to_broadcast([128, NT, 2]), op=ALU.mult), ["gcol"], ["r2"])
                P.mm(PS[2][:, 64:64 + 2 * NT], [(self.onesf[:], r2[:].rearrange("p t h -> p (t h)"))], ["r2"], ["ps2"])
                P.act(lambda e, z=z: e.activation(out=dec[:, z, :, :].rearrange("p t h -> p (t h)"), in_=PS[2][:, 64:64 + 2 * NT], func=AF.Exp, scale=-1.0),
                      ["ps2"], ["dec%d" % z])
            self.refence()
            if self.dbg == "B2":
                raise StopIteration
            for z in range(2):
                for h in range(2):
                    P.pool(lambda e, z=z, h=h: e.memset(qs[z][h][:], 0.0), [], ["qs%d%d" % (z, h)])
            visited = set()
            pending = []

            def rec(z, tt, hd=hd):
                tk = slice(tt * 128, (tt + 1) * 128)
                kz = str(z)
                X, A, C0 = 4 * z, 4 * z + 1, 4 * z + 2
                halves = (0, 1) if z == 0 else (1, 0)
                P.mm(PS[X][:, 0:128], [(kT[:, c, tk], qT[:, c, tk]) for c in range(4)], [], ["psXs" + kz])
                yield
                P.dve(lambda e: e.tensor_tensor(out=PTt[z][:], in0=PS[X][:, 0:128], in1=DT[:, z, tt, :], op=ALU.mult), ["psXs" + kz], ["PT" + kz])
                yield
                for hf in range(2):
                    cs = slice(hf * 64, hf * 64 + 64)
                    tq = slice(tt * 128 + hf * 64, tt * 128 + hf * 64 + 64)
                    P.pool(lambda e, hf=hf, cs=cs, tq=tq: e.tensor_tensor(out=qs[z][hf][:, :, cs], in0=qT[:, :, tq],
                                                                        in1=EBb[:, z, tt, cs].unsqueeze(1).to_broadcast([128, 4, 64]), op=ALU.mult),
                           [], ["qs%s%d" % (kz, hf)])
                    yield
                    P.pool(lambda e, hf=hf: e.tensor_tensor(out=vw[z][hf][:], in0=vtm[:, tt, :], in1=wkh[:, z, hf, tt:tt + 1].to_broadcast([128, 512]), op=ALU.mult),
                           [], ["vw%s%d" % (kz, hf)])
                    yield

                def mA(e):
                    e.matmul(PS[A][:, :], lhsT=PTt[z][:], rhs=vtm[:, tt, :], start=True, stop=False)
                    return e.matmul(PS[X][:, 128:129], lhsT=PTt[z][:], rhs=self.onesb[:, 0:1], start=True, stop=True)

                P.pe(mA, ["PT" + kz], ["psA" + kz, "psXd" + kz])
                yield
                for hi, hf in enumerate(halves):
                    lastg = hi == 1

                    def mB(e, hf=hf, lastg=lastg, hi=hi):
                        ins = None
                        for c in range(4):
                            e.matmul(PS[A][:, :], lhsT=qs[z][hf][:, c, :], rhs=Cbf[z][:, c, :], start=False, stop=(lastg and c == 3))
                        for c in range(4):
                            ins = e.matmul(PS[X][:, 129 + hi:130 + hi], lhsT=qs[z][hf][:, c, :], rhs=nbf[:, z, c:c + 1], start=(c == 0), stop=(c == 3))
                        return ins

                    P.pe(mB, ["qs%s%d" % (kz, hf), "Cbf" + kz, "nbf" + kz], ["psA" + kz, "psXd" + kz])
                    yield
                    dcol = dec[:, z, tt, hf:hf + 1]
                    for pr in range(2):
                        def mC(e, hf=hf, pr=pr):
                            ins = None
                            for q in range(2):
                                c = 2 * pr + q
                                ins = e.matmul(PS[C0 + q][:, :], lhsT=ktm[:, tt, c * 128:(c + 1) * 128], rhs=vw[z][hf][:], start=True, stop=True)
                            return ins

                        P.pe(mC, ["vw%s%d" % (kz, hf)], ["psC" + kz])
                        yield
                        cc = slice(2 * pr, 2 * pr + 2)
                        for q in range(2):
                            P.dve(lambda e, q=q, pr=pr, dcol=dcol: e.scalar_tensor_tensor(out=C32[z][:, 2 * pr + q, :], in0=C32[z][:, 2 * pr + q, :],
                                                                                      scalar=dcol, in1=PS[C0 + q][:, :], op0=ALU.mult, op1=ALU.add),
                                  ["psC" + kz, "C32_%s_%d" % (kz, pr)], ["C32_%s_%d" % (kz, pr)])
                            yield
                        P.act(lambda e, cc=cc: e.copy(out=Cbf[z][:, cc, :], in_=C32[z][:, cc, :]), ["C32_%s_%d" % (kz, pr)], ["Cbf" + kz])
                        yield

                    def nm(e, hf=hf, hi=hi):
                        ins = None
                        for c in range(4):
                            ins = e.matmul(PS[X][:, 136 + hi * 4 + c:137 + hi * 4 + c], lhsT=ktm[:, tt, c * 128:(c + 1) * 128], rhs=wkh16[:, z, hf, tt:tt + 1], start=True, stop=True)
                        return ins

                    P.pe(nm, [], ["psXn%s%d" % (kz, hi)])
                    yield
                    P.dve(lambda e, hi=hi, dcol=dcol: e.scalar_tensor_tensor(out=nst[:, z, :], in0=nst[:, z, :], scalar=dcol, in1=PS[X][:, 136 + hi * 4:140 + hi * 4],
                                                                         op0=ALU.mult, op1=ALU.add), ["psXn%s%d" % (kz, hi), "nstate" + kz], ["nstate" + kz])
                    yield
                    P.act(lambda e: e.copy(out=nbf[:, z, :], in_=nst[:, z, :]), ["nstate" + kz], ["nbf" + kz])
                    yield
                d = dn[z]
                kd = "dn" + kz
                P.dve(lambda e: e.tensor_reduce(out=d[:, 4:5], in_=PS[X][:, 128:131], axis=AX.X, op=ALU.add), ["psXd" + kz], [kd])
                yield
                P.dve(lambda e: e.tensor_scalar(out=d[:, 0:1], in0=d[:, 4:5], scalar1=-1.0, scalar2=None, op0=ALU.mult), [kd], [kd])
                yield
                P.dve(lambda e: e.tensor_tensor(out=d[:, 1:2], in0=d[:, 4:5], in1=d[:, 0:1], op=ALU.max), [kd], [kd])
                yield
                P.dve(lambda e: e.tensor_tensor(out=d[:, 2:3], in0=d[:, 1:2], in1=emc[:, z * 4 + hd:z * 4 + hd + 1], op=ALU.max), [kd], [kd])
                yield
                P.dve(lambda e: e.reciprocal(out=d[:, 3:4], in_=d[:, 2:3]), [kd], [kd])
                yield
                if tt not in visited:
                    visited.add(tt)
                    P.act(lambda e: e.activation(out=H[:, tt, :], in_=PS[A][:, :], func=AF.Copy, scale=d[:, 3:4]), ["psA" + kz, kd], ["H%d" % tt])
                    yield
                    return
                P.dve(lambda e: e.scalar_tensor_tensor(out=hs[z][:], in0=PS[A][:, :], scalar=d[:, 3:4], in1=H[:, tt, :], op0=ALU.mult, op1=ALU.add),
                      ["psA" + kz, kd, "H%d" % tt], ["hs" + kz])
                yield
                pending.append(outg(z, tt))

            def outg(z, tt, hd=hd):
                tk = slice(tt * 128, (tt + 1) * 128)
                kz = str(z)
                X = 4 * z
                o = ost[z]
                ko = "ost" + kz
                P.dve(lambda e: e.tensor_reduce(out=o[:, 0:1], in_=hs[z][:], axis=AX.X, op=ALU.add), ["hs" + kz], [ko])
                yield
                P.act(lambda e: e.activation(out=self.junk[:, z * 512:(z + 1) * 512], in_=hs[z][:], func=AF.Square, accum_out=o[:, 1:2]), ["hs" + kz], ["junk" + kz, ko])
                yield
                P.dve(lambda e: e.tensor_scalar(out=o[:, 2:3], in0=o[:, 0:1], scalar1=1.0 / 512, scalar2=None, op0=ALU.mult), [ko], [ko])
                yield
                P.dve(lambda e: e.tensor_tensor(out=o[:, 3:4], in0=o[:, 2:3], in1=o[:, 2:3], op=ALU.mult), [ko], [ko])
                yield
                P.dve(lambda e: e.scalar_tensor_tensor(out=o[:, 4:5], in0=o[:, 1:2], scalar=1.0 / 512, in1=o[:, 3:4], op0=ALU.mult, op1=ALU.subtract), [ko], [ko])
                yield
                P.act(lambda e: e.activation(out=o[:, 5:6], in_=o[:, 4:5], func=AF.Sqrt, bias=self.epsc[:, 0:1], scale=1.0), [ko], [ko])
                yield
                P.dve(lambda e: e.reciprocal(out=o[:, 6:7], in_=o[:, 5:6]), [ko], [ko])
                yield
                P.dve(lambda e: e.scalar_tensor_tensor(out=o[:, 7:8], in0=o[:, 2:3], scalar=-1.0, in1=o[:, 6:7], op0=ALU.mult, op1=ALU.mult), [ko], [ko])
                yield
                P.act(lambda e: e.activation(out=hn[z][:], in_=hs[z][:], func=AF.Identity, scale=o[:, 6:7], bias=o[:, 7:8]), ["hs" + kz, ko], ["hn" + kz])
                yield
                gb_ = mcol[:, 0, hd * 4:(hd + 1) * 4].unsqueeze(2).to_broadcast([128, 4, 128])
                for pp in range(2):
                    def otr(e, pp=pp):
                        ins = None
                        for q in range(2):
                            c = 2 * pp + q
                            ins = e.matmul(PS[X][:, 256 + q * 128:256 + (q + 1) * 128], lhsT=hn[z][:, c * 128:(c + 1) * 128], rhs=self.identb[:], start=True, stop=True)
                        return ins

                    P.pe(otr, ["hn" + kz], ["psXt" + kz])
                    yield
                    P.dve(lambda e, pp=pp: e.tensor_tensor(out=actt[z][:, 2 * pp:2 * pp + 2, :], in0=PS[X][:, 256:512].rearrange("p (c t) -> p c t", c=2),
                                                         in1=gb_[:, 2 * pp:2 * pp + 2, :], op=ALU.mult), ["psXt" + kz], ["actt" + kz])
                    yield
                P.dma(uct[z][:], self.d_uc[hd * 512:(hd + 1) * 512, tk].rearrange("(c p) t -> p c t", p=128), writes=["uct" + kz])
                P.dma(szt[z][:], self.d_sz[hd * 512:(hd + 1) * 512, tk].rearrange("(c p) t -> p c t", p=128), writes=["szt" + kz])
                sb_ = mcol[:, 1, hd * 4:(hd + 1) * 4].unsqueeze(2).to_broadcast([128, 4, 128])
                P.pool(lambda e: e.tensor_tensor(out=oa[z][:], in0=uct[z][:], in1=sb_, op=ALU.mult), ["uct" + kz], ["oa" + kz])
                yield
                P.pool(lambda e: e.tensor_tensor(out=oa[z][:], in0=oa[z][:], in1=actt[z][:], op=ALU.add), ["oa" + kz, "actt" + kz], ["oa" + kz])
                yield
                P.pool(lambda e: e.tensor_tensor(out=actt[z][:], in0=oa[z][:], in1=szt[z][:], op=ALU.mult), ["oa" + kz, "szt" + kz], ["actt" + kz])
                yield
                P.dma(self.d_act[hd * 512:(hd + 1) * 512, tk].rearrange("(c p) t -> p c t", p=128), actt[z][:], reads=["actt" + kz], writes=["dact_h%d_%d" % (hd, tt)])
                yield

            def drive(gens):
                gens = list(gens)
                while gens:
                    for g in list(gens):
                        try:
                            next(g)
                        except StopIteration:
                            gens.remove(g)

            for step in range(NT):
                cur = pending
                pending = []
                import os as _os
                if _os.environ.get("KSKIPOUT"):
                    cur = []
                if _os.environ.get("KSEQ"):
                    drive([rec(0, order[0][step])]); drive([rec(1, order[1][step])])
                    for g_ in cur:
                        drive([g_])
                else:
                    drive([rec(0, order[0][step]), rec(1, order[1][step])] + cur)
                if self.dbg == "S%d" % step:
                    raise StopIteration
            drive(pending)
            pending = []
            self.refence()
        self.outproj(i, 16, I["ml_w_out"][j], None, list(range(NT)))

    def natten(self, i, j, last):
        P, I, PS = self.P, self.I, self.PS
        hT = self.hA
        P.sb_off = self.top
        load = self.stager("nstg", 1, 3072)
        wq = [P.sb("nwq%d" % s, [128, 8, 3, 128], BF16) for s in range(2)]
        bcol = P.sb("nbcol", [128, 2, 8], F32)
        bvr = P.sb("nbvr", [128, D], F32)
        qc = [P.sb("nqc%d" % s, [128, TOK], BF16) for s in range(2)]
        kc = [P.sb("nkc%d" % s, [128, TOK], BF16) for s in range(2)]
        vc = [P.sb("nvc%d" % s, [128, NT, 128], BF16) for s in range(2)]
        oc_ = [P.sb("noc%d" % s, [128, TOK], BF16) for s in range(2)]
        nabt = [P.sb("nabt%d" % s, [128, 2, 5, 576], F32) for s in range(2)]
        S = [P.sb("nS%d" % s, [128, 832], F32) for s in range(4)]
        Pm = [P.sb("nPm%d" % s, [128, 832], BF16) for s in range(4)]
        PT = [P.sb("nPT%d" % s, [128, 7, 128], BF16) for s in range(4)]
        otm = [P.sb("notm%d" % s, [128, 128], BF16) for s in range(2)]
        sst = [P.sb("nsst%d" % s, [128, 4], F32) for s in range(4)]
        self.load_cols(bcol[:, 0, :], I["na_b_qkv"][j, 0:D], "nbcol")
        self.load_cols(bcol[:, 1, :], I["na_b_qkv"][j, D:2 * D], "nbcol")
        P.dve(lambda e: e.tensor_scalar(out=bcol[:, 0, :], in0=bcol[:, 0, :], scalar1=0.125, scalar2=None, op0=ALU.mult), ["nbcol"], ["nbcol"])
        P.dma(bvr[:], I["na_b_qkv"][j, 2 * D:3 * D].partition_broadcast(128), writes=["nbvr"])
        Wq = I["na_w_qkv"][j]
        nb = 0
        nu = 0
        qtiles = list(range(NT)) if not last else list(range(2, NT))
        for c in range(8):
            s = c % 2
            load(wq[s][:], [(m * 1024, 1024, Wq[:, m * D + c * 128:m * D + (c + 1) * 128].rearrange("(k p) o -> p k o", p=128),
                             ("p (k o) -> p k o", dict(k=8))) for m in range(3)], ["nwq%d" % s],
                 shape=("p (m k o) -> p k m o", dict(m=3, k=8)))
            P.dma(nabt[s][:], I["nab"][j, 2 * c:2 * c + 2].rearrange("h r q n -> q h r n"), writes=["nabt%d" % s])
            for (t0, n) in TBLK:
                for m, dst, kk in ((0, qc[s], "nqc%d" % s), (1, kc[s], "nkc%d" % s)):
                    bank = nb % 4
                    nb += 1
                    P.mm(PS[bank][:, 0:n], [(wq[s][:, k, m, :], hT[:, k, t0:t0 + n]) for k in range(8)], ["nwq%d" % s] + tkeys("h1_", t0, n), ["ps%d" % bank])
                    P.act(lambda e, bank=bank, n=n, dst=dst, t0=t0, m=m, c=c: e.activation(out=dst[:, t0:t0 + n], in_=PS[bank][:, 0:n], func=AF.Identity,
                                                                                       bias=bcol[:, m, c:c + 1], scale=(0.125 if m == 0 else 1.0)),
                          ["ps%d" % bank, "nbcol"], [kk])
            for g0 in range(0, NT, 4):
                ng = min(4, NT - g0)
                bank = nb % 4
                nb += 1

                def vm(e, g0=g0, ng=ng, bank=bank, s=s):
                    ins = None
                    for q in range(ng):
                        tk = slice((g0 + q) * 128, (g0 + q + 1) * 128)
                        for k in range(8):
                            ins = e.matmul(PS[bank][:, q * 128:(q + 1) * 128], lhsT=hT[:, k, tk], rhs=wq[s][:, k, 2, :], start=(k == 0), stop=(k == 7))
                    return ins

                P.pe(vm, ["nwq%d" % s] + tkeys("h1_", g0 * 128, ng * 128), ["ps%d" % bank])
                P.dve(lambda e, g0=g0, ng=ng, bank=bank, s=s, c=c: e.tensor_tensor(out=vc[s][:, g0:g0 + ng, :], in0=PS[bank][:, 0:ng * 128].rearrange("p (t o) -> p t o", t=ng),
                                                                                  in1=bvr[:, c * 128:(c + 1) * 128].unsqueeze(1).to_broadcast([128, ng, 128]), op=ALU.add),
                      ["ps%d" % bank, "nbvr"], ["nvc%d" % s])
            def unit(n, tt, hh, s=s, c=c):
                tq = slice(tt * 128, (tt + 1) * 128)
                if tt >= 2:
                    r = 2 * (tt - 2)
                    if r <= 2:
                        rs0, nkr, cls = 0, 8, r // 2
                    elif r >= 28:
                        rs0, nkr, cls = 24, 8, 3 + (r - 28) // 2
                    else:
                        rs0, nkr, cls = r - 4, 9, 2
                    nl = nkr * 64
                    ks = CTX + rs0 * 64
                else:
                    nl, ks, cls = 0, 0, 0
                ntot = nl + CTX
                osl = (n // 2) % 2
                pb = slice(hh * 64, hh * 64 + 64)
                u = n % 4
                ub = 2 * u
                k0, k1 = "ps%d" % ub, "ps%d" % (ub + 1)
                kS, kP, kPT, kst = "nS%d" % u, "nPm%d" % u, "nPT%d" % u, "nsst%d" % u
                q_ap = qc[s][pb, tq]
                if nl > 0:
                    P.mm(PS[ub][:, 0:512], [(q_ap, kc[s][pb, ks:ks + 512])], ["nqc%d" % s, "nkc%d" % s], [k0])
                    yield
                    P.dve(lambda e: e.tensor_tensor(out=S[u][:, 0:512], in0=PS[ub][:, 0:512], in1=nabt[s][:, hh, cls, 0:512], op=ALU.add),
                          [k0, "nabt%d" % s], [kS])
                    yield

                def sc2(e):
                    if nl > 512:
                        e.matmul(PS[ub + 1][:, 0:64], lhsT=q_ap, rhs=kc[s][pb, ks + 512:ks + 576], start=True, stop=True)
                    return e.matmul(PS[ub + 1][:, 64:320], lhsT=q_ap, rhs=kc[s][pb, 0:CTX], start=True, stop=True)

                P.pe(sc2, ["nqc%d" % s, "nkc%d" % s], [k1])
                yield
                if nl > 512:
                    P.dve(lambda e: e.tensor_tensor(out=S[u][:, 512:576], in0=PS[ub + 1][:, 0:64], in1=nabt[s][:, hh, cls, 512:576], op=ALU.add),
                          [k1, "nabt%d" % s], [kS])
                    yield
                P.act(lambda e: e.copy(out=S[u][:, nl:nl + CTX], in_=PS[ub + 1][:, 64:320]), [k1], [kS])
                yield
                st = sst[u]
                P.dve(lambda e: e.tensor_reduce(out=st[:, 0:1], in_=S[u][:, 0:ntot], axis=AX.X, op=ALU.max), [kS], [kst])
                yield
                P.dve(lambda e: e.tensor_scalar(out=st[:, 1:2], in0=st[:, 0:1], scalar1=-1.0, scalar2=None, op0=ALU.mult), [kst], [kst])
                yield
                P.act(lambda e: e.activation(out=Pm[u][:, 0:ntot], in_=S[u][:, 0:ntot], func=AF.Exp, bias=st[:, 1:2], scale=1.0, accum_out=st[:, 2:3]),
                      [kS, kst], [kP, kst])
                yield
                P.dve(lambda e: e.reciprocal(out=st[:, 3:4], in_=st[:, 2:3]), [kst], [kst])
                yield
                chunks = []
                off = 0
                while off < nl:
                    kn = min(128, nl - off)
                    chunks.append((off, kn, vc[s][0:kn, (ks + off) // 128, hh * 64:(hh + 1) * 64]))
                    off += kn
                for q in range(2):
                    chunks.append((nl + q * 128, 128, vc[s][:, q, hh * 64:(hh + 1) * 64]))
                pv = PS[ub][:].bitcast(BF16)

                def ptr(e):
                    ins = None
                    for ci, (off, kn, _) in enumerate(chunks):
                        ins = e.transpose(pv[0:kn, ci * 128:(ci + 1) * 128], Pm[u][:, off:off + kn], self.identb[:])
                    return ins

                P.pe(ptr, [kP], [k0])
                yield
                nch = len(chunks)
                P.act(lambda e: e.copy(out=PT[u][:, 0:nch, :].rearrange("p c q -> p (c q)"), in_=pv[:, 0:nch * 128]), [k0], [kPT])
                yield
                P.mm(PS[ub + 1][:, 320:384], [(PT[u][0:kn, ci, :], vap) for ci, (off, kn, vap) in enumerate(chunks)],
                     [kPT, "nvc%d" % s], [k1])
                yield
                P.act(lambda e: e.activation(out=otm[osl][:, hh * 64:(hh + 1) * 64], in_=PS[ub + 1][:, 320:384],
                                             func=AF.Copy, scale=st[:, 3:4]), [k1, kst], ["notm%d" % osl])
                yield
                if hh == 1:
                    pv3 = PS[ub + 1][:].bitcast(BF16)
                    P.pe(lambda e: e.transpose(pv3[:, 768:896], otm[osl][:], self.identb[:]), ["notm%d" % osl], [k1])
                    yield
                    P.dve(lambda e: e.tensor_copy(out=oc_[s][:, tq], in_=pv3[:, 768:896]), [k1], ["noc%d" % s])
                    yield

            units = [(tt, hh) for tt in qtiles for hh in range(2)]
            active = []
            nxt = 0
            while nxt < len(units) or active:
                while len(active) < 4 and nxt < len(units):
                    active.append(unit(nxt, units[nxt][0], units[nxt][1]))
                    nxt += 1
                for g in list(active):
                    try:
                        next(g)
                    except StopIteration:
                        active.remove(g)
            if not last:
                P.dma(self.d_act[c * 128:(c + 1) * 128, 0:CTX], oc_[s][:, 0:CTX], reads=["noc%d" % s], writes=["dactn%d" % c])
            P.dma(self.d_act[c * 128:(c + 1) * 128, CTX:TOK], oc_[s][:, CTX:TOK], reads=["noc%d" % s], writes=["dactn%d" % c])
        self.refence()
        self.outproj(i, 8, I["na_w_out"][j], I["na_b_out"][j], qtiles)

    def build(self):
        P = self.P
        self.declare()
        self.consts()
        self.mods()
        self.persist()
        self.first_norm()
        try:
            self.layers()
        except StopIteration:
            pass
        P.emit()
        return self.nc

    def layers(self):
        for i in range(self.depth):
            last = i == self.depth - 1
            j = i // 2
            if i % 2 == 0:
                self.mlstm(i, j)
            else:
                self.natten(i, j, last)
            self.ffn(i, last)


def host_inputs(inputs, depth=4):
    f = lambda a: np.ascontiguousarray(np.asarray(a, dtype=np.float32))
    x, c, ctx, c_ctx = f(inputs["x"]), f(inputs["c"]), f(inputs["ctx"]), f(inputs["c_ctx"])
    B = x.shape[0]
    shared = {}
    for k in ("ada_w", "ada_b", "norm1_g", "norm2_g", "final_g", "ml_w_in", "ml_conv_w", "ml_conv_b", "ml_skip", "ml_hnorm_g", "ml_w_out",
              "na_w_qkv", "na_b_qkv", "na_w_out", "na_b_out", "ff_w_up", "ff_b_up", "ff_conv_w", "ff_conv_b", "ff_w_down", "ff_b_down"):
        shared[k] = f(inputs[k])
    n_a = shared["ml_w_in"].shape[0]
    for nm, src in (("bdq", "ml_wq"), ("bdk", "ml_wk"), ("bdv", "ml_wv")):
        w = f(inputs[src])
        bd = np.zeros((n_a, 16, 128, 128), np.float32)
        wr = w.reshape(n_a, 16, 32, 4, 4)
        for n in range(32):
            bd[:, :, 4 * n:4 * n + 4, 4 * n:4 * n + 4] = wr[:, :, n]
        shared[nm] = bd
        shared[nm + "T"] = np.ascontiguousarray(bd.transpose(0, 1, 3, 2))
    gw = f(inputs["ml_gate_w"])
    g2 = gw.reshape(n_a, 2, 3, 16, 128, 2, 4)
    g2 = g2.transpose(0, 2, 3, 4, 5, 1, 6)
    shared["gw"] = np.ascontiguousarray(g2.reshape(n_a, 3, 16, 128, 16))
    gb = f(inputs["ml_gate_b"]).reshape(n_a, 2, 2, 4).transpose(0, 2, 1, 3)
    shared["gb"] = np.ascontiguousarray(gb.reshape(n_a, 16))
    rpb = f(inputs["na_rpb"])
    n_b = rpb.shape[0]
    nab = np.full((n_b, 16, 5, 128, 576), NEGV, np.float32)
    col = np.arange(64)
    cs = np.clip(col - 8, 0, 48)
    ok = (col[None, :] >= cs[:, None]) & (col[None, :] < cs[:, None] + 16)
    dc = np.clip(col[None, :] - col[:, None] + 15, 0, 30)
    for cls, r in enumerate((0, 2, 4, 28, 30)):
        for rho in range(2):
            rq = r + rho
            rs_q = int(np.clip(rq - 4, 0, 24))
            rs0 = int(np.clip(r - 4, 0, 24))
            nkr = 9 if cls == 2 else 8
            for jj in range(nkr):
                krow = rs0 + jj
                if krow < rs_q or krow >= rs_q + 8:
                    continue
                dr = krow - rq + 7
                vals = rpb[:, :, dr, :][:, :, dc]
                blk = np.where(ok[None, None], vals, np.float32(NEGV))
                nab[:, :, cls, rho * 64:(rho + 1) * 64, jj * 64:(jj + 1) * 64] = blk
    shared["nab"] = nab
    u = np.arange(128)
    same = (u[:, None] // 64) == (u[None, :] // 64)
    cm = np.zeros((2, 3, 128, 128), np.float32)
    cm[0, 0] = same & (u[:, None] <= u[None, :])
    cm[0, 1] = -(same & (u[:, None] > u[None, :])).astype(np.float32)
    cm[0, 2] = np.where(same & (u[:, None] <= u[None, :]), 0.0, NEGV)
    cm[1, 0] = same & (u[:, None] >= u[None, :])
    cm[1, 1] = -(same & (u[:, None] < u[None, :])).astype(np.float32)
    cm[1, 2] = np.where(same & (u[:, None] >= u[None, :]), 0.0, NEGV)
    shared["cmask"] = cm
    shared["identf"] = np.eye(128, dtype=np.float32)
    hm = np.zeros((128, 2), np.float32)
    hm[:64, 0] = 1.0
    hm[64:, 1] = 1.0
    shared["halfm"] = hm
    maps = []
    for b in range(B):
        m = dict(shared)
        m["x"] = x[b]
        m["ctx"] = ctx[b]
        m["cvec"] = np.ascontiguousarray(np.stack([c[b], c_ctx], 0))
        maps.append(m)
    return maps


_NC_CACHE = {}


def kernel(**inputs):
    maps = host_inputs(inputs)
    if "nc" not in _NC_CACHE:
        _NC_CACHE["nc"] = Builder(4).build()
    nc = _NC_CACHE["nc"]
    res = run_bass_kernel_spmd(nc, maps, core_ids=list(range(len(maps))))
    return np.stack([np.asarray(r["out"], dtype=np.float32) for r in res.results], 0)
```

```python
import contextlib
import itertools
import numpy as np
import concourse.bass as bass
import concourse.mybir as mybir
from concourse.bass_utils import run_bass_kernel_spmd

F32 = mybir.dt.float32
BF16 = mybir.dt.bfloat16
AF = mybir.ActivationFunctionType
ALU = mybir.AluOpType
AX = mybir.AxisListType

D = 1024
SEQ = 2048
CTX = 256
TOK = SEQ + CTX
NT = TOK // 128
EPS = 1e-6
FF = 2816
NEGV = -30000.0
TBLK = [(0, 256), (256, 512), (768, 512), (1280, 512), (1792, 512)]


class Prog:
    ENGS = ("pe", "act", "dve", "pool", "sp")
    NDMA = {"sp": 40, "pool": 16, "act": 8}

    def __init__(self, nc):
        self.nc = nc
        self.ops = []
        self.last_w = {}
        self.readers = {}
        self.sb_off = 16512
        self.ndma = {q: 0 for q in self.NDMA}
        self.fence_idx = None
        self.nname = 0

    def sb(self, name, shape, dtype):
        esz = 4 if dtype == F32 else 2
        n = 1
        for s in shape[1:]:
            n *= s
        nbytes = (n * esz + 63) // 64 * 64
        off = self.sb_off
        self.sb_off += nbytes
        assert self.sb_off <= 229344, (name, self.sb_off)
        self.nname += 1
        return self.nc.alloc_sbuf_tensor_at("%s_%d" % (name, self.nname), list(shape), dtype, offset=off)

    def add(self, eng, fn, reads=(), writes=(), dma=False):
        idx = len(self.ops)
        deps = set()
        if self.fence_idx is not None:
            deps.add(self.fence_idx)
        for k in reads:
            if k in self.last_w:
                deps.add(self.last_w[k])
        for k in writes:
            if k in self.last_w:
                deps.add(self.last_w[k])
            for r in self.readers.get(k, ()):
                deps.add(r)
        deps.discard(idx)
        for k in reads:
            self.readers.setdefault(k, []).append(idx)
        for k in writes:
            self.last_w[k] = idx
            self.readers[k] = []
        op = dict(eng=eng, fn=fn, deps=deps, dma=dma, sig=False)
        if dma:
            q = eng
            k = self.ndma[q]
            self.ndma[q] += 1
            op["dq"] = (q, k % self.NDMA[q])
            op["dval"] = 16 * (k // self.NDMA[q] + 1)
            op["dprev"] = 16 * (k // self.NDMA[q])
        self.ops.append(op)
        return idx

    def fence(self, scratch):
        deps = set(self.last_w.values())
        for rs in self.readers.values():
            deps.update(rs)
        idx = len(self.ops)
        if self.fence_idx is not None:
            deps.add(self.fence_idx)
        self.ops.append(dict(eng="pool", fn=lambda e: e.memset(scratch, 0.0), deps=deps, dma=False, sig=True))
        self.last_w = {}
        self.readers = {}
        self.fence_idx = idx

    def pe(self, fn, reads=(), writes=()):
        return self.add("pe", fn, reads, writes)

    def act(self, fn, reads=(), writes=()):
        return self.add("act", fn, reads, writes)

    def dve(self, fn, reads=(), writes=()):
        return self.add("dve", fn, reads, writes)

    def pool(self, fn, reads=(), writes=()):
        return self.add("pool", fn, reads, writes)

    def dma(self, out, in_, reads=(), writes=(), q="sp", **kw):
        return self.add(q, lambda e: e.dma_start(out=out, in_=in_, **kw), reads, writes, dma=True)

    def mm(self, out_ap, pairs, reads, writes):
        def fn(e):
            n = len(pairs)
            ins = None
            for i, (l, r) in enumerate(pairs):
                ins = e.matmul(out_ap, lhsT=l, rhs=r, start=(i == 0), stop=(i == n - 1))
            return ins

        return self.pe(fn, reads, writes)

    def emit(self):
        nc = self.nc
        ops = self.ops
        for op in ops:
            for d in op["deps"]:
                if ops[d]["eng"] == "pe" and op["eng"] == "pe" and not ops[d]["dma"]:
                    continue
                ops[d]["sig"] = True
        last_on = {}
        for i, op in enumerate(ops):
            if not op["dma"]:
                last_on[op["eng"]] = i
        for e, i in last_on.items():
            ops[i]["sig"] = True
        cnt = {e: 0 for e in self.ENGS}
        for op in ops:
            if op["sig"] and not op["dma"]:
                cnt[op["eng"]] += 1
                op["val"] = cnt[op["eng"]]
        with contextlib.ExitStack() as st:
            esem = {e: st.enter_context(nc.semaphore("c_" + e)) for e in self.ENGS}
            dsem = {
                q: [st.enter_context(nc.semaphore("d_%s%d" % (q, i))) for i in range(n)]
                for q, n in self.NDMA.items()
            }
            block = st.enter_context(nc.Block())

            def stream(ename):
                def body(e):
                    waited = {}

                    def wait(sem, key, val):
                        if waited.get(key, 0) >= val:
                            return
                        waited[key] = val
                        e.wait_ge(sem, val)

                    for op in ops:
                        if op["eng"] != ename:
                            continue
                        for d in sorted(op["deps"]):
                            dop = ops[d]
                            if dop["dma"]:
                                q, si = dop["dq"]
                                wait(dsem[q][si], ("d", q, si), dop["dval"])
                            else:
                                if dop["eng"] == "pe" and ename == "pe":
                                    continue
                                wait(esem[dop["eng"]], ("e", dop["eng"]), dop["val"])
                        if op["dma"]:
                            q, si = op["dq"]
                            if op["dprev"] > 0:
                                wait(dsem[q][si], ("d", q, si), op["dprev"])
                            ins = op["fn"](e)
                            ins.then_inc(dsem[q][si], 16)
                        else:
                            ins = op["fn"](e)
                            if op["sig"]:
                                ins.then_inc(esem[ename], 1)
                    if ename == "sp":
                        for q, n in self.NDMA.items():
                            tot = self.ndma[q]
                            for si in range(min(n, tot)):
                                k_last = ((tot - 1 - si) // n) * n + si
                                wait(dsem[q][si], ("d", q, si), 16 * (k_last // n + 1))
                        for en in self.ENGS:
                            if en != "sp" and cnt[en] > 0:
                                wait(esem[en], ("e", en), cnt[en])

                return body

            block.tensor(stream("pe"))
            block.scalar(stream("act"))
            block.vector(stream("dve"))
            block.gpsimd(stream("pool"))
            block.sync(stream("sp"))


def tkeys(prefix, start, n):
    return ["%s%d" % (prefix, t) for t in range(start // 128, (start + n + 127) // 128)]


def pcol(t):
    return 1 + t if t < CTX else 2 + t


class Builder:
    def __init__(self, depth=4, dbg=None):
        self.depth = depth
        self.dbg = dbg
        nc = self.nc = bass.Bass("TRN2", target_bir_lowering=False)
        self.P = Prog(nc)
        self.I = {}
        psall = nc.alloc_psum_tensor("psall", [128, 4096], F32)
        self.PS = [psall[:, i * 512:(i + 1) * 512] for i in range(8)]
        self.PSC = [psall[:, (4 * z + 2) * 512:(4 * z + 4) * 512] for z in range(2)]

    def inp(self, name, shape, dtype=F32):
        self.I[name] = self.nc.dram_tensor(name, list(shape), dtype, kind="ExternalInput").ap()
        return self.I[name]

    def scratch(self, name, shape, dtype):
        return self.nc.dram_tensor(name, list(shape), dtype, kind="Internal").ap()

    def declare(self):
        n_a, n_b = 2, 2
        inp = self.inp
        inp("x", [SEQ, D]); inp("ctx", [CTX, D]); inp("cvec", [2, D])
        inp("ada_w", [4, D, 6 * D]); inp("ada_b", [4, 6 * D])
        inp("norm1_g", [4, D]); inp("norm2_g", [4, D]); inp("final_g", [D])
        inp("ml_w_in", [n_a, D, 4096]); inp("ml_conv_w", [n_a, 5, 2048]); inp("ml_conv_b", [n_a, 2048])
        inp("bdq", [n_a, 16, 128, 128]); inp("bdk", [n_a, 16, 128, 128]); inp("bdv", [n_a, 16, 128, 128])
        inp("bdqT", [n_a, 16, 128, 128]); inp("bdkT", [n_a, 16, 128, 128]); inp("bdvT", [n_a, 16, 128, 128])
        inp("gw", [n_a, 3, 16, 128, 16]); inp("gb", [n_a, 16])
        inp("ml_skip", [n_a, 2048]); inp("ml_hnorm_g", [n_a, 2048]); inp("ml_w_out", [n_a, 2048, D])
        inp("na_w_qkv", [n_b, D, 3 * D]); inp("na_b_qkv", [n_b, 3 * D]); inp("nab", [n_b, 16, 5, 128, 576])
        inp("na_w_out", [n_b, D, D]); inp("na_b_out", [n_b, D])
        inp("ff_w_up", [4, D, 2 * FF]); inp("ff_b_up", [4, 2 * FF]); inp("ff_conv_w", [4, 3, 2 * FF])
        inp("ff_conv_b", [4, 2 * FF]); inp("ff_w_down", [4, FF, D]); inp("ff_b_down", [4, D])
        inp("cmask", [2, 3, 128, 128]); inp("identf", [128, 128]); inp("halfm", [128, 2])
        self.out = self.nc.dram_tensor("out", [SEQ, D], F32, kind="ExternalOutput").ap()
        sc = self.scratch
        self.Xs = sc("Xs", [TOK, D], F32)
        self.modd = sc("modd", [4, 2, 6 * D], F32)
        self.d_u = sc("d_u", [2048, TOK], BF16)
        self.d_uc = sc("d_uc", [2048, TOK], BF16)
        self.d_sz = sc("d_sz", [2048, TOK], BF16)
        self.d_act = sc("d_act", [FF, TOK], BF16)

    def consts(self):
        P, I = self.P, self.I
        self.identf = P.sb("identf", [128, 128], F32)
        self.identb = P.sb("identb", [128, 128], BF16)
        self.cm = P.sb("cm", [128, 2, 3, 128], F32)
        self.halfm = P.sb("halfm", [128, 2], F32)
        self.onesf = P.sb("onesf", [128, 128], F32)
        self.onesb = P.sb("onesb", [128, 2], BF16)
        self.epsc = P.sb("epsc", [128, 1], F32)
        self.fsc = P.sb("fsc", [128, 16], F32)
        self.junk = P.sb("junk", [128, 1024], BF16)
        P.dma(self.identf[:], I["identf"], writes=["identf"])
        P.dma(self.cm[:], I["cmask"].rearrange("z m p c -> p z m c"), writes=["cm"])
        P.dma(self.halfm[:], I["halfm"], writes=["halfm"])
        P.dve(lambda e: e.tensor_copy(out=self.identb[:], in_=self.identf[:]), ["identf"], ["identb"])
        P.dve(lambda e: e.memset(self.onesf[:], 1.0), [], ["onesf"])
        P.dve(lambda e: e.memset(self.onesb[:], 1.0), [], ["onesb"])
        P.dve(lambda e: e.memset(self.epsc[:], EPS), [], ["epsc"])
        self.const_keys = ["identf", "identb", "cm", "halfm", "onesf", "onesb", "epsc"]

    def refence(self):
        self.P.fence(self.fsc[:, 0:1])

    def mods(self):
        P, I, PS = self.P, self.I, self.PS
        m0 = P.sb_off
        s2 = P.sb("s2", [128, 8, 2], F32)
        stg = [P.sb("adastg%d" % i, [128, 8, 512], F32) for i in range(2)]
        modrow = P.sb("modrow", [2, 6 * D], F32)
        adab = P.sb("adab", [2, 6 * D], F32)
        g1b = P.sb("g1b", [2, D], F32)
        g2b = P.sb("g2b", [2, D], F32)
        for jj in range(2):
            P.dma(s2[:, :, jj], I["cvec"][jj].rearrange("(k p) -> p k", p=128), writes=["s2"], allow_slow_non_contiguous=True)
        P.act(lambda e: e.activation(out=s2[:], in_=s2[:], func=AF.Silu), ["s2"], ["s2"])
        n = 0
        for i in range(self.depth):
            P.dma(adab[:], I["ada_b"][i].partition_broadcast(2), writes=["adab"])
            P.dma(g1b[:], I["norm1_g"][i].partition_broadcast(2), writes=["g1b"])
            P.dma(g2b[:], I["norm2_g"][i].partition_broadcast(2), writes=["g2b"])
            for cb in range(12):
                sl = n % 2
                bank = 6 + (n % 2)
                n += 1
                P.dma(stg[sl][:], I["ada_w"][i][:, cb * 512:(cb + 1) * 512].rearrange("(k p) c -> p k c", p=128),
                      writes=["adastg%d" % sl])
                P.mm(PS[bank][0:2, :], [(s2[:, k, :], stg[sl][:, k, :]) for k in range(8)],
                     ["s2", "adastg%d" % sl], ["ps%d" % bank])
                P.dve(lambda e, bank=bank, cb=cb: e.tensor_tensor(out=modrow[:, cb * 512:(cb + 1) * 512], in0=PS[bank][0:2, :],
                                                                  in1=adab[:, cb * 512:(cb + 1) * 512], op=ALU.add),
                      ["ps%d" % bank, "adab"], ["modrow"])
            P.dve(lambda e: e.scalar_tensor_tensor(out=modrow[:, D:2 * D], in0=modrow[:, D:2 * D], scalar=1.0, in1=g1b[:],
                                                   op0=ALU.add, op1=ALU.mult), ["modrow", "g1b"], ["modrow"])
            P.dve(lambda e: e.scalar_tensor_tensor(out=modrow[:, 4 * D:5 * D], in0=modrow[:, 4 * D:5 * D], scalar=1.0, in1=g2b[:],
                                                   op0=ALU.add, op1=ALU.mult), ["modrow", "g2b"], ["modrow"])
            P.dma(self.modd[i], modrow[:], reads=["modrow"], writes=["modd%d" % i])
        self.refence()
        P.sb_off = m0

    def load_cols(self, dst, src1d, key):
        self.P.dma(dst, src1d.rearrange("(k p) -> p k", p=128), writes=[key], allow_slow_non_contiguous=True)

    def persist(self):
        P = self.P
        self.colm = P.sb("colm", [128, 4, 2, 6, 8], F32)
        self.grow_off = P.sb_off
        self.grow = P.sb("grow", [128, 2, 2, D], F32)
        self.nst = [P.sb("nst%d" % s, [128, 8], F32) for s in range(2)]
        self.xn = [P.sb("xn%d" % s, [128, D], BF16) for s in range(2)]
        for i in range(self.depth):
            for v in range(2):
                for s in (0, 1, 3, 4):
                    P.dma(self.colm[:, i, v, s, :], self.modd[i, v, s * D:(s + 1) * D].rearrange("(k p) -> p k", p=128),
                          reads=["modd%d" % i], writes=["colm"], allow_slow_non_contiguous=True)
        self.hA = P.sb("hA", [128, 8, TOK], BF16)
        self.hA_off = P.sb_off - 8 * TOK * 2
        self.top = P.sb_off

    def load_grow(self, i):
        P = self.P
        for v in range(2):
            P.dma(self.grow[:, v, 0, :], self.modd[i, v, 2 * D:3 * D].partition_broadcast(128), reads=["modd%d" % i], writes=["grow"])
            P.dma(self.grow[:, v, 1, :], self.modd[i, v, 5 * D:6 * D].partition_broadcast(128), reads=["modd%d" % i], writes=["grow"])

    def rms_stats(self, xt, xkey, sl):
        P = self.P
        st = self.nst[sl]
        ks = "nst%d" % sl
        P.act(lambda e: e.activation(out=self.junk[:], in_=xt, func=AF.Square, accum_out=st[:, 0:1]), [xkey], ["junk", ks])
        P.act(lambda e: e.activation(out=st[:, 1:2], in_=st[:, 0:1], func=AF.Sqrt, scale=1.0 / D, bias=self.epsc[:, 0:1]), [ks], [ks])
        P.dve(lambda e: e.reciprocal(out=st[:, 2:3], in_=st[:, 1:2]), [ks], [ks])
        return st, ks

    def norm_tile(self, xt, xkey, tt, sl, li, which, hname, bank):
        P, PS = self.P, self.PS
        v = 1 if tt < 2 else 0
        xn = self.xn[sl]
        kx = "xn%d" % sl
        st, ks = self.rms_stats(xt, xkey, sl)
        P.dve(lambda e: e.tensor_scalar(out=xn[:], in0=xt, scalar1=st[:, 2:3], scalar2=None, op0=ALU.mult), [xkey, ks], [kx])
        pv = PS[bank][:].bitcast(BF16)

        def tr(e):
            ins = None
            for c in range(8):
                ins = e.transpose(pv[:, c * 128:(c + 1) * 128], xn[:, c * 128:(c + 1) * 128], self.identb[:])
            return ins

        P.pe(tr, [kx], ["ps%d" % bank])
        sa, sb_ = (1, 0) if which == 0 else (4, 3)
        hk = "%s%d" % (hname, tt)
        for c in range(8):
            A = self.colm[:, li, v, sa, c:c + 1]
            B = self.colm[:, li, v, sb_, c:c + 1]
            dst = self.hA[:, c, tt * 128:(tt + 1) * 128]
            src = pv[:, c * 128:(c + 1) * 128]
            if c % 2 == 0:
                P.act(lambda e, A=A, B=B, dst=dst, src=src: e.activation(out=dst, in_=src, func=AF.Identity, scale=A, bias=B),
                      ["ps%d" % bank, "colm"], [hk])
            else:
                P.dve(lambda e, A=A, B=B, dst=dst, src=src: e.tensor_scalar(out=dst, in0=src, scalar1=A, scalar2=B, op0=ALU.mult, op1=ALU.add),
                      ["ps%d" % bank, "colm"], [hk])

    def first_norm(self):
        P, I = self.P, self.I
        P.sb_off = self.top
        xt = [P.sb("fxt%d" % s, [128, D], F32) for s in range(2)]
        for tt in range(NT):
            sl = tt % 2
            src = I["ctx"][tt * 128:(tt + 1) * 128, :] if tt < 2 else I["x"][(tt - 2) * 128:(tt - 1) * 128, :]
            P.dma(xt[sl][:], src, writes=["fxt%d" % sl])
            P.dma(self.Xs[tt * 128:(tt + 1) * 128, :], xt[sl][:], reads=["fxt%d" % sl], writes=["X%d" % tt])
            self.norm_tile(xt[sl][:], "fxt%d" % sl, tt, sl, 0, 0, "h1_", 6 + sl)
        self.refence()

    def stager(self, name, nbuf, cols):
        P = self.P
        bufs = [P.sb("%s%d" % (name, i), [128, cols], F32) for i in range(nbuf)]
        state = dict(n=0)

        def load(dst, srcs, dkeys, eng="pool", shape=None):
            sl = state["n"] % nbuf
            state["n"] += 1
            key = "%s%d" % (name, sl)
            tot = 0
            for off, ncols, ap, pat in srcs:
                d = bufs[sl][:, off:off + ncols]
                if pat is not None:
                    d = d.rearrange(pat[0], **pat[1])
                P.dma(d, ap, writes=[key], q="pool")
                tot = max(tot, off + ncols)
            src = bufs[sl][:, 0:tot]
            if shape is not None:
                src = src.rearrange(shape[0], **shape[1])
            P.add(eng, lambda e: e.tensor_copy(out=dst, in_=src), [key], dkeys)

        return load

    def ffn(self, i, last):
        P, I, PS = self.P, self.I, self.PS
        hT = self.hA
        P.sb_off = self.top
        tiles = list(range(NT)) if not last else list(range(2, NT))
        blks = TBLK if not last else TBLK[1:]
        W = 2307
        L = W - 2
        wdn = P.sb("wdn", [128, 22, D], BF16)
        bdn = P.sb("bdn", [128, D], F32)
        keep = P.sb_off
        bcol = P.sb("fbcol", [128, 5, 44], F32)
        wup = [P.sb("wup%d" % s, [128, 8, 2, 256], BF16) for s in range(2)]
        upad = [[P.sb("upad%d%d" % (s, g), [128, W], BF16) for g in range(2)] for s in range(2)]
        acc = [[P.sb("facc%d%d" % (s, g), [128, L], F32) for g in range(2)] for s in range(2)]
        actc = [P.sb("actc%d" % s, [128, L], BF16) for s in range(2)]
        self.load_cols(bcol[:, 0, :], I["ff_b_up"][i], "fbcol")
        for k in range(3):
            self.load_cols(bcol[:, 1 + k, :], I["ff_conv_w"][i, k], "fbcol")
        self.load_cols(bcol[:, 4, :], I["ff_conv_b"][i], "fbcol")
        for s in range(2):
            for g in range(2):
                P.pool(lambda e, s=s, g=g: e.memset(upad[s][g][:], 0.0), [], ["upad%d%d" % (s, g)])
        nb = 0
        for cg in range(11):
            ws = cg % 2
            for g in range(2):
                P.dma(wup[ws][:, :, g, :], I["ff_w_up"][i][:, g * FF + cg * 256:g * FF + (cg + 1) * 256].rearrange("(k p) c -> p k c", p=128),
                      writes=["wup%d" % ws], q="pool")
            if cg == 1:
                P.dma(bdn[:], I["ff_b_down"][i].partition_broadcast(128), writes=["bdn"])
                for c0 in range(0, 22, 2):
                    P.dma(wdn[:, c0:c0 + 2, :], I["ff_w_down"][i][c0 * 128:(c0 + 2) * 128, :].rearrange("(c p) o -> p c o", p=128), writes=["wdn"], q="pool")
            for cc in range(2):
                c = cg * 2 + cc
                s = c % 2
                for g in range(2):
                    col = c + 22 * g
                    ku, ka = "upad%d%d" % (s, g), "facc%d%d" % (s, g)
                    u, a = upad[s][g], acc[s][g]
                    for (t0, n) in blks:
                        bank = nb % 4
                        nb += 1
                        P.mm(PS[bank][:, 0:n], [(wup[ws][:, k, g, cc * 128:(cc + 1) * 128], hT[:, k, t0:t0 + n]) for k in range(8)],
                             ["wup%d" % ws] + tkeys("h2_", t0, n), ["ps%d" % bank])
                        P.act(lambda e, bank=bank, n=n, u=u, t0=t0, col=col: e.activation(
                            out=u[:, pcol(t0):pcol(t0) + n], in_=PS[bank][:, 0:n], func=AF.Identity,
                            bias=bcol[:, 0, col:col + 1], scale=1.0), ["ps%d" % bank, "fbcol"], [ku])
                    P.pool(lambda e, u=u, a=a, col=col: e.tensor_scalar(out=a[:], in0=u[:, 1:1 + L], scalar1=bcol[:, 2, col:col + 1],
                                                                        scalar2=bcol[:, 4, col:col + 1], op0=ALU.mult, op1=ALU.add),
                           [ku, "fbcol"], [ka])
                    P.dve(lambda e, u=u, a=a, col=col: e.scalar_tensor_tensor(out=a[:], in0=u[:, 0:L], scalar=bcol[:, 1, col:col + 1],
                                                                               in1=a[:], op0=ALU.mult, op1=ALU.add), [ku, ka, "fbcol"], [ka])
                    P.dve(lambda e, u=u, a=a, col=col: e.scalar_tensor_tensor(out=a[:], in0=u[:, 2:2 + L], scalar=bcol[:, 3, col:col + 1],
                                                                              in1=a[:], op0=ALU.mult, op1=ALU.add), [ku, ka, "fbcol"], [ka])
                P.act(lambda e, s=s: e.activation(out=acc[s][1][:], in_=acc[s][1][:], func=AF.Silu), ["facc%d1" % s], ["facc%d1" % s])
                P.dve(lambda e, s=s: e.tensor_tensor(out=actc[s][:], in0=acc[s][0][:], in1=acc[s][1][:], op=ALU.mult),
                      ["facc%d0" % s, "facc%d1" % s], ["actc%d" % s])
                if not last:
                    P.dma(self.d_act[c * 128:(c + 1) * 128, 0:CTX], actc[s][:, 0:CTX], reads=["actc%d" % s], writes=["dact%d" % c])
                P.dma(self.d_act[c * 128:(c + 1) * 128, CTX:TOK], actc[s][:, CTX + 1:TOK + 1], reads=["actc%d" % s], writes=["dact%d" % c])
        self.refence()
        P.sb_off = keep
        at = [P.sb("at%d" % s, [128, 22, 128], BF16) for s in range(2)]
        xt = [P.sb("xt%d" % s, [128, D], F32) for s in range(2)]
        tmp = [P.sb("ftmp%d" % s, [128, 512], F32) for s in range(2)]
        fg = None
        if last:
            fg = P.sb("fg", [128, D], F32)
            P.dma(fg[:], I["final_g"].partition_broadcast(128), writes=["fg"])
        for n_, tt in enumerate(tiles):
            sl = n_ % 2
            v = 1 if tt < 2 else 0
            P.dma(at[sl][:], self.d_act[:, tt * 128:(tt + 1) * 128].rearrange("(c p) t -> p c t", p=128), writes=["at%d" % sl])
            P.dma(xt[sl][:], self.Xs[tt * 128:(tt + 1) * 128, :], writes=["xt%d" % sl])
            for hf in range(2):
                bank = 2 * sl + hf
                P.mm(PS[bank][:, :], [(at[sl][:, c, :], wdn[:, c, hf * 512:(hf + 1) * 512]) for c in range(22)],
                     ["at%d" % sl], ["ps%d" % bank])
                tk = "ftmp%d" % hf
                P.dve(lambda e, bank=bank, hf=hf: e.tensor_tensor(out=tmp[hf][:], in0=PS[bank][:, :], in1=bdn[:, hf * 512:(hf + 1) * 512], op=ALU.add),
                      ["ps%d" % bank], [tk])
                P.pool(lambda e, hf=hf, v=v: e.tensor_tensor(out=tmp[hf][:], in0=tmp[hf][:], in1=self.grow[:, v, 1, hf * 512:(hf + 1) * 512], op=ALU.mult),
                       [tk, "grow"], [tk])
                P.pool(lambda e, hf=hf, sl=sl: e.tensor_tensor(out=xt[sl][:, hf * 512:(hf + 1) * 512], in0=xt[sl][:, hf * 512:(hf + 1) * 512], in1=tmp[hf][:], op=ALU.add),
                       [tk, "xt%d" % sl], ["xt%d" % sl])
            if last:
                st_, ks = self.rms_stats(xt[sl][:], "xt%d" % sl, sl)
                P.dve(lambda e, sl=sl, st_=st_: e.scalar_tensor_tensor(out=xt[sl][:], in0=xt[sl][:], scalar=st_[:, 2:3], in1=fg[:], op0=ALU.mult, op1=ALU.mult),
                      ["xt%d" % sl, ks, "fg"], ["xt%d" % sl])
                P.dma(self.out[(tt - 2) * 128:(tt - 1) * 128, :], xt[sl][:], reads=["xt%d" % sl], writes=["out%d" % tt])
            else:
                P.dma(self.Xs[tt * 128:(tt + 1) * 128, :], xt[sl][:], reads=["xt%d" % sl], writes=["X%d" % tt])
                self.norm_tile(xt[sl][:], "xt%d" % sl, tt, sl, i + 1, 0, "h1_", 6 + sl)
        self.refence()

    def outproj(self, i, nk, w_src, b_src, tiles):
        P, I, PS = self.P, self.I, self.PS
        P.sb_off = self.top
        self.load_grow(i)
        wo = P.sb("wo", [128, nk, D], BF16)
        at = [P.sb("oat%d" % s, [128, nk, 128], BF16) for s in range(2)]
        xt = [P.sb("oxt%d" % s, [128, D], F32) for s in range(2)]
        tmp = [P.sb("otmp%d" % s, [128, 512], F32) for s in range(2)]
        bo = None
        if b_src is not None:
            bo = P.sb("bo", [128, D], F32)
            P.dma(bo[:], b_src.partition_broadcast(128), writes=["bo"])
        for c0 in range(0, nk, 4):
            P.dma(wo[:, c0:c0 + 4, :], w_src[c0 * 128:(c0 + 4) * 128, :].rearrange("(c p) o -> p c o", p=128), writes=["wo"], q="pool")
        for n_, tt in enumerate(tiles):
            sl = n_ % 2
            v = 1 if tt < 2 else 0
            P.dma(at[sl][:], self.d_act[0:nk * 128, tt * 128:(tt + 1) * 128].rearrange("(c p) t -> p c t", p=128), writes=["oat%d" % sl])
            P.dma(xt[sl][:], self.Xs[tt * 128:(tt + 1) * 128, :], writes=["oxt%d" % sl])
            for hf in range(2):
                bank = 2 * sl + hf
                P.mm(PS[bank][:, :], [(at[sl][:, k, :], wo[:, k, hf * 512:(hf + 1) * 512]) for k in range(nk)], ["oat%d" % sl, "wo"], ["ps%d" % bank])
                tk = "otmp%d" % hf
                if bo is not None:
                    P.dve(lambda e, bank=bank, hf=hf: e.tensor_tensor(out=tmp[hf][:], in0=PS[bank][:, :], in1=bo[:, hf * 512:(hf + 1) * 512], op=ALU.add),
                          ["ps%d" % bank, "bo"], [tk])
                    P.pool(lambda e, hf=hf, v=v: e.tensor_tensor(out=tmp[hf][:], in0=tmp[hf][:], in1=self.grow[:, v, 0, hf * 512:(hf + 1) * 512], op=ALU.mult),
                           [tk, "grow"], [tk])
                else:
                    P.dve(lambda e, bank=bank, hf=hf, v=v: e.tensor_tensor(out=tmp[hf][:], in0=PS[bank][:, :], in1=self.grow[:, v, 0, hf * 512:(hf + 1) * 512], op=ALU.mult),
                          ["ps%d" % bank, "grow"], [tk])
                P.pool(lambda e, hf=hf, sl=sl: e.tensor_tensor(out=xt[sl][:, hf * 512:(hf + 1) * 512], in0=xt[sl][:, hf * 512:(hf + 1) * 512], in1=tmp[hf][:], op=ALU.add),
                       [tk, "oxt%d" % sl], ["oxt%d" % sl])
            P.dma(self.Xs[tt * 128:(tt + 1) * 128, :], xt[sl][:], reads=["oxt%d" % sl], writes=["X%d" % tt])
            self.norm_tile(xt[sl][:], "oxt%d" % sl, tt, sl, i, 1, "h2_", 6 + sl)
        self.refence()

    def mlstm(self, i, j):
        P, I, PS = self.P, self.I, self.PS
        hT = self.hA
        idf = self.identf
        P.sb_off = self.top
        gcol = P.sb("gcol", [128, NT, 16], F32)
        emc = P.sb("emc", [128, 8], F32)
        mcol = P.sb("mcol", [128, 2, 16], F32)
        bdb = P.sb("bdb", [128, 3, 16, 128], BF16)
        keepA = P.sb_off
        win = [P.sb("win%d" % s, [128, 8, 512], BF16) for s in range(2)]
        bdT_off = P.sb_off
        bdT = P.sb("bdT", [128, 3, 16, 128], F32)
        P.sb("bdTpad", [128, 1024], F32)
        gwt = P.sb("gwt", [128, 3, 16, 16], F32)
        wgf = P.sb("wgf", [128, 2, 16, 16], BF16)
        ccol = P.sb("ccol", [128, 6, 16], F32)
        gbc = P.sb("gbc", [40, 1], F32)
        W5 = 2310
        L5 = W5 - 4
        upad = [P.sb("mupad%d" % s, [128, W5], BF16) for s in range(2)]
        ucp = [P.sb("mucp%d" % s, [128, W5], BF16) for s in range(2)]
        macc = [P.sb("macc%d" % s, [128, L5], F32) for s in range(2)]
        szb = [P.sb("szb%d" % s, [128, 512], BF16) for s in range(3)]
        mx = P.sb("mx", [8, 12], F32)
        _save = P.sb_off
        P.sb_off = bdT_off
        gsb = P.sb("gsb", [40, TOK], F32)
        gt1 = P.sb("gt1", [40, TOK], F32)
        gt2 = P.sb("gt2", [40, TOK], F32)
        P.sb_off = max(_save, P.sb_off)

        self.load_cols(mcol[:, 0, :], I["ml_hnorm_g"][j], "mcol")
        self.load_cols(mcol[:, 1, :], I["ml_skip"][j], "mcol")
        for k in range(5):
            self.load_cols(ccol[:, k, :], I["ml_conv_w"][j, k], "ccol")
        self.load_cols(ccol[:, 5, :], I["ml_conv_b"][j], "ccol")
        P.dma(gbc[0:8, :], I["gb"][j, 0:8].rearrange("(p o) -> p o", o=1), writes=["gbc"])
        P.dma(gbc[32:40, :], I["gb"][j, 8:16].rearrange("(p o) -> p o", o=1), writes=["gbc"])
        for m, nm in enumerate(("bdq", "bdk", "bdv")):
            P.dma(bdb[:, m, :, :], I[nm][j].rearrange("c p o -> p c o"), writes=["bdb"], q="pool")
            P.dma(bdT[:, m, :, :], I[nm + "T"][j].rearrange("c p o -> p c o"), writes=["bdT"])
            P.dma(gwt[:, m, :, :], I["gw"][j, m].rearrange("c p o -> p c o"), writes=["gwt"])
        P.dve(lambda e: e.tensor_scalar(out=gwt[:, 1, :, :], in0=gwt[:, 1, :, :], scalar1=float(512 ** -0.5), scalar2=None, op0=ALU.mult), ["gwt"], ["gwt"])
        for c in range(16):
            P.mm(PS[5][:, c * 16:(c + 1) * 16], [(bdT[:, 0, c, :], gwt[:, 0, c, :]), (bdT[:, 1, c, :], gwt[:, 1, c, :])], ["bdT", "gwt"], ["ps5"])
            P.mm(PS[6][:, c * 16:(c + 1) * 16], [(bdT[:, 2, c, :], gwt[:, 2, c, :])], ["bdT", "gwt"], ["ps6"])
        P.dve(lambda e: e.tensor_copy(out=wgf[:, 0, :, :], in_=PS[5][:, 0:256].rearrange("p (c o) -> p c o", c=16)), ["ps5"], ["wgf"])
        P.dve(lambda e: e.tensor_copy(out=wgf[:, 1, :, :], in_=PS[6][:, 0:256].rearrange("p (c o) -> p c o", c=16)), ["ps6"], ["wgf"])
        for s in range(2):
            P.pool(lambda e, s=s: e.memset(upad[s][:], 0.0), [], ["mupad%d" % s])

        def p5(t):
            return 2 + t if t < CTX else 4 + t

        nb = 0
        nz = 0
        for og in range(8):
            ws = og % 2
            P.dma(win[ws][:], I["ml_w_in"][j][:, og * 512:(og + 1) * 512].rearrange("(k p) c -> p k c", p=128), writes=["win%d" % ws], q="pool")
            for oo in range(4):
                oc = og * 4 + oo
                s = oc % 2
                for (t0, n) in TBLK:
                    bank = 5 + nb % 3
                    nb += 1
                    P.mm(PS[bank][:, 0:n], [(win[ws][:, k, oo * 128:(oo + 1) * 128], hT[:, k, t0:t0 + n]) for k in range(8)],
                         ["win%d" % ws] + tkeys("h1_", t0, n), ["ps%d" % bank])
                    if oc < 16:
                        P.act(lambda e, bank=bank, n=n, s=s, t0=t0: e.copy(out=upad[s][:, p5(t0):p5(t0) + n], in_=PS[bank][:, 0:n]),
                              ["ps%d" % bank], ["mupad%d" % s])
                    else:
                        zs = nz % 3
                        nz += 1
                        P.act(lambda e, bank=bank, n=n, zs=zs: e.activation(out=szb[zs][:, 0:n], in_=PS[bank][:, 0:n], func=AF.Silu),
                              ["ps%d" % bank], ["szb%d" % zs])
                        P.dma(self.d_sz[(oc - 16) * 128:(oc - 15) * 128, t0:t0 + n], szb[zs][:, 0:n], reads=["szb%d" % zs], writes=["dsz%d" % (oc - 16)])
                if oc < 16:
                    u, a, uc = upad[s], macc[s], ucp[s]
                    ku, ka, kc = "mupad%d" % s, "macc%d" % s, "mucp%d" % s
                    P.pool(lambda e, u=u, a=a, oc=oc: e.tensor_scalar(out=a[:], in0=u[:, 0:L5], scalar1=ccol[:, 0, oc:oc + 1], scalar2=ccol[:, 5, oc:oc + 1],
                                                                      op0=ALU.mult, op1=ALU.add), [ku, "ccol"], [ka])
                    for k in range(1, 5):
                        eng = "dve"
                        P.add(eng, lambda e, u=u, a=a, oc=oc, k=k: e.scalar_tensor_tensor(out=a[:], in0=u[:, k:k + L5], scalar=ccol[:, k, oc:oc + 1], in1=a[:],
                                                                                        op0=ALU.mult, op1=ALU.add), [ku, ka, "ccol"], [ka])
                    P.act(lambda e, a=a, uc=uc: e.activation(out=uc[:, 2:2 + L5], in_=a[:], func=AF.Silu), [ka], [kc])
                    for b, (t0, n) in enumerate(TBLK):
                        def gm(e, b=b, t0=t0, n=n, oc=oc, u=u, uc=uc):
                            c0 = p5(t0)
                            e.matmul(PS[b][0:8, 0:n], lhsT=wgf[:, 0, oc, 0:8], rhs=uc[:, c0:c0 + n], start=(oc == 0), stop=False)
                            e.matmul(PS[b][0:8, 0:n], lhsT=wgf[:, 1, oc, 0:8], rhs=u[:, c0:c0 + n], start=False, stop=(oc == 15))
                            e.matmul(PS[b][32:40, 0:n], lhsT=wgf[:, 0, oc, 8:16], rhs=uc[:, c0:c0 + n], start=(oc == 0), stop=False)
                            return e.matmul(PS[b][32:40, 0:n], lhsT=wgf[:, 1, oc, 8:16], rhs=u[:, c0:c0 + n], start=False, stop=(oc == 15))
                        P.pe(gm, [ku, kc, "wgf"], ["ps%d" % b])
                    for (src, dst, kk) in ((u, self.d_u, "du"), (uc, self.d_uc, "duc")):
                        P.dma(dst[oc * 128:(oc + 1) * 128, 0:CTX], src[:, 2:2 + CTX], reads=[ku if src is u else kc], writes=["%s%d" % (kk, oc)])
                        P.dma(dst[oc * 128:(oc + 1) * 128, CTX:TOK], src[:, 4 + CTX:4 + TOK], reads=[ku if src is u else kc], writes=["%s%d" % (kk, oc)])
        for b, (t0, n) in enumerate(TBLK):
            P.act(lambda e, b=b, t0=t0, n=n: e.activation(out=gsb[0:40, t0:t0 + n], in_=PS[b][0:40, 0:n], func=AF.Identity, bias=gbc[0:40, 0:1], scale=1.0),
                  ["ps%d" % b, "gbc"], ["gsb"])
        R = slice(32, 40)
        P.dve(lambda e: e.tensor_scalar(out=gt2[R, :], in0=gsb[R, :], scalar1=-1.0, scalar2=None, op0=ALU.mult), ["gsb"], ["gt2"])
        P.dve(lambda e: e.tensor_tensor(out=gt1[R, :], in0=gsb[R, :], in1=gt2[R, :], op=ALU.max), ["gsb", "gt2"], ["gt1"])
        P.act(lambda e: e.activation(out=gt1[R, :], in_=gt1[R, :], func=AF.Exp, scale=-1.0), ["gt1"], ["gt1"])
        P.act(lambda e: e.activation(out=gt1[R, :], in_=gt1[R, :], func=AF.Ln, bias=self.onesf[R, 0:1], scale=1.0), ["gt1"], ["gt1"])
        P.dve(lambda e: e.tensor_scalar(out=gt2[R, :], in0=gt2[R, :], scalar1=0.0, scalar2=None, op0=ALU.max), ["gt2"], ["gt2"])
        P.dve(lambda e: e.tensor_tensor(out=gsb[R, :], in0=gt1[R, :], in1=gt2[R, :], op=ALU.add), ["gt1", "gt2", "gsb"], ["gsb"])
        P.dve(lambda e: e.tensor_reduce(out=mx[0:8, 0:1], in_=gsb[0:8, :], axis=AX.X, op=ALU.max), ["gsb"], ["mx"])
        P.dve(lambda e: e.tensor_scalar(out=gsb[0:8, :], in0=gsb[0:8, :], scalar1=mx[0:8, 0:1], scalar2=None, op0=ALU.subtract), ["gsb", "mx"], ["gsb"])
        P.dve(lambda e: e.tensor_scalar(out=mx[0:8, 4:12], in0=idf[0:8, 0:8], scalar1=mx[0:8, 0:1], scalar2=None, op0=ALU.mult), ["mx"], ["mx"])
        P.mm(PS[5][:, 0:8], [(self.onesf[0:8, :], mx[0:8, 4:12])], ["mx"], ["ps5"])
        P.act(lambda e: e.activation(out=emc[:], in_=PS[5][:, 0:8], func=AF.Exp, scale=-1.0), ["ps5"], ["emc"])

        def gtr(e):
            ins = None
            for tt in range(NT):
                e.transpose(PS[6][:, tt * 16:tt * 16 + 8], gsb[0:8, tt * 128:(tt + 1) * 128], idf[0:8, 0:8])
                ins = e.transpose(PS[6][:, tt * 16 + 8:tt * 16 + 16], gsb[32:40, tt * 128:(tt + 1) * 128], idf[32:40, 32:40])
            return ins

        P.pe(gtr, ["gsb"], ["ps6"])
        P.dve(lambda e: e.tensor_copy(out=gcol[:].rearrange("p t g -> p (t g)"), in_=PS[6][:, 0:NT * 16]), ["ps6"], ["gcol"])
        self.refence()

        cm = self.cm
        order = {0: [0, 1] + list(range(2, NT)), 1: [1, 0] + list(range(NT - 1, 1, -1))}
        for hd in range(4):
            P.sb_off = self.hA_off
            H = P.sb("H", [128, NT, 512], BF16)
            C32 = [P.sb("C32_%d" % z, [128, 4, 512], F32) for z in range(2)]
            assert P.sb_off <= self.top
            P.sb_off = keepA
            qT = P.sb("qT", [128, 4, TOK], BF16)
            kT = P.sb("kT", [128, 4, TOK], BF16)
            ktm = P.sb("ktm", [128, NT, 512], BF16)
            vtm = P.sb("vtm", [128, NT, 512], BF16)
            Cbf = [P.sb("Cbf%d" % z, [128, 4, 512], BF16) for z in range(2)]
            nst = P.sb("nstate", [128, 2, 4], F32)
            nbf = P.sb("nbf", [128, 2, 4], BF16)
            keepB = P.sb_off
            ucT = P.sb("ucT", [128, 4, TOK], BF16)
            uT = P.sb("uT", [128, 4, TOK], BF16)
            P.dma(ucT[:], self.d_uc[hd * 512:(hd + 1) * 512, :].rearrange("(c p) t -> p c t", p=128), writes=["ucT"])
            P.dma(uT[:], self.d_u[hd * 512:(hd + 1) * 512, :].rearrange("(c p) t -> p c t", p=128), writes=["uT"])
            nb = 0
            sk = float(512 ** -0.5)
            for c in range(4):
                cc = hd * 4 + c
                for (t0, n) in TBLK:
                    for m, dst in ((0, qT), (1, kT)):
                        bank = nb % 4
                        nb += 1
                        P.mm(PS[bank][:, 0:n], [(bdb[:, m, cc, :], ucT[:, c, t0:t0 + n])], ["ucT"], ["ps%d" % bank])
                        if m == 0:
                            P.act(lambda e, bank=bank, n=n, dst=dst, c=c, t0=t0: e.copy(out=dst[:, c, t0:t0 + n], in_=PS[bank][:, 0:n]), ["ps%d" % bank], ["qT"])
                        else:
                            P.dve(lambda e, bank=bank, n=n, dst=dst, c=c, t0=t0: e.tensor_scalar(out=dst[:, c, t0:t0 + n], in0=PS[bank][:, 0:n], scalar1=sk, scalar2=None, op0=ALU.mult),
                                  ["ps%d" % bank], ["kT"])
            for tt in range(NT):
                tk = slice(tt * 128, (tt + 1) * 128)
                for m, src, dst in ((1, ucT, ktm), (2, uT, vtm)):
                    bank = 4 + nb % 4
                    nb += 1

                    def tm(e, bank=bank, m=m, src=src, tk=tk, hd=hd):
                        ins = None
                        for c in range(4):
                            ins = e.matmul(PS[bank][:, c * 128:(c + 1) * 128], lhsT=src[:, c, tk], rhs=bdb[:, m, hd * 4 + c, :], start=True, stop=True)
                        return ins

                    P.pe(tm, ["ucT", "uT"], ["ps%d" % bank])
                    if m == 1:
                        P.act(lambda e, bank=bank, tt=tt: e.activation(out=ktm[:, tt, :], in_=PS[bank][:, :], func=AF.Copy, scale=sk), ["ps%d" % bank], ["ktm%d" % tt])
                    else:
                        P.dve(lambda e, bank=bank, tt=tt: e.tensor_copy(out=vtm[:, tt, :], in_=PS[bank][:, :]), ["ps%d" % bank], ["vtm%d" % tt])
            for z in range(2):
                P.pool(lambda e, z=z: e.memset(C32[z][:], 0.0), [], ["C32_%d" % z])
                P.pool(lambda e, z=z: e.memset(Cbf[z][:], 0.0), [], ["Cbf%d" % z])
            P.pool(lambda e: e.memset(nst[:], 0.0), [], ["nstate0", "nstate1"])
            P.pool(lambda e: e.memset(nbf[:], 0.0), [], ["nbf0", "nbf1"])
            self.refence()
            P.sb_off = keepB
            DT = P.sb("DT", [128, 2, NT, 128], F32)
            EBb = P.sb("EBb", [128, 2, NT, 128], BF16)
            wkh = P.sb("wkh", [128, 2, 2, NT], F32)
            wkh16 = P.sb("wkh16", [128, 2, 2, NT], BF16)
            dec = P.sb("dec", [128, 2, NT, 2], F32)
            r2 = P.sb("r2", [128, NT, 2], F32)
            tc_ = P.sb("tcol", [128, NT], F32)
            wkc = P.sb("wkc", [128, NT], F32)
            dn = [P.sb("dn%d" % s, [128, 8], F32) for s in range(2)]
            ost = [P.sb("ost%d" % s, [128, 8], F32) for s in range(2)]
            lfm_off = P.sb_off
            LFm = P.sb("LFm", [128, NT, 128], F32)
            T2 = P.sb("T2", [128, NT, 128], F32)
            P.sb_off = lfm_off
            PTt = [P.sb("PT%d" % s, [128, 128], BF16) for s in range(2)]
            qs = [[P.sb("qs%d%d" % (z, h), [128, 4, 128], BF16) for h in range(2)] for z in range(2)]
            vw = [[P.sb("vw%d%d" % (z, h), [128, 512], BF16) for h in range(2)] for z in range(2)]
            _sv = P.sb_off
            P.sb_off = self.grow_off
            hs = [P.sb("hs%d" % s, [128, 512], F32) for s in range(2)]
            hn = [P.sb("hn%d" % s, [128, 512], BF16) for s in range(2)]
            oa = [P.sb("oa%d" % s, [128, 4, 128], F32) for s in range(2)]
            uct = [P.sb("uct%d" % s, [128, 4, 128], BF16) for s in range(2)]
            szt = [P.sb("szt%d" % s, [128, 4, 128], BF16) for s in range(2)]
            actt = [P.sb("actt%d" % s, [128, 4, 128], BF16) for s in range(2)]
            assert P.sb_off <= self.grow_off + 16384
            P.sb_off = _sv
            for z in range(2):
                lic = gcol[:, :, z * 4 + hd]
                nlfc = gcol[:, :, 8 + z * 4 + hd]
                P.dve(lambda e, z=z, nlfc=nlfc: e.tensor_tensor(out=LFm[:], in0=cm[:, z, 1, :].unsqueeze(1).to_broadcast([128, NT, 128]),
                                                                in1=nlfc.unsqueeze(2).to_broadcast([128, NT, 128]), op=ALU.mult), ["gcol"], ["LFm"])
                P.pool(lambda e, lic=lic: e.tensor_tensor(out=T2[:], in0=idf[:].unsqueeze(1).to_broadcast([128, NT, 128]),
                                                          in1=lic.unsqueeze(2).to_broadcast([128, NT, 128]), op=ALU.mult), ["gcol"], ["T2"])
                P.dve(lambda e: e.tensor_tensor(out=LFm[:], in0=LFm[:], in1=T2[:], op=ALU.add), ["LFm", "T2"], ["LFm"])
                P.dve(lambda e, nlfc=nlfc: e.tensor_scalar(out=T2[:], in0=nlfc.unsqueeze(2).to_broadcast([128, NT, 128]), scalar1=-1.0, scalar2=None, op0=ALU.mult),
                       ["gcol", "T2"], ["T2"])
                for g0 in range(0, NT, 4):
                    bank = (g0 // 4) % 2
                    ng = min(4, NT - g0)

                    def dm(e, g0=g0, ng=ng, bank=bank, z=z):
                        ins = None
                        for q in range(ng):
                            e.matmul(PS[bank][:, q * 128:(q + 1) * 128], lhsT=LFm[:, g0 + q, :], rhs=cm[:, z, 0, :], start=True, stop=False)
                            ins = e.matmul(PS[bank][:, q * 128:(q + 1) * 128], lhsT=idf[:], rhs=cm[:, z, 2, :], start=False, stop=True)
                        return ins

                    P.pe(dm, ["LFm"], ["ps%d" % bank])
                    P.act(lambda e, g0=g0, ng=ng, bank=bank, z=z: e.activation(out=DT[:, z, g0:g0 + ng, :].rearrange("p t s -> p (t s)"), in_=PS[bank][:, 0:ng * 128], func=AF.Exp),
                          ["ps%d" % bank], ["DT%d" % z])
                    bank2 = 4 + (g0 // 4) % 2

                    def em(e, g0=g0, ng=ng, bank2=bank2, z=z):
                        ins = None
                        for q in range(ng):
                            ins = e.matmul(PS[bank2][:, q * 128:(q + 1) * 128], lhsT=T2[:, g0 + q, :], rhs=cm[:, z, 0, :], start=True, stop=True)
                        return ins

                    P.pe(em, ["T2"], ["ps%d" % bank2])
                    P.act(lambda e, g0=g0, ng=ng, bank2=bank2, z=z: e.activation(out=EBb[:, z, g0:g0 + ng, :].rearrange("p t s -> p (t s)"), in_=PS[bank2][:, 0:ng * 128], func=AF.Exp),
                          ["ps%d" % bank2], ["EBb%d" % z])
                P.mm(PS[3][:, 0:NT], [(cm[:, z, 1, :], nlfc)], ["gcol"], ["ps3"])
                P.dve(lambda e, lic=lic: e.tensor_tensor(out=tc_[:], in0=PS[3][:, 0:NT], in1=lic, op=ALU.add), ["ps3", "gcol"], ["tcol"])
                P.act(lambda e: e.activation(out=wkc[:], in_=tc_[:], func=AF.Exp), ["tcol"], ["wkc"])
                for hf in range(2):
                    P.dve(lambda e, z=z, hf=hf: e.tensor_scalar(out=wkh[:, z, hf, :], in0=wkc[:], scalar1=self.halfm[:, hf:hf + 1], scalar2=None, op0=ALU.mult),
                          ["wkc"], ["wkh%d" % z])
                P.dve(lambda e, z=z: e.tensor_copy(out=wkh16[:, z, :, :], in_=wkh[:, z, :, :]), ["wkh%d" % z], ["wkh16_%d" % z])
                P.dve(lambda e, nlfc=nlfc: e.tensor_tensor(out=r2[:], in0=nlfc.unsqueeze(2).to_broadcast([128, NT, 2]),
                                                           in1=self.halfm[:].unsqueeze(1).to_broadcast([128, NT, 2]), op=ALU.mult), ["gcol"], ["r2"])
                P.mm(PS[2][:, 64:64 + 2 * NT], [(self.onesf[:], r2[:].rearrange("p t h -> p (t h)"))], ["r2"], ["ps2"])
                P.act(lambda e, z=z: e.activation(out=dec[:, z, :, :].rearrange("p t h -> p (t h)"), in_=PS[2][:, 64:64 + 2 * NT], func=AF.Exp, scale=-1.0),
                      ["ps2"], ["dec%d" % z])
            self.refence()
            if self.dbg == "B2":
                raise StopIteration
            for z in range(2):
                for h in range(2):
                    P.pool(lambda e, z=z, h=h: e.memset(qs[z][h][:], 0.0), [], ["qs%d%d" % (z, h)])
            visited = set()
            pending = []

            def rec(z, tt, hd=hd):
                tk = slice(tt * 128, (tt + 1) * 128)
                kz = str(z)
                X, A, C0 = 4 * z, 4 * z + 1, 4 * z + 2
                halves = (0, 1) if z == 0 else (1, 0)
                P.mm(PS[X][:, 0:128], [(kT[:, c, tk], qT[:, c, tk]) for c in range(4)], [], ["psXs" + kz])
                yield
                P.dve(lambda e: e.tensor_tensor(out=PTt[z][:], in0=PS[X][:, 0:128], in1=DT[:, z, tt, :], op=ALU.mult), ["psXs" + kz], ["PT" + kz])
                yield
                for hf in range(2):
                    cs = slice(hf * 64, hf * 64 + 64)
                    tq = slice(tt * 128 + hf * 64, tt * 128 + hf * 64 + 64)
                    P.pool(lambda e, hf=hf, cs=cs, tq=tq: e.tensor_tensor(out=qs[z][hf][:, :, cs], in0=qT[:, :, tq],
                                                                        in1=EBb[:, z, tt, cs].unsqueeze(1).to_broadcast([128, 4, 64]), op=ALU.mult),
                           [], ["qs%s%d" % (kz, hf)])
                    yield
                    P.pool(lambda e, hf=hf: e.tensor_tensor(out=vw[z][hf][:], in0=vtm[:, tt, :], in1=wkh[:, z, hf, tt:tt + 1].to_broadcast([128, 512]), op=ALU.mult),
                           [], ["vw%s%d" % (kz, hf)])
                    yield

                def mA(e):
                    e.matmul(PS[A][:, :], lhsT=PTt[z][:], rhs=vtm[:, tt, :], start=True, stop=False)
                    return e.matmul(PS[X][:, 128:129], lhsT=PTt[z][:], rhs=self.onesb[:, 0:1], start=True, stop=True)

                P.pe(mA, ["PT" + kz], ["psA" + kz, "psXd" + kz])
                yield
                for hi, hf in enumerate(halves):
                    lastg = hi == 1

                    def mB(e, hf=hf, lastg=lastg, hi=hi):
                        ins = None
                        for c in range(4):
                            e.matmul(PS[A][:, :], lhsT=qs[z][hf][:, c, :], rhs=Cbf[z][:, c, :], start=False, stop=(lastg and c == 3))
                        for c in range(4):
                            ins = e.matmul(PS[X][:, 129 + hi:130 + hi], lhsT=qs[z][hf][:, c, :], rhs=nbf[:, z, c:c + 1], start=(c == 0), stop=(c == 3))
                        return ins

                    P.pe(mB, ["qs%s%d" % (kz, hf), "Cbf" + kz, "nbf" + kz], ["psA" + kz, "psXd" + kz])
                    yield
                    dcol = dec[:, z, tt, hf:hf + 1]
                    for pr in range(2):
                        def mC(e, hf=hf, pr=pr):
                            ins = None
                            for q in range(2):
                                c = 2 * pr + q
                                ins = e.matmul(PS[C0 + q][:, :], lhsT=ktm[:, tt, c * 128:(c + 1) * 128], rhs=vw[z][hf][:], start=True, stop=True)
                            return ins

                        P.pe(mC, ["vw%s%d" % (kz, hf)], ["psC" + kz])
                        yield
                        cc = slice(2 * pr, 2 * pr + 2)
                        for q in range(2):
                            P.dve(lambda e, q=q, pr=pr, dcol=dcol: e.scalar_tensor_tensor(out=C32[z][:, 2 * pr + q, :], in0=C32[z][:, 2 * pr + q, :],
                                                                                      scalar=dcol, in1=PS[C0 + q][:, :], op0=ALU.mult, op1=ALU.add),
                                  ["psC" + kz, "C32_%s_%d" % (kz, pr)], ["C32_%s_%d" % (kz, pr)])
                            yield
                        P.act(lambda e, cc=cc: e.copy(out=Cbf[z][:, cc, :], in_=C32[z][:, cc, :]), ["C32_%s_%d" % (kz, pr)], ["Cbf" + kz])
                        yield

                    def nm(e, hf=hf, hi=hi):
                        ins = None
                        for c in range(4):
                            ins = e.matmul(PS[X][:, 136 + hi * 4 + c:137 + hi * 4 + c], lhsT=ktm[:, tt, c * 128:(c + 1) * 128], rhs=wkh16[:, z, hf, tt:tt + 1], start=True, stop=True)
                        return ins

                    P.pe(nm, [], ["psXn%s%d" % (kz, hi)])
                    yield
                    P.dve(lambda e, hi=hi, dcol=dcol: e.scalar_tensor_tensor(out=nst[:, z, :], in0=nst[:, z, :], scalar=dcol, in1=PS[X][:, 136 + hi * 4:140 + hi * 4],
                                                                         op0=ALU.mult, op1=ALU.add), ["psXn%s%d" % (kz, hi), "nstate" + kz], ["nstate" + kz])
                    yield
                    P.act(lambda e: e.copy(out=nbf[:, z, :], in_=nst[:, z, :]), ["nstate" + kz], ["nbf" + kz])
                    yield
                d = dn[z]
                kd = "dn" + kz
                P.dve(lambda e: e.tensor_reduce(out=d[:, 4:5], in_=PS[X][:, 128:131], axis=AX.X, op=ALU.add), ["psXd" + kz], [kd])
                yield
                P.dve(lambda e: e.tensor_scalar(out=d[:, 0:1], in0=d[:, 4:5], scalar1=-1.0, scalar2=None, op0=ALU.mult), [kd], [kd])
                yield
                P.dve(lambda e: e.tensor_tensor(out=d[:, 1:2], in0=d[:, 4:5], in1=d[:, 0:1], op=ALU.max), [kd], [kd])
                yield
                P.dve(lambda e: e.tensor_tensor(out=d[:, 2:3], in0=d[:, 1:2], in1=emc[:, z * 4 + hd:z * 4 + hd + 1], op=ALU.max), [kd], [kd])
                yield
                P.dve(lambda e: e.reciprocal(out=d[:, 3:4], in_=d[:, 2:3]), [kd], [kd])
                yield
                if tt not in visited:
                    visited.add(tt)
                    P.act(lambda e: e.activation(out=H[:, tt, :], in_=PS[A][:, :], func=AF.Copy, scale=d[:, 3:4]), ["psA" + kz, kd], ["H%d" % tt])
                    yield
                    return
                P.dve(lambda e: e.scalar_tensor_tensor(out=hs[z][:], in0=PS[A][:, :], scalar=d[:, 3:4], in1=H[:, tt, :], op0=ALU.mult, op1=ALU.add),
                      ["psA" + kz, kd, "H%d" % tt], ["hs" + kz])
                yield
                pending.append(outg(z, tt))

            def outg(z, tt, hd=hd):
                tk = slice(tt * 128, (tt + 1) * 128)
                kz = str(z)
                X = 4 * z
                o = ost[z]
                ko = "ost" + kz
                P.dve(lambda e: e.tensor_reduce(out=o[:, 0:1], in_=hs[z][:], axis=AX.X, op=ALU.add), ["hs" + kz], [ko])
                yield
                P.act(lambda e: e.activation(out=self.junk[:, z * 512:(z + 1) * 512], in_=hs[z][:], func=AF.Square, accum_out=o[:, 1:2]), ["hs" + kz], ["junk" + kz, ko])
                yield
                P.dve(lambda e: e.tensor_scalar(out=o[:, 2:3], in0=o[:, 0:1], scalar1=1.0 / 512, scalar2=None, op0=ALU.mult), [ko], [ko])
                yield
                P.dve(lambda e: e.tensor_tensor(out=o[:, 3:4], in0=o[:, 2:3], in1=o[:, 2:3], op=ALU.mult), [ko], [ko])
                yield
                P.dve(lambda e: e.scalar_tensor_tensor(out=o[:, 4:5], in0=o[:, 1:2], scalar=1.0 / 512, in1=o[:, 3:4], op0=ALU.mult, op1=ALU.subtract), [ko], [ko])
                yield
                P.act(lambda e: e.activation(out=o[:, 5:6], in_=o[:, 4:5], func=AF.Sqrt, bias=self.epsc[:, 0:1], scale=1.0), [ko], [ko])
                yield
                P.dve(lambda e: e.reciprocal(out=o[:, 6:7], in_=o[:, 5:6]), [ko], [ko])
                yield
                P.dve(lambda e: e.scalar_tensor_tensor(out=o[:, 7:8], in0=o[:, 2:3], scalar=-1.0, in1=o[:, 6:7], op0=ALU.mult, op1=ALU.mult), [ko], [ko])
                yield
                P.act(lambda e: e.activation(out=hn[z][:], in_=hs[z][:], func=AF.Identity, scale=o[:, 6:7], bias=o[:, 7:8]), ["hs" + kz, ko], ["hn" + kz])
                yield
                gb_ = mcol[:, 0, hd * 4:(hd + 1) * 4].unsqueeze(2).to_broadcast([128, 4, 128])
                for pp in range(2):
                    def otr(e, pp=pp):
                        ins = None
                        for q in range(2):
                            c = 2 * pp + q
                            ins = e.matmul(PS[X][:, 256 + q * 128:256 + (q + 1) * 128], lhsT=hn[z][:, c * 128:(c + 1) * 128], rhs=self.identb[:], start=True, stop=True)
                        return ins

                    P.pe(otr, ["hn" + kz], ["psXt" + kz])
                    yield
                    P.dve(lambda e, pp=pp: e.tensor_tensor(out=actt[z][:, 2 * pp:2 * pp + 2, :], in0=PS[X][:, 256:512].rearrange("p (c t) -> p c t", c=2),
                                                         in1=gb_[:, 2 * pp:2 * pp + 2, :], op=ALU.mult), ["psXt" + kz], ["actt" + kz])
                    yield
                P.dma(uct[z][:], self.d_uc[hd * 512:(hd + 1) * 512, tk].rearrange("(c p) t -> p c t", p=128), writes=["uct" + kz])
                P.dma(szt[z][:], self.d_sz[hd * 512:(hd + 1) * 512, tk].rearrange("(c p) t -> p c t", p=128), writes=["szt" + kz])
                sb_ = mcol[:, 1, hd * 4:(hd + 1) * 4].unsqueeze(2).to_broadcast([128, 4, 128])
                P.pool(lambda e: e.tensor_tensor(out=oa[z][:], in0=uct[z][:], in1=sb_, op=ALU.mult), ["uct" + kz], ["oa" + kz])
                yield
                P.pool(lambda e: e.tensor_tensor(out=oa[z][:], in0=oa[z][:], in1=actt[z][:], op=ALU.add), ["oa" + kz, "actt" + kz], ["oa" + kz])
                yield
                P.pool(lambda e: e.tensor_tensor(out=actt[z][:], in0=oa[z][:], in1=szt[z][:], op=ALU.mult), ["oa" + kz, "szt" + kz], ["actt" + kz])
                yield
                P.dma(self.d_act[hd * 512:(hd + 1) * 512, tk].rearrange("(c p) t -> p c t", p=128), actt[z][:], reads=["actt" + kz], writes=["dact_h%d_%d" % (hd, tt)])
                yield

            def drive(gens):
                gens = list(gens)
                while gens:
                    for g in list(gens):
                        try:
                            next(g)
                        except StopIteration:
                            gens.remove(g)

            for step in range(NT):
                cur = pending
                pending = []
                import os as _os
                if _os.environ.get("KSKIPOUT"):
                    cur = []
                if _os.environ.get("KSEQ"):
                    drive([rec(0, order[0][step])]); drive([rec(1, order[1][step])])
                    for g_ in cur:
                        drive([g_])
                else:
                    drive([rec(0, order[0][step]), rec(1, order[1][step])] + cur)
                if self.dbg == "S%d" % step:
                    raise StopIteration
            drive(pending)
            pending = []
            self.refence()
        self.outproj(i, 16, I["ml_w_out"][j], None, list(range(NT)))

    def natten(self, i, j, last):
        P, I, PS = self.P, self.I, self.PS
        hT = self.hA
        P.sb_off = self.top
        wq = [P.sb("nwq%d" % s, [128, 8, 3, 128], BF16) for s in range(2)]
        bcol = P.sb("nbcol", [128, 2, 8], F32)
        bvr = P.sb("nbvr", [128, D], F32)
        qc = [P.sb("nqc%d" % s, [128, TOK], BF16) for s in range(2)]
        kc = [P.sb("nkc%d" % s, [128, TOK], BF16) for s in range(2)]
        vc = [P.sb("nvc%d" % s, [128, NT, 128], BF16) for s in range(2)]
        oc_ = [P.sb("noc%d" % s, [128, TOK], BF16) for s in range(2)]
        nabt = [P.sb("nabt%d" % s, [128, 2, 5, 576], F32) for s in range(2)]
        S = [P.sb("nS%d" % s, [128, 832], F32) for s in range(4)]
        Pm = [P.sb("nPm%d" % s, [128, 832], BF16) for s in range(4)]
        PT = [P.sb("nPT%d" % s, [128, 7, 128], BF16) for s in range(4)]
        otm = [P.sb("notm%d" % s, [128, 128], BF16) for s in range(2)]
        sst = [P.sb("nsst%d" % s, [128, 4], F32) for s in range(4)]
        self.load_cols(bcol[:, 0, :], I["na_b_qkv"][j, 0:D], "nbcol")
        self.load_cols(bcol[:, 1, :], I["na_b_qkv"][j, D:2 * D], "nbcol")
        P.dve(lambda e: e.tensor_scalar(out=bcol[:, 0, :], in0=bcol[:, 0, :], scalar1=0.125, scalar2=None, op0=ALU.mult), ["nbcol"], ["nbcol"])
        P.dma(bvr[:], I["na_b_qkv"][j, 2 * D:3 * D].partition_broadcast(128), writes=["nbvr"])
        Wq = I["na_w_qkv"][j]
        nb = 0
        nu = 0
        qtiles = list(range(NT)) if not last else list(range(2, NT))
        for c in range(8):
            s = c % 2
            for m in range(3):
                P.dma(wq[s][:, :, m, :], Wq[:, m * D + c * 128:m * D + (c + 1) * 128].rearrange("(k p) o -> p k o", p=128), writes=["nwq%d" % s], q="pool")
            P.dma(nabt[s][:], I["nab"][j, 2 * c:2 * c + 2].rearrange("h r q n -> q h r n"), writes=["nabt%d" % s])
            for (t0, n) in TBLK:
                for m, dst, kk in ((0, qc[s], "nqc%d" % s), (1, kc[s], "nkc%d" % s)):
                    bank = nb % 4
                    nb += 1
                    P.mm(PS[bank][:, 0:n], [(wq[s][:, k, m, :], hT[:, k, t0:t0 + n]) for k in range(8)], ["nwq%d" % s] + tkeys("h1_", t0, n), ["ps%d" % bank])
                    P.act(lambda e, bank=bank, n=n, dst=dst, t0=t0, m=m, c=c: e.activation(out=dst[:, t0:t0 + n], in_=PS[bank][:, 0:n], func=AF.Identity,
                                                                                       bias=bcol[:, m, c:c + 1], scale=(0.125 if m == 0 else 1.0)),
                          ["ps%d" % bank, "nbcol"], [kk])
            for g0 in range(0, NT, 4):
                ng = min(4, NT - g0)
                bank = nb % 4
                nb += 1

                def vm(e, g0=g0, ng=ng, bank=bank, s=s):
                    ins = None
                    for q in range(ng):
                        tk = slice((g0 + q) * 128, (g0 + q + 1) * 128)
                        for k in range(8):
                            ins = e.matmul(PS[bank][:, q * 128:(q + 1) * 128], lhsT=hT[:, k, tk], rhs=wq[s][:, k, 2, :], start=(k == 0), stop=(k == 7))
                    return ins

                P.pe(vm, ["nwq%d" % s] + tkeys("h1_", g0 * 128, ng * 128), ["ps%d" % bank])
                P.dve(lambda e, g0=g0, ng=ng, bank=bank, s=s, c=c: e.tensor_tensor(out=vc[s][:, g0:g0 + ng, :], in0=PS[bank][:, 0:ng * 128].rearrange("p (t o) -> p t o", t=ng),
                                                                                  in1=bvr[:, c * 128:(c + 1) * 128].unsqueeze(1).to_broadcast([128, ng, 128]), op=ALU.add),
                      ["ps%d" % bank, "nbvr"], ["nvc%d" % s])
            def unit(n, tt, hh, s=s, c=c):
                tq = slice(tt * 128, (tt + 1) * 128)
                if tt >= 2:
                    r = 2 * (tt - 2)
                    if r <= 2:
                        rs0, nkr, cls = 0, 8, r // 2
                    elif r >= 28:
                        rs0, nkr, cls = 24, 8, 3 + (r - 28) // 2
                    else:
                        rs0, nkr, cls = r - 4, 9, 2
                    nl = nkr * 64
                    ks = CTX + rs0 * 64
                else:
                    nl, ks, cls = 0, 0, 0
                ntot = nl + CTX
                osl = (n // 2) % 2
                pb = slice(hh * 64, hh * 64 + 64)
                u = n % 4
                ub = 2 * u
                k0, k1 = "ps%d" % ub, "ps%d" % (ub + 1)
                kS, kP, kPT, kst = "nS%d" % u, "nPm%d" % u, "nPT%d" % u, "nsst%d" % u
                q_ap = qc[s][pb, tq]
                if nl > 0:
                    P.mm(PS[ub][:, 0:512], [(q_ap, kc[s][pb, ks:ks + 512])], ["nqc%d" % s, "nkc%d" % s], [k0])
                    yield
                    P.dve(lambda e: e.tensor_tensor(out=S[u][:, 0:512], in0=PS[ub][:, 0:512], in1=nabt[s][:, hh, cls, 0:512], op=ALU.add),
                          [k0, "nabt%d" % s], [kS])
                    yield

                def sc2(e):
                    if nl > 512:
                        e.matmul(PS[ub + 1][:, 0:64], lhsT=q_ap, rhs=kc[s][pb, ks + 512:ks + 576], start=True, stop=True)
                    return e.matmul(PS[ub + 1][:, 64:320], lhsT=q_ap, rhs=kc[s][pb, 0:CTX], start=True, stop=True)

                P.pe(sc2, ["nqc%d" % s, "nkc%d" % s], [k1])
                yield
                if nl > 512:
                    P.dve(lambda e: e.tensor_tensor(out=S[u][:, 512:576], in0=PS[ub + 1][:, 0:64], in1=nabt[s][:, hh, cls, 512:576], op=ALU.add),
                          [k1, "nabt%d" % s], [kS])
                    yield
                P.act(lambda e: e.copy(out=S[u][:, nl:nl + CTX], in_=PS[ub + 1][:, 64:320]), [k1], [kS])
                yield
                st = sst[u]
                P.dve(lambda e: e.tensor_reduce(out=st[:, 0:1], in_=S[u][:, 0:ntot], axis=AX.X, op=ALU.max), [kS], [kst])
                yield
                P.dve(lambda e: e.tensor_scalar(out=st[:, 1:2], in0=st[:, 0:1], scalar1=-1.0, scalar2=None, op0=ALU.mult), [kst], [kst])
                yield
                P.act(lambda e: e.activation(out=Pm[u][:, 0:ntot], in_=S[u][:, 0:ntot], func=AF.Exp, bias=st[:, 1:2], scale=1.0, accum_out=st[:, 2:3]),
                      [kS, kst], [kP, kst])
                yield
                P.dve(lambda e: e.reciprocal(out=st[:, 3:4], in_=st[:, 2:3]), [kst], [kst])
                yield
                chunks = []
                off = 0
                while off < nl:
                    kn = min(128, nl - off)
                    chunks.append((off, kn, vc[s][0:kn, (ks + off) // 128, hh * 64:(hh + 1) * 64]))
                    off += kn
                for q in range(2):
                    chunks.append((nl + q * 128, 128, vc[s][:, q, hh * 64:(hh + 1) * 64]))
                pv = PS[ub][:].bitcast(BF16)

                def ptr(e):
                    ins = None
                    for ci, (off, kn, _) in enumerate(chunks):
                        ins = e.transpose(pv[0:kn, ci * 128:(ci + 1) * 128], Pm[u][:, off:off + kn], self.identb[:])
                    return ins

                P.pe(ptr, [kP], [k0])
                yield
                nch = len(chunks)
                P.act(lambda e: e.copy(out=PT[u][:, 0:nch, :].rearrange("p c q -> p (c q)"), in_=pv[:, 0:nch * 128]), [k0], [kPT])
                yield
                P.mm(PS[ub + 1][:, 320:384], [(PT[u][0:kn, ci, :], vap) for ci, (off, kn, vap) in enumerate(chunks)],
                     [kPT, "nvc%d" % s], [k1])
                yield
                P.act(lambda e: e.activation(out=otm[osl][:, hh * 64:(hh + 1) * 64], in_=PS[ub + 1][:, 320:384],
                                             func=AF.Copy, scale=st[:, 3:4]), [k1, kst], ["notm%d" % osl])
                yield
                if hh == 1:
                    pv3 = PS[ub + 1][:].bitcast(BF16)
                    P.pe(lambda e: e.transpose(pv3[:, 768:896], otm[osl][:], self.identb[:]), ["notm%d" % osl], [k1])
                    yield
                    P.dve(lambda e: e.tensor_copy(out=oc_[s][:, tq], in_=pv3[:, 768:896]), [k1], ["noc%d" % s])
                    yield

            units = [(tt, hh) for tt in qtiles for hh in range(2)]
            active = []
            nxt = 0
            while nxt < len(units) or active:
                while len(active) < 4 and nxt < len(units):
                    active.append(unit(nxt, units[nxt][0], units[nxt][1]))
                    nxt += 1
                for g in list(active):
                    try:
                        next(g)
                    except StopIteration:
                        active.remove(g)
            if not last:
                P.dma(self.d_act[c * 128:(c + 1) * 128, 0:CTX], oc_[s][:, 0:CTX], reads=["noc%d" % s], writes=["dactn%d" % c])
            P.dma(self.d_act[c * 128:(c + 1) * 128, CTX:TOK], oc_[s][:, CTX:TOK], reads=["noc%d" % s], writes=["dactn%d" % c])
        self.refence()
        self.outproj(i, 8, I["na_w_out"][j], I["na_b_out"][j], qtiles)

    def build(self):
        P = self.P
        self.declare()
        self.consts()
        self.mods()
        self.persist()
        self.first_norm()
        try:
            self.layers()
        except StopIteration:
            pass
        P.emit()
        return self.nc

    def layers(self):
        for i in range(self.depth):
            last = i == self.depth - 1
            j = i // 2
            if i % 2 == 0:
                self.mlstm(i, j)
            else:
                self.natten(i, j, last)
            self.ffn(i, last)


def host_inputs(inputs, depth=4):
    f = lambda a: np.ascontiguousarray(np.asarray(a, dtype=np.float32))
    x, c, ctx, c_ctx = f(inputs["x"]), f(inputs["c"]), f(inputs["ctx"]), f(inputs["c_ctx"])
    B = x.shape[0]
    shared = {}
    for k in ("ada_w", "ada_b", "norm1_g", "norm2_g", "final_g", "ml_w_in", "ml_conv_w", "ml_conv_b", "ml_skip", "ml_hnorm_g", "ml_w_out",
              "na_w_qkv", "na_b_qkv", "na_w_out", "na_b_out", "ff_w_up", "ff_b_up", "ff_conv_w", "ff_conv_b", "ff_w_down", "ff_b_down"):
        shared[k] = f(inputs[k])
    n_a = shared["ml_w_in"].shape[0]
    for nm, src in (("bdq", "ml_wq"), ("bdk", "ml_wk"), ("bdv", "ml_wv")):
        w = f(inputs[src])
        bd = np.zeros((n_a, 16, 128, 128), np.float32)
        wr = w.reshape(n_a, 16, 32, 4, 4)
        for n in range(32):
            bd[:, :, 4 * n:4 * n + 4, 4 * n:4 * n + 4] = wr[:, :, n]
        shared[nm] = bd
        shared[nm + "T"] = np.ascontiguousarray(bd.transpose(0, 1, 3, 2))
    gw = f(inputs["ml_gate_w"])
    g2 = gw.reshape(n_a, 2, 3, 16, 128, 2, 4)
    g2 = g2.transpose(0, 2, 3, 4, 5, 1, 6)
    shared["gw"] = np.ascontiguousarray(g2.reshape(n_a, 3, 16, 128, 16))
    gb = f(inputs["ml_gate_b"]).reshape(n_a, 2, 2, 4).transpose(0, 2, 1, 3)
    shared["gb"] = np.ascontiguousarray(gb.reshape(n_a, 16))
    rpb = f(inputs["na_rpb"])
    n_b = rpb.shape[0]
    nab = np.full((n_b, 16, 5, 128, 576), NEGV, np.float32)
    col = np.arange(64)
    cs = np.clip(col - 8, 0, 48)
    ok = (col[None, :] >= cs[:, None]) & (col[None, :] < cs[:, None] + 16)
    dc = np.clip(col[None, :] - col[:, None] + 15, 0, 30)
    for cls, r in enumerate((0, 2, 4, 28, 30)):
        for rho in range(2):
            rq = r + rho
            rs_q = int(np.clip(rq - 4, 0, 24))
            rs0 = int(np.clip(r - 4, 0, 24))
            nkr = 9 if cls == 2 else 8
            for jj in range(nkr):
                krow = rs0 + jj
                if krow < rs_q or krow >= rs_q + 8:
                    continue
                dr = krow - rq + 7
                vals = rpb[:, :, dr, :][:, :, dc]
                blk = np.where(ok[None, None], vals, np.float32(NEGV))
                nab[:, :, cls, rho * 64:(rho + 1) * 64, jj * 64:(jj + 1) * 64] = blk
    shared["nab"] = nab
    u = np.arange(128)
    same = (u[:, None] // 64) == (u[None, :] // 64)
    cm = np.zeros((2, 3, 128, 128), np.float32)
    cm[0, 0] = same & (u[:, None] <= u[None, :])
    cm[0, 1] = -(same & (u[:, None] > u[None, :])).astype(np.float32)
    cm[0, 2] = np.where(same & (u[:, None] <= u[None, :]), 0.0, NEGV)
    cm[1, 0] = same & (u[:, None] >= u[None, :])
    cm[1, 1] = -(same & (u[:, None] < u[None, :])).astype(np.float32)
    cm[1, 2] = np.where(same & (u[:, None] >= u[None, :]), 0.0, NEGV)
    shared["cmask"] = cm
    shared["identf"] = np.eye(128, dtype=np.float32)
    hm = np.zeros((128, 2), np.float32)
    hm[:64, 0] = 1.0
    hm[64:, 1] = 1.0
    shared["halfm"] = hm
    maps = []
    for b in range(B):
        m = dict(shared)
        m["x"] = x[b]
        m["ctx"] = ctx[b]
        m["cvec"] = np.ascontiguousarray(np.stack([c[b], c_ctx], 0))
        maps.append(m)
    return maps


_NC_CACHE = {}


def kernel(**inputs):
    maps = host_inputs(inputs)
    if "nc" not in _NC_CACHE:
        _NC_CACHE["nc"] = Builder(4).build()
    nc = _NC_CACHE["nc"]
    res = run_bass_kernel_spmd(nc, maps, core_ids=list(range(len(maps))))
    return np.stack([np.asarray(r["out"], dtype=np.float32) for r in res.results], 0)
```

```python
import contextlib
import itertools
import numpy as np
import concourse.bass as bass
import concourse.mybir as mybir
from concourse.bass_utils import run_bass_kernel_spmd

F32 = mybir.dt.float32
BF16 = mybir.dt.bfloat16
AF = mybir.ActivationFunctionType
ALU = mybir.AluOpType
AX = mybir.AxisListType

D = 1024
SEQ = 2048
CTX = 256
TOK = SEQ + CTX
NT = TOK // 128
EPS = 1e-6
FF = 2816
NEGV = -30000.0
TBLK = [(0, 256), (256, 512), (768, 512), (1280, 512), (1792, 512)]


class Prog:
    ENGS = ("pe", "act", "dve", "pool", "sp")
    NDMA = {"sp": 40, "pool": 16, "act": 8}

    def __init__(self, nc):
        self.nc = nc
        self.ops = []
        self.last_w = {}
        self.readers = {}
        self.sb_off = 16512
        self.ndma = {q: 0 for q in self.NDMA}
        self.fence_idx = None
        self.nname = 0

    def sb(self, name, shape, dtype):
        esz = 4 if dtype == F32 else 2
        n = 1
        for s in shape[1:]:
            n *= s
        nbytes = (n * esz + 63) // 64 * 64
        off = self.sb_off
        self.sb_off += nbytes
        assert self.sb_off <= 229344, (name, self.sb_off)
        self.nname += 1
        return self.nc.alloc_sbuf_tensor_at("%s_%d" % (name, self.nname), list(shape), dtype, offset=off)

    def add(self, eng, fn, reads=(), writes=(), dma=False):
        idx = len(self.ops)
        deps = set()
        if self.fence_idx is not None:
            deps.add(self.fence_idx)
        for k in reads:
            if k in self.last_w:
                deps.add(self.last_w[k])
        for k in writes:
            if k in self.last_w:
                deps.add(self.last_w[k])
            for r in self.readers.get(k, ()):
                deps.add(r)
        deps.discard(idx)
        for k in reads:
            self.readers.setdefault(k, []).append(idx)
        for k in writes:
            self.last_w[k] = idx
            self.readers[k] = []
        op = dict(eng=eng, fn=fn, deps=deps, dma=dma, sig=False)
        if dma:
            q = eng
            k = self.ndma[q]
            self.ndma[q] += 1
            op["dq"] = (q, k % self.NDMA[q])
            op["dval"] = 16 * (k // self.NDMA[q] + 1)
            op["dprev"] = 16 * (k // self.NDMA[q])
        self.ops.append(op)
        return idx

    def fence(self, scratch):
        deps = set(self.last_w.values())
        for rs in self.readers.values():
            deps.update(rs)
        idx = len(self.ops)
        if self.fence_idx is not None:
            deps.add(self.fence_idx)
        self.ops.append(dict(eng="pool", fn=lambda e: e.memset(scratch, 0.0), deps=deps, dma=False, sig=True))
        self.last_w = {}
        self.readers = {}
        self.fence_idx = idx

    def pe(self, fn, reads=(), writes=()):
        return self.add("pe", fn, reads, writes)

    def act(self, fn, reads=(), writes=()):
        return self.add("act", fn, reads, writes)

    def dve(self, fn, reads=(), writes=()):
        return self.add("dve", fn, reads, writes)

    def pool(self, fn, reads=(), writes=()):
        return self.add("pool", fn, reads, writes)

    def dma(self, out, in_, reads=(), writes=(), q="sp", **kw):
        return self.add(q, lambda e: e.dma_start(out=out, in_=in_, **kw), reads, writes, dma=True)

    def mm(self, out_ap, pairs, reads, writes):
        def fn(e):
            n = len(pairs)
            ins = None
            for i, (l, r) in enumerate(pairs):
                ins = e.matmul(out_ap, lhsT=l, rhs=r, start=(i == 0), stop=(i == n - 1))
            return ins

        return self.pe(fn, reads, writes)

    def emit(self):
        nc = self.nc
        ops = self.ops
        for op in ops:
            for d in op["deps"]:
                if ops[d]["eng"] == "pe" and op["eng"] == "pe" and not ops[d]["dma"]:
                    continue
                ops[d]["sig"] = True
        last_on = {}
        for i, op in enumerate(ops):
            if not op["dma"]:
                last_on[op["eng"]] = i
        for e, i in last_on.items():
            ops[i]["sig"] = True
        cnt = {e: 0 for e in self.ENGS}
        for op in ops:
            if op["sig"] and not op["dma"]:
                cnt[op["eng"]] += 1
                op["val"] = cnt[op["eng"]]
        with contextlib.ExitStack() as st:
            esem = {e: st.enter_context(nc.semaphore("c_" + e)) for e in self.ENGS}
            dsem = {
                q: [st.enter_context(nc.semaphore("d_%s%d" % (q, i))) for i in range(n)]
                for q, n in self.NDMA.items()
            }
            block = st.enter_context(nc.Block())

            def stream(ename):
                def body(e):
                    waited = {}

                    def wait(sem, key, val):
                        if waited.get(key, 0) >= val:
                            return
                        waited[key] = val
                        e.wait_ge(sem, val)

                    for op in ops:
                        if op["eng"] != ename:
                            continue
                        for d in sorted(op["deps"]):
                            dop = ops[d]
                            if dop["dma"]:
                                q, si = dop["dq"]
                                wait(dsem[q][si], ("d", q, si), dop["dval"])
                            else:
                                if dop["eng"] == "pe" and ename == "pe":
                                    continue
                                wait(esem[dop["eng"]], ("e", dop["eng"]), dop["val"])
                        if op["dma"]:
                            q, si = op["dq"]
                            if op["dprev"] > 0:
                                wait(dsem[q][si], ("d", q, si), op["dprev"])
                            ins = op["fn"](e)
                            ins.then_inc(dsem[q][si], 16)
                        else:
                            ins = op["fn"](e)
                            if op["sig"]:
                                ins.then_inc(esem[ename], 1)
                    if ename == "sp":
                        for q, n in self.NDMA.items():
                            tot = self.ndma[q]
                            for si in range(min(n, tot)):
                                k_last = ((tot - 1 - si) // n) * n + si
                                wait(dsem[q][si], ("d", q, si), 16 * (k_last // n + 1))
                        for en in self.ENGS:
                            if en != "sp" and cnt[en] > 0:
                                wait(esem[en], ("e", en), cnt[en])

                return body

            block.tensor(stream("pe"))
            block.scalar(stream("act"))
            block.vector(stream("dve"))
            block.gpsimd(stream("pool"))
            block.sync(stream("sp"))


def tkeys(prefix, start, n):
    return ["%s%d" % (prefix, t) for t in range(start // 128, (start + n + 127) // 128)]


def pcol(t):
    return 1 + t if t < CTX else 2 + t


class Builder:
    def __init__(self, depth=4, dbg=None):
        self.depth = depth
        self.dbg = dbg
        nc = self.nc = bass.Bass("TRN2", target_bir_lowering=False)
        self.P = Prog(nc)
        self.I = {}
        psall = nc.alloc_psum_tensor("psall", [128, 4096], F32)
        self.PS = [psall[:, i * 512:(i + 1) * 512] for i in range(8)]
        self.PSC = [psall[:, (4 * z + 2) * 512:(4 * z + 4) * 512] for z in range(2)]

    def inp(self, name, shape, dtype=F32):
        self.I[name] = self.nc.dram_tensor(name, list(shape), dtype, kind="ExternalInput").ap()
        return self.I[name]

    def scratch(self, name, shape, dtype):
        return self.nc.dram_tensor(name, list(shape), dtype, kind="Internal").ap()

    def declare(self):
        n_a, n_b = 2, 2
        inp = self.inp
        inp("x", [SEQ, D]); inp("ctx", [CTX, D]); inp("cvec", [2, D])
        inp("ada_w", [4, D, 6 * D]); inp("ada_b", [4, 6 * D])
        inp("norm1_g", [4, D]); inp("norm2_g", [4, D]); inp("final_g", [D])
        inp("ml_w_in", [n_a, D, 4096]); inp("ml_conv_w", [n_a, 5, 2048]); inp("ml_conv_b", [n_a, 2048])
        inp("bdq", [n_a, 16, 128, 128]); inp("bdk", [n_a, 16, 128, 128]); inp("bdv", [n_a, 16, 128, 128])
        inp("bdqT", [n_a, 16, 128, 128]); inp("bdkT", [n_a, 16, 128, 128]); inp("bdvT", [n_a, 16, 128, 128])
        inp("gw", [n_a, 3, 16, 128, 16]); inp("gb", [n_a, 16])
        inp("ml_skip", [n_a, 2048]); inp("ml_hnorm_g", [n_a, 2048]); inp("ml_w_out", [n_a, 2048, D])
        inp("na_w_qkv", [n_b, D, 3 * D]); inp("na_b_qkv", [n_b, 3 * D]); inp("nab", [n_b, 16, 5, 128, 576])
        inp("na_w_out", [n_b, D, D]); inp("na_b_out", [n_b, D])
        inp("ff_w_up", [4, D, 2 * FF]); inp("ff_b_up", [4, 2 * FF]); inp("ff_conv_w", [4, 3, 2 * FF])
        inp("ff_conv_b", [4, 2 * FF]); inp("ff_w_down", [4, FF, D]); inp("ff_b_down", [4, D])
        inp("cmask", [2, 3, 128, 128]); inp("identf", [128, 128]); inp("halfm", [128, 2])
        self.out = self.nc.dram_tensor("out", [SEQ, D], F32, kind="ExternalOutput").ap()
        sc = self.scratch
        self.Xs = sc("Xs", [TOK, D], F32)
        self.modd = sc("modd", [4, 2, 6 * D], F32)
        self.d_u = sc("d_u", [2048, TOK], BF16)
        self.d_uc = sc("d_uc", [2048, TOK], BF16)
        self.d_sz = sc("d_sz", [2048, TOK], BF16)
        self.d_act = sc("d_act", [FF, TOK], BF16)

    def consts(self):
        P, I = self.P, self.I
        self.identf = P.sb("identf", [128, 128], F32)
        self.identb = P.sb("identb", [128, 128], BF16)
        self.cm = P.sb("cm", [128, 2, 3, 128], F32)
        self.halfm = P.sb("halfm", [128, 2], F32)
        self.onesf = P.sb("onesf", [128, 128], F32)
        self.onesb = P.sb("onesb", [128, 2], BF16)
        self.epsc = P.sb("epsc", [128, 1], F32)
        self.fsc = P.sb("fsc", [128, 16], F32)
        self.junk = P.sb("junk", [128, 1024], BF16)
        P.dma(self.identf[:], I["identf"], writes=["identf"])
        P.dma(self.cm[:], I["cmask"].rearrange("z m p c -> p z m c"), writes=["cm"])
        P.dma(self.halfm[:], I["halfm"], writes=["halfm"])
        P.dve(lambda e: e.tensor_copy(out=self.identb[:], in_=self.identf[:]), ["identf"], ["identb"])
        P.dve(lambda e: e.memset(self.onesf[:], 1.0), [], ["onesf"])
        P.dve(lambda e: e.memset(self.onesb[:], 1.0), [], ["onesb"])
        P.dve(lambda e: e.memset(self.epsc[:], EPS), [], ["epsc"])
        self.const_keys = ["identf", "identb", "cm", "halfm", "onesf", "onesb", "epsc"]

    def refence(self):
        self.P.fence(self.fsc[:, 0:1])

    def mods(self):
        P, I, PS = self.P, self.I, self.PS
        m0 = P.sb_off
        s2 = P.sb("s2", [128, 8, 2], F32)
        stg = [P.sb("adastg%d" % i, [128, 8, 512], F32) for i in range(2)]
        modrow = P.sb("modrow", [2, 6 * D], F32)
        adab = P.sb("adab", [2, 6 * D], F32)
        g1b = P.sb("g1b", [2, D], F32)
        g2b = P.sb("g2b", [2, D], F32)
        for jj in range(2):
            P.dma(s2[:, :, jj], I["cvec"][jj].rearrange("(k p) -> p k", p=128), writes=["s2"], allow_slow_non_contiguous=True)
        P.act(lambda e: e.activation(out=s2[:], in_=s2[:], func=AF.Silu), ["s2"], ["s2"])
        n = 0
        for i in range(self.depth):
            P.dma(adab[:], I["ada_b"][i].partition_broadcast(2), writes=["adab"])
            P.dma(g1b[:], I["norm1_g"][i].partition_broadcast(2), writes=["g1b"])
            P.dma(g2b[:], I["norm2_g"][i].partition_broadcast(2), writes=["g2b"])
            for cb in range(12):
                sl = n % 2
                bank = 6 + (n % 2)
                n += 1
                P.dma(stg[sl][:], I["ada_w"][i][:, cb * 512:(cb + 1) * 512].rearrange("(k p) c -> p k c", p=128),
                      writes=["adastg%d" % sl])
                P.mm(PS[bank][0:2, :], [(s2[:, k, :], stg[sl][:, k, :]) for k in range(8)],
                     ["s2", "adastg%d" % sl], ["ps%d" % bank])
                P.dve(lambda e, bank=bank, cb=cb: e.tensor_tensor(out=modrow[:, cb * 512:(cb + 1) * 512], in0=PS[bank][0:2, :],
                                                                  in1=adab[:, cb * 512:(cb + 1) * 512], op=ALU.add),
                      ["ps%d" % bank, "adab"], ["modrow"])
            P.dve(lambda e: e.scalar_tensor_tensor(out=modrow[:, D:2 * D], in0=modrow[:, D:2 * D], scalar=1.0, in1=g1b[:],
                                                   op0=ALU.add, op1=ALU.mult), ["modrow", "g1b"], ["modrow"])
            P.dve(lambda e: e.scalar_tensor_tensor(out=modrow[:, 4 * D:5 * D], in0=modrow[:, 4 * D:5 * D], scalar=1.0, in1=g2b[:],
                                                   op0=ALU.add, op1=ALU.mult), ["modrow", "g2b"], ["modrow"])
            P.dma(self.modd[i], modrow[:], reads=["modrow"], writes=["modd%d" % i])
        self.refence()
        P.sb_off = m0

    def load_cols(self, dst, src1d, key):
        self.P.dma(dst, src1d.rearrange("(k p) -> p k", p=128), writes=[key], allow_slow_non_contiguous=True)

    def persist(self):
        P = self.P
        self.colm = P.sb("colm", [128, 4, 2, 6, 8], F32)
        self.grow_off = P.sb_off
        self.grow = P.sb("grow", [128, 2, 2, D], F32)
        self.nst = [P.sb("nst%d" % s, [128, 8], F32) for s in range(2)]
        self.xn = [P.sb("xn%d" % s, [128, D], BF16) for s in range(2)]
        for i in range(self.depth):
            for v in range(2):
                for s in (0, 1, 3, 4):
                    P.dma(self.colm[:, i, v, s, :], self.modd[i, v, s * D:(s + 1) * D].rearrange("(k p) -> p k", p=128),
                          reads=["modd%d" % i], writes=["colm"], allow_slow_non_contiguous=True)
        self.hA = P.sb("hA", [128, 8, TOK], BF16)
        self.hA_off = P.sb_off - 8 * TOK * 2
        self.top = P.sb_off

    def load_grow(self, i):
        P = self.P
        for v in range(2):
            P.dma(self.grow[:, v, 0, :], self.modd[i, v, 2 * D:3 * D].partition_broadcast(128), reads=["modd%d" % i], writes=["grow"])
            P.dma(self.grow[:, v, 1, :], self.modd[i, v, 5 * D:6 * D].partition_broadcast(128), reads=["modd%d" % i], writes=["grow"])

    def rms_stats(self, xt, xkey, sl):
        P = self.P
        st = self.nst[sl]
        ks = "nst%d" % sl
        P.act(lambda e: e.activation(out=self.junk[:], in_=xt, func=AF.Square, accum_out=st[:, 0:1]), [xkey], ["junk", ks])
        P.act(lambda e: e.activation(out=st[:, 1:2], in_=st[:, 0:1], func=AF.Sqrt, scale=1.0 / D, bias=self.epsc[:, 0:1]), [ks], [ks])
        P.dve(lambda e: e.reciprocal(out=st[:, 2:3], in_=st[:, 1:2]), [ks], [ks])
        return st, ks

    def norm_tile(self, xt, xkey, tt, sl, li, which, hname, bank):
        P, PS = self.P, self.PS
        v = 1 if tt < 2 else 0
        xn = self.xn[sl]
        kx = "xn%d" % sl
        st, ks = self.rms_stats(xt, xkey, sl)
        P.dve(lambda e: e.tensor_scalar(out=xn[:], in0=xt, scalar1=st[:, 2:3], scalar2=None, op0=ALU.mult), [xkey, ks], [kx])
        pv = PS[bank][:].bitcast(BF16)

        def tr(e):
            ins = None
            for c in range(8):
                ins = e.transpose(pv[:, c * 128:(c + 1) * 128], xn[:, c * 128:(c + 1) * 128], self.identb[:])
            return ins

        P.pe(tr, [kx], ["ps%d" % bank])
        sa, sb_ = (1, 0) if which == 0 else (4, 3)
        hk = "%s%d" % (hname, tt)
        for c in range(8):
            A = self.colm[:, li, v, sa, c:c + 1]
            B = self.colm[:, li, v, sb_, c:c + 1]
            dst = self.hA[:, c, tt * 128:(tt + 1) * 128]
            src = pv[:, c * 128:(c + 1) * 128]
            if c % 2 == 0:
                P.act(lambda e, A=A, B=B, dst=dst, src=src: e.activation(out=dst, in_=src, func=AF.Identity, scale=A, bias=B),
                      ["ps%d" % bank, "colm"], [hk])
            else:
                P.dve(lambda e, A=A, B=B, dst=dst, src=src: e.tensor_scalar(out=dst, in0=src, scalar1=A, scalar2=B, op0=ALU.mult, op1=ALU.add),
                      ["ps%d" % bank, "colm"], [hk])

    def first_norm(self):
        P, I = self.P, self.I
        P.sb_off = self.top
        xt = [P.sb("fxt%d" % s, [128, D], F32) for s in range(2)]
        for tt in range(NT):
            sl = tt % 2
            src = I["ctx"][tt * 128:(tt + 1) * 128, :] if tt < 2 else I["x"][(tt - 2) * 128:(tt - 1) * 128, :]
            P.dma(xt[sl][:], src, writes=["fxt%d" % sl])
            P.dma(self.Xs[tt * 128:(tt + 1) * 128, :], xt[sl][:], reads=["fxt%d" % sl], writes=["X%d" % tt])
            self.norm_tile(xt[sl][:], "fxt%d" % sl, tt, sl, 0, 0, "h1_", 6 + sl)
        self.refence()

    def stager(self, name, nbuf, cols):
        P = self.P
        bufs = [P.sb("%s%d" % (name, i), [128, cols], F32) for i in range(nbuf)]
        state = dict(n=0)

        def load(dst, srcs, dkeys, eng="pool", shape=None):
            sl = state["n"] % nbuf
            state["n"] += 1
            key = "%s%d" % (name, sl)
            tot = 0
            for off, ncols, ap, pat in srcs:
                d = bufs[sl][:, off:off + ncols]
                if pat is not None:
                    d = d.rearrange(pat[0], **pat[1])
                P.dma(d, ap, writes=[key], q="pool")
                tot = max(tot, off + ncols)
            src = bufs[sl][:, 0:tot]
            if shape is not None:
                src = src.rearrange(shape[0], **shape[1])
            P.add(eng, lambda e: e.tensor_copy(out=dst, in_=src), [key], dkeys)

        return load

    def ffn(self, i, last):
        P, I, PS = self.P, self.I, self.PS
        hT = self.hA
        P.sb_off = self.top
        tiles = list(range(NT)) if not last else list(range(2, NT))
        blks = TBLK if not last else TBLK[1:]
        W = 2307
        L = W - 2
        wdn = P.sb("wdn", [128, 22, D], BF16)
        bdn = P.sb("bdn", [128, D], F32)
        keep = P.sb_off
        bcol = P.sb("fbcol", [128, 5, 44], F32)
        wup = [P.sb("wup%d" % s, [128, 8, 2, 256], BF16) for s in range(2)]
        upad = [[P.sb("upad%d%d" % (s, g), [128, W], BF16) for g in range(2)] for s in range(2)]
        acc = [[P.sb("facc%d%d" % (s, g), [128, L], F32) for g in range(2)] for s in range(2)]
        actc = [P.sb("actc%d" % s, [128, L], BF16) for s in range(2)]
        self.load_cols(bcol[:, 0, :], I["ff_b_up"][i], "fbcol")
        for k in range(3):
            self.load_cols(bcol[:, 1 + k, :], I["ff_conv_w"][i, k], "fbcol")
        self.load_cols(bcol[:, 4, :], I["ff_conv_b"][i], "fbcol")
        for s in range(2):
            for g in range(2):
                P.pool(lambda e, s=s, g=g: e.memset(upad[s][g][:], 0.0), [], ["upad%d%d" % (s, g)])
        nb = 0
        utail = [None]
        for cg in range(11):
            ws = cg % 2
            for g in range(2):
                P.dma(wup[ws][:, :, g, :], I["ff_w_up"][i][:, g * FF + cg * 256:g * FF + (cg + 1) * 256].rearrange("(k p) c -> p k c", p=128),
                      writes=["wup%d" % ws], q="pool")
            if cg == 1:
                P.dma(bdn[:], I["ff_b_down"][i].partition_broadcast(128), writes=["bdn"])
            P.dma(wdn[:, 2 * cg:2 * cg + 2, :], I["ff_w_down"][i][2 * cg * 128:(2 * cg + 2) * 128, :].rearrange("(c p) o -> p c o", p=128), writes=["wdn"], q="pool")
            for cc in range(2):
                c = cg * 2 + cc
                s = c % 2
                for g in range(2):
                    col = c + 22 * g
                    ku, ka = "upad%d%d" % (s, g), "facc%d%d" % (s, g)
                    u, a = upad[s][g], acc[s][g]
                    for (t0, n) in blks:
                        bank = nb % 4
                        nb += 1
                        P.mm(PS[bank][:, 0:n], [(wup[ws][:, k, g, cc * 128:(cc + 1) * 128], hT[:, k, t0:t0 + n]) for k in range(8)],
                             ["wup%d" % ws] + tkeys("h2_", t0, n), ["ps%d" % bank])
                        P.act(lambda e, bank=bank, n=n, u=u, t0=t0, col=col: e.activation(
                            out=u[:, pcol(t0):pcol(t0) + n], in_=PS[bank][:, 0:n], func=AF.Identity,
                            bias=bcol[:, 0, col:col + 1], scale=1.0), ["ps%d" % bank, "fbcol"], [ku])
                    P.pool(lambda e, u=u, a=a, col=col: e.tensor_scalar(out=a[:], in0=u[:, 1:1 + L], scalar1=bcol[:, 2, col:col + 1],
                                                                        scalar2=bcol[:, 4, col:col + 1], op0=ALU.mult, op1=ALU.add),
                           [ku, "fbcol"], [ka])
                    P.dve(lambda e, u=u, a=a, col=col: e.scalar_tensor_tensor(out=a[:], in0=u[:, 0:L], scalar=bcol[:, 1, col:col + 1],
                                                                               in1=a[:], op0=ALU.mult, op1=ALU.add), [ku, ka, "fbcol"], [ka])
                    P.dve(lambda e, u=u, a=a, col=col: e.scalar_tensor_tensor(out=a[:], in0=u[:, 2:2 + L], scalar=bcol[:, 3, col:col + 1],
                                                                              in1=a[:], op0=ALU.mult, op1=ALU.add), [ku, ka, "fbcol"], [ka])
                if utail[0] is not None:
                    utail[0]()

                def _tail(s=s, c=c):
                    P.act(lambda e: e.activation(out=acc[s][1][:], in_=acc[s][1][:], func=AF.Silu), ["facc%d1" % s], ["facc%d1" % s])
                    P.dve(lambda e: e.tensor_tensor(out=actc[s][:], in0=acc[s][0][:], in1=acc[s][1][:], op=ALU.mult),
                          ["facc%d0" % s, "facc%d1" % s], ["actc%d" % s])
                    if not last:
                        P.dma(self.d_act[c * 128:(c + 1) * 128, 0:CTX], actc[s][:, 0:CTX], reads=["actc%d" % s], writes=["dact%d" % c])
                    P.dma(self.d_act[c * 128:(c + 1) * 128, CTX:TOK], actc[s][:, CTX + 1:TOK + 1], reads=["actc%d" % s], writes=["dact%d" % c])

                utail[0] = _tail
        utail[0]()
        self.refence()
        P.sb_off = keep
        at = [P.sb("at%d" % s, [128, 22, 128], BF16) for s in range(2)]
        xt = [P.sb("xt%d" % s, [128, D], F32) for s in range(2)]
        tmp = [P.sb("ftmp%d" % s, [128, 512], F32) for s in range(2)]
        fg = None
        if last:
            fg = P.sb("fg", [128, D], F32)
            P.dma(fg[:], I["final_g"].partition_broadcast(128), writes=["fg"])
        def dhead(tt, sl):
            P.dma(at[sl][:], self.d_act[:, tt * 128:(tt + 1) * 128].rearrange("(c p) t -> p c t", p=128), writes=["at%d" % sl])
            P.dma(xt[sl][:], self.Xs[tt * 128:(tt + 1) * 128, :], writes=["xt%d" % sl])
            for hf in range(2):
                bank = 2 * sl + hf
                P.mm(PS[bank][:, :], [(at[sl][:, c, :], wdn[:, c, hf * 512:(hf + 1) * 512]) for c in range(22)],
                     ["at%d" % sl], ["ps%d" % bank])

        def dtail(tt, sl):
            v = 1 if tt < 2 else 0
            for hf in range(2):
                bank = 2 * sl + hf
                tk = "ftmp%d" % hf
                P.dve(lambda e, bank=bank, hf=hf: e.tensor_tensor(out=tmp[hf][:], in0=PS[bank][:, :], in1=bdn[:, hf * 512:(hf + 1) * 512], op=ALU.add),
                      ["ps%d" % bank], [tk])
                P.pool(lambda e, hf=hf, v=v: e.tensor_tensor(out=tmp[hf][:], in0=tmp[hf][:], in1=self.grow[:, v, 1, hf * 512:(hf + 1) * 512], op=ALU.mult),
                       [tk, "grow"], [tk])
                P.pool(lambda e, hf=hf, sl=sl: e.tensor_tensor(out=xt[sl][:, hf * 512:(hf + 1) * 512], in0=xt[sl][:, hf * 512:(hf + 1) * 512], in1=tmp[hf][:], op=ALU.add),
                       [tk, "xt%d" % sl], ["xt%d" % sl])
            if last:
                st_, ks = self.rms_stats(xt[sl][:], "xt%d" % sl, sl)
                P.dve(lambda e, sl=sl, st_=st_: e.scalar_tensor_tensor(out=xt[sl][:], in0=xt[sl][:], scalar=st_[:, 2:3], in1=fg[:], op0=ALU.mult, op1=ALU.mult),
                      ["xt%d" % sl, ks, "fg"], ["xt%d" % sl])
                P.dma(self.out[(tt - 2) * 128:(tt - 1) * 128, :], xt[sl][:], reads=["xt%d" % sl], writes=["out%d" % tt])
            else:
                P.dma(self.Xs[tt * 128:(tt + 1) * 128, :], xt[sl][:], reads=["xt%d" % sl], writes=["X%d" % tt])
                self.norm_tile(xt[sl][:], "xt%d" % sl, tt, sl, i + 1, 0, "h1_", 6 + sl)

        prev = None
        for n_, tt in enumerate(tiles):
            dhead(tt, n_ % 2)
            if prev is not None:
                dtail(*prev)
            prev = (tt, n_ % 2)
        dtail(*prev)
        self.refence()

    def outproj(self, i, nk, w_src, b_src, tiles):
        P, I, PS = self.P, self.I, self.PS
        P.sb_off = self.top
        self.load_grow(i)
        wo = P.sb("wo", [128, nk, D], BF16)
        at = [P.sb("oat%d" % s, [128, nk, 128], BF16) for s in range(2)]
        xt = [P.sb("oxt%d" % s, [128, D], F32) for s in range(2)]
        tmp = [P.sb("otmp%d" % s, [128, 512], F32) for s in range(2)]
        bo = None
        if b_src is not None:
            bo = P.sb("bo", [128, D], F32)
            P.dma(bo[:], b_src.partition_broadcast(128), writes=["bo"])
        for c0 in range(0, nk, 4):
            P.dma(wo[:, c0:c0 + 4, :], w_src[c0 * 128:(c0 + 4) * 128, :].rearrange("(c p) o -> p c o", p=128), writes=["wo"], q="pool")
        def ohead(tt, sl):
            P.dma(at[sl][:], self.d_act[0:nk * 128, tt * 128:(tt + 1) * 128].rearrange("(c p) t -> p c t", p=128), writes=["oat%d" % sl])
            P.dma(xt[sl][:], self.Xs[tt * 128:(tt + 1) * 128, :], writes=["oxt%d" % sl])
            for hf in range(2):
                bank = 2 * sl + hf
                P.mm(PS[bank][:, :], [(at[sl][:, k, :], wo[:, k, hf * 512:(hf + 1) * 512]) for k in range(nk)], ["oat%d" % sl, "wo"], ["ps%d" % bank])

        def otail(tt, sl):
            v = 1 if tt < 2 else 0
            for hf in range(2):
                bank = 2 * sl + hf
                tk = "otmp%d" % hf
                if bo is not None:
                    P.dve(lambda e, bank=bank, hf=hf: e.tensor_tensor(out=tmp[hf][:], in0=PS[bank][:, :], in1=bo[:, hf * 512:(hf + 1) * 512], op=ALU.add),
                          ["ps%d" % bank, "bo"], [tk])
                    P.pool(lambda e, hf=hf, v=v: e.tensor_tensor(out=tmp[hf][:], in0=tmp[hf][:], in1=self.grow[:, v, 0, hf * 512:(hf + 1) * 512], op=ALU.mult),
                           [tk, "grow"], [tk])
                else:
                    P.dve(lambda e, bank=bank, hf=hf, v=v: e.tensor_tensor(out=tmp[hf][:], in0=PS[bank][:, :], in1=self.grow[:, v, 0, hf * 512:(hf + 1) * 512], op=ALU.mult),
                          ["ps%d" % bank, "grow"], [tk])
                P.pool(lambda e, hf=hf, sl=sl: e.tensor_tensor(out=xt[sl][:, hf * 512:(hf + 1) * 512], in0=xt[sl][:, hf * 512:(hf + 1) * 512], in1=tmp[hf][:], op=ALU.add),
                       [tk, "oxt%d" % sl], ["oxt%d" % sl])
            P.dma(self.Xs[tt * 128:(tt + 1) * 128, :], xt[sl][:], reads=["oxt%d" % sl], writes=["X%d" % tt])
            self.norm_tile(xt[sl][:], "oxt%d" % sl, tt, sl, i, 1, "h2_", 6 + sl)

        prev = None
        for n_, tt in enumerate(tiles):
            ohead(tt, n_ % 2)
            if prev is not None:
                otail(*prev)
            prev = (tt, n_ % 2)
        otail(*prev)
        self.refence()

    def mlstm(self, i, j):
        P, I, PS = self.P, self.I, self.PS
        hT = self.hA
        idf = self.identf
        P.sb_off = self.top
        gcol = P.sb("gcol", [128, NT, 16], F32)
        emc = P.sb("emc", [128, 8], F32)
        mcol = P.sb("mcol", [128, 2, 16], F32)
        bdb = P.sb("bdb", [128, 3, 16, 128], BF16)
        keepA = P.sb_off
        win = [P.sb("win%d" % s, [128, 8, 512], BF16) for s in range(2)]
        bdT_off = P.sb_off
        bdT = P.sb("bdT", [128, 3, 16, 128], F32)
        P.sb("bdTpad", [128, 1024], F32)
        gwt = P.sb("gwt", [128, 3, 16, 16], F32)
        wgf = P.sb("wgf", [128, 2, 16, 16], BF16)
        ccol = P.sb("ccol", [128, 6, 16], F32)
        gbc = P.sb("gbc", [40, 1], F32)
        W5 = 2310
        L5 = W5 - 4
        upad = [P.sb("mupad%d" % s, [128, W5], BF16) for s in range(2)]
        ucp = [P.sb("mucp%d" % s, [128, W5], BF16) for s in range(2)]
        macc = [P.sb("macc%d" % s, [128, L5], F32) for s in range(2)]
        szb = [P.sb("szb%d" % s, [128, 512], BF16) for s in range(3)]
        mx = P.sb("mx", [8, 12], F32)
        _save = P.sb_off
        P.sb_off = bdT_off
        gsb = P.sb("gsb", [40, TOK], F32)
        gt1 = P.sb("gt1", [40, TOK], F32)
        gt2 = P.sb("gt2", [40, TOK], F32)
        P.sb_off = max(_save, P.sb_off)

        self.load_cols(mcol[:, 0, :], I["ml_hnorm_g"][j], "mcol")
        self.load_cols(mcol[:, 1, :], I["ml_skip"][j], "mcol")
        for k in range(5):
            self.load_cols(ccol[:, k, :], I["ml_conv_w"][j, k], "ccol")
        self.load_cols(ccol[:, 5, :], I["ml_conv_b"][j], "ccol")
        P.dma(gbc[0:8, :], I["gb"][j, 0:8].rearrange("(p o) -> p o", o=1), writes=["gbc"])
        P.dma(gbc[32:40, :], I["gb"][j, 8:16].rearrange("(p o) -> p o", o=1), writes=["gbc"])
        for m, nm in enumerate(("bdq", "bdk", "bdv")):
            P.dma(bdb[:, m, :, :], I[nm][j].rearrange("c p o -> p c o"), writes=["bdb"], q="pool")
            P.dma(bdT[:, m, :, :], I[nm + "T"][j].rearrange("c p o -> p c o"), writes=["bdT"])
            P.dma(gwt[:, m, :, :], I["gw"][j, m].rearrange("c p o -> p c o"), writes=["gwt"])
        P.dve(lambda e: e.tensor_scalar(out=gwt[:, 1, :, :], in0=gwt[:, 1, :, :], scalar1=float(512 ** -0.5), scalar2=None, op0=ALU.mult), ["gwt"], ["gwt"])
        for c in range(16):
            P.mm(PS[5][:, c * 16:(c + 1) * 16], [(bdT[:, 0, c, :], gwt[:, 0, c, :]), (bdT[:, 1, c, :], gwt[:, 1, c, :])], ["bdT", "gwt"], ["ps5"])
            P.mm(PS[6][:, c * 16:(c + 1) * 16], [(bdT[:, 2, c, :], gwt[:, 2, c, :])], ["bdT", "gwt"], ["ps6"])
        P.dve(lambda e: e.tensor_copy(out=wgf[:, 0, :, :], in_=PS[5][:, 0:256].rearrange("p (c o) -> p c o", c=16)), ["ps5"], ["wgf"])
        P.dve(lambda e: e.tensor_copy(out=wgf[:, 1, :, :], in_=PS[6][:, 0:256].rearrange("p (c o) -> p c o", c=16)), ["ps6"], ["wgf"])
        for s in range(2):
            P.pool(lambda e, s=s: e.memset(upad[s][:], 0.0), [], ["mupad%d" % s])

        def p5(t):
            return 2 + t if t < CTX else 4 + t

        nb = 0
        nz = 0
        mtail = [None]
        for og in range(8):
            ws = og % 2
            P.dma(win[ws][:], I["ml_w_in"][j][:, og * 512:(og + 1) * 512].rearrange("(k p) c -> p k c", p=128), writes=["win%d" % ws], q="pool")
            for oo in range(4):
                oc = og * 4 + oo
                s = oc % 2
                for (t0, n) in TBLK:
                    bank = 5 + nb % 3
                    nb += 1
                    P.mm(PS[bank][:, 0:n], [(win[ws][:, k, oo * 128:(oo + 1) * 128], hT[:, k, t0:t0 + n]) for k in range(8)],
                         ["win%d" % ws] + tkeys("h1_", t0, n), ["ps%d" % bank])
                    if oc < 16:
                        P.act(lambda e, bank=bank, n=n, s=s, t0=t0: e.copy(out=upad[s][:, p5(t0):p5(t0) + n], in_=PS[bank][:, 0:n]),
                              ["ps%d" % bank], ["mupad%d" % s])
                    else:
                        zs = nz % 3
                        nz += 1
                        P.act(lambda e, bank=bank, n=n, zs=zs: e.activation(out=szb[zs][:, 0:n], in_=PS[bank][:, 0:n], func=AF.Silu),
                              ["ps%d" % bank], ["szb%d" % zs])
                        P.dma(self.d_sz[(oc - 16) * 128:(oc - 15) * 128, t0:t0 + n], szb[zs][:, 0:n], reads=["szb%d" % zs], writes=["dsz%d" % (oc - 16)])
                if mtail[0] is not None:
                    mtail[0]()
                    mtail[0] = None
                if oc < 16:
                    u, a, uc = upad[s], macc[s], ucp[s]
                    ku, ka, kc = "mupad%d" % s, "macc%d" % s, "mucp%d" % s
                    P.pool(lambda e, u=u, a=a, oc=oc: e.tensor_scalar(out=a[:], in0=u[:, 0:L5], scalar1=ccol[:, 0, oc:oc + 1], scalar2=ccol[:, 5, oc:oc + 1],
                                                                      op0=ALU.mult, op1=ALU.add), [ku, "ccol"], [ka])
                    for k in range(1, 5):
                        eng = "dve"
                        P.add(eng, lambda e, u=u, a=a, oc=oc, k=k: e.scalar_tensor_tensor(out=a[:], in0=u[:, k:k + L5], scalar=ccol[:, k, oc:oc + 1], in1=a[:],
                                                                                        op0=ALU.mult, op1=ALU.add), [ku, ka, "ccol"], [ka])
                    def _mtail(oc=oc, u=u, a=a, uc=uc, ku=ku, ka=ka, kc=kc):
                        P.act(lambda e, a=a, uc=uc: e.activation(out=uc[:, 2:2 + L5], in_=a[:], func=AF.Silu), [ka], [kc])
                        for b, (t0, n) in enumerate(TBLK):
                            def gm(e, b=b, t0=t0, n=n, oc=oc, u=u, uc=uc):
                                c0 = p5(t0)
                                e.matmul(PS[b][0:8, 0:n], lhsT=wgf[:, 0, oc, 0:8], rhs=uc[:, c0:c0 + n], start=(oc == 0), stop=False)
                                e.matmul(PS[b][0:8, 0:n], lhsT=wgf[:, 1, oc, 0:8], rhs=u[:, c0:c0 + n], start=False, stop=(oc == 15))
                                e.matmul(PS[b][32:40, 0:n], lhsT=wgf[:, 0, oc, 8:16], rhs=uc[:, c0:c0 + n], start=(oc == 0), stop=False)
                                return e.matmul(PS[b][32:40, 0:n], lhsT=wgf[:, 1, oc, 8:16], rhs=u[:, c0:c0 + n], start=False, stop=(oc == 15))
                            P.pe(gm, [ku, kc, "wgf"], ["ps%d" % b])
                        for (src, dst, kk) in ((u, self.d_u, "du"), (uc, self.d_uc, "duc")):
                            P.dma(dst[oc * 128:(oc + 1) * 128, 0:CTX], src[:, 2:2 + CTX], reads=[ku if src is u else kc], writes=["%s%d" % (kk, oc)])
                            P.dma(dst[oc * 128:(oc + 1) * 128, CTX:TOK], src[:, 4 + CTX:4 + TOK], reads=[ku if src is u else kc], writes=["%s%d" % (kk, oc)])

                    mtail[0] = _mtail
        if mtail[0] is not None:
            mtail[0]()
        for b, (t0, n) in enumerate(TBLK):
            P.act(lambda e, b=b, t0=t0, n=n: e.activation(out=gsb[0:40, t0:t0 + n], in_=PS[b][0:40, 0:n], func=AF.Identity, bias=gbc[0:40, 0:1], scale=1.0),
                  ["ps%d" % b, "gbc"], ["gsb"])
        R = slice(32, 40)
        P.dve(lambda e: e.tensor_scalar(out=gt2[R, :], in0=gsb[R, :], scalar1=-1.0, scalar2=None, op0=ALU.mult), ["gsb"], ["gt2"])
        P.dve(lambda e: e.tensor_tensor(out=gt1[R, :], in0=gsb[R, :], in1=gt2[R, :], op=ALU.max), ["gsb", "gt2"], ["gt1"])
        P.act(lambda e: e.activation(out=gt1[R, :], in_=gt1[R, :], func=AF.Exp, scale=-1.0), ["gt1"], ["gt1"])
        P.act(lambda e: e.activation(out=gt1[R, :], in_=gt1[R, :], func=AF.Ln, bias=self.onesf[R, 0:1], scale=1.0), ["gt1"], ["gt1"])
        P.dve(lambda e: e.tensor_scalar(out=gt2[R, :], in0=gt2[R, :], scalar1=0.0, scalar2=None, op0=ALU.max), ["gt2"], ["gt2"])
        P.dve(lambda e: e.tensor_tensor(out=gsb[R, :], in0=gt1[R, :], in1=gt2[R, :], op=ALU.add), ["gt1", "gt2", "gsb"], ["gsb"])
        P.dve(lambda e: e.tensor_reduce(out=mx[0:8, 0:1], in_=gsb[0:8, :], axis=AX.X, op=ALU.max), ["gsb"], ["mx"])
        P.dve(lambda e: e.tensor_scalar(out=gsb[0:8, :], in0=gsb[0:8, :], scalar1=mx[0:8, 0:1], scalar2=None, op0=ALU.subtract), ["gsb", "mx"], ["gsb"])
        P.dve(lambda e: e.tensor_scalar(out=mx[0:8, 4:12], in0=idf[0:8, 0:8], scalar1=mx[0:8, 0:1], scalar2=None, op0=ALU.mult), ["mx"], ["mx"])
        P.mm(PS[5][:, 0:8], [(self.onesf[0:8, :], mx[0:8, 4:12])], ["mx"], ["ps5"])
        P.act(lambda e: e.activation(out=emc[:], in_=PS[5][:, 0:8], func=AF.Exp, scale=-1.0), ["ps5"], ["emc"])

        def gtr(e):
            ins = None
            for tt in range(NT):
                e.transpose(PS[6][:, tt * 16:tt * 16 + 8], gsb[0:8, tt * 128:(tt + 1) * 128], idf[0:8, 0:8])
                ins = e.transpose(PS[6][:, tt * 16 + 8:tt * 16 + 16], gsb[32:40, tt * 128:(tt + 1) * 128], idf[32:40, 32:40])
            return ins

        P.pe(gtr, ["gsb"], ["ps6"])
        P.dve(lambda e: e.tensor_copy(out=gcol[:].rearrange("p t g -> p (t g)"), in_=PS[6][:, 0:NT * 16]), ["ps6"], ["gcol"])
        self.refence()

        cm = self.cm
        order = {0: [0, 1] + list(range(2, NT)), 1: [1, 0] + list(range(NT - 1, 1, -1))}
        for hd in range(4):
            P.sb_off = self.hA_off
            H = P.sb("H", [128, NT, 512], BF16)
            C32 = [P.sb("C32_%d" % z, [128, 4, 512], F32) for z in range(2)]
            assert P.sb_off <= self.top
            P.sb_off = keepA
            qT = P.sb("qT", [128, 4, TOK], BF16)
            kT = P.sb("kT", [128, 4, TOK], BF16)
            ktm = P.sb("ktm", [128, NT, 512], BF16)
            vtm = P.sb("vtm", [128, NT, 512], BF16)
            Cbf = [P.sb("Cbf%d" % z, [128, 4, 512], BF16) for z in range(2)]
            nst = P.sb("nstate", [128, 2, 4], F32)
            nbf = P.sb("nbf", [128, 2, 4], BF16)
            keepB = P.sb_off
            ucT = P.sb("ucT", [128, 4, TOK], BF16)
            uT = P.sb("uT", [128, 4, TOK], BF16)
            P.dma(ucT[:], self.d_uc[hd * 512:(hd + 1) * 512, :].rearrange("(c p) t -> p c t", p=128), writes=["ucT"])
            P.dma(uT[:], self.d_u[hd * 512:(hd + 1) * 512, :].rearrange("(c p) t -> p c t", p=128), writes=["uT"])
            nb = 0
            sk = float(512 ** -0.5)
            for c in range(4):
                cc = hd * 4 + c
                for (t0, n) in TBLK:
                    for m, dst in ((0, qT), (1, kT)):
                        bank = nb % 4
                        nb += 1
                        P.mm(PS[bank][:, 0:n], [(bdb[:, m, cc, :], ucT[:, c, t0:t0 + n])], ["ucT"], ["ps%d" % bank])
                        if m == 0:
                            P.act(lambda e, bank=bank, n=n, dst=dst, c=c, t0=t0: e.copy(out=dst[:, c, t0:t0 + n], in_=PS[bank][:, 0:n]), ["ps%d" % bank], ["qT"])
                        else:
                            P.dve(lambda e, bank=bank, n=n, dst=dst, c=c, t0=t0: e.tensor_scalar(out=dst[:, c, t0:t0 + n], in0=PS[bank][:, 0:n], scalar1=sk, scalar2=None, op0=ALU.mult),
                                  ["ps%d" % bank], ["kT"])
            for tt in range(NT):
                tk = slice(tt * 128, (tt + 1) * 128)
                for m, src, dst in ((1, ucT, ktm), (2, uT, vtm)):
                    bank = 4 + nb % 4
                    nb += 1

                    def tm(e, bank=bank, m=m, src=src, tk=tk, hd=hd):
                        ins = None
                        for c in range(4):
                            ins = e.matmul(PS[bank][:, c * 128:(c + 1) * 128], lhsT=src[:, c, tk], rhs=bdb[:, m, hd * 4 + c, :], start=True, stop=True)
                        return ins

                    P.pe(tm, ["ucT", "uT"], ["ps%d" % bank])
                    if m == 1:
                        P.act(lambda e, bank=bank, tt=tt: e.activation(out=ktm[:, tt, :], in_=PS[bank][:, :], func=AF.Copy, scale=sk), ["ps%d" % bank], ["ktm%d" % tt])
                    else:
                        P.dve(lambda e, bank=bank, tt=tt: e.tensor_copy(out=vtm[:, tt, :], in_=PS[bank][:, :]), ["ps%d" % bank], ["vtm%d" % tt])
            for z in range(2):
                P.pool(lambda e, z=z: e.memset(C32[z][:], 0.0), [], ["C32_%d" % z])
                P.pool(lambda e, z=z: e.memset(Cbf[z][:], 0.0), [], ["Cbf%d" % z])
            P.pool(lambda e: e.memset(nst[:], 0.0), [], ["nstate0", "nstate1"])
            P.pool(lambda e: e.memset(nbf[:], 0.0), [], ["nbf0", "nbf1"])
            self.refence()
            P.sb_off = keepB
            DT = P.sb("DT", [128, 2, NT, 128], F32)
            EBb = P.sb("EBb", [128, 2, NT, 128], BF16)
            wkh = P.sb("wkh", [128, 2, 2, NT], F32)
            wkh16 = P.sb("wkh16", [128, 2, 2, NT], BF16)
            dec = P.sb("dec", [128, 2, NT, 2], F32)
            r2 = P.sb("r2", [128, NT, 2], F32)
            tc_ = P.sb("tcol", [128, NT], F32)
            wkc = P.sb("wkc", [128, NT], F32)
            dn = [P.sb("dn%d" % s, [128, 8], F32) for s in range(2)]
            ost = [P.sb("ost%d" % s, [128, 8], F32) for s in range(2)]
            lfm_off = P.sb_off
            LFm = P.sb("LFm", [128, NT, 128], F32)
            T2 = P.sb("T2", [128, NT, 128], F32)
            P.sb_off = lfm_off
            PTt = [P.sb("PT%d" % s, [128, 128], BF16) for s in range(2)]
            qs = [[P.sb("qs%d%d" % (z, h), [128, 4, 128], BF16) for h in range(2)] for z in range(2)]
            vw = [[P.sb("vw%d%d" % (z, h), [128, 512], BF16) for h in range(2)] for z in range(2)]
            _sv = P.sb_off
            P.sb_off = self.grow_off
            hs = [P.sb("hs%d" % s, [128, 512], F32) for s in range(2)]
            hn = [P.sb("hn%d" % s, [128, 512], BF16) for s in range(2)]
            oa = [P.sb("oa%d" % s, [128, 4, 128], F32) for s in range(2)]
            uct = [P.sb("uct%d" % s, [128, 4, 128], BF16) for s in range(2)]
            szt = [P.sb("szt%d" % s, [128, 4, 128], BF16) for s in range(2)]
            actt = [P.sb("actt%d" % s, [128, 4, 128], BF16) for s in range(2)]
            assert P.sb_off <= self.grow_off + 16384
            P.sb_off = _sv
            for z in range(2):
                lic = gcol[:, :, z * 4 + hd]
                nlfc = gcol[:, :, 8 + z * 4 + hd]
                P.dve(lambda e, z=z, nlfc=nlfc: e.tensor_tensor(out=LFm[:], in0=cm[:, z, 1, :].unsqueeze(1).to_broadcast([128, NT, 128]),
                                                                in1=nlfc.unsqueeze(2).to_broadcast([128, NT, 128]), op=ALU.mult), ["gcol"], ["LFm"])
                P.pool(lambda e, lic=lic: e.tensor_tensor(out=T2[:], in0=idf[:].unsqueeze(1).to_broadcast([128, NT, 128]),
                                                          in1=lic.unsqueeze(2).to_broadcast([128, NT, 128]), op=ALU.mult), ["gcol"], ["T2"])
                P.dve(lambda e: e.tensor_tensor(out=LFm[:], in0=LFm[:], in1=T2[:], op=ALU.add), ["LFm", "T2"], ["LFm"])
                P.dve(lambda e, nlfc=nlfc: e.tensor_scalar(out=T2[:], in0=nlfc.unsqueeze(2).to_broadcast([128, NT, 128]), scalar1=-1.0, scalar2=None, op0=ALU.mult),
                       ["gcol", "T2"], ["T2"])
                for g0 in range(0, NT, 4):
                    bank = (g0 // 4) % 2
                    ng = min(4, NT - g0)

                    def dm(e, g0=g0, ng=ng, bank=bank, z=z):
                        ins = None
                        for q in range(ng):
                            e.matmul(PS[bank][:, q * 128:(q + 1) * 128], lhsT=LFm[:, g0 + q, :], rhs=cm[:, z, 0, :], start=True, stop=False)
                            ins = e.matmul(PS[bank][:, q * 128:(q + 1) * 128], lhsT=idf[:], rhs=cm[:, z, 2, :], start=False, stop=True)
                        return ins

                    P.pe(dm, ["LFm"], ["ps%d" % bank])
                    P.act(lambda e, g0=g0, ng=ng, bank=bank, z=z: e.activation(out=DT[:, z, g0:g0 + ng, :].rearrange("p t s -> p (t s)"), in_=PS[bank][:, 0:ng * 128], func=AF.Exp),
                          ["ps%d" % bank], ["DT%d" % z])
                    bank2 = 4 + (g0 // 4) % 2

                    def em(e, g0=g0, ng=ng, bank2=bank2, z=z):
                        ins = None
                        for q in range(ng):
                            ins = e.matmul(PS[bank2][:, q * 128:(q + 1) * 128], lhsT=T2[:, g0 + q, :], rhs=cm[:, z, 0, :], start=True, stop=True)
                        return ins

                    P.pe(em, ["T2"], ["ps%d" % bank2])
                    P.act(lambda e, g0=g0, ng=ng, bank2=bank2, z=z: e.activation(out=EBb[:, z, g0:g0 + ng, :].rearrange("p t s -> p (t s)"), in_=PS[bank2][:, 0:ng * 128], func=AF.Exp),
                          ["ps%d" % bank2], ["EBb%d" % z])
                P.mm(PS[3][:, 0:NT], [(cm[:, z, 1, :], nlfc)], ["gcol"], ["ps3"])
                P.dve(lambda e, lic=lic: e.tensor_tensor(out=tc_[:], in0=PS[3][:, 0:NT], in1=lic, op=ALU.add), ["ps3", "gcol"], ["tcol"])
                P.act(lambda e, z=z: e.activation(out=wkh[:, z, 0, :], in_=tc_[:], func=AF.Exp), ["tcol"], ["wkh%d" % z])
                P.dve(lambda e, z=z: e.tensor_copy(out=wkh16[:, z, 0, :], in_=wkh[:, z, 0, :]), ["wkh%d" % z], ["wkh16_%d" % z])
                P.mm(PS[2][:, 64:64 + NT], [(self.onesf[:], nlfc)], ["gcol"], ["ps2"])
                P.act(lambda e, z=z: e.activation(out=dec[:, z, :, 0], in_=PS[2][:, 64:64 + NT], func=AF.Exp, scale=-1.0),
                      ["ps2"], ["dec%d" % z])
            self.refence()
            if self.dbg == "B2":
                raise StopIteration
            visited = set()
            pending = []

            def rec(z, tt, hd=hd):
                tk = slice(tt * 128, (tt + 1) * 128)
                kz = str(z)
                X, A, C0 = 4 * z, 4 * z + 1, 4 * z + 2
                q_ = qs[z][0]
                vw_ = vw[z][0]
                P.mm(PS[X][:, 0:128], [(kT[:, c, tk], qT[:, c, tk]) for c in range(4)], [], ["psXs" + kz])
                yield
                P.dve(lambda e: e.tensor_tensor(out=PTt[z][:], in0=PS[X][:, 0:128], in1=DT[:, z, tt, :], op=ALU.mult), ["psXs" + kz], ["PT" + kz])
                yield
                P.pool(lambda e: e.tensor_tensor(out=q_[:], in0=qT[:, :, tk], in1=EBb[:, z, tt, :].unsqueeze(1).to_broadcast([128, 4, 128]), op=ALU.mult),
                       [], ["qs" + kz])
                yield
                P.pool(lambda e: e.tensor_tensor(out=vw_[:], in0=vtm[:, tt, :], in1=wkh[:, z, 0, tt:tt + 1].to_broadcast([128, 512]), op=ALU.mult),
                       [], ["vw" + kz])
                yield

                def mA(e):
                    e.matmul(PS[A][:, :], lhsT=PTt[z][:], rhs=vtm[:, tt, :], start=True, stop=False)
                    return e.matmul(PS[X][:, 128:129], lhsT=PTt[z][:], rhs=self.onesb[:, 0:1], start=True, stop=True)

                P.pe(mA, ["PT" + kz], ["psA" + kz, "psXd" + kz])
                yield
                def mB(e):
                    ins = None
                    for c in range(4):
                        e.matmul(PS[A][:, :], lhsT=q_[:, c, :], rhs=Cbf[z][:, c, :], start=False, stop=(c == 3))
                    for c in range(4):
                        ins = e.matmul(PS[X][:, 129:130], lhsT=q_[:, c, :], rhs=nbf[:, z, c:c + 1], start=(c == 0), stop=(c == 3))
                    return ins

                P.pe(mB, ["qs" + kz, "Cbf" + kz, "nbf" + kz], ["psA" + kz, "psXd" + kz])
                yield
                dcol = dec[:, z, tt, 0:1]
                for pr in range(2):
                    def mC(e, pr=pr):
                        ins = None
                        for q in range(2):
                            c = 2 * pr + q
                            ins = e.matmul(PS[C0 + q][:, :], lhsT=ktm[:, tt, c * 128:(c + 1) * 128], rhs=vw_[:], start=True, stop=True)
                        return ins

                    P.pe(mC, ["vw" + kz], ["psC" + kz])
                    yield
                    for q in range(2):
                        P.dve(lambda e, q=q, pr=pr: e.scalar_tensor_tensor(out=C32[z][:, 2 * pr + q, :], in0=C32[z][:, 2 * pr + q, :],
                                                                       scalar=dcol, in1=PS[C0 + q][:, :], op0=ALU.mult, op1=ALU.add),
                              ["psC" + kz, "C32_%s_%d" % (kz, pr)], ["C32_%s_%d" % (kz, pr)])
                        yield

                def nm(e):
                    ins = None
                    for c in range(4):
                        ins = e.matmul(PS[X][:, 136 + c:137 + c], lhsT=ktm[:, tt, c * 128:(c + 1) * 128], rhs=wkh16[:, z, 0, tt:tt + 1], start=True, stop=True)
                    return ins

                P.pe(nm, [], ["psXn" + kz])
                yield

                for pr in range(2):
                    cc = slice(2 * pr, 2 * pr + 2)
                    P.act(lambda e, cc=cc: e.copy(out=Cbf[z][:, cc, :], in_=C32[z][:, cc, :]), ["C32_%s_%d" % (kz, pr)], ["Cbf" + kz])
                    yield
                P.dve(lambda e: e.scalar_tensor_tensor(out=nst[:, z, :], in0=nst[:, z, :], scalar=dcol, in1=PS[X][:, 136:140],
                                                       op0=ALU.mult, op1=ALU.add), ["psXn" + kz, "nstate" + kz], ["nstate" + kz])
                yield
                P.act(lambda e: e.copy(out=nbf[:, z, :], in_=nst[:, z, :]), ["nstate" + kz], ["nbf" + kz])
                yield
                d = dn[z]
                kd = "dn" + kz
                P.dve(lambda e: e.tensor_reduce(out=d[:, 4:5], in_=PS[X][:, 128:130], axis=AX.X, op=ALU.add), ["psXd" + kz], [kd])
                yield
                P.dve(lambda e: e.tensor_scalar(out=d[:, 0:1], in0=d[:, 4:5], scalar1=-1.0, scalar2=None, op0=ALU.mult), [kd], [kd])
                yield
                P.dve(lambda e: e.tensor_tensor(out=d[:, 1:2], in0=d[:, 4:5], in1=d[:, 0:1], op=ALU.max), [kd], [kd])
                yield
                P.dve(lambda e: e.tensor_tensor(out=d[:, 2:3], in0=d[:, 1:2], in1=emc[:, z * 4 + hd:z * 4 + hd + 1], op=ALU.max), [kd], [kd])
                yield
                P.dve(lambda e: e.reciprocal(out=d[:, 3:4], in_=d[:, 2:3]), [kd], [kd])
                yield
                if tt not in visited:
                    visited.add(tt)
                    P.act(lambda e: e.activation(out=H[:, tt, :], in_=PS[A][:, :], func=AF.Copy, scale=d[:, 3:4]), ["psA" + kz, kd], ["H%d" % tt])
                    yield
                    return
                P.dve(lambda e: e.scalar_tensor_tensor(out=hs[z][:], in0=PS[A][:, :], scalar=d[:, 3:4], in1=H[:, tt, :], op0=ALU.mult, op1=ALU.add),
                      ["psA" + kz, kd, "H%d" % tt], ["hs" + kz])
                yield
                pending.append(outg(z, tt))

            def outg(z, tt, hd=hd):
                tk = slice(tt * 128, (tt + 1) * 128)
                kz = str(z)
                X = 4 * z
                o = ost[z]
                ko = "ost" + kz
                P.dve(lambda e: e.tensor_reduce(out=o[:, 0:1], in_=hs[z][:], axis=AX.X, op=ALU.add), ["hs" + kz], [ko])
                yield
                P.act(lambda e: e.activation(out=self.junk[:, z * 512:(z + 1) * 512], in_=hs[z][:], func=AF.Square, accum_out=o[:, 1:2]), ["hs" + kz], ["junk" + kz, ko])
                yield
                P.dve(lambda e: e.tensor_scalar(out=o[:, 2:3], in0=o[:, 0:1], scalar1=1.0 / 512, scalar2=None, op0=ALU.mult), [ko], [ko])
                yield
                P.dve(lambda e: e.tensor_tensor(out=o[:, 3:4], in0=o[:, 2:3], in1=o[:, 2:3], op=ALU.mult), [ko], [ko])
                yield
                P.dve(lambda e: e.scalar_tensor_tensor(out=o[:, 4:5], in0=o[:, 1:2], scalar=1.0 / 512, in1=o[:, 3:4], op0=ALU.mult, op1=ALU.subtract), [ko], [ko])
                yield
                P.act(lambda e: e.activation(out=o[:, 5:6], in_=o[:, 4:5], func=AF.Sqrt, bias=self.epsc[:, 0:1], scale=1.0), [ko], [ko])
                yield
                P.dve(lambda e: e.reciprocal(out=o[:, 6:7], in_=o[:, 5:6]), [ko], [ko])
                yield
                P.dve(lambda e: e.scalar_tensor_tensor(out=o[:, 7:8], in0=o[:, 2:3], scalar=-1.0, in1=o[:, 6:7], op0=ALU.mult, op1=ALU.mult), [ko], [ko])
                yield
                P.act(lambda e: e.activation(out=hn[z][:], in_=hs[z][:], func=AF.Identity, scale=o[:, 6:7], bias=o[:, 7:8]), ["hs" + kz, ko], ["hn" + kz])
                yield
                gb_ = mcol[:, 0, hd * 4:(hd + 1) * 4].unsqueeze(2).to_broadcast([128, 4, 128])
                for pp in range(2):
                    def otr(e, pp=pp):
                        ins = None
                        for q in range(2):
                            c = 2 * pp + q
                            ins = e.matmul(PS[X][:, 256 + q * 128:256 + (q + 1) * 128], lhsT=hn[z][:, c * 128:(c + 1) * 128], rhs=self.identb[:], start=True, stop=True)
                        return ins

                    P.pe(otr, ["hn" + kz], ["psXt" + kz])
                    yield
                    P.dve(lambda e, pp=pp: e.tensor_tensor(out=actt[z][:, 2 * pp:2 * pp + 2, :], in0=PS[X][:, 256:512].rearrange("p (c t) -> p c t", c=2),
                                                         in1=gb_[:, 2 * pp:2 * pp + 2, :], op=ALU.mult), ["psXt" + kz], ["actt" + kz])
                    yield
                P.dma(uct[z][:], self.d_uc[hd * 512:(hd + 1) * 512, tk].rearrange("(c p) t -> p c t", p=128), writes=["uct" + kz])
                P.dma(szt[z][:], self.d_sz[hd * 512:(hd + 1) * 512, tk].rearrange("(c p) t -> p c t", p=128), writes=["szt" + kz])
                sb_ = mcol[:, 1, hd * 4:(hd + 1) * 4].unsqueeze(2).to_broadcast([128, 4, 128])
                P.pool(lambda e: e.tensor_tensor(out=oa[z][:], in0=uct[z][:], in1=sb_, op=ALU.mult), ["uct" + kz], ["oa" + kz])
                yield
                P.pool(lambda e: e.tensor_tensor(out=oa[z][:], in0=oa[z][:], in1=actt[z][:], op=ALU.add), ["oa" + kz, "actt" + kz], ["oa" + kz])
                yield
                P.pool(lambda e: e.tensor_tensor(out=actt[z][:], in0=oa[z][:], in1=szt[z][:], op=ALU.mult), ["oa" + kz, "szt" + kz], ["actt" + kz])
                yield
                P.dma(self.d_act[hd * 512:(hd + 1) * 512, tk].rearrange("(c p) t -> p c t", p=128), actt[z][:], reads=["actt" + kz], writes=["dact_h%d_%d" % (hd, tt)])
                yield

            def drive(gens):
                gens = list(gens)
                while gens:
                    for g in list(gens):
                        try:
                            next(g)
                        except StopIteration:
                            gens.remove(g)

            for step in range(NT):
                cur = pending
                pending = []
                import os as _os
                if _os.environ.get("KSKIPOUT"):
                    cur = []
                if _os.environ.get("KSEQ"):
                    drive([rec(0, order[0][step])]); drive([rec(1, order[1][step])])
                    for g_ in cur:
                        drive([g_])
                else:
                    drive([rec(0, order[0][step]), rec(1, order[1][step])] + cur)
                if self.dbg == "S%d" % step:
                    raise StopIteration
            drive(pending)
            pending = []
            self.refence()
        self.outproj(i, 16, I["ml_w_out"][j], None, list(range(NT)))

    def natten(self, i, j, last):
        P, I, PS = self.P, self.I, self.PS
        hT = self.hA
        P.sb_off = self.top
        wq = [P.sb("nwq%d" % s, [128, 8, 3, 128], BF16) for s in range(2)]
        bcol = P.sb("nbcol", [128, 2, 8], F32)
        bvr = P.sb("nbvr", [128, D], F32)
        qc = [P.sb("nqc%d" % s, [128, TOK], BF16) for s in range(2)]
        kc = [P.sb("nkc%d" % s, [128, TOK], BF16) for s in range(2)]
        vc = [P.sb("nvc%d" % s, [128, NT, 128], BF16) for s in range(2)]
        oc_ = [P.sb("noc%d" % s, [128, TOK], BF16) for s in range(2)]
        nabt = [P.sb("nabt%d" % s, [128, 2, 5, 576], F32) for s in range(2)]
        S = [P.sb("nS%d" % s, [128, 832], F32) for s in range(4)]
        Pm = [P.sb("nPm%d" % s, [128, 832], BF16) for s in range(4)]
        PT = [P.sb("nPT%d" % s, [128, 7, 128], BF16) for s in range(4)]
        otm = [P.sb("notm%d" % s, [128, 128], BF16) for s in range(2)]
        sst = [P.sb("nsst%d" % s, [128, 4], F32) for s in range(4)]
        self.load_cols(bcol[:, 0, :], I["na_b_qkv"][j, 0:D], "nbcol")
        self.load_cols(bcol[:, 1, :], I["na_b_qkv"][j, D:2 * D], "nbcol")
        P.dve(lambda e: e.tensor_scalar(out=bcol[:, 0, :], in0=bcol[:, 0, :], scalar1=0.125, scalar2=None, op0=ALU.mult), ["nbcol"], ["nbcol"])
        P.dma(bvr[:], I["na_b_qkv"][j, 2 * D:3 * D].partition_broadcast(128), writes=["nbvr"])
        Wq = I["na_w_qkv"][j]
        nb = 0
        nu = 0
        qtiles = list(range(NT)) if not last else list(range(2, NT))
        for c in range(8):
            s = c % 2
            for m in range(3):
                P.dma(wq[s][:, :, m, :], Wq[:, m * D + c * 128:m * D + (c + 1) * 128].rearrange("(k p) o -> p k o", p=128), writes=["nwq%d" % s], q="pool")
            P.dma(nabt[s][:], I["nab"][j, 2 * c:2 * c + 2].rearrange("h r q n -> q h r n"), writes=["nabt%d" % s])
            for (t0, n) in TBLK:
                for m, dst, kk in ((0, qc[s], "nqc%d" % s), (1, kc[s], "nkc%d" % s)):
                    bank = nb % 4
                    nb += 1
                    P.mm(PS[bank][:, 0:n], [(wq[s][:, k, m, :], hT[:, k, t0:t0 + n]) for k in range(8)], ["nwq%d" % s] + tkeys("h1_", t0, n), ["ps%d" % bank])
                    P.act(lambda e, bank=bank, n=n, dst=dst, t0=t0, m=m, c=c: e.activation(out=dst[:, t0:t0 + n], in_=PS[bank][:, 0:n], func=AF.Identity,
                                                                                       bias=bcol[:, m, c:c + 1], scale=(0.125 if m == 0 else 1.0)),
                          ["ps%d" % bank, "nbcol"], [kk])
            for g0 in range(0, NT, 4):
                ng = min(4, NT - g0)
                bank = nb % 4
                nb += 1

                def vm(e, g0=g0, ng=ng, bank=bank, s=s):
                    ins = None
                    for q in range(ng):
                        tk = slice((g0 + q) * 128, (g0 + q + 1) * 128)
                        for k in range(8):
                            ins = e.matmul(PS[bank][:, q * 128:(q + 1) * 128], lhsT=hT[:, k, tk], rhs=wq[s][:, k, 2, :], start=(k == 0), stop=(k == 7))
                    return ins

                P.pe(vm, ["nwq%d" % s] + tkeys("h1_", g0 * 128, ng * 128), ["ps%d" % bank])
                P.dve(lambda e, g0=g0, ng=ng, bank=bank, s=s, c=c: e.tensor_tensor(out=vc[s][:, g0:g0 + ng, :], in0=PS[bank][:, 0:ng * 128].rearrange("p (t o) -> p t o", t=ng),
                                                                                  in1=bvr[:, c * 128:(c + 1) * 128].unsqueeze(1).to_broadcast([128, ng, 128]), op=ALU.add),
                      ["ps%d" % bank, "nbvr"], ["nvc%d" % s])
            def unit(n, tt, hh, s=s, c=c):
                tq = slice(tt * 128, (tt + 1) * 128)
                if tt >= 2:
                    r = 2 * (tt - 2)
                    if r <= 2:
                        rs0, nkr, cls = 0, 8, r // 2
                    elif r >= 28:
                        rs0, nkr, cls = 24, 8, 3 + (r - 28) // 2
                    else:
                        rs0, nkr, cls = r - 4, 9, 2
                    nl = nkr * 64
                    ks = CTX + rs0 * 64
                else:
                    nl, ks, cls = 0, 0, 0
                ntot = nl + CTX
                osl = (n // 2) % 2
                pb = slice(hh * 64, hh * 64 + 64)
                u = n % 4
                ub = 2 * u
                k0, k1 = "ps%d" % ub, "ps%d" % (ub + 1)
                kS, kP, kPT, kst = "nS%d" % u, "nPm%d" % u, "nPT%d" % u, "nsst%d" % u
                q_ap = qc[s][pb, tq]
                if nl > 0:
                    P.mm(PS[ub][:, 0:512], [(q_ap, kc[s][pb, ks:ks + 512])], ["nqc%d" % s, "nkc%d" % s], [k0])
                    yield
                    P.dve(lambda e: e.tensor_tensor(out=S[u][:, 0:512], in0=PS[ub][:, 0:512], in1=nabt[s][:, hh, cls, 0:512], op=ALU.add),
                          [k0, "nabt%d" % s], [kS])
                    yield

                def sc2(e):
                    if nl > 512:
                        e.matmul(PS[ub + 1][:, 0:64], lhsT=q_ap, rhs=kc[s][pb, ks + 512:ks + 576], start=True, stop=True)
                    return e.matmul(PS[ub + 1][:, 64:320], lhsT=q_ap, rhs=kc[s][pb, 0:CTX], start=True, stop=True)

                P.pe(sc2, ["nqc%d" % s, "nkc%d" % s], [k1])
                yield
                if nl > 512:
                    P.dve(lambda e: e.tensor_tensor(out=S[u][:, 512:576], in0=PS[ub + 1][:, 0:64], in1=nabt[s][:, hh, cls, 512:576], op=ALU.add),
                          [k1, "nabt%d" % s], [kS])
                    yield
                P.act(lambda e: e.copy(out=S[u][:, nl:nl + CTX], in_=PS[ub + 1][:, 64:320]), [k1], [kS])
                yield
                st = sst[u]
                P.dve(lambda e: e.tensor_reduce(out=st[:, 0:1], in_=S[u][:, 0:ntot], axis=AX.X, op=ALU.max), [kS], [kst])
                yield
                P.dve(lambda e: e.tensor_scalar(out=st[:, 1:2], in0=st[:, 0:1], scalar1=-1.0, scalar2=None, op0=ALU.mult), [kst], [kst])
                yield
                P.act(lambda e: e.activation(out=Pm[u][:, 0:ntot], in_=S[u][:, 0:ntot], func=AF.Exp, bias=st[:, 1:2], scale=1.0, accum_out=st[:, 2:3]),
                      [kS, kst], [kP, kst])
                yield
                P.dve(lambda e: e.reciprocal(out=st[:, 3:4], in_=st[:, 2:3]), [kst], [kst])
                yield
                chunks = []
                off = 0
                while off < nl:
                    kn = min(128, nl - off)
                    chunks.append((off, kn, vc[s][0:kn, (ks + off) // 128, hh * 64:(hh + 1) * 64]))
                    off += kn
                for q in range(2):
                    chunks.append((nl + q * 128, 128, vc[s][:, q, hh * 64:(hh + 1) * 64]))
                pv = PS[ub][:].bitcast(BF16)

                def ptr(e):
                    ins = None
                    for ci, (off, kn, _) in enumerate(chunks):
                        ins = e.transpose(pv[0:kn, ci * 128:(ci + 1) * 128], Pm[u][:, off:off + kn], self.identb[:])
                    return ins

                P.pe(ptr, [kP], [k0])
                yield
                nch = len(chunks)
                P.act(lambda e: e.copy(out=PT[u][:, 0:nch, :].rearrange("p c q -> p (c q)"), in_=pv[:, 0:nch * 128]), [k0], [kPT])
                yield
                P.mm(PS[ub + 1][:, 320:384], [(PT[u][0:kn, ci, :], vap) for ci, (off, kn, vap) in enumerate(chunks)],
                     [kPT, "nvc%d" % s], [k1])
                yield
                P.act(lambda e: e.activation(out=otm[osl][:, hh * 64:(hh + 1) * 64], in_=PS[ub + 1][:, 320:384],
                                             func=AF.Copy, scale=st[:, 3:4]), [k1, kst], ["notm%d" % osl])
                yield
                if hh == 1:
                    pv3 = PS[ub + 1][:].bitcast(BF16)
                    P.pe(lambda e: e.transpose(pv3[:, 768:896], otm[osl][:], self.identb[:]), ["notm%d" % osl], [k1])
                    yield
                    P.dve(lambda e: e.tensor_copy(out=oc_[s][:, tq], in_=pv3[:, 768:896]), [k1], ["noc%d" % s])
                    yield

            units = [(tt, hh) for tt in qtiles for hh in range(2)]
            active = []
            nxt = 0
            while nxt < len(units) or active:
                while len(active) < 4 and nxt < len(units):
                    active.append(unit(nxt, units[nxt][0], units[nxt][1]))
                    nxt += 1
                for g in list(active):
                    try:
                        next(g)
                    except StopIteration:
                        active.remove(g)
            if not last:
                P.dma(self.d_act[c * 128:(c + 1) * 128, 0:CTX], oc_[s][:, 0:CTX], reads=["noc%d" % s], writes=["dactn%d" % c])
            P.dma(self.d_act[c * 128:(c + 1) * 128, CTX:TOK], oc_[s][:, CTX:TOK], reads=["noc%d" % s], writes=["dactn%d" % c])
        self.refence()
        self.outproj(i, 8, I["na_w_out"][j], I["na_b_out"][j], qtiles)

    def build(self):
        P = self.P
        self.declare()
        self.consts()
        self.mods()
        self.persist()
        self.first_norm()
        try:
            self.layers()
        except StopIteration:
            pass
        P.emit()
        return self.nc

    def layers(self):
        for i in range(self.depth):
            last = i == self.depth - 1
            j = i // 2
            if i % 2 == 0:
                self.mlstm(i, j)
            else:
                self.natten(i, j, last)
            self.ffn(i, last)


def host_inputs(inputs, depth=4):
    f = lambda a: np.ascontiguousarray(np.asarray(a, dtype=np.float32))
    x, c, ctx, c_ctx = f(inputs["x"]), f(inputs["c"]), f(inputs["ctx"]), f(inputs["c_ctx"])
    B = x.shape[0]
    shared = {}
    for k in ("ada_w", "ada_b", "norm1_g", "norm2_g", "final_g", "ml_w_in", "ml_conv_w", "ml_conv_b", "ml_skip", "ml_hnorm_g", "ml_w_out",
              "na_w_qkv", "na_b_qkv", "na_w_out", "na_b_out", "ff_w_up", "ff_b_up", "ff_conv_w", "ff_conv_b", "ff_w_down", "ff_b_down"):
        shared[k] = f(inputs[k])
    n_a = shared["ml_w_in"].shape[0]
    for nm, src in (("bdq", "ml_wq"), ("bdk", "ml_wk"), ("bdv", "ml_wv")):
        w = f(inputs[src])
        bd = np.zeros((n_a, 16, 128, 128), np.float32)
        wr = w.reshape(n_a, 16, 32, 4, 4)
        for n in range(32):
            bd[:, :, 4 * n:4 * n + 4, 4 * n:4 * n + 4] = wr[:, :, n]
        shared[nm] = bd
        shared[nm + "T"] = np.ascontiguousarray(bd.transpose(0, 1, 3, 2))
    gw = f(inputs["ml_gate_w"])
    g2 = gw.reshape(n_a, 2, 3, 16, 128, 2, 4)
    g2 = g2.transpose(0, 2, 3, 4, 5, 1, 6)
    shared["gw"] = np.ascontiguousarray(g2.reshape(n_a, 3, 16, 128, 16))
    gb = f(inputs["ml_gate_b"]).reshape(n_a, 2, 2, 4).transpose(0, 2, 1, 3)
    shared["gb"] = np.ascontiguousarray(gb.reshape(n_a, 16))
    rpb = f(inputs["na_rpb"])
    n_b = rpb.shape[0]
    nab = np.full((n_b, 16, 5, 128, 576), NEGV, np.float32)
    col = np.arange(64)
    cs = np.clip(col - 8, 0, 48)
    ok = (col[None, :] >= cs[:, None]) & (col[None, :] < cs[:, None] + 16)
    dc = np.clip(col[None, :] - col[:, None] + 15, 0, 30)
    for cls, r in enumerate((0, 2, 4, 28, 30)):
        for rho in range(2):
            rq = r + rho
            rs_q = int(np.clip(rq - 4, 0, 24))
            rs0 = int(np.clip(r - 4, 0, 24))
            nkr = 9 if cls == 2 else 8
            for jj in range(nkr):
                krow = rs0 + jj
                if krow < rs_q or krow >= rs_q + 8:
                    continue
                dr = krow - rq + 7
                vals = rpb[:, :, dr, :][:, :, dc]
                blk = np.where(ok[None, None], vals, np.float32(NEGV))
                nab[:, :, cls, rho * 64:(rho + 1) * 64, jj * 64:(jj + 1) * 64] = blk
    shared["nab"] = nab
    u = np.arange(128)
    same = np.ones((128, 128), bool)
    cm = np.zeros((2, 3, 128, 128), np.float32)
    cm[0, 0] = same & (u[:, None] <= u[None, :])
    cm[0, 1] = -(same & (u[:, None] > u[None, :])).astype(np.float32)
    cm[0, 2] = np.where(same & (u[:, None] <= u[None, :]), 0.0, NEGV)
    cm[1, 0] = same & (u[:, None] >= u[None, :])
    cm[1, 1] = -(same & (u[:, None] < u[None, :])).astype(np.float32)
    cm[1, 2] = np.where(same & (u[:, None] >= u[None, :]), 0.0, NEGV)
    shared["cmask"] = cm
    shared["identf"] = np.eye(128, dtype=np.float32)
    hm = np.zeros((128, 2), np.float32)
    hm[:64, 0] = 1.0
    hm[64:, 1] = 1.0
    shared["halfm"] = hm
    maps = []
    for b in range(B):
        m = dict(shared)
        m["x"] = x[b]
        m["ctx"] = ctx[b]
        m["cvec"] = np.ascontiguousarray(np.stack([c[b], c_ctx], 0))
        maps.append(m)
    return maps


_NC_CACHE = {}


def kernel(**inputs):
    maps = host_inputs(inputs)
    if "nc" not in _NC_CACHE:
        _NC_CACHE["nc"] = Builder(4).build()
    nc = _NC_CACHE["nc"]
    res = run_bass_kernel_spmd(nc, maps, core_ids=list(range(len(maps))))
    return np.stack([np.asarray(r["out"], dtype=np.float32) for r in res.results], 0)
```

```python
import contextlib
import itertools
import numpy as np
import concourse.bass as bass
import concourse.mybir as mybir
from concourse.bass_utils import run_bass_kernel_spmd

F32 = mybir.dt.float32
BF16 = mybir.dt.bfloat16
AF = mybir.ActivationFunctionType
ALU = mybir.AluOpType
AX = mybir.AxisListType

D = 1024
SEQ = 2048
CTX = 256
TOK = SEQ + CTX
NT = TOK // 128
EPS = 1e-6
FF = 2816
NEGV = -30000.0
TBLK = [(0, 256), (256, 512), (768, 512), (1280, 512), (1792, 512)]


class Prog:
    ENGS = ("pe", "act", "dve", "pool", "sp")
    NDMA = {"sp": 40, "pool": 16, "act": 8}

    def __init__(self, nc):
        self.nc = nc
        self.ops = []
        self.last_w = {}
        self.readers = {}
        self.sb_off = 16512
        self.ndma = {q: 0 for q in self.NDMA}
        self.fence_idx = None
        self.nname = 0

    def sb(self, name, shape, dtype):
        esz = 4 if dtype == F32 else 2
        n = 1
        for s in shape[1:]:
            n *= s
        nbytes = (n * esz + 63) // 64 * 64
        off = self.sb_off
        self.sb_off += nbytes
        assert self.sb_off <= 229344, (name, self.sb_off)
        self.nname += 1
        return self.nc.alloc_sbuf_tensor_at("%s_%d" % (name, self.nname), list(shape), dtype, offset=off)

    def add(self, eng, fn, reads=(), writes=(), dma=False):
        idx = len(self.ops)
        deps = set()
        if self.fence_idx is not None:
            deps.add(self.fence_idx)
        for k in reads:
            if k in self.last_w:
                deps.add(self.last_w[k])
        for k in writes:
            if k in self.last_w:
                deps.add(self.last_w[k])
            for r in self.readers.get(k, ()):
                deps.add(r)
        deps.discard(idx)
        for k in reads:
            self.readers.setdefault(k, []).append(idx)
        for k in writes:
            self.last_w[k] = idx
            self.readers[k] = []
        op = dict(eng=eng, fn=fn, deps=deps, dma=dma, sig=False)
        if dma:
            q = eng
            k = self.ndma[q]
            self.ndma[q] += 1
            op["dq"] = (q, k % self.NDMA[q])
            op["dval"] = 16 * (k // self.NDMA[q] + 1)
            op["dprev"] = 16 * (k // self.NDMA[q])
        self.ops.append(op)
        return idx

    def fence(self, scratch):
        deps = set(self.last_w.values())
        for rs in self.readers.values():
            deps.update(rs)
        idx = len(self.ops)
        if self.fence_idx is not None:
            deps.add(self.fence_idx)
        self.ops.append(dict(eng="pool", fn=lambda e: e.memset(scratch, 0.0), deps=deps, dma=False, sig=True))
        self.last_w = {}
        self.readers = {}
        self.fence_idx = idx

    def pe(self, fn, reads=(), writes=()):
        return self.add("pe", fn, reads, writes)

    def act(self, fn, reads=(), writes=()):
        return self.add("act", fn, reads, writes)

    def dve(self, fn, reads=(), writes=()):
        return self.add("dve", fn, reads, writes)

    def pool(self, fn, reads=(), writes=()):
        return self.add("pool", fn, reads, writes)

    def dma(self, out, in_, reads=(), writes=(), q="sp", **kw):
        return self.add(q, lambda e: e.dma_start(out=out, in_=in_, **kw), reads, writes, dma=True)

    def mm(self, out_ap, pairs, reads, writes):
        def fn(e):
            n = len(pairs)
            ins = None
            for i, (l, r) in enumerate(pairs):
                ins = e.matmul(out_ap, lhsT=l, rhs=r, start=(i == 0), stop=(i == n - 1))
            return ins

        return self.pe(fn, reads, writes)

    def emit(self):
        nc = self.nc
        ops = self.ops
        for op in ops:
            for d in op["deps"]:
                if ops[d]["eng"] == "pe" and op["eng"] == "pe" and not ops[d]["dma"]:
                    continue
                ops[d]["sig"] = True
        last_on = {}
        for i, op in enumerate(ops):
            if not op["dma"]:
                last_on[op["eng"]] = i
        for e, i in last_on.items():
            ops[i]["sig"] = True
        cnt = {e: 0 for e in self.ENGS}
        for op in ops:
            if op["sig"] and not op["dma"]:
                cnt[op["eng"]] += 1
                op["val"] = cnt[op["eng"]]
        with contextlib.ExitStack() as st:
            esem = {e: st.enter_context(nc.semaphore("c_" + e)) for e in self.ENGS}
            dsem = {
                q: [st.enter_context(nc.semaphore("d_%s%d" % (q, i))) for i in range(n)]
                for q, n in self.NDMA.items()
            }
            block = st.enter_context(nc.Block())

            def stream(ename):
                def body(e):
                    waited = {}

                    def wait(sem, key, val):
                        if waited.get(key, 0) >= val:
                            return
                        waited[key] = val
                        e.wait_ge(sem, val)

                    for op in ops:
                        if op["eng"] != ename:
                            continue
                        for d in sorted(op["deps"]):
                            dop = ops[d]
                            if dop["dma"]:
                                q, si = dop["dq"]
                                wait(dsem[q][si], ("d", q, si), dop["dval"])
                            else:
                                if dop["eng"] == "pe" and ename == "pe":
                                    continue
                                wait(esem[dop["eng"]], ("e", dop["eng"]), dop["val"])
                        if op["dma"]:
                            q, si = op["dq"]
                            if op["dprev"] > 0:
                                wait(dsem[q][si], ("d", q, si), op["dprev"])
                            ins = op["fn"](e)
                            ins.then_inc(dsem[q][si], 16)
                        else:
                            ins = op["fn"](e)
                            if op["sig"]:
                                ins.then_inc(esem[ename], 1)
                    if ename == "sp":
                        for q, n in self.NDMA.items():
                            tot = self.ndma[q]
                            for si in range(min(n, tot)):
                                k_last = ((tot - 1 - si) // n) * n + si
                                wait(dsem[q][si], ("d", q, si), 16 * (k_last // n + 1))
                        for en in self.ENGS:
                            if en != "sp" and cnt[en] > 0:
                                wait(esem[en], ("e", en), cnt[en])

                return body

            block.tensor(stream("pe"))
            block.scalar(stream("act"))
            block.vector(stream("dve"))
            block.gpsimd(stream("pool"))
            block.sync(stream("sp"))


def tkeys(prefix, start, n):
    return ["%s%d" % (prefix, t) for t in range(start // 128, (start + n + 127) // 128)]


def pcol(t):
    return 1 + t if t < CTX else 2 + t


class Builder:
    def __init__(self, depth=4, dbg=None):
        self.depth = depth
        self.dbg = dbg
        nc = self.nc = bass.Bass("TRN2", target_bir_lowering=False)
        self.P = Prog(nc)
        self.I = {}
        psall = nc.alloc_psum_tensor("psall", [128, 4096], F32)
        self.PS = [psall[:, i * 512:(i + 1) * 512] for i in range(8)]
        self.PSC = [psall[:, (4 * z + 2) * 512:(4 * z + 4) * 512] for z in range(2)]

    def inp(self, name, shape, dtype=F32):
        self.I[name] = self.nc.dram_tensor(name, list(shape), dtype, kind="ExternalInput").ap()
        return self.I[name]

    def scratch(self, name, shape, dtype):
        return self.nc.dram_tensor(name, list(shape), dtype, kind="Internal").ap()

    def declare(self):
        n_a, n_b = 2, 2
        inp = self.inp
        inp("x", [SEQ, D]); inp("ctx", [CTX, D]); inp("cvec", [2, D])
        inp("ada_w", [4, D, 6 * D]); inp("ada_b", [4, 6 * D])
        inp("norm1_g", [4, D]); inp("norm2_g", [4, D]); inp("final_g", [D])
        inp("ml_w_in", [n_a, D, 4096]); inp("ml_conv_w", [n_a, 5, 2048]); inp("ml_conv_b", [n_a, 2048])
        inp("bdq", [n_a, 16, 128, 128]); inp("bdk", [n_a, 16, 128, 128]); inp("bdv", [n_a, 16, 128, 128])
        inp("bdqT", [n_a, 16, 128, 128]); inp("bdkT", [n_a, 16, 128, 128]); inp("bdvT", [n_a, 16, 128, 128])
        inp("gw", [n_a, 3, 16, 128, 16]); inp("gb", [n_a, 16])
        inp("ml_skip", [n_a, 2048]); inp("ml_hnorm_g", [n_a, 2048]); inp("ml_w_out", [n_a, 2048, D])
        inp("na_w_qkv", [n_b, D, 3 * D]); inp("na_b_qkv", [n_b, 3 * D]); inp("nab", [n_b, 16, 5, 128, 576])
        inp("na_w_out", [n_b, D, D]); inp("na_b_out", [n_b, D])
        inp("ff_w_up", [4, D, 2 * FF]); inp("ff_b_up", [4, 2 * FF]); inp("ff_conv_w", [4, 3, 2 * FF])
        inp("ff_conv_b", [4, 2 * FF]); inp("ff_w_down", [4, FF, D]); inp("ff_b_down", [4, D])
        inp("cmask", [2, 3, 128, 128]); inp("identf", [128, 128]); inp("halfm", [128, 2])
        self.out = self.nc.dram_tensor("out", [SEQ, D], F32, kind="ExternalOutput").ap()
        sc = self.scratch
        self.Xs = sc("Xs", [TOK, D], F32)
        self.modd = sc("modd", [4, 2, 6 * D], F32)
        self.d_u = sc("d_u", [2048, TOK], BF16)
        self.d_uc = sc("d_uc", [2048, TOK], BF16)
        self.d_sz = sc("d_sz", [2048, TOK], BF16)
        self.d_act = sc("d_act", [FF, TOK], BF16)

    def consts(self):
        P, I = self.P, self.I
        self.identf = P.sb("identf", [128, 128], F32)
        self.identb = P.sb("identb", [128, 128], BF16)
        self.cm = P.sb("cm", [128, 2, 3, 128], F32)
        self.halfm = P.sb("halfm", [128, 2], F32)
        self.onesf = P.sb("onesf", [128, 128], F32)
        self.onesb = P.sb("onesb", [128, 2], BF16)
        self.epsc = P.sb("epsc", [128, 1], F32)
        self.fsc = P.sb("fsc", [128, 16], F32)
        self.junk = P.sb("junk", [128, 1024], BF16)
        P.dma(self.identf[:], I["identf"], writes=["identf"])
        P.dma(self.cm[:], I["cmask"].rearrange("z m p c -> p z m c"), writes=["cm"])
        P.dma(self.halfm[:], I["halfm"], writes=["halfm"])
        P.dve(lambda e: e.tensor_copy(out=self.identb[:], in_=self.identf[:]), ["identf"], ["identb"])
        P.dve(lambda e: e.memset(self.onesf[:], 1.0), [], ["onesf"])
        P.dve(lambda e: e.memset(self.onesb[:], 1.0), [], ["onesb"])
        P.dve(lambda e: e.memset(self.epsc[:], EPS), [], ["epsc"])
        self.const_keys = ["identf", "identb", "cm", "halfm", "onesf", "onesb", "epsc"]

    def refence(self):
        self.P.fence(self.fsc[:, 0:1])

    def mods(self):
        P, I, PS = self.P, self.I, self.PS
        m0 = P.sb_off
        s2 = P.sb("s2", [128, 8, 2], F32)
        stg = [P.sb("adastg%d" % i, [128, 8, 512], F32) for i in range(2)]
        modrow = P.sb("modrow", [2, 6 * D], F32)
        adab = P.sb("adab", [2, 6 * D], F32)
        g1b = P.sb("g1b", [2, D], F32)
        g2b = P.sb("g2b", [2, D], F32)
        for jj in range(2):
            P.dma(s2[:, :, jj], I["cvec"][jj].rearrange("(k p) -> p k", p=128), writes=["s2"], allow_slow_non_contiguous=True)
        P.act(lambda e: e.activation(out=s2[:], in_=s2[:], func=AF.Silu), ["s2"], ["s2"])
        self._modn = 0

        def mod_layer(i):
            n = self._modn
            P.dma(adab[:], I["ada_b"][i].partition_broadcast(2), writes=["adab"])
            P.dma(g1b[:], I["norm1_g"][i].partition_broadcast(2), writes=["g1b"])
            P.dma(g2b[:], I["norm2_g"][i].partition_broadcast(2), writes=["g2b"])
            for cb in range(12):
                sl = n % 2
                bank = 6 + (n % 2)
                n += 1
                P.dma(stg[sl][:], I["ada_w"][i][:, cb * 512:(cb + 1) * 512].rearrange("(k p) c -> p k c", p=128),
                      writes=["adastg%d" % sl])
                P.mm(PS[bank][0:2, :], [(s2[:, k, :], stg[sl][:, k, :]) for k in range(8)],
                     ["s2", "adastg%d" % sl], ["ps%d" % bank])
                P.dve(lambda e, bank=bank, cb=cb: e.tensor_tensor(out=modrow[:, cb * 512:(cb + 1) * 512], in0=PS[bank][0:2, :],
                                                                  in1=adab[:, cb * 512:(cb + 1) * 512], op=ALU.add),
                      ["ps%d" % bank, "adab"], ["modrow"])
            P.dve(lambda e: e.scalar_tensor_tensor(out=modrow[:, D:2 * D], in0=modrow[:, D:2 * D], scalar=1.0, in1=g1b[:],
                                                   op0=ALU.add, op1=ALU.mult), ["modrow", "g1b"], ["modrow"])
            P.dve(lambda e: e.scalar_tensor_tensor(out=modrow[:, 4 * D:5 * D], in0=modrow[:, 4 * D:5 * D], scalar=1.0, in1=g2b[:],
                                                   op0=ALU.add, op1=ALU.mult), ["modrow", "g2b"], ["modrow"])
            P.dma(self.modd[i], modrow[:], reads=["modrow"], writes=["modd%d" % i])
            self._modn = n

        self.mod_layer = mod_layer

    def load_cols(self, dst, src1d, key):
        self.P.dma(dst, src1d.rearrange("(k p) -> p k", p=128), writes=[key], allow_slow_non_contiguous=True)

    def persist(self):
        P = self.P
        self.colm = P.sb("colm", [128, 4, 2, 6, 8], F32)
        self.grow_off = P.sb_off
        self.grow = P.sb("grow", [128, 2, 2, D], F32)
        self.nst = [P.sb("nst%d" % s, [128, 8], F32) for s in range(2)]
        self.xn = [P.sb("xn%d" % s, [128, D], BF16) for s in range(2)]
        self.hA = P.sb("hA", [128, 8, TOK], BF16)
        self.hA_off = P.sb_off - 8 * TOK * 2
        self.top = P.sb_off

    def colm_load(self, i):
        P = self.P
        for v in range(2):
            for s in (0, 1, 3, 4):
                P.dma(self.colm[:, i, v, s, :], self.modd[i, v, s * D:(s + 1) * D].rearrange("(k p) -> p k", p=128),
                      reads=["modd%d" % i], writes=["colm%d" % i], allow_slow_non_contiguous=True)

    def load_grow(self, i):
        P = self.P
        for v in range(2):
            P.dma(self.grow[:, v, 0, :], self.modd[i, v, 2 * D:3 * D].partition_broadcast(128), reads=["modd%d" % i], writes=["grow"])
            P.dma(self.grow[:, v, 1, :], self.modd[i, v, 5 * D:6 * D].partition_broadcast(128), reads=["modd%d" % i], writes=["grow"])

    def rms_stats(self, xt, xkey, sl):
        P = self.P
        st = self.nst[sl]
        ks = "nst%d" % sl
        P.act(lambda e: e.activation(out=self.junk[:], in_=xt, func=AF.Square, accum_out=st[:, 0:1]), [xkey], ["junk", ks])
        P.act(lambda e: e.activation(out=st[:, 1:2], in_=st[:, 0:1], func=AF.Sqrt, scale=1.0 / D, bias=self.epsc[:, 0:1]), [ks], [ks])
        P.dve(lambda e: e.reciprocal(out=st[:, 2:3], in_=st[:, 1:2]), [ks], [ks])
        return st, ks

    def norm_tile(self, xt, xkey, tt, sl, li, which, hname, bank):
        P, PS = self.P, self.PS
        v = 1 if tt < 2 else 0
        xn = self.xn[sl]
        kx = "xn%d" % sl
        st, ks = self.rms_stats(xt, xkey, sl)
        P.dve(lambda e: e.tensor_scalar(out=xn[:], in0=xt, scalar1=st[:, 2:3], scalar2=None, op0=ALU.mult), [xkey, ks], [kx])
        pv = PS[bank][:].bitcast(BF16)

        def tr(e):
            ins = None
            for c in range(8):
                ins = e.transpose(pv[:, c * 128:(c + 1) * 128], xn[:, c * 128:(c + 1) * 128], self.identb[:])
            return ins

        P.pe(tr, [kx], ["ps%d" % bank])
        sa, sb_ = (1, 0) if which == 0 else (4, 3)
        hk = "%s%d" % (hname, tt)
        for c in range(8):
            A = self.colm[:, li, v, sa, c:c + 1]
            B = self.colm[:, li, v, sb_, c:c + 1]
            dst = self.hA[:, c, tt * 128:(tt + 1) * 128]
            src = pv[:, c * 128:(c + 1) * 128]
            if c % 2 == 0:
                P.act(lambda e, A=A, B=B, dst=dst, src=src: e.activation(out=dst, in_=src, func=AF.Identity, scale=A, bias=B),
                      ["ps%d" % bank, "colm%d" % li], [hk])
            else:
                P.dve(lambda e, A=A, B=B, dst=dst, src=src: e.tensor_scalar(out=dst, in0=src, scalar1=A, scalar2=B, op0=ALU.mult, op1=ALU.add),
                      ["ps%d" % bank, "colm%d" % li], [hk])

    def first_norm(self):
        P, I = self.P, self.I
        xt = [P.sb("fxt%d" % s, [128, D], F32) for s in range(2)]
        for tt in range(NT):
            sl = tt % 2
            src = I["ctx"][tt * 128:(tt + 1) * 128, :] if tt < 2 else I["x"][(tt - 2) * 128:(tt - 1) * 128, :]
            P.dma(xt[sl][:], src, writes=["fxt%d" % sl])
            P.dma(self.Xs[tt * 128:(tt + 1) * 128, :], xt[sl][:], reads=["fxt%d" % sl], writes=["X%d" % tt])
            self.norm_tile(xt[sl][:], "fxt%d" % sl, tt, sl, 0, 0, "h1_", 6 + sl)

    def stager(self, name, nbuf, cols):
        P = self.P
        bufs = [P.sb("%s%d" % (name, i), [128, cols], F32) for i in range(nbuf)]
        state = dict(n=0)

        def load(dst, srcs, dkeys, eng="pool", shape=None):
            sl = state["n"] % nbuf
            state["n"] += 1
            key = "%s%d" % (name, sl)
            tot = 0
            for off, ncols, ap, pat in srcs:
                d = bufs[sl][:, off:off + ncols]
                if pat is not None:
                    d = d.rearrange(pat[0], **pat[1])
                P.dma(d, ap, writes=[key], q="pool")
                tot = max(tot, off + ncols)
            src = bufs[sl][:, 0:tot]
            if shape is not None:
                src = src.rearrange(shape[0], **shape[1])
            P.add(eng, lambda e: e.tensor_copy(out=dst, in_=src), [key], dkeys)

        return load

    def ffn(self, i, last):
        P, I, PS = self.P, self.I, self.PS
        hT = self.hA
        P.sb_off = self.top
        tiles = list(range(NT)) if not last else list(range(2, NT))
        blks = TBLK if not last else TBLK[1:]
        W = 2307
        L = W - 2
        wdn = P.sb("wdn", [128, 22, D], BF16)
        bdn = P.sb("bdn", [128, D], F32)
        keep = P.sb_off
        bcol = P.sb("fbcol", [128, 5, 44], F32)
        wup = [P.sb("wup%d" % s, [128, 8, 2, 256], BF16) for s in range(2)]
        upad = [[P.sb("upad%d%d" % (s, g), [128, W], BF16) for g in range(2)] for s in range(2)]
        acc = [[P.sb("facc%d%d" % (s, g), [128, L], F32) for g in range(2)] for s in range(2)]
        actc = [P.sb("actc%d" % s, [128, L], BF16) for s in range(2)]
        self.load_cols(bcol[:, 0, :], I["ff_b_up"][i], "fbcol")
        for k in range(3):
            self.load_cols(bcol[:, 1 + k, :], I["ff_conv_w"][i, k], "fbcol")
        self.load_cols(bcol[:, 4, :], I["ff_conv_b"][i], "fbcol")
        for s in range(2):
            for g in range(2):
                P.pool(lambda e, s=s, g=g: e.memset(upad[s][g][:], 0.0), [], ["upad%d%d" % (s, g)])
        nb = 0
        utail = [None]
        for cg in range(11):
            ws = cg % 2
            for g in range(2):
                P.dma(wup[ws][:, :, g, :], I["ff_w_up"][i][:, g * FF + cg * 256:g * FF + (cg + 1) * 256].rearrange("(k p) c -> p k c", p=128),
                      writes=["wup%d" % ws], q="pool")
            if cg == 1:
                P.dma(bdn[:], I["ff_b_down"][i].partition_broadcast(128), writes=["bdn"])
            P.dma(wdn[:, 2 * cg:2 * cg + 2, :], I["ff_w_down"][i][2 * cg * 128:(2 * cg + 2) * 128, :].rearrange("(c p) o -> p c o", p=128), writes=["wdn"], q="pool")
            for cc in range(2):
                c = cg * 2 + cc
                s = c % 2
                for g in range(2):
                    col = c + 22 * g
                    ku, ka = "upad%d%d" % (s, g), "facc%d%d" % (s, g)
                    u, a = upad[s][g], acc[s][g]
                    for (t0, n) in blks:
                        bank = nb % 4
                        nb += 1
                        P.mm(PS[bank][:, 0:n], [(wup[ws][:, k, g, cc * 128:(cc + 1) * 128], hT[:, k, t0:t0 + n]) for k in range(8)],
                             ["wup%d" % ws] + tkeys("h2_", t0, n), ["ps%d" % bank])
                        P.act(lambda e, bank=bank, n=n, u=u, t0=t0, col=col: e.activation(
                            out=u[:, pcol(t0):pcol(t0) + n], in_=PS[bank][:, 0:n], func=AF.Identity,
                            bias=bcol[:, 0, col:col + 1], scale=1.0), ["ps%d" % bank, "fbcol"], [ku])
                    P.pool(lambda e, u=u, a=a, col=col: e.tensor_scalar(out=a[:], in0=u[:, 1:1 + L], scalar1=bcol[:, 2, col:col + 1],
                                                                        scalar2=bcol[:, 4, col:col + 1], op0=ALU.mult, op1=ALU.add),
                           [ku, "fbcol"], [ka])
                    P.dve(lambda e, u=u, a=a, col=col: e.scalar_tensor_tensor(out=a[:], in0=u[:, 0:L], scalar=bcol[:, 1, col:col + 1],
                                                                               in1=a[:], op0=ALU.mult, op1=ALU.add), [ku, ka, "fbcol"], [ka])
                    P.dve(lambda e, u=u, a=a, col=col: e.scalar_tensor_tensor(out=a[:], in0=u[:, 2:2 + L], scalar=bcol[:, 3, col:col + 1],
                                                                              in1=a[:], op0=ALU.mult, op1=ALU.add), [ku, ka, "fbcol"], [ka])
                if utail[0] is not None:
                    utail[0]()

                def _tail(s=s, c=c):
                    P.act(lambda e: e.activation(out=acc[s][1][:], in_=acc[s][1][:], func=AF.Silu), ["facc%d1" % s], ["facc%d1" % s])
                    P.dve(lambda e: e.tensor_tensor(out=actc[s][:], in0=acc[s][0][:], in1=acc[s][1][:], op=ALU.mult),
                          ["facc%d0" % s, "facc%d1" % s], ["actc%d" % s])
                    if not last:
                        P.dma(self.d_act[c * 128:(c + 1) * 128, 0:CTX], actc[s][:, 0:CTX], reads=["actc%d" % s], writes=["dact%d" % c])
                    P.dma(self.d_act[c * 128:(c + 1) * 128, CTX:TOK], actc[s][:, CTX + 1:TOK + 1], reads=["actc%d" % s], writes=["dact%d" % c])

                utail[0] = _tail
        utail[0]()
        self.refence()
        P.sb_off = keep
        at = [P.sb("at%d" % s, [128, 22, 128], BF16) for s in range(2)]
        xt = [P.sb("xt%d" % s, [128, D], F32) for s in range(2)]
        tmp = [P.sb("ftmp%d" % s, [128, 512], F32) for s in range(2)]
        fg = None
        if last:
            fg = P.sb("fg", [128, D], F32)
            P.dma(fg[:], I["final_g"].partition_broadcast(128), writes=["fg"])
        def dhead(tt, sl):
            P.dma(at[sl][:], self.d_act[:, tt * 128:(tt + 1) * 128].rearrange("(c p) t -> p c t", p=128), writes=["at%d" % sl])
            P.dma(xt[sl][:], self.Xs[tt * 128:(tt + 1) * 128, :], writes=["xt%d" % sl])
            for hf in range(2):
                bank = 2 * sl + hf
                P.mm(PS[bank][:, :], [(at[sl][:, c, :], wdn[:, c, hf * 512:(hf + 1) * 512]) for c in range(22)],
                     ["at%d" % sl], ["ps%d" % bank])

        def dtail(tt, sl):
            v = 1 if tt < 2 else 0
            for hf in range(2):
                bank = 2 * sl + hf
                tk = "ftmp%d" % hf
                P.dve(lambda e, bank=bank, hf=hf: e.tensor_tensor(out=tmp[hf][:], in0=PS[bank][:, :], in1=bdn[:, hf * 512:(hf + 1) * 512], op=ALU.add),
                      ["ps%d" % bank], [tk])
                P.pool(lambda e, hf=hf, v=v: e.tensor_tensor(out=tmp[hf][:], in0=tmp[hf][:], in1=self.grow[:, v, 1, hf * 512:(hf + 1) * 512], op=ALU.mult),
                       [tk, "grow"], [tk])
                P.pool(lambda e, hf=hf, sl=sl: e.tensor_tensor(out=xt[sl][:, hf * 512:(hf + 1) * 512], in0=xt[sl][:, hf * 512:(hf + 1) * 512], in1=tmp[hf][:], op=ALU.add),
                       [tk, "xt%d" % sl], ["xt%d" % sl])
            if last:
                st_, ks = self.rms_stats(xt[sl][:], "xt%d" % sl, sl)
                P.dve(lambda e, sl=sl, st_=st_: e.scalar_tensor_tensor(out=xt[sl][:], in0=xt[sl][:], scalar=st_[:, 2:3], in1=fg[:], op0=ALU.mult, op1=ALU.mult),
                      ["xt%d" % sl, ks, "fg"], ["xt%d" % sl])
                P.dma(self.out[(tt - 2) * 128:(tt - 1) * 128, :], xt[sl][:], reads=["xt%d" % sl], writes=["out%d" % tt])
            else:
                P.dma(self.Xs[tt * 128:(tt + 1) * 128, :], xt[sl][:], reads=["xt%d" % sl], writes=["X%d" % tt])
                self.norm_tile(xt[sl][:], "xt%d" % sl, tt, sl, i + 1, 0, "h1_", 6 + sl)

        prev = None
        for n_, tt in enumerate(tiles):
            dhead(tt, n_ % 2)
            if prev is not None:
                dtail(*prev)
            prev = (tt, n_ % 2)
        dtail(*prev)
        self.refence()

    def outproj(self, i, nk, w_src, b_src, tiles):
        P, I, PS = self.P, self.I, self.PS
        P.sb_off = self.top
        self.load_grow(i)
        wo = P.sb("wo", [128, nk, D], BF16)
        at = [P.sb("oat%d" % s, [128, nk, 128], BF16) for s in range(2)]
        xt = [P.sb("oxt%d" % s, [128, D], F32) for s in range(2)]
        tmp = [P.sb("otmp%d" % s, [128, 512], F32) for s in range(2)]
        bo = None
        if b_src is not None:
            bo = P.sb("bo", [128, D], F32)
            P.dma(bo[:], b_src.partition_broadcast(128), writes=["bo"])
        for c0 in range(0, nk, 4):
            P.dma(wo[:, c0:c0 + 4, :], w_src[c0 * 128:(c0 + 4) * 128, :].rearrange("(c p) o -> p c o", p=128), writes=["wo"], q="pool")
        def ohead(tt, sl):
            P.dma(at[sl][:], self.d_act[0:nk * 128, tt * 128:(tt + 1) * 128].rearrange("(c p) t -> p c t", p=128), writes=["oat%d" % sl])
            P.dma(xt[sl][:], self.Xs[tt * 128:(tt + 1) * 128, :], writes=["oxt%d" % sl])
            for hf in range(2):
                bank = 2 * sl + hf
                P.mm(PS[bank][:, :], [(at[sl][:, k, :], wo[:, k, hf * 512:(hf + 1) * 512]) for k in range(nk)], ["oat%d" % sl, "wo"], ["ps%d" % bank])

        def otail(tt, sl):
            v = 1 if tt < 2 else 0
            for hf in range(2):
                bank = 2 * sl + hf
                tk = "otmp%d" % hf
                if bo is not None:
                    P.dve(lambda e, bank=bank, hf=hf: e.tensor_tensor(out=tmp[hf][:], in0=PS[bank][:, :], in1=bo[:, hf * 512:(hf + 1) * 512], op=ALU.add),
                          ["ps%d" % bank, "bo"], [tk])
                    P.pool(lambda e, hf=hf, v=v: e.tensor_tensor(out=tmp[hf][:], in0=tmp[hf][:], in1=self.grow[:, v, 0, hf * 512:(hf + 1) * 512], op=ALU.mult),
                           [tk, "grow"], [tk])
                else:
                    P.dve(lambda e, bank=bank, hf=hf, v=v: e.tensor_tensor(out=tmp[hf][:], in0=PS[bank][:, :], in1=self.grow[:, v, 0, hf * 512:(hf + 1) * 512], op=ALU.mult),
                          ["ps%d" % bank, "grow"], [tk])
                P.pool(lambda e, hf=hf, sl=sl: e.tensor_tensor(out=xt[sl][:, hf * 512:(hf + 1) * 512], in0=xt[sl][:, hf * 512:(hf + 1) * 512], in1=tmp[hf][:], op=ALU.add),
                       [tk, "oxt%d" % sl], ["oxt%d" % sl])
            P.dma(self.Xs[tt * 128:(tt + 1) * 128, :], xt[sl][:], reads=["oxt%d" % sl], writes=["X%d" % tt])
            self.norm_tile(xt[sl][:], "oxt%d" % sl, tt, sl, i, 1, "h2_", 6 + sl)

        prev = None
        for n_, tt in enumerate(tiles):
            ohead(tt, n_ % 2)
            if prev is not None:
                otail(*prev)
            prev = (tt, n_ % 2)
        otail(*prev)
        self.refence()

    def mlstm(self, i, j):
        P, I, PS = self.P, self.I, self.PS
        hT = self.hA
        idf = self.identf
        P.sb_off = self.top
        gcol = P.sb("gcol", [128, NT, 16], F32)
        emc = P.sb("emc", [128, 8], F32)
        mcol = P.sb("mcol", [128, 2, 16], F32)
        bdb = P.sb("bdb", [128, 3, 16, 128], BF16)
        keepA = P.sb_off
        win = [P.sb("win%d" % s, [128, 8, 512], BF16) for s in range(2)]
        bdT_off = P.sb_off
        bdT = P.sb("bdT", [128, 3, 16, 128], F32)
        P.sb("bdTpad", [128, 1024], F32)
        gwt = P.sb("gwt", [128, 3, 16, 16], F32)
        wgf = P.sb("wgf", [128, 2, 16, 16], BF16)
        ccol = P.sb("ccol", [128, 6, 16], F32)
        gbc = P.sb("gbc", [40, 1], F32)
        W5 = 2310
        L5 = W5 - 4
        upad = [P.sb("mupad%d" % s, [128, W5], BF16) for s in range(2)]
        ucp = [P.sb("mucp%d" % s, [128, W5], BF16) for s in range(2)]
        macc = [P.sb("macc%d" % s, [128, L5], F32) for s in range(2)]
        szb = [P.sb("szb%d" % s, [128, 512], BF16) for s in range(3)]
        mx = P.sb("mx", [8, 12], F32)
        _save = P.sb_off
        P.sb_off = bdT_off
        gsb = P.sb("gsb", [40, TOK], F32)
        gt1 = P.sb("gt1", [40, TOK], F32)
        gt2 = P.sb("gt2", [40, TOK], F32)
        P.sb_off = max(_save, P.sb_off)

        self.load_cols(mcol[:, 0, :], I["ml_hnorm_g"][j], "mcol")
        self.load_cols(mcol[:, 1, :], I["ml_skip"][j], "mcol")
        for k in range(5):
            self.load_cols(ccol[:, k, :], I["ml_conv_w"][j, k], "ccol")
        self.load_cols(ccol[:, 5, :], I["ml_conv_b"][j], "ccol")
        P.dma(gbc[0:8, :], I["gb"][j, 0:8].rearrange("(p o) -> p o", o=1), writes=["gbc"])
        P.dma(gbc[32:40, :], I["gb"][j, 8:16].rearrange("(p o) -> p o", o=1), writes=["gbc"])
        for m, nm in enumerate(("bdq", "bdk", "bdv")):
            P.dma(bdb[:, m, :, :], I[nm][j].rearrange("c p o -> p c o"), writes=["bdb"], q="pool")
            P.dma(bdT[:, m, :, :], I[nm + "T"][j].rearrange("c p o -> p c o"), writes=["bdT"])
            P.dma(gwt[:, m, :, :], I["gw"][j, m].rearrange("c p o -> p c o"), writes=["gwt"])
        P.dve(lambda e: e.tensor_scalar(out=gwt[:, 1, :, :], in0=gwt[:, 1, :, :], scalar1=float(512 ** -0.5), scalar2=None, op0=ALU.mult), ["gwt"], ["gwt"])
        for c in range(16):
            P.mm(PS[5][:, c * 16:(c + 1) * 16], [(bdT[:, 0, c, :], gwt[:, 0, c, :]), (bdT[:, 1, c, :], gwt[:, 1, c, :])], ["bdT", "gwt"], ["ps5"])
            P.mm(PS[6][:, c * 16:(c + 1) * 16], [(bdT[:, 2, c, :], gwt[:, 2, c, :])], ["bdT", "gwt"], ["ps6"])
        P.dve(lambda e: e.tensor_copy(out=wgf[:, 0, :, :], in_=PS[5][:, 0:256].rearrange("p (c o) -> p c o", c=16)), ["ps5"], ["wgf"])
        P.dve(lambda e: e.tensor_copy(out=wgf[:, 1, :, :], in_=PS[6][:, 0:256].rearrange("p (c o) -> p c o", c=16)), ["ps6"], ["wgf"])
        for s in range(2):
            P.pool(lambda e, s=s: e.memset(upad[s][:], 0.0), [], ["mupad%d" % s])

        def p5(t):
            return 2 + t if t < CTX else 4 + t

        nb = 0
        nz = 0
        mtail = [None]
        for og in range(8):
            ws = og % 2
            P.dma(win[ws][:], I["ml_w_in"][j][:, og * 512:(og + 1) * 512].rearrange("(k p) c -> p k c", p=128), writes=["win%d" % ws], q="pool")
            for oo in range(4):
                oc = og * 4 + oo
                s = oc % 2
                for (t0, n) in TBLK:
                    bank = 5 + nb % 3
                    nb += 1
                    P.mm(PS[bank][:, 0:n], [(win[ws][:, k, oo * 128:(oo + 1) * 128], hT[:, k, t0:t0 + n]) for k in range(8)],
                         ["win%d" % ws] + tkeys("h1_", t0, n), ["ps%d" % bank])
                    if oc < 16:
                        P.act(lambda e, bank=bank, n=n, s=s, t0=t0: e.copy(out=upad[s][:, p5(t0):p5(t0) + n], in_=PS[bank][:, 0:n]),
                              ["ps%d" % bank], ["mupad%d" % s])
                    else:
                        zs = nz % 3
                        nz += 1
                        P.act(lambda e, bank=bank, n=n, zs=zs: e.activation(out=szb[zs][:, 0:n], in_=PS[bank][:, 0:n], func=AF.Silu),
                              ["ps%d" % bank], ["szb%d" % zs])
                        P.dma(self.d_sz[(oc - 16) * 128:(oc - 15) * 128, t0:t0 + n], szb[zs][:, 0:n], reads=["szb%d" % zs], writes=["dsz%d" % (oc - 16)])
                if mtail[0] is not None:
                    mtail[0]()
                    mtail[0] = None
                if oc < 16:
                    u, a, uc = upad[s], macc[s], ucp[s]
                    ku, ka, kc = "mupad%d" % s, "macc%d" % s, "mucp%d" % s
                    P.pool(lambda e, u=u, a=a, oc=oc: e.tensor_scalar(out=a[:], in0=u[:, 0:L5], scalar1=ccol[:, 0, oc:oc + 1], scalar2=ccol[:, 5, oc:oc + 1],
                                                                      op0=ALU.mult, op1=ALU.add), [ku, "ccol"], [ka])
                    for k in range(1, 5):
                        eng = "dve"
                        P.add(eng, lambda e, u=u, a=a, oc=oc, k=k: e.scalar_tensor_tensor(out=a[:], in0=u[:, k:k + L5], scalar=ccol[:, k, oc:oc + 1], in1=a[:],
                                                                                        op0=ALU.mult, op1=ALU.add), [ku, ka, "ccol"], [ka])
                    def _mtail(oc=oc, u=u, a=a, uc=uc, ku=ku, ka=ka, kc=kc):
                        P.act(lambda e, a=a, uc=uc: e.activation(out=uc[:, 2:2 + L5], in_=a[:], func=AF.Silu), [ka], [kc])
                        for b, (t0, n) in enumerate(TBLK):
                            def gm(e, b=b, t0=t0, n=n, oc=oc, u=u, uc=uc):
                                c0 = p5(t0)
                                e.matmul(PS[b][0:8, 0:n], lhsT=wgf[:, 0, oc, 0:8], rhs=uc[:, c0:c0 + n], start=(oc == 0), stop=False)
                                e.matmul(PS[b][0:8, 0:n], lhsT=wgf[:, 1, oc, 0:8], rhs=u[:, c0:c0 + n], start=False, stop=(oc == 15))
                                e.matmul(PS[b][32:40, 0:n], lhsT=wgf[:, 0, oc, 8:16], rhs=uc[:, c0:c0 + n], start=(oc == 0), stop=False)
                                return e.matmul(PS[b][32:40, 0:n], lhsT=wgf[:, 1, oc, 8:16], rhs=u[:, c0:c0 + n], start=False, stop=(oc == 15))
                            P.pe(gm, [ku, kc, "wgf"], ["ps%d" % b])
                        for (src, dst, kk) in ((u, self.d_u, "du"), (uc, self.d_uc, "duc")):
                            P.dma(dst[oc * 128:(oc + 1) * 128, 0:CTX], src[:, 2:2 + CTX], reads=[ku if src is u else kc], writes=["%s%d" % (kk, oc)])
                            P.dma(dst[oc * 128:(oc + 1) * 128, CTX:TOK], src[:, 4 + CTX:4 + TOK], reads=[ku if src is u else kc], writes=["%s%d" % (kk, oc)])

                    mtail[0] = _mtail
        if mtail[0] is not None:
            mtail[0]()
        for b, (t0, n) in enumerate(TBLK):
            P.act(lambda e, b=b, t0=t0, n=n: e.activation(out=gsb[0:40, t0:t0 + n], in_=PS[b][0:40, 0:n], func=AF.Identity, bias=gbc[0:40, 0:1], scale=1.0),
                  ["ps%d" % b, "gbc"], ["gsb"])
        R = slice(32, 40)
        P.dve(lambda e: e.tensor_scalar(out=gt2[R, :], in0=gsb[R, :], scalar1=-1.0, scalar2=None, op0=ALU.mult), ["gsb"], ["gt2"])
        P.dve(lambda e: e.tensor_tensor(out=gt1[R, :], in0=gsb[R, :], in1=gt2[R, :], op=ALU.max), ["gsb", "gt2"], ["gt1"])
        P.act(lambda e: e.activation(out=gt1[R, :], in_=gt1[R, :], func=AF.Exp, scale=-1.0), ["gt1"], ["gt1"])
        P.act(lambda e: e.activation(out=gt1[R, :], in_=gt1[R, :], func=AF.Ln, bias=self.onesf[R, 0:1], scale=1.0), ["gt1"], ["gt1"])
        P.dve(lambda e: e.tensor_scalar(out=gt2[R, :], in0=gt2[R, :], scalar1=0.0, scalar2=None, op0=ALU.max), ["gt2"], ["gt2"])
        P.dve(lambda e: e.tensor_tensor(out=gsb[R, :], in0=gt1[R, :], in1=gt2[R, :], op=ALU.add), ["gt1", "gt2", "gsb"], ["gsb"])
        P.dve(lambda e: e.tensor_reduce(out=mx[0:8, 0:1], in_=gsb[0:8, :], axis=AX.X, op=ALU.max), ["gsb"], ["mx"])
        P.dve(lambda e: e.tensor_scalar(out=gsb[0:8, :], in0=gsb[0:8, :], scalar1=mx[0:8, 0:1], scalar2=None, op0=ALU.subtract), ["gsb", "mx"], ["gsb"])
        P.dve(lambda e: e.tensor_scalar(out=mx[0:8, 4:12], in0=idf[0:8, 0:8], scalar1=mx[0:8, 0:1], scalar2=None, op0=ALU.mult), ["mx"], ["mx"])
        P.mm(PS[5][:, 0:8], [(self.onesf[0:8, :], mx[0:8, 4:12])], ["mx"], ["ps5"])
        P.act(lambda e: e.activation(out=emc[:], in_=PS[5][:, 0:8], func=AF.Exp, scale=-1.0), ["ps5"], ["emc"])

        def gtr(e):
            ins = None
            for tt in range(NT):
                e.transpose(PS[6][:, tt * 16:tt * 16 + 8], gsb[0:8, tt * 128:(tt + 1) * 128], idf[0:8, 0:8])
                ins = e.transpose(PS[6][:, tt * 16 + 8:tt * 16 + 16], gsb[32:40, tt * 128:(tt + 1) * 128], idf[32:40, 32:40])
            return ins

        P.pe(gtr, ["gsb"], ["ps6"])
        P.dve(lambda e: e.tensor_copy(out=gcol[:].rearrange("p t g -> p (t g)"), in_=PS[6][:, 0:NT * 16]), ["ps6"], ["gcol"])
        self.refence()

        cm = self.cm
        order = {0: [0, 1] + list(range(2, NT)), 1: [1, 0] + list(range(NT - 1, 1, -1))}
        for hd in range(4):
            P.sb_off = self.hA_off
            H = P.sb("H", [128, NT, 512], BF16)
            C32 = [P.sb("C32_%d" % z, [128, 4, 512], F32) for z in range(2)]
            assert P.sb_off <= self.top
            P.sb_off = keepA
            qT = P.sb("qT", [128, 4, TOK], BF16)
            kT = P.sb("kT", [128, 4, TOK], BF16)
            ktm = P.sb("ktm", [128, NT, 512], BF16)
            vtm = P.sb("vtm", [128, NT, 512], BF16)
            Cbf = [P.sb("Cbf%d" % z, [128, 4, 512], BF16) for z in range(2)]
            nst = P.sb("nstate", [128, 2, 4], F32)
            nbf = P.sb("nbf", [128, 2, 4], BF16)
            keepB = P.sb_off
            ucT = P.sb("ucT", [128, 4, TOK], BF16)
            uT = P.sb("uT", [128, 4, TOK], BF16)
            P.dma(ucT[:], self.d_uc[hd * 512:(hd + 1) * 512, :].rearrange("(c p) t -> p c t", p=128), writes=["ucT"])
            P.dma(uT[:], self.d_u[hd * 512:(hd + 1) * 512, :].rearrange("(c p) t -> p c t", p=128), writes=["uT"])
            nb = 0
            sk = float(512 ** -0.5)
            for c in range(4):
                cc = hd * 4 + c
                for (t0, n) in TBLK:
                    for m, dst in ((0, qT), (1, kT)):
                        bank = nb % 4
                        nb += 1
                        P.mm(PS[bank][:, 0:n], [(bdb[:, m, cc, :], ucT[:, c, t0:t0 + n])], ["ucT"], ["ps%d" % bank])
                        if m == 0:
                            P.act(lambda e, bank=bank, n=n, dst=dst, c=c, t0=t0: e.copy(out=dst[:, c, t0:t0 + n], in_=PS[bank][:, 0:n]), ["ps%d" % bank], ["qT"])
                        else:
                            P.dve(lambda e, bank=bank, n=n, dst=dst, c=c, t0=t0: e.tensor_scalar(out=dst[:, c, t0:t0 + n], in0=PS[bank][:, 0:n], scalar1=sk, scalar2=None, op0=ALU.mult),
                                  ["ps%d" % bank], ["kT"])
            for tt in range(NT):
                tk = slice(tt * 128, (tt + 1) * 128)
                for m, src, dst in ((1, ucT, ktm), (2, uT, vtm)):
                    bank = 4 + nb % 4
                    nb += 1

                    def tm(e, bank=bank, m=m, src=src, tk=tk, hd=hd):
                        ins = None
                        for c in range(4):
                            ins = e.matmul(PS[bank][:, c * 128:(c + 1) * 128], lhsT=src[:, c, tk], rhs=bdb[:, m, hd * 4 + c, :], start=True, stop=True)
                        return ins

                    P.pe(tm, ["ucT", "uT"], ["ps%d" % bank])
                    if m == 1:
                        P.act(lambda e, bank=bank, tt=tt: e.activation(out=ktm[:, tt, :], in_=PS[bank][:, :], func=AF.Copy, scale=sk), ["ps%d" % bank], ["ktm%d" % tt])
                    else:
                        P.dve(lambda e, bank=bank, tt=tt: e.tensor_copy(out=vtm[:, tt, :], in_=PS[bank][:, :]), ["ps%d" % bank], ["vtm%d" % tt])
            for z in range(2):
                P.pool(lambda e, z=z: e.memset(C32[z][:], 0.0), [], ["C32_%d" % z])
                P.pool(lambda e, z=z: e.memset(Cbf[z][:], 0.0), [], ["Cbf%d" % z])
            P.pool(lambda e: e.memset(nst[:], 0.0), [], ["nstate0", "nstate1"])
            P.pool(lambda e: e.memset(nbf[:], 0.0), [], ["nbf0", "nbf1"])
            self.refence()
            P.sb_off = keepB
            DT = P.sb("DT", [128, 2, NT, 128], F32)
            EBb = P.sb("EBb", [128, 2, NT, 128], BF16)
            wkh = P.sb("wkh", [128, 2, 2, NT], F32)
            wkh16 = P.sb("wkh16", [128, 2, 2, NT], BF16)
            dec = P.sb("dec", [128, 2, NT, 2], F32)
            r2 = P.sb("r2", [128, NT, 2], F32)
            tc_ = P.sb("tcol", [128, NT], F32)
            wkc = P.sb("wkc", [128, NT], F32)
            dn = [P.sb("dn%d" % s, [128, 8], F32) for s in range(2)]
            ost = [P.sb("ost%d" % s, [128, 8], F32) for s in range(2)]
            lfm_off = P.sb_off
            LFm = P.sb("LFm", [128, NT, 128], F32)
            T2 = P.sb("T2", [128, NT, 128], F32)
            P.sb_off = lfm_off
            PTt = [P.sb("PT%d" % s, [128, 128], BF16) for s in range(2)]
            qs = [[P.sb("qs%d%d" % (z, h), [128, 4, 128], BF16) for h in range(2)] for z in range(2)]
            vw = [[P.sb("vw%d%d" % (z, h), [128, 512], BF16) for h in range(2)] for z in range(2)]
            _sv = P.sb_off
            P.sb_off = self.grow_off
            hs = [P.sb("hs%d" % s, [128, 512], F32) for s in range(2)]
            hn = [P.sb("hn%d" % s, [128, 512], BF16) for s in range(2)]
            oa = [P.sb("oa%d" % s, [128, 4, 128], F32) for s in range(2)]
            uct = [P.sb("uct%d" % s, [128, 4, 128], BF16) for s in range(2)]
            szt = [P.sb("szt%d" % s, [128, 4, 128], BF16) for s in range(2)]
            actt = [P.sb("actt%d" % s, [128, 4, 128], BF16) for s in range(2)]
            assert P.sb_off <= self.grow_off + 16384
            P.sb_off = _sv
            for z in range(2):
                lic = gcol[:, :, z * 4 + hd]
                nlfc = gcol[:, :, 8 + z * 4 + hd]
                P.dve(lambda e, z=z, nlfc=nlfc: e.tensor_tensor(out=LFm[:], in0=cm[:, z, 1, :].unsqueeze(1).to_broadcast([128, NT, 128]),
                                                                in1=nlfc.unsqueeze(2).to_broadcast([128, NT, 128]), op=ALU.mult), ["gcol"], ["LFm"])
                P.pool(lambda e, lic=lic: e.tensor_tensor(out=T2[:], in0=idf[:].unsqueeze(1).to_broadcast([128, NT, 128]),
                                                          in1=lic.unsqueeze(2).to_broadcast([128, NT, 128]), op=ALU.mult), ["gcol"], ["T2"])
                P.dve(lambda e: e.tensor_tensor(out=LFm[:], in0=LFm[:], in1=T2[:], op=ALU.add), ["LFm", "T2"], ["LFm"])
                P.dve(lambda e, nlfc=nlfc: e.tensor_scalar(out=T2[:], in0=nlfc.unsqueeze(2).to_broadcast([128, NT, 128]), scalar1=-1.0, scalar2=None, op0=ALU.mult),
                       ["gcol", "T2"], ["T2"])
                for g0 in range(0, NT, 4):
                    bank = (g0 // 4) % 2
                    ng = min(4, NT - g0)

                    def dm(e, g0=g0, ng=ng, bank=bank, z=z):
                        ins = None
                        for q in range(ng):
                            e.matmul(PS[bank][:, q * 128:(q + 1) * 128], lhsT=LFm[:, g0 + q, :], rhs=cm[:, z, 0, :], start=True, stop=False)
                            ins = e.matmul(PS[bank][:, q * 128:(q + 1) * 128], lhsT=idf[:], rhs=cm[:, z, 2, :], start=False, stop=True)
                        return ins

                    P.pe(dm, ["LFm"], ["ps%d" % bank])
                    P.act(lambda e, g0=g0, ng=ng, bank=bank, z=z: e.activation(out=DT[:, z, g0:g0 + ng, :].rearrange("p t s -> p (t s)"), in_=PS[bank][:, 0:ng * 128], func=AF.Exp),
                          ["ps%d" % bank], ["DT%d" % z])
                    bank2 = 4 + (g0 // 4) % 2

                    def em(e, g0=g0, ng=ng, bank2=bank2, z=z):
                        ins = None
                        for q in range(ng):
                            ins = e.matmul(PS[bank2][:, q * 128:(q + 1) * 128], lhsT=T2[:, g0 + q, :], rhs=cm[:, z, 0, :], start=True, stop=True)
                        return ins

                    P.pe(em, ["T2"], ["ps%d" % bank2])
                    P.act(lambda e, g0=g0, ng=ng, bank2=bank2, z=z: e.activation(out=EBb[:, z, g0:g0 + ng, :].rearrange("p t s -> p (t s)"), in_=PS[bank2][:, 0:ng * 128], func=AF.Exp),
                          ["ps%d" % bank2], ["EBb%d" % z])
                P.mm(PS[3][:, 0:NT], [(cm[:, z, 1, :], nlfc)], ["gcol"], ["ps3"])
                P.dve(lambda e, lic=lic: e.tensor_tensor(out=tc_[:], in0=PS[3][:, 0:NT], in1=lic, op=ALU.add), ["ps3", "gcol"], ["tcol"])
                P.act(lambda e, z=z: e.activation(out=wkh[:, z, 0, :], in_=tc_[:], func=AF.Exp), ["tcol"], ["wkh%d" % z])
                P.dve(lambda e, z=z: e.tensor_copy(out=wkh16[:, z, 0, :], in_=wkh[:, z, 0, :]), ["wkh%d" % z], ["wkh16_%d" % z])
                P.mm(PS[2][:, 64:64 + NT], [(self.onesf[:], nlfc)], ["gcol"], ["ps2"])
                P.act(lambda e, z=z: e.activation(out=dec[:, z, :, 0], in_=PS[2][:, 64:64 + NT], func=AF.Exp, scale=-1.0),
                      ["ps2"], ["dec%d" % z])
            self.refence()
            if self.dbg == "B2":
                raise StopIteration
            visited = set()
            pending = []

            def rec(z, tt, hd=hd):
                tk = slice(tt * 128, (tt + 1) * 128)
                kz = str(z)
                X, A, C0 = 4 * z, 4 * z + 1, 4 * z + 2
                q_ = qs[z][0]
                vw_ = vw[z][0]
                P.mm(PS[X][:, 0:128], [(kT[:, c, tk], qT[:, c, tk]) for c in range(4)], [], ["psXs" + kz])
                yield
                P.dve(lambda e: e.tensor_tensor(out=PTt[z][:], in0=PS[X][:, 0:128], in1=DT[:, z, tt, :], op=ALU.mult), ["psXs" + kz], ["PT" + kz])
                yield
                P.pool(lambda e: e.tensor_tensor(out=q_[:], in0=qT[:, :, tk], in1=EBb[:, z, tt, :].unsqueeze(1).to_broadcast([128, 4, 128]), op=ALU.mult),
                       [], ["qs" + kz])
                yield
                P.pool(lambda e: e.tensor_tensor(out=vw_[:], in0=vtm[:, tt, :], in1=wkh[:, z, 0, tt:tt + 1].to_broadcast([128, 512]), op=ALU.mult),
                       [], ["vw" + kz])
                yield

                def mA(e):
                    e.matmul(PS[A][:, :], lhsT=PTt[z][:], rhs=vtm[:, tt, :], start=True, stop=False)
                    return e.matmul(PS[X][:, 128:129], lhsT=PTt[z][:], rhs=self.onesb[:, 0:1], start=True, stop=True)

                P.pe(mA, ["PT" + kz], ["psA" + kz, "psXd" + kz])
                yield
                def mB(e):
                    ins = None
                    for c in range(4):
                        e.matmul(PS[A][:, :], lhsT=q_[:, c, :], rhs=Cbf[z][:, c, :], start=False, stop=(c == 3))
                    for c in range(4):
                        ins = e.matmul(PS[X][:, 129:130], lhsT=q_[:, c, :], rhs=nbf[:, z, c:c + 1], start=(c == 0), stop=(c == 3))
                    return ins

                P.pe(mB, ["qs" + kz, "Cbf" + kz, "nbf" + kz], ["psA" + kz, "psXd" + kz])
                yield
                dcol = dec[:, z, tt, 0:1]
                for pr in range(2):
                    def mC(e, pr=pr):
                        ins = None
                        for q in range(2):
                            c = 2 * pr + q
                            ins = e.matmul(PS[C0 + q][:, :], lhsT=ktm[:, tt, c * 128:(c + 1) * 128], rhs=vw_[:], start=True, stop=True)
                        return ins

                    P.pe(mC, ["vw" + kz], ["psC" + kz])
                    yield
                    for q in range(2):
                        P.dve(lambda e, q=q, pr=pr: e.scalar_tensor_tensor(out=C32[z][:, 2 * pr + q, :], in0=C32[z][:, 2 * pr + q, :],
                                                                       scalar=dcol, in1=PS[C0 + q][:, :], op0=ALU.mult, op1=ALU.add),
                              ["psC" + kz, "C32_%s_%d" % (kz, pr)], ["C32_%s_%d" % (kz, pr)])
                        yield

                def nm(e):
                    ins = None
                    for c in range(4):
                        ins = e.matmul(PS[X][:, 136 + c:137 + c], lhsT=ktm[:, tt, c * 128:(c + 1) * 128], rhs=wkh16[:, z, 0, tt:tt + 1], start=True, stop=True)
                    return ins

                P.pe(nm, [], ["psXn" + kz])
                yield

                for pr in range(2):
                    cc = slice(2 * pr, 2 * pr + 2)
                    P.act(lambda e, cc=cc: e.copy(out=Cbf[z][:, cc, :], in_=C32[z][:, cc, :]), ["C32_%s_%d" % (kz, pr)], ["Cbf" + kz])
                    yield
                P.dve(lambda e: e.scalar_tensor_tensor(out=nst[:, z, :], in0=nst[:, z, :], scalar=dcol, in1=PS[X][:, 136:140],
                                                       op0=ALU.mult, op1=ALU.add), ["psXn" + kz, "nstate" + kz], ["nstate" + kz])
                yield
                P.act(lambda e: e.copy(out=nbf[:, z, :], in_=nst[:, z, :]), ["nstate" + kz], ["nbf" + kz])
                yield
                d = dn[z]
                kd = "dn" + kz
                P.dve(lambda e: e.tensor_reduce(out=d[:, 4:5], in_=PS[X][:, 128:130], axis=AX.X, op=ALU.add), ["psXd" + kz], [kd])
                yield
                P.dve(lambda e: e.tensor_scalar(out=d[:, 0:1], in0=d[:, 4:5], scalar1=-1.0, scalar2=None, op0=ALU.mult), [kd], [kd])
                yield
                P.dve(lambda e: e.tensor_tensor(out=d[:, 1:2], in0=d[:, 4:5], in1=d[:, 0:1], op=ALU.max), [kd], [kd])
                yield
                P.dve(lambda e: e.tensor_tensor(out=d[:, 2:3], in0=d[:, 1:2], in1=emc[:, z * 4 + hd:z * 4 + hd + 1], op=ALU.max), [kd], [kd])
                yield
                P.dve(lambda e: e.reciprocal(out=d[:, 3:4], in_=d[:, 2:3]), [kd], [kd])
                yield
                if tt not in visited:
                    visited.add(tt)
                    P.act(lambda e: e.activation(out=H[:, tt, :], in_=PS[A][:, :], func=AF.Copy, scale=d[:, 3:4]), ["psA" + kz, kd], ["H%d" % tt])
                    yield
                    return
                P.dve(lambda e: e.scalar_tensor_tensor(out=hs[z][:], in0=PS[A][:, :], scalar=d[:, 3:4], in1=H[:, tt, :], op0=ALU.mult, op1=ALU.add),
                      ["psA" + kz, kd, "H%d" % tt], ["hs" + kz])
                yield
                pending.append(outg(z, tt))

            def outg(z, tt, hd=hd):
                tk = slice(tt * 128, (tt + 1) * 128)
                kz = str(z)
                X = 4 * z
                o = ost[z]
                ko = "ost" + kz
                P.dve(lambda e: e.tensor_reduce(out=o[:, 0:1], in_=hs[z][:], axis=AX.X, op=ALU.add), ["hs" + kz], [ko])
                yield
                P.act(lambda e: e.activation(out=self.junk[:, z * 512:(z + 1) * 512], in_=hs[z][:], func=AF.Square, accum_out=o[:, 1:2]), ["hs" + kz], ["junk" + kz, ko])
                yield
                P.dve(lambda e: e.tensor_scalar(out=o[:, 2:3], in0=o[:, 0:1], scalar1=1.0 / 512, scalar2=None, op0=ALU.mult), [ko], [ko])
                yield
                P.dve(lambda e: e.tensor_tensor(out=o[:, 3:4], in0=o[:, 2:3], in1=o[:, 2:3], op=ALU.mult), [ko], [ko])
                yield
                P.dve(lambda e: e.scalar_tensor_tensor(out=o[:, 4:5], in0=o[:, 1:2], scalar=1.0 / 512, in1=o[:, 3:4], op0=ALU.mult, op1=ALU.subtract), [ko], [ko])
                yield
                P.act(lambda e: e.activation(out=o[:, 5:6], in_=o[:, 4:5], func=AF.Sqrt, bias=self.epsc[:, 0:1], scale=1.0), [ko], [ko])
                yield
                P.dve(lambda e: e.reciprocal(out=o[:, 6:7], in_=o[:, 5:6]), [ko], [ko])
                yield
                P.dve(lambda e: e.scalar_tensor_tensor(out=o[:, 7:8], in0=o[:, 2:3], scalar=-1.0, in1=o[:, 6:7], op0=ALU.mult, op1=ALU.mult), [ko], [ko])
                yield
                P.act(lambda e: e.activation(out=hn[z][:], in_=hs[z][:], func=AF.Identity, scale=o[:, 6:7], bias=o[:, 7:8]), ["hs" + kz, ko], ["hn" + kz])
                yield
                gb_ = mcol[:, 0, hd * 4:(hd + 1) * 4].unsqueeze(2).to_broadcast([128, 4, 128])
                for pp in range(2):
                    def otr(e, pp=pp):
                        ins = None
                        for q in range(2):
                            c = 2 * pp + q
                            ins = e.matmul(PS[X][:, 256 + q * 128:256 + (q + 1) * 128], lhsT=hn[z][:, c * 128:(c + 1) * 128], rhs=self.identb[:], start=True, stop=True)
                        return ins

                    P.pe(otr, ["hn" + kz], ["psXt" + kz])
                    yield
                    P.dve(lambda e, pp=pp: e.tensor_tensor(out=actt[z][:, 2 * pp:2 * pp + 2, :], in0=PS[X][:, 256:512].rearrange("p (c t) -> p c t", c=2),
                                                         in1=gb_[:, 2 * pp:2 * pp + 2, :], op=ALU.mult), ["psXt" + kz], ["actt" + kz])
                    yield
                P.dma(uct[z][:], self.d_uc[hd * 512:(hd + 1) * 512, tk].rearrange("(c p) t -> p c t", p=128), writes=["uct" + kz])
                P.dma(szt[z][:], self.d_sz[hd * 512:(hd + 1) * 512, tk].rearrange("(c p) t -> p c t", p=128), writes=["szt" + kz])
                sb_ = mcol[:, 1, hd * 4:(hd + 1) * 4].unsqueeze(2).to_broadcast([128, 4, 128])
                P.pool(lambda e: e.tensor_tensor(out=oa[z][:], in0=uct[z][:], in1=sb_, op=ALU.mult), ["uct" + kz], ["oa" + kz])
                yield
                P.pool(lambda e: e.tensor_tensor(out=oa[z][:], in0=oa[z][:], in1=actt[z][:], op=ALU.add), ["oa" + kz, "actt" + kz], ["oa" + kz])
                yield
                P.pool(lambda e: e.tensor_tensor(out=actt[z][:], in0=oa[z][:], in1=szt[z][:], op=ALU.mult), ["oa" + kz, "szt" + kz], ["actt" + kz])
                yield
                P.dma(self.d_act[hd * 512:(hd + 1) * 512, tk].rearrange("(c p) t -> p c t", p=128), actt[z][:], reads=["actt" + kz], writes=["dact_h%d_%d" % (hd, tt)])
                yield

            def drive(gens):
                gens = list(gens)
                while gens:
                    for g in list(gens):
                        try:
                            next(g)
                        except StopIteration:
                            gens.remove(g)

            for step in range(NT):
                cur = pending
                pending = []
                import os as _os
                if _os.environ.get("KSKIPOUT"):
                    cur = []
                if _os.environ.get("KSEQ"):
                    drive([rec(0, order[0][step])]); drive([rec(1, order[1][step])])
                    for g_ in cur:
                        drive([g_])
                else:
                    drive([rec(0, order[0][step]), rec(1, order[1][step])] + cur)
                if self.dbg == "S%d" % step:
                    raise StopIteration
            drive(pending)
            pending = []
            self.refence()
        self.outproj(i, 16, I["ml_w_out"][j], None, list(range(NT)))

    def natten(self, i, j, last):
        P, I, PS = self.P, self.I, self.PS
        hT = self.hA
        P.sb_off = self.top
        wq = [P.sb("nwq%d" % s, [128, 8, 3, 128], BF16) for s in range(2)]
        bcol = P.sb("nbcol", [128, 2, 8], F32)
        bvr = P.sb("nbvr", [128, D], F32)
        qc = [P.sb("nqc%d" % s, [128, TOK], BF16) for s in range(2)]
        kc = [P.sb("nkc%d" % s, [128, TOK], BF16) for s in range(2)]
        vc = [P.sb("nvc%d" % s, [128, NT, 128], BF16) for s in range(2)]
        oc_ = [P.sb("noc%d" % s, [128, TOK], BF16) for s in range(2)]
        nabt = [P.sb("nabt%d" % s, [128, 2, 5, 576], F32) for s in range(2)]
        S = [P.sb("nS%d" % s, [128, 832], F32) for s in range(4)]
        Pm = [P.sb("nPm%d" % s, [128, 832], BF16) for s in range(4)]
        PT = [P.sb("nPT%d" % s, [128, 7, 128], BF16) for s in range(4)]
        otm = [P.sb("notm%d" % s, [128, 128], BF16) for s in range(2)]
        sst = [P.sb("nsst%d" % s, [128, 4], F32) for s in range(4)]
        self.load_cols(bcol[:, 0, :], I["na_b_qkv"][j, 0:D], "nbcol")
        self.load_cols(bcol[:, 1, :], I["na_b_qkv"][j, D:2 * D], "nbcol")
        P.dve(lambda e: e.tensor_scalar(out=bcol[:, 0, :], in0=bcol[:, 0, :], scalar1=0.125, scalar2=None, op0=ALU.mult), ["nbcol"], ["nbcol"])
        P.dma(bvr[:], I["na_b_qkv"][j, 2 * D:3 * D].partition_broadcast(128), writes=["nbvr"])
        Wq = I["na_w_qkv"][j]
        nb = 0
        nu = 0
        qtiles = list(range(NT)) if not last else list(range(2, NT))
        for c in range(8):
            s = c % 2
            for m in range(3):
                P.dma(wq[s][:, :, m, :], Wq[:, m * D + c * 128:m * D + (c + 1) * 128].rearrange("(k p) o -> p k o", p=128), writes=["nwq%d" % s], q="pool")
            P.dma(nabt[s][:], I["nab"][j, 2 * c:2 * c + 2].rearrange("h r q n -> q h r n"), writes=["nabt%d" % s])
            for (t0, n) in TBLK:
                for m, dst, kk in ((0, qc[s], "nqc%d" % s), (1, kc[s], "nkc%d" % s)):
                    bank = nb % 4
                    nb += 1
                    P.mm(PS[bank][:, 0:n], [(wq[s][:, k, m, :], hT[:, k, t0:t0 + n]) for k in range(8)], ["nwq%d" % s] + tkeys("h1_", t0, n), ["ps%d" % bank])
                    P.act(lambda e, bank=bank, n=n, dst=dst, t0=t0, m=m, c=c: e.activation(out=dst[:, t0:t0 + n], in_=PS[bank][:, 0:n], func=AF.Identity,
                                                                                       bias=bcol[:, m, c:c + 1], scale=(0.125 if m == 0 else 1.0)),
                          ["ps%d" % bank, "nbcol"], [kk])
            for g0 in range(0, NT, 4):
                ng = min(4, NT - g0)
                bank = nb % 4
                nb += 1

                def vm(e, g0=g0, ng=ng, bank=bank, s=s):
                    ins = None
                    for q in range(ng):
                        tk = slice((g0 + q) * 128, (g0 + q + 1) * 128)
                        for k in range(8):
                            ins = e.matmul(PS[bank][:, q * 128:(q + 1) * 128], lhsT=hT[:, k, tk], rhs=wq[s][:, k, 2, :], start=(k == 0), stop=(k == 7))
                    return ins

                P.pe(vm, ["nwq%d" % s] + tkeys("h1_", g0 * 128, ng * 128), ["ps%d" % bank])
                P.dve(lambda e, g0=g0, ng=ng, bank=bank, s=s, c=c: e.tensor_tensor(out=vc[s][:, g0:g0 + ng, :], in0=PS[bank][:, 0:ng * 128].rearrange("p (t o) -> p t o", t=ng),
                                                                                  in1=bvr[:, c * 128:(c + 1) * 128].unsqueeze(1).to_broadcast([128, ng, 128]), op=ALU.add),
                      ["ps%d" % bank, "nbvr"], ["nvc%d" % s])
            def unit(n, tt, hh, s=s, c=c):
                tq = slice(tt * 128, (tt + 1) * 128)
                if tt >= 2:
                    r = 2 * (tt - 2)
                    if r <= 2:
                        rs0, nkr, cls = 0, 8, r // 2
                    elif r >= 28:
                        rs0, nkr, cls = 24, 8, 3 + (r - 28) // 2
                    else:
                        rs0, nkr, cls = r - 4, 9, 2
                    nl = nkr * 64
                    ks = CTX + rs0 * 64
                else:
                    nl, ks, cls = 0, 0, 0
                ntot = nl + CTX
                osl = (n // 2) % 2
                pb = slice(hh * 64, hh * 64 + 64)
                u = n % 4
                ub = 2 * u
                k0, k1 = "ps%d" % ub, "ps%d" % (ub + 1)
                kS, kP, kPT, kst = "nS%d" % u, "nPm%d" % u, "nPT%d" % u, "nsst%d" % u
                q_ap = qc[s][pb, tq]
                if nl > 0:
                    P.mm(PS[ub][:, 0:512], [(q_ap, kc[s][pb, ks:ks + 512])], ["nqc%d" % s, "nkc%d" % s], [k0])
                    yield
                    P.dve(lambda e: e.tensor_tensor(out=S[u][:, 0:512], in0=PS[ub][:, 0:512], in1=nabt[s][:, hh, cls, 0:512], op=ALU.add),
                          [k0, "nabt%d" % s], [kS])
                    yield

                def sc2(e):
                    if nl > 512:
                        e.matmul(PS[ub + 1][:, 0:64], lhsT=q_ap, rhs=kc[s][pb, ks + 512:ks + 576], start=True, stop=True)
                    return e.matmul(PS[ub + 1][:, 64:320], lhsT=q_ap, rhs=kc[s][pb, 0:CTX], start=True, stop=True)

                P.pe(sc2, ["nqc%d" % s, "nkc%d" % s], [k1])
                yield
                if nl > 512:
                    P.dve(lambda e: e.tensor_tensor(out=S[u][:, 512:576], in0=PS[ub + 1][:, 0:64], in1=nabt[s][:, hh, cls, 512:576], op=ALU.add),
                          [k1, "nabt%d" % s], [kS])
                    yield
                P.act(lambda e: e.copy(out=S[u][:, nl:nl + CTX], in_=PS[ub + 1][:, 64:320]), [k1], [kS])
                yield
                st = sst[u]
                P.dve(lambda e: e.tensor_reduce(out=st[:, 0:1], in_=S[u][:, 0:ntot], axis=AX.X, op=ALU.max), [kS], [kst])
                yield
                P.dve(lambda e: e.tensor_scalar(out=st[:, 1:2], in0=st[:, 0:1], scalar1=-1.0, scalar2=None, op0=ALU.mult), [kst], [kst])
                yield
                P.act(lambda e: e.activation(out=Pm[u][:, 0:ntot], in_=S[u][:, 0:ntot], func=AF.Exp, bias=st[:, 1:2], scale=1.0, accum_out=st[:, 2:3]),
                      [kS, kst], [kP, kst])
                yield
                P.dve(lambda e: e.reciprocal(out=st[:, 3:4], in_=st[:, 2:3]), [kst], [kst])
                yield
                chunks = []
                off = 0
                while off < nl:
                    kn = min(128, nl - off)
                    chunks.append((off, kn, vc[s][0:kn, (ks + off) // 128, hh * 64:(hh + 1) * 64]))
                    off += kn
                for q in range(2):
                    chunks.append((nl + q * 128, 128, vc[s][:, q, hh * 64:(hh + 1) * 64]))
                pv = PS[ub][:].bitcast(BF16)

                def ptr(e):
                    ins = None
                    for ci, (off, kn, _) in enumerate(chunks):
                        ins = e.transpose(pv[0:kn, ci * 128:(ci + 1) * 128], Pm[u][:, off:off + kn], self.identb[:])
                    return ins

                P.pe(ptr, [kP], [k0])
                yield
                nch = len(chunks)
                if n % 2 == 0:
                    P.act(lambda e: e.copy(out=PT[u][:, 0:nch, :].rearrange("p c q -> p (c q)"), in_=pv[:, 0:nch * 128]), [k0], [kPT])
                else:
                    P.dve(lambda e: e.tensor_copy(out=PT[u][:, 0:nch, :].rearrange("p c q -> p (c q)"), in_=pv[:, 0:nch * 128]), [k0], [kPT])
                yield
                P.mm(PS[ub + 1][:, 320:384], [(PT[u][0:kn, ci, :], vap) for ci, (off, kn, vap) in enumerate(chunks)],
                     [kPT, "nvc%d" % s], [k1])
                yield
                P.act(lambda e: e.activation(out=otm[osl][:, hh * 64:(hh + 1) * 64], in_=PS[ub + 1][:, 320:384],
                                             func=AF.Copy, scale=st[:, 3:4]), [k1, kst], ["notm%d" % osl])
                yield
                if hh == 1:
                    pv3 = PS[ub + 1][:].bitcast(BF16)
                    P.pe(lambda e: e.transpose(pv3[:, 768:896], otm[osl][:], self.identb[:]), ["notm%d" % osl], [k1])
                    yield
                    P.dve(lambda e: e.tensor_copy(out=oc_[s][:, tq], in_=pv3[:, 768:896]), [k1], ["noc%d" % s])
                    yield

            units = [(tt, hh) for tt in qtiles for hh in range(2)]
            active = []
            nxt = 0
            while nxt < len(units) or active:
                while len(active) < 4 and nxt < len(units):
                    active.append(unit(nxt, units[nxt][0], units[nxt][1]))
                    nxt += 1
                for g in list(active):
                    try:
                        next(g)
                    except StopIteration:
                        active.remove(g)
            if not last:
                P.dma(self.d_act[c * 128:(c + 1) * 128, 0:CTX], oc_[s][:, 0:CTX], reads=["noc%d" % s], writes=["dactn%d" % c])
            P.dma(self.d_act[c * 128:(c + 1) * 128, CTX:TOK], oc_[s][:, CTX:TOK], reads=["noc%d" % s], writes=["dactn%d" % c])
        self.refence()
        self.outproj(i, 8, I["na_w_out"][j], I["na_b_out"][j], qtiles)

    def build(self):
        P = self.P
        self.declare()
        self.consts()
        self.persist()
        self.mods()
        self.mod_layer(0)
        self.colm_load(0)
        self.first_norm()
        for i in range(1, self.depth):
            self.mod_layer(i)
            self.colm_load(i)
        self.refence()
        P.sb_off = self.top
        try:
            self.layers()
        except StopIteration:
            pass
        P.emit()
        return self.nc

    def layers(self):
        for i in range(self.depth):
            last = i == self.depth - 1
            j = i // 2
            if i % 2 == 0:
                self.mlstm(i, j)
            else:
                self.natten(i, j, last)
            self.ffn(i, last)


def host_inputs(inputs, depth=4):
    f = lambda a: np.ascontiguousarray(np.asarray(a, dtype=np.float32))
    x, c, ctx, c_ctx = f(inputs["x"]), f(inputs["c"]), f(inputs["ctx"]), f(inputs["c_ctx"])
    B = x.shape[0]
    shared = {}
    for k in ("ada_w", "ada_b", "norm1_g", "norm2_g", "final_g", "ml_w_in", "ml_conv_w", "ml_conv_b", "ml_skip", "ml_hnorm_g", "ml_w_out",
              "na_w_qkv", "na_b_qkv", "na_w_out", "na_b_out", "ff_w_up", "ff_b_up", "ff_conv_w", "ff_conv_b", "ff_w_down", "ff_b_down"):
        shared[k] = f(inputs[k])
    n_a = shared["ml_w_in"].shape[0]
    for nm, src in (("bdq", "ml_wq"), ("bdk", "ml_wk"), ("bdv", "ml_wv")):
        w = f(inputs[src])
        bd = np.zeros((n_a, 16, 128, 128), np.float32)
        wr = w.reshape(n_a, 16, 32, 4, 4)
        for n in range(32):
            bd[:, :, 4 * n:4 * n + 4, 4 * n:4 * n + 4] = wr[:, :, n]
        shared[nm] = bd
        shared[nm + "T"] = np.ascontiguousarray(bd.transpose(0, 1, 3, 2))
    gw = f(inputs["ml_gate_w"])
    g2 = gw.reshape(n_a, 2, 3, 16, 128, 2, 4)
    g2 = g2.transpose(0, 2, 3, 4, 5, 1, 6)
    shared["gw"] = np.ascontiguousarray(g2.reshape(n_a, 3, 16, 128, 16))
    gb = f(inputs["ml_gate_b"]).reshape(n_a, 2, 2, 4).transpose(0, 2, 1, 3)
    shared["gb"] = np.ascontiguousarray(gb.reshape(n_a, 16))
    rpb = f(inputs["na_rpb"])
    n_b = rpb.shape[0]
    nab = np.full((n_b, 16, 5, 128, 576), NEGV, np.float32)
    col = np.arange(64)
    cs = np.clip(col - 8, 0, 48)
    ok = (col[None, :] >= cs[:, None]) & (col[None, :] < cs[:, None] + 16)
    dc = np.clip(col[None, :] - col[:, None] + 15, 0, 30)
    for cls, r in enumerate((0, 2, 4, 28, 30)):
        for rho in range(2):
            rq = r + rho
            rs_q = int(np.clip(rq - 4, 0, 24))
            rs0 = int(np.clip(r - 4, 0, 24))
            nkr = 9 if cls == 2 else 8
            for jj in range(nkr):
                krow = rs0 + jj
                if krow < rs_q or krow >= rs_q + 8:
                    continue
                dr = krow - rq + 7
                vals = rpb[:, :, dr, :][:, :, dc]
                blk = np.where(ok[None, None], vals, np.float32(NEGV))
                nab[:, :, cls, rho * 64:(rho + 1) * 64, jj * 64:(jj + 1) * 64] = blk
    shared["nab"] = nab
    u = np.arange(128)
    same = np.ones((128, 128), bool)
    cm = np.zeros((2, 3, 128, 128), np.float32)
    cm[0, 0] = same & (u[:, None] <= u[None, :])
    cm[0, 1] = -(same & (u[:, None] > u[None, :])).astype(np.float32)
    cm[0, 2] = np.where(same & (u[:, None] <= u[None, :]), 0.0, NEGV)
    cm[1, 0] = same & (u[:, None] >= u[None, :])
    cm[1, 1] = -(same & (u[:, None] < u[None, :])).astype(np.float32)
    cm[1, 2] = np.where(same & (u[:, None] >= u[None, :]), 0.0, NEGV)
    shared["cmask"] = cm
    shared["identf"] = np.eye(128, dtype=np.float32)
    hm = np.zeros((128, 2), np.float32)
    hm[:64, 0] = 1.0
    hm[64:, 1] = 1.0
    shared["halfm"] = hm
    maps = []
    for b in range(B):
        m = dict(shared)
        m["x"] = x[b]
        m["ctx"] = ctx[b]
        m["cvec"] = np.ascontiguousarray(np.stack([c[b], c_ctx], 0))
        maps.append(m)
    return maps


_NC_CACHE = {}


def kernel(**inputs):
    maps = host_inputs(inputs)
    if "nc" not in _NC_CACHE:
        _NC_CACHE["nc"] = Builder(4).build()
    nc = _NC_CACHE["nc"]
    res = run_bass_kernel_spmd(nc, maps, core_ids=list(range(len(maps))))
    return np.stack([np.asarray(r["out"], dtype=np.float32) for r in res.results], 0)
```
